# Optimizing a Trainium2 kernel written in Bass

```python
import math
import jax, jax.numpy as jnp
from jax import lax
import numpy as np

D_MODEL = 1024
BATCH = 16
SEQ = 2048
DEPTH = 4
DEC_BATCH = 8
DEC_SEQ = 8192
PAST_LEN = 128

HEAD_DIM = 64
A_HEADS = 8
A_KV_HEADS = 2
A_WINDOW = 128
A_BLOCK = 128
B_HEADS = 8
GRID_W = 64
NB_MAX_ROWS = 8
NB_COLS = 16
NB_COL_BLOCK = 16
NB_COL_SLAB = 32
ROPE_THETA = 10000.0
A_Q_W = A_HEADS * HEAD_DIM
A_KV_W = A_KV_HEADS * HEAD_DIM
B_W = B_HEADS * HEAD_DIM
ATTN_IN_W = A_Q_W + 2 * A_KV_W + 3 * B_W
ATTN_CAT_W = A_Q_W + B_W
C_HEADS = 8
C_KEY_DIM = D_MODEL // C_HEADS
C_WIDTH = C_HEADS * C_KEY_DIM
C_CHUNK = 64
N_GROUPS = 4
EXPERTS_PER_GROUP = 8
N_EXPERTS = N_GROUPS * EXPERTS_PER_GROUP
TOP_K_IN_GROUP = 2
D_EXPERT = D_MODEL // 2
MOE_BLOCK = 128
N_ATTN_LAYERS = (DEPTH + 1) // 2
N_REC_LAYERS = DEPTH // 2
DN_ALPHA = (2 * DEPTH) ** 0.25
DN_BETA = (8 * DEPTH) ** -0.25
LN_EPS = 1e-5
RMS_EPS = 1e-6

kernel_name = 'hybrid_bidir_encoder_swa_natten_hgrn2_hmoe'


def layer_norm(x, g, b):
    xf = x.astype(jnp.float32)
    mu = jnp.mean(xf, axis=-1, keepdims=True)
    var = jnp.mean(jnp.square(xf - mu), axis=-1, keepdims=True)
    return ((xf - mu) * lax.rsqrt(var + LN_EPS) * g.astype(jnp.float32) + b.astype(jnp.float32)).astype(x.dtype)


def rope(x, pos):
    inv = jnp.power(ROPE_THETA, -jnp.arange(0, HEAD_DIM, 2, dtype=jnp.float32) / HEAD_DIM)
    ang = pos.astype(jnp.float32)[:, None] * inv[None, :]
    cos = jnp.concatenate([jnp.cos(ang), jnp.cos(ang)], axis=-1)[None, :, None, :]
    sin = jnp.concatenate([jnp.sin(ang), jnp.sin(ang)], axis=-1)[None, :, None, :]
    x1, x2 = jnp.split(x, 2, axis=-1)
    rot = jnp.concatenate([-x2, x1], axis=-1)
    return (x * cos + rot * sin).astype(x.dtype)


def window_attention(q, k, v, sink):
    bsz, t = q.shape[0], q.shape[1]
    grp = A_HEADS // A_KV_HEADS
    n_blk = t // A_BLOCK
    span = A_BLOCK + 2 * A_WINDOW
    kp = jnp.pad(k, ((0, 0), (A_WINDOW, A_WINDOW), (0, 0), (0, 0)))
    vp = jnp.pad(v, ((0, 0), (A_WINDOW, A_WINDOW), (0, 0), (0, 0)))
    sink_l = sink.astype(jnp.float32).reshape(A_KV_HEADS, grp)
    scale = HEAD_DIM ** -0.5

    def block(j):
        q0 = j * A_BLOCK
        qb = lax.dynamic_slice_in_dim(q, q0, A_BLOCK, axis=1).reshape(bsz, A_BLOCK, A_KV_HEADS, grp, HEAD_DIM)
        kb = lax.dynamic_slice_in_dim(kp, q0, span, axis=1)
        vb = lax.dynamic_slice_in_dim(vp, q0, span, axis=1)
        qpos = q0 + jnp.arange(A_BLOCK)
        kpos = q0 - A_WINDOW + jnp.arange(span)
        valid = (jnp.abs(qpos[:, None] - kpos[None, :]) <= A_WINDOW) & (kpos >= 0)[None, :] & (kpos < t)[None, :]
        s = jnp.einsum('bqhgd,bkhd->bhgqk', qb, kb, preferred_element_type=jnp.float32) * scale
        s = jnp.where(valid, s, -jnp.inf)
        sk = jnp.broadcast_to(sink_l[None, :, :, None, None], (bsz, A_KV_HEADS, grp, A_BLOCK, 1))
        p = jax.nn.softmax(jnp.concatenate([s, sk], axis=-1), axis=-1)[..., :span]
        o = jnp.einsum('bhgqk,bkhd->bqhgd', p.astype(v.dtype), vb)
        return o.reshape(bsz, A_BLOCK, A_Q_W)

    out = lax.map(block, jnp.arange(n_blk))
    return out.transpose(1, 0, 2, 3).reshape(bsz, t, A_Q_W)


def neighbourhood_attention(q, k, v, rel_bias):
    bsz, t = q.shape[0], q.shape[1]
    rows = t // GRID_W
    wr = min(NB_MAX_ROWS, rows)
    ncb = GRID_W // NB_COL_BLOCK
    qg = q.reshape(bsz, rows, ncb, NB_COL_BLOCK, B_HEADS, HEAD_DIM)
    kg = k.reshape(bsz, rows, GRID_W, B_HEADS, HEAD_DIM)
    vg = v.reshape(bsz, rows, GRID_W, B_HEADS, HEAD_DIM)
    cb = np.arange(ncb)
    slab_start = np.clip(cb * NB_COL_BLOCK - NB_COLS // 2, 0, GRID_W - NB_COL_SLAB)
    kcol = slab_start[:, None] + np.arange(NB_COL_SLAB)
    qcol = cb[:, None] * NB_COL_BLOCK + np.arange(NB_COL_BLOCK)
    qwin = np.clip(qcol - NB_COLS // 2, 0, GRID_W - NB_COLS)
    col_ok = (kcol[:, None, :] >= qwin[:, :, None]) & (kcol[:, None, :] < qwin[:, :, None] + NB_COLS)
    ok = np.broadcast_to(col_ok[:, None, :, None, :], (ncb, 1, NB_COL_BLOCK, wr, NB_COL_SLAB)).reshape(ncb, 1, NB_COL_BLOCK, wr * NB_COL_SLAB)
    dc = np.clip(kcol[:, None, :] - qcol[:, :, None], -(NB_COLS - 1), NB_COLS - 1) + NB_COLS - 1
    col_bias = rel_bias.astype(jnp.float32)[:, :, dc]
    scale = HEAD_DIM ** -0.5

    def row(r):
        rs = jnp.clip(r - wr // 2, 0, rows - wr)
        qr = lax.dynamic_index_in_dim(qg, r, axis=1, keepdims=False)
        ks = lax.dynamic_slice_in_dim(kg, rs, wr, axis=1)
        vs = lax.dynamic_slice_in_dim(vg, rs, wr, axis=1)
        kb = ks[:, :, kcol].transpose(0, 2, 1, 3, 4, 5).reshape(bsz, ncb, wr * NB_COL_SLAB, B_HEADS, HEAD_DIM)
        vb = vs[:, :, kcol].transpose(0, 2, 1, 3, 4, 5).reshape(bsz, ncb, wr * NB_COL_SLAB, B_HEADS, HEAD_DIM)
        s = jnp.einsum('bjqhd,bjkhd->bjhqk', qr, kb, preferred_element_type=jnp.float32) * scale
        dr = rs + jnp.arange(wr) - r + NB_MAX_ROWS - 1
        bias = jnp.take(col_bias, dr, axis=1)
        bias = bias.transpose(2, 0, 3, 1, 4).reshape(ncb, B_HEADS, NB_COL_BLOCK, wr * NB_COL_SLAB)
        s = jnp.where(ok, s + bias, -jnp.inf)
        p = jax.nn.softmax(s, axis=-1)
        o = jnp.einsum('bjhqk,bjkhd->bjqhd', p.astype(v.dtype), vb)
        return o.reshape(bsz, GRID_W, B_W)

    out = lax.map(row, jnp.arange(rows))
    return out.transpose(1, 0, 2, 3).reshape(bsz, t, B_W)


def attention_mixer(h, w_in, sink, rel_bias, w_out):
    bsz, t, _ = h.shape
    proj = jnp.einsum('btd,de->bte', h, w_in)
    cuts = [A_Q_W, A_Q_W + A_KV_W, A_Q_W + 2 * A_KV_W, A_Q_W + 2 * A_KV_W + B_W, A_Q_W + 2 * A_KV_W + 2 * B_W]
    qa, ka, va, qb, kb, vb = jnp.split(proj, cuts, axis=-1)
    pos = jnp.arange(t)
    qa = rope(qa.reshape(bsz, t, A_HEADS, HEAD_DIM), pos)
    ka = rope(ka.reshape(bsz, t, A_KV_HEADS, HEAD_DIM), pos)
    va = va.reshape(bsz, t, A_KV_HEADS, HEAD_DIM)
    oa = window_attention(qa, ka, va, sink)
    ob = neighbourhood_attention(qb.reshape(bsz, t, B_HEADS, HEAD_DIM), kb.reshape(bsz, t, B_HEADS, HEAD_DIM), vb.reshape(bsz, t, B_HEADS, HEAD_DIM), rel_bias)
    return jnp.einsum('bte,ed->btd', jnp.concatenate([oa, ob], axis=-1), w_out)


def hgrn_direction(q, z, v, lb):
    bsz, nh, t, dk = q.shape
    dv = v.shape[-1]
    nc = t // C_CHUNK
    lbb = lb[None, :, None, :]
    f = lbb + (1.0 - lbb) * jax.nn.sigmoid(z)
    logf = jnp.log(f)
    k = 1.0 - f

    def chunks(a):
        return a.reshape(bsz, nh, nc, C_CHUNK, a.shape[-1]).transpose(2, 0, 1, 3, 4)

    qc, kc, vc = chunks(q), chunks(k), chunks(v)
    bc = jnp.cumsum(chunks(logf), axis=3)
    incl = np.tril(np.ones((C_CHUNK, C_CHUNK), dtype=bool))

    def step(s_state, xs):
        qi, ki, vi, bi = xs
        diff = bi[:, :, :, None, :] - bi[:, :, None, :, :]
        dec = jnp.exp(jnp.where(incl[:, :, None], diff, -jnp.inf))
        a = jnp.einsum('bhtk,bhsk,bhtsk->bhts', qi, ki, dec)
        bl = bi[:, :, -1, :]
        o = jnp.einsum('bhts,bhsv->bhtv', a, vi) + jnp.einsum('bhtk,bhkv->bhtv', qi * jnp.exp(bi), s_state)
        s_state = jnp.exp(bl)[..., None] * s_state + jnp.einsum('bhsk,bhsv->bhkv', ki * jnp.exp(bl[:, :, None, :] - bi), vi)
        return s_state, o

    s0 = jnp.zeros((bsz, nh, dk, dv), jnp.float32)
    _, o = lax.scan(step, s0, (qc, kc, vc, bc))
    return o.transpose(1, 2, 0, 3, 4).reshape(bsz, nh, t, dv)


def hgrn_mixer(h, w_in, lb, gnorm, w_out):
    bsz, t, _ = h.shape
    proj = jnp.einsum('btd,de->bte', h, w_in).astype(jnp.float32)
    q, zf, zb, v, g = jnp.split(proj, 5, axis=-1)

    def heads(a):
        return a.reshape(bsz, t, C_HEADS, C_KEY_DIM).transpose(0, 2, 1, 3)

    q, zf, zb, v = heads(q), heads(zf), heads(zb), heads(v)
    lbh = lb.reshape(C_HEADS, C_KEY_DIM)
    o_f = hgrn_direction(q, zf, v, lbh)
    o_b = jnp.flip(hgrn_direction(jnp.flip(q, axis=2), jnp.flip(zb, axis=2), jnp.flip(v, axis=2), lbh), axis=2)
    o = (o_f + o_b).transpose(0, 2, 1, 3)
    o = o * lax.rsqrt(jnp.mean(jnp.square(o), axis=-1, keepdims=True) + RMS_EPS)
    o = o.reshape(bsz, t, C_WIDTH) * gnorm.astype(jnp.float32) * jax.nn.silu(g)
    return jnp.einsum('bte,ed->btd', o.astype(h.dtype), w_out)


def hierarchical_moe(h, w_rg, b_rg, w_re, b_re, w_gate, w_up, w_down):
    n_tok, d = h.shape
    hf = h.astype(jnp.float32)
    g_prob = jax.nn.softmax(hf @ w_rg.astype(jnp.float32) + b_rg.astype(jnp.float32), axis=-1)
    p_grp, grp = lax.top_k(g_prob, 1)
    e_logits = (hf @ w_re.astype(jnp.float32)).reshape(n_tok, N_GROUPS, EXPERTS_PER_GROUP) + b_re.astype(jnp.float32)
    e_logits = jnp.take_along_axis(e_logits, grp[:, :, None], axis=1)[:, 0]
    top_val, top_idx = lax.top_k(e_logits, TOP_K_IN_GROUP)
    gate = jax.nn.softmax(top_val, axis=-1) * p_grp
    n_asg = n_tok * TOP_K_IN_GROUP
    eid = (grp * EXPERTS_PER_GROUP + top_idx).reshape(n_asg)
    tok = jnp.repeat(jnp.arange(n_tok, dtype=jnp.int32), TOP_K_IN_GROUP)
    wt = gate.reshape(n_asg)
    order = jnp.argsort(eid)
    eid_s, tok_s, wt_s = eid[order], tok[order], wt[order]
    counts = jax.ops.segment_sum(jnp.ones_like(eid_s), eid_s, num_segments=N_EXPERTS)
    starts = jnp.cumsum(counts) - counts
    padded = (counts + MOE_BLOCK - 1) // MOE_BLOCK * MOE_BLOCK
    pad_ends = jnp.cumsum(padded)
    pad_starts = pad_ends - padded
    rank = jnp.arange(n_asg, dtype=jnp.int32) - starts[eid_s]
    dest = pad_starts[eid_s] + rank
    cap = -(-n_asg // MOE_BLOCK) * MOE_BLOCK + N_EXPERTS * MOE_BLOCK
    n_blk = cap // MOE_BLOCK
    xbuf = jnp.zeros((cap, d), h.dtype).at[dest].set(h[tok_s])
    blk_exp = jnp.minimum(jnp.searchsorted(pad_ends, jnp.arange(n_blk, dtype=jnp.int32) * MOE_BLOCK, side='right'), N_EXPERTS - 1)

    def run_block(args):
        xb, e = args
        return (jax.nn.silu(xb @ w_gate[e]) * (xb @ w_up[e])) @ w_down[e]

    ybuf = lax.map(run_block, (xbuf.reshape(n_blk, MOE_BLOCK, d), blk_exp)).reshape(cap, d)
    y = jnp.zeros((n_tok, d), jnp.float32).at[tok_s].add(ybuf[dest].astype(jnp.float32) * wt_s[:, None])
    return y.astype(h.dtype)


def encoder_trunk(x, c, ada_w, ada_b, ln_g, ln_b, attn_w_in, attn_sink, nat_rel_bias, attn_w_out, rec_w_in, rec_lb, rec_gnorm, rec_w_out, router_w_group, router_b_group, router_w_expert, router_b_expert, expert_w_gate, expert_w_up, expert_w_down):
    bsz, t, d = x.shape
    p_lb = jax.nn.softmax(rec_lb.astype(jnp.float32), axis=0)
    lower_bounds = jnp.cumsum(p_lb, axis=0) - p_lb[0:1]
    cs = jax.nn.silu(c)
    for l in range(DEPTH):
        mod = cs @ ada_w[l] + ada_b[l]
        sh1, sc1, g1, sh2, sc2, g2 = [m[:, None, :] for m in jnp.split(mod, 6, axis=-1)]
        hmix = x * (1 + sc1) + sh1
        if l % 2 == 0:
            i = l // 2
            y = attention_mixer(hmix, attn_w_in[i], attn_sink[i], nat_rel_bias[i], attn_w_out[i])
        else:
            i = l // 2
            y = hgrn_mixer(hmix, rec_w_in[i], lower_bounds[i], rec_gnorm[i], rec_w_out[i])
        x = layer_norm(DN_ALPHA * x + (1 + g1) * y, ln_g[l, 0], ln_b[l, 0])
        hffn = x * (1 + sc2) + sh2
        y = hierarchical_moe(hffn.reshape(bsz * t, d), router_w_group[l], router_b_group[l], router_w_expert[l], router_b_expert[l], expert_w_gate[l], expert_w_up[l], expert_w_down[l]).reshape(bsz, t, d)
        x = layer_norm(DN_ALPHA * x + (1 + g2) * y, ln_g[l, 1], ln_b[l, 1])
    return x


def setup_inputs(seed: int = 0) -> dict:
    key = jax.random.key(seed)
    ks = jax.random.split(key, 24)
    f32 = jnp.float32
    nrm = lambda k, s: jax.random.normal(k, s, f32)
    d = D_MODEL
    return {
        'x_prompt': nrm(ks[0], (BATCH, SEQ, d)),
        'x_sample': nrm(ks[1], (DEC_BATCH, DEC_SEQ, d)),
        'c_prompt': nrm(ks[2], (BATCH, d)),
        'c_sample': nrm(ks[3], (DEC_BATCH, d)),
        'ada_w': nrm(ks[4], (DEPTH, d, 6 * d)) * (0.2 * d ** -0.5),
        'ada_b': nrm(ks[5], (DEPTH, 6 * d)) * 0.02,
        'ln_g': 1.0 + 0.02 * nrm(ks[6], (DEPTH, 2, d)),
        'ln_b': 0.02 * nrm(ks[7], (DEPTH, 2, d)),
        'attn_w_in': nrm(ks[8], (N_ATTN_LAYERS, d, ATTN_IN_W)) * d ** -0.5,
        'attn_sink': nrm(ks[9], (N_ATTN_LAYERS, A_HEADS)) * 0.5,
        'nat_rel_bias': nrm(ks[10], (N_ATTN_LAYERS, B_HEADS, 2 * NB_MAX_ROWS - 1, 2 * NB_COLS - 1)) * 0.1,
        'attn_w_out': nrm(ks[11], (N_ATTN_LAYERS, ATTN_CAT_W, d)) * (ATTN_CAT_W ** -0.5 * DN_BETA),
        'rec_w_in': nrm(ks[12], (N_REC_LAYERS, d, 5 * C_WIDTH)) * d ** -0.5,
        'rec_lb': nrm(ks[13], (N_REC_LAYERS, C_WIDTH)) * 0.5,
        'rec_gnorm': 1.0 + 0.02 * nrm(ks[14], (N_REC_LAYERS, C_WIDTH)),
        'rec_w_out': nrm(ks[15], (N_REC_LAYERS, C_WIDTH, d)) * (C_WIDTH ** -0.5 * DN_BETA),
        'router_w_group': nrm(ks[16], (DEPTH, d, N_GROUPS)) * d ** -0.5,
        'router_b_group': nrm(ks[17], (DEPTH, N_GROUPS)) * 0.01,
        'router_w_expert': nrm(ks[18], (DEPTH, d, N_EXPERTS)) * d ** -0.5,
        'router_b_expert': nrm(ks[19], (DEPTH, N_GROUPS, EXPERTS_PER_GROUP)) * 0.01,
        'expert_w_gate': nrm(ks[20], (DEPTH, N_EXPERTS, d, D_EXPERT)) * d ** -0.5,
        'expert_w_up': nrm(ks[21], (DEPTH, N_EXPERTS, d, D_EXPERT)) * d ** -0.5,
        'expert_w_down': nrm(ks[22], (DEPTH, N_EXPERTS, D_EXPERT, d)) * (D_EXPERT ** -0.5 * DN_BETA),
    }


def reference(x_prompt, x_sample, c_prompt, c_sample, ada_w, ada_b, ln_g, ln_b, attn_w_in, attn_sink, nat_rel_bias, attn_w_out, rec_w_in, rec_lb, rec_gnorm, rec_w_out, router_w_group, router_b_group, router_w_expert, router_b_expert, expert_w_gate, expert_w_up, expert_w_down):
    y_prompt = encoder_trunk(x_prompt, c_prompt, ada_w, ada_b, ln_g, ln_b, attn_w_in, attn_sink, nat_rel_bias, attn_w_out, rec_w_in, rec_lb, rec_gnorm, rec_w_out, router_w_group, router_b_group, router_w_expert, router_b_expert, expert_w_gate, expert_w_up, expert_w_down)
    y_sample = encoder_trunk(x_sample, c_sample, ada_w, ada_b, ln_g, ln_b, attn_w_in, attn_sink, nat_rel_bias, attn_w_out, rec_w_in, rec_lb, rec_gnorm, rec_w_out, router_w_group, router_b_group, router_w_expert, router_b_expert, expert_w_gate, expert_w_up, expert_w_down)
    return (y_prompt, y_sample)
```

```python
import math
from collections import Counter
from contextlib import ExitStack
import numpy as np
import ml_dtypes
import concourse.bass as bass
import concourse.mybir as mybir
from concourse.bass_utils import run_bass_kernel_spmd

F32 = mybir.dt.float32
BF16 = mybir.dt.bfloat16
I32 = mybir.dt.int32
AF = mybir.ActivationFunctionType
ALU = mybir.AluOpType
AX = mybir.AxisListType

D = 1024
HD = 64
N_GROUPS = 4
EPG = 8
NEXP = 32
DEXP = 512
LN_EPS = 1e-5
RMS_EPS = 1e-6
ROPE_THETA = 10000.0


class KB:
    def __init__(self, nc):
        self.nc = nc
        self.engs = {"pe": nc.tensor, "act": nc.scalar, "dve": nc.vector, "pool": nc.gpsimd, "sp": nc.sync}
        self.sems = {}
        self.base = {}
        for e in self.engs:
            self.sems[("e", e)] = nc.alloc_semaphore(name="S_" + e)
            self.base[("e", e)] = 0
        self.chan_owner = {}
        self.nphase = 0

    def sig(self, key):
        if key not in self.sems:
            self.sems[key] = self.nc.alloc_semaphore(name="C_" + str(key[1]))
            self.base[key] = 0
        return self.sems[key]


class Phase:
    def __init__(self, kb, name, n_iter=None):
        self.kb = kb
        self.name = name
        self.n_iter = n_iter
        self.ops = []

    def op(self, eng, fn, r=(), w=(), chan=None):
        self.ops.append(dict(eng=eng, fn=fn, r=tuple(r), w=tuple(w), chan=chan))

    def dma(self, eng, chan, fn, r=(), w=()):
        self.op(eng, fn, r, w, chan=chan)

    def run(self):
        kb = self.kb
        nc = kb.nc
        ops = self.ops
        m = len(ops)
        if m == 0:
            return
        loop = self.n_iter is not None
        N = self.n_iter if loop else 1
        sig_of = [("c", o["chan"]) if o["chan"] else ("e", o["eng"]) for o in ops]
        for j, o in enumerate(ops):
            if o["chan"]:
                own = kb.chan_owner.setdefault(o["chan"], o["eng"])
                assert own == o["eng"], f"chan {o['chan']} issued from two engines"
        per_iter = Counter(sig_of)
        kidx = []
        cnt = Counter()
        for j in range(m):
            kidx.append(cnt[sig_of[j]])
            cnt[sig_of[j]] += 1
        last_w = {}
        readers = {}
        deps = [set() for _ in range(m)]
        for it in ((0, 1) if loop else (0,)):
            for j, o in enumerate(ops):
                d = set()
                for b in o["r"]:
                    if b in last_w:
                        d.add(last_w[b] + ("raw",))
                for b in o["w"]:
                    if b in last_w:
                        d.add(last_w[b] + ("waw",))
                    for rd in readers.get(b, []):
                        d.add(rd + ("war",))
                if it == (1 if loop else 0):
                    deps[j] = {(jj, itt - it, ty) for (itt, jj, ty) in d if (itt, jj) != (it, j)}
                for b in o["r"]:
                    readers.setdefault(b, []).append((it, j))
                for b in o["w"]:
                    last_w[b] = (it, j)
                    readers[b] = []
        waits = []
        for j, o in enumerate(ops):
            need = {}
            for (jj, off, ty) in deps[j]:
                p = ops[jj]
                s = sig_of[jj]
                if s[0] == "e" and sig_of[j][0] == "e" and p["eng"] == o["eng"]:
                    if o["eng"] == "pe" or ty != "raw":
                        continue
                n_s = per_iter[s]
                if s[0] == "c":
                    if off == 0:
                        q = sum(1 for x in range(j) if sig_of[x] == s)
                        const = q * 16
                    else:
                        const = 0
                else:
                    const = (kidx[jj] + 1) if off == 0 else (kidx[jj] + 1 - n_s)
                if s not in need or const > need[s]:
                    need[s] = const
            waits.append(need)
        known = {}
        for j, o in enumerate(ops):
            kn = known.setdefault(o["eng"], {})
            for s in list(waits[j]):
                if s in kn and kn[s] >= waits[j][s]:
                    del waits[j][s]
                else:
                    kn[s] = waits[j][s]
        T0 = {}
        for s, n_s in per_iter.items():
            kb.sig(s)
            inc = 16 if s[0] == "c" else 1
            T0[s] = kb.base[s] + (n_s * inc if (loop and s[0] == "e") else 0)
        engines_used = []
        for o in ops:
            if o["eng"] not in engines_used:
                engines_used.append(o["eng"])

        def free_ids(E):
            hs, ids = [], []
            while True:
                try:
                    kb.probe_cnt = getattr(kb, "probe_cnt", 0) + 1
                    h = E.alloc_register(f"probe{kb.probe_cnt}")
                except Exception:
                    break
                hs.append(h)
                ids.append(nc.lookup_reg(h).reg_id)
            for h in hs:
                E.free_register(h)
            return set(ids)

        def emit_engine(ename):
            E = kb.engs[ename]
            before = free_ids(E)
            emit_engine_inner(ename)
            leaked = sorted(before - free_ids(E))
            for rid in leaked:
                kb.probe_cnt += 1
                h = nc.add_register(E.engine, f"leak{kb.probe_cnt}", rid)
                E.free_register(h)

        def emit_engine_inner(ename):
            E = kb.engs[ename]
            mine = [j for j, o in enumerate(ops) if o["eng"] == ename]
            wsigs = []
            for j in mine:
                for s in waits[j]:
                    if s not in wsigs:
                        wsigs.append(s)

            def body(i, regs, tmp):
                for j in mine:
                    o = ops[j]
                    for s, const in waits[j].items():
                        if i is None:
                            E.wait_ge(kb.sig(s), T0[s] + const)
                        elif const == 0:
                            E.wait_ge(kb.sig(s), regs[s])
                        else:
                            E.reg_add(tmp, regs[s], const)
                            E.wait_ge(kb.sig(s), tmp)
                    ins = o["fn"](i)
                    s = sig_of[j]
                    ins.then_inc(kb.sig(s), 16 if s[0] == "c" else 1)

            if loop:
                if ("e", ename) in per_iter:
                    E.sem_inc(kb.sig(("e", ename)), per_iter[("e", ename)])
                regs = {}
                for k, s in enumerate(wsigs):
                    regs[s] = E.alloc_register(f"w{kb.nphase}_{ename}_{k}")
                    E.reg_mov(regs[s], T0[s])
                tmp = E.alloc_register(f"wt{kb.nphase}_{ename}")
                with E.Fori(0, N) as i:
                    body(i, regs, tmp)
                    for s in wsigs:
                        E.reg_add(regs[s], regs[s], per_iter[s] * (16 if s[0] == "c" else 1))
                for s in wsigs:
                    E.free_register(regs[s])
                E.free_register(tmp)
            else:
                body(None, None, None)
            for s, n_s in per_iter.items():
                owner = kb.chan_owner[s[1]] if s[0] == "c" else s[1]
                if owner == ename:
                    inc = 16 if s[0] == "c" else 1
                    E.wait_ge(kb.sig(s), T0[s] + N * n_s * inc)

        with nc.Block() as blk:
            reg = {"pe": blk.tensor, "act": blk.scalar, "dve": blk.vector, "pool": blk.gpsimd, "sp": blk.sync}
            for ename in engines_used:
                reg[ename](lambda eng, ename=ename: emit_engine(ename))
        for s, n_s in per_iter.items():
            inc = 16 if s[0] == "c" else 1
            kb.base[s] = T0[s] + N * n_s * inc
        nc.all_engine_barrier()
        kb.nphase += 1


def _bf(a):
    return np.ascontiguousarray(a).astype(ml_dtypes.bfloat16)


def host_constants(cfg):
    seqs = cfg["seqs"]
    NT = sum(seqs)
    c = {}
    c["ident_f"] = np.eye(128, dtype=np.float32)
    c["ident_b"] = _bf(np.eye(128, dtype=np.float32))
    pos = np.concatenate([np.arange(t) for t in seqs]).astype(np.float32)
    inv = np.power(ROPE_THETA, -np.arange(0, HD, 2, dtype=np.float32) / HD).astype(np.float32)
    d = np.arange(128) % 64
    ang = pos[None, :] * inv[d % 32][:, None]
    c["rope_cos"] = np.cos(ang).astype(np.float32)
    sgn = np.where(d < 32, -1.0, 1.0).astype(np.float32)
    c["rope_sin"] = (np.sin(ang) * sgn[:, None]).astype(np.float32)
    j = np.arange(128)[:, None]
    i = np.arange(128)[None, :]
    c["wmask"] = _bf(np.stack([(j >= i), (j <= i)], axis=1).astype(np.float32))
    kc = np.arange(64)
    qc = np.arange(64)
    qwin = np.clip(qc - 8, 0, 48)
    col_ok = (kc[:, None] >= qwin[None, :]) & (kc[:, None] < qwin[None, :] + 16)
    m = np.zeros((128, 9, 128), np.float32)
    for t in range(9):
        for b in range(2):
            for a in range(2):
                ok = col_ok.astype(np.float32)
                if t == 7 and (a == 1 and b == 0):
                    ok = ok * 0
                if t == 8 and not (a == 1 and b == 0):
                    ok = ok * 0
                m[b * 64:(b + 1) * 64, t, a * 64:(a + 1) * 64] = ok
    c["namask"] = m
    s = np.arange(64)[:, None]
    t = np.arange(64)[None, :]
    c["hmask"] = _bf(np.stack([(s <= t), (s >= t)], axis=1).astype(np.float32))
    sm = np.ones((128, 8, 128), np.float32)
    sm[:, :, 0] = 0
    sm[:, :, 64] = 0
    c["scanmask"] = sm.reshape(128, 1024)
    c["iota32"] = np.tile(np.arange(32, dtype=np.float32)[None, :], (128, 1))
    c["iota_blk"] = np.tile((np.arange(256, dtype=np.float32) * 128.0)[None, :], (128, 1))
    c["iota_p"] = np.arange(128, dtype=np.float32).reshape(128, 1)
    c["tri_su"] = _bf((np.arange(128)[:, None] < np.arange(128)[None, :]).astype(np.float32))
    c["ones_b"] = _bf(np.ones((128, 128), np.float32))
    return c


NA_TABLE_DELTA = [-3, -2, -1, 0, 1, 2, 3, -2, 2]


def na_bias_tables(rel_bias):
    L = rel_bias.shape[0]
    kc = np.arange(64)[:, None]
    qc = np.arange(64)[None, :]
    dc = np.clip(kc - qc, -15, 15) + 15
    out = np.zeros((L, 128, 9, 8, 128), np.float32)
    for t, dl in enumerate(NA_TABLE_DELTA):
        for b in range(2):
            for a in range(2):
                dr = 2 * dl + b - a + 7
                g = rel_bias[:, :, dr, :][:, :, dc]
                out[:, b * 64:(b + 1) * 64, t, :, a * 64:(a + 1) * 64] = g.transpose(0, 2, 1, 3)
    return out


def attn_w_layout(w_in):
    L = w_in.shape[0]
    qa0, ka0, va0, qb0, kb0, vb0 = 0, 512, 640, 768, 1280, 1792
    sw = (np.arange(64) + 32) % 64
    cols = []
    for jc in range(4):
        for h in (jc, 4 + jc):
            cols += list(qa0 + h * 64 + np.arange(64))
    for jc in range(4):
        for h in (jc, 4 + jc):
            cols += list(qa0 + h * 64 + sw)
    for h in range(2):
        cols += list(ka0 + h * 64 + np.arange(64))
    for h in range(2):
        cols += list(ka0 + h * 64 + sw)
    cols += list(qb0 + np.arange(512))
    cols += list(kb0 + np.arange(512))
    cols = np.array(cols)
    vcols = np.concatenate([va0 + np.arange(128), vb0 + np.arange(512)])
    return np.ascontiguousarray(w_in[:, :, cols]), np.ascontiguousarray(w_in[:, :, vcols])


class B:
    def __init__(self, cfg):
        self.cfg = cfg
        self.nc = bass.Bass("TRN2", target_bir_lowering=False)
        self.kb = KB(self.nc)
        self.dram = {}
        self.uid = 0

    def din(self, name, shape, dt=F32):
        t = self.nc.dram_tensor(name, list(shape), dt, kind="ExternalInput")
        self.dram[name] = t
        return t

    def dout(self, name, shape, dt=F32):
        t = self.nc.dram_tensor(name, list(shape), dt, kind="ExternalOutput")
        self.dram[name] = t
        return t

    def dscr(self, name, shape, dt=F32):
        if self.cfg.get("debug"):
            return self.dout(name, shape, dt)
        t = self.nc.dram_tensor(name, list(shape), dt)
        self.dram[name] = t
        return t


def un(b, name):
    b.uid += 1
    return f"{name}_{b.uid}"


def _ap(x, i):
    return x(i) if callable(x) else x


def dma(p, nc, eng, chan, out, in_, r=(), w=(), slow=False):
    E = {"sp": nc.sync, "act": nc.scalar, "pool": nc.gpsimd}[eng]
    if slow:
        p.dma(eng, chan, lambda i: E.dma_start(out=_ap(out, i), in_=_ap(in_, i), allow_slow_non_contiguous=True), r=r, w=w)
    else:
        p.dma(eng, chan, lambda i: E.dma_start(out=_ap(out, i), in_=_ap(in_, i)), r=r, w=w)


def eng_of(nc, e):
    return {"dve": nc.vector, "pool": nc.gpsimd, "act": nc.scalar}[e]


def tt(p, nc, e, out, in0, in1, op, r=(), w=()):
    E = eng_of(nc, e)
    p.op(e, lambda i: E.tensor_tensor(_ap(out, i), _ap(in0, i), _ap(in1, i), op), r=r, w=w)


def ts(p, nc, e, out, in0, s1, s2, op0, op1=None, r=(), w=()):
    E = eng_of(nc, e)
    if op1 is None:
        p.op(e, lambda i: E.tensor_scalar(_ap(out, i), _ap(in0, i), s1, None, op0), r=r, w=w)
    else:
        p.op(e, lambda i: E.tensor_scalar(_ap(out, i), _ap(in0, i), s1, s2, op0, op1), r=r, w=w)


def cp(p, nc, e, out, in_, r=(), w=()):
    if e == "act":
        p.op(e, lambda i: nc.scalar.copy(_ap(out, i), _ap(in_, i)), r=r, w=w)
    else:
        E = eng_of(nc, e)
        p.op(e, lambda i: E.tensor_copy(_ap(out, i), _ap(in_, i)), r=r, w=w)


def act(p, nc, out, in_, func, bias=None, scale=None, r=(), w=()):
    kw = {}
    if bias is not None:
        kw["bias"] = bias
    if scale is not None:
        kw["scale"] = scale
    p.op("act", lambda i: nc.scalar.activation(_ap(out, i), _ap(in_, i), func, **kw), r=r, w=w)


def mm(p, nc, out, lhsT, rhs, start, stop, r=(), w=()):
    p.op("pe", lambda i: nc.tensor.matmul(_ap(out, i), lhsT=_ap(lhsT, i), rhs=_ap(rhs, i), start=start, stop=stop), r=r, w=w)


def tr(p, nc, out, in_, ident, r=(), w=()):
    p.op("pe", lambda i: nc.tensor.transpose(_ap(out, i), _ap(in_, i), ident), r=r, w=w)


def load_weight_bf16(b, p, dst, src, ncols, tag, stg, kdim=8):
    nc = b.nc
    engs = ["dve", "act", "pool"]
    c0 = 0
    k = 0
    while c0 < ncols:
        cw = min(512, ncols - c0)
        st = stg[k % 2]
        sname = f"stg{k % 2}"
        dma(p, nc, "sp" if k % 2 == 0 else "act", f"wstg{k % 2}", st[:, :, 0:cw], src[:, :, c0:c0 + cw], w=[sname])
        cp(p, nc, engs[k % 3], dst[:, :, c0:c0 + cw], st[:, :, 0:cw], r=[sname], w=[tag + str(k)])
        c0 += cw
        k += 1


def layer_norm_tail(b, p, z, zname, lng, lnb, out, outname, scr, tg):
    nc = b.nc
    sq, st = scr["sq"], scr["st"]
    p.op("dve", lambda i: nc.vector.tensor_reduce(st[:, 0:1], z[:], AX.X, ALU.add), r=[zname], w=[tg + "s0"])
    tt(p, nc, "pool", sq[:], z[:], z[:], ALU.mult, r=[zname], w=[tg + "sq"])
    p.op("dve", lambda i: nc.vector.tensor_reduce(st[:, 1:2], sq[:], AX.X, ALU.add), r=[tg + "sq"], w=[tg + "s1"])
    ts(p, nc, "dve", st[:, 2:3], st[:, 0:1], 1.0 / D, None, ALU.mult, r=[tg + "s0"], w=[tg + "s2"])
    ts(p, nc, "dve", st[:, 3:4], st[:, 1:2], 1.0 / D, None, ALU.mult, r=[tg + "s1"], w=[tg + "s3"])
    tt(p, nc, "dve", st[:, 4:5], st[:, 2:3], st[:, 2:3], ALU.mult, r=[tg + "s2"], w=[tg + "s4"])
    tt(p, nc, "dve", st[:, 5:6], st[:, 3:4], st[:, 4:5], ALU.subtract, r=[tg + "s3", tg + "s4"], w=[tg + "s5"])
    act(p, nc, st[:, 6:7], st[:, 5:6], AF.Ln, bias=b.eps_ln[:, 0:1], r=[tg + "s5"], w=[tg + "s6"])
    act(p, nc, st[:, 6:7], st[:, 6:7], AF.Exp, scale=-0.5, r=[tg + "s6"], w=[tg + "s6"])
    p.op("dve", lambda i: nc.vector.scalar_tensor_tensor(st[:, 7:8], st[:, 2:3], -1.0, st[:, 6:7], ALU.mult, ALU.mult),
         r=[tg + "s2", tg + "s6"], w=[tg + "s7"])
    act(p, nc, sq[:], z[:], AF.Identity, bias=st[:, 7:8], scale=st[:, 6:7], r=[zname, tg + "s6", tg + "s7", tg + "sq"], w=[tg + "sq"])
    tt(p, nc, "dve", sq[:], sq[:], lng[:], ALU.mult, r=[tg + "sq"], w=[tg + "sq"])
    tt(p, nc, "pool", out[:], sq[:], lnb[:], ALU.add, r=[tg + "sq"], w=[outname])


def build_program(cfg):
    seqs = cfg["seqs"]
    NT = sum(seqs)
    NTILES = NT // 128
    depth = cfg["depth"]
    LA = (depth + 1) // 2
    LR = depth // 2
    stop = cfg.get("stop")
    alpha = float((2 * cfg.get("dn_depth", depth)) ** 0.25)
    b = B(cfg)
    nc = b.nc
    kb = b.kb

    x_in = b.din("x", [NT, D])
    cT_in = b.din("cT", [128, 3, 8])
    ada_w = b.din("ada_w", [depth, D, 6 * D])
    ada_b = b.din("ada_b", [depth, 6 * D])
    ln_g = b.din("ln_g", [depth, 2, D])
    ln_b = b.din("ln_b", [depth, 2, D])
    a_wfm = b.din("attn_wfm", [LA, D, 2304])
    a_wv = b.din("attn_wv", [LA, D, 640])
    a_sink = b.din("attn_sink", [LA, 8])
    a_rb = b.din("na_rb", [LA, 128, 9, 8, 128])
    a_wout = b.din("attn_w_out", [LA, D, D])
    r_win = b.din("rec_w_in", [max(LR, 1), D, 5 * D])
    r_lb = b.din("rec_lb", [max(LR, 1), D])
    r_gn = b.din("rec_gnorm", [max(LR, 1), D])
    r_wout = b.din("rec_w_out", [max(LR, 1), D, D])
    ro_w = b.din("router_w", [depth, D, 36])
    ro_b = b.din("router_b", [depth, 36])
    e_wg = b.din("e_wg", [depth * NEXP * 128, 4096])
    e_wu = b.din("e_wu", [depth * NEXP * 128, 4096])
    e_wd = b.din("e_wd", [depth * NEXP * 128, 4096])
    cn = {}
    for name, shape, dt in [("ident_f", [128, 128], F32), ("ident_b", [128, 128], BF16), ("rope_cos", [128, NT], F32),
                            ("rope_sin", [128, NT], F32), ("wmask", [128, 2, 128], BF16), ("namask", [128, 9, 128], F32),
                            ("hmask", [64, 2, 64], BF16), ("scanmask", [128, 1024], F32), ("iota32", [128, 32], F32),
                            ("iota_blk", [128, 256], F32), ("iota_p", [128, 1], F32), ("tri_su", [128, 128], BF16),
                            ("ones_b", [128, 128], BF16)]:
        cn[name] = b.din(name, shape, dt)
    y_out = b.dout("y", [NT, D])
    X = b.dscr("X", [NT, D])
    MODT = b.dscr("MODT", [NTILES, 6 * D])
    FM = b.dscr("FM", [13, 128, NT], BF16)
    VE = b.dscr("VE", [NT, 720], BF16)

    ODBG = b.dout("ODBG", [NT, D], BF16) if cfg.get("debug") else None
    seq_tile0 = [0]
    for t in seqs[:-1]:
        seq_tile0.append(seq_tile0[-1] + t // 128)

    with ExitStack() as gs:
        def sb(name, shape, dt=F32, stack=gs):
            return stack.enter_context(nc.sbuf_tensor(un(b, name), list(shape), dt))

        def ps(name, shape, dt=F32, stack=gs):
            return stack.enter_context(nc.psum_tensor(un(b, name), list(shape), dt))

        identf = sb("identf", [128, 128])
        identb = sb("identb", [128, 128], BF16)
        CS = sb("CS", [128, 24, 128])
        epst = sb("epst", [128, 2])
        b.eps_ln = epst
        csb = sb("csb", [128, 24])
        p = Phase(kb, "setup")
        p.op("pool", lambda i: nc.gpsimd.memset(epst[:, 0:1], LN_EPS), w=["epst0"])
        p.op("pool", lambda i: nc.gpsimd.memset(epst[:, 1:2], RMS_EPS), w=["epst1"])
        dma(p, nc, "sp", "k0", identf[:], cn["ident_f"][:, :], w=["identf"])
        dma(p, nc, "sp", "k1", identb[:], cn["ident_b"][:, :], w=["identb"])
        dma(p, nc, "sp", "k2", csb[:], cT_in.ap().rearrange("p s k -> p (s k)"), w=["csb"])
        act(p, nc, csb[:], csb[:], AF.Silu, r=["csb"], w=["csb"])
        cp(p, nc, "dve", CS[:], csb[:].unsqueeze(2).to_broadcast([128, 24, 128]), r=["csb"], w=["CS"])
        p.run()

        p = Phase(kb, "xcopy")
        nchunk = max(1, NT // 1024)
        rows = NT // nchunk
        for k in range(nchunk):
            dma(p, nc, ["sp", "act", "pool"][k % 3], f"xc{k % 3}", X[k * rows:(k + 1) * rows, :], x_in[k * rows:(k + 1) * rows, :])
        p.run()

        for l in range(depth):
            with ExitStack() as st:
                aw = sb("aw", [128, 8, 512], stack=st)
                ab = sb("ab", [128, 512], stack=st)
                mo = [sb(f"mo{s}", [128, 512], stack=st) for s in range(3)]
                pm = [ps(f"pm{s}", [128, 512], stack=st) for s in range(3)]
                p = Phase(kb, f"mod{l}", n_iter=12)
                dma(p, nc, "sp", "aw", aw[:], lambda i: ada_w[l].rearrange("(kc p) n -> p kc n", p=128)[:, :, bass.ds(i * 512, 512)], w=["aw"])
                dma(p, nc, "act", "ab", ab[:], lambda i: ada_b[l:l + 1, bass.ds(i * 512, 512)].partition_broadcast(128), w=["ab"])
                for s in range(3):
                    for kc in range(8):
                        mm(p, nc, pm[s][:], CS[:, s * 8 + kc, :], aw[:, kc, :], kc == 0, kc == 7, r=["aw"], w=[f"pm{s}"])
                    tt(p, nc, "dve", mo[s][:], pm[s][:], ab[:], ALU.add, r=[f"pm{s}", "ab"], w=[f"mo{s}"])
                    nts = seqs[s] // 128
                    dma(p, nc, "pool", f"mo{s}", lambda i, s=s, nts=nts: MODT[seq_tile0[s]:seq_tile0[s] + nts, bass.ds(i * 512, 512)],
                        mo[s][0:nts, :], r=[f"mo{s}"])
                p.run()
            if stop == "mod":
                break
            if l % 2 == 0:
                build_attention_layer(b, l, l // 2, dict(X=X, MODT=MODT, FM=FM, VE=VE, a_wfm=a_wfm, a_wv=a_wv, a_sink=a_sink,
                                                         a_rb=a_rb, a_wout=a_wout, ln_g=ln_g, ln_b=ln_b, cn=cn, identb=identb,
                                                         identf=identf, alpha=alpha, seq_tile0=seq_tile0, ODBG=ODBG))
            else:
                build_hgrn_layer(b, l, l // 2, dict(X=X, MODT=MODT, r_win=r_win, r_lb=r_lb, r_gn=r_gn, r_wout=r_wout,
                                                    ln_g=ln_g, ln_b=ln_b, cn=cn, identb=identb, identf=identf, alpha=alpha,
                                                    seq_tile0=seq_tile0))
            if stop in (f"mix{l}", "attw", "A1", "attpre", "A2e", "H1", "H2"):
                break
            build_moe_layer(b, l, dict(X=X, MODT=MODT, ro_w=ro_w, ro_b=ro_b, e_wg=e_wg, e_wu=e_wu, e_wd=e_wd, ln_g=ln_g,
                                       ln_b=ln_b, cn=cn, identb=identb, identf=identf, alpha=alpha))
            if stop in (f"moe{l}", "moeD", "moeS", "moeB"):
                break

        p = Phase(kb, "ycopy")
        for k in range(nchunk):
            dma(p, nc, ["sp", "act", "pool"][k % 3], f"yc{k % 3}", y_out[k * rows:(k + 1) * rows, :], X[k * rows:(k + 1) * rows, :])
        p.run()
    return nc


def build_attention_layer(b, l, la, g):
    nc, kb = b.nc, b.kb
    seqs = b.cfg["seqs"]
    NT = sum(seqs)
    X, MODT, FM, VE, cn = g["X"], g["MODT"], g["FM"], g["VE"], g["cn"]
    identb = g["identb"]
    alpha = g["alpha"]
    seq_tile0 = g["seq_tile0"]

    with ExitStack() as st:
        def sb(name, shape, dt=F32):
            return st.enter_context(nc.sbuf_tensor(un(b, name), list(shape), dt))

        def ps(name, shape, dt=F32):
            return st.enter_context(nc.psum_tensor(un(b, name), list(shape), dt))

        wfm = sb("wfm", [128, 8, 2304], BF16)
        wv = sb("wv", [128, 8, 640], BF16)
        stg = [sb("stg0", [128, 8, 512]), sb("stg1", [128, 8, 512])]
        veall = sb("veall", [128, 4, 10, 72], BF16)
        p = Phase(kb, f"attw{l}")
        load_weight_bf16(b, p, wfm, g["a_wfm"][la].rearrange("(kc p) n -> p kc n", p=128), 2304, "wfm", stg)
        load_weight_bf16(b, p, wv, g["a_wv"][la].rearrange("(kc p) n -> p kc n", p=128), 640, "wv", stg)
        p.op("pool", lambda i: nc.gpsimd.memset(veall[:], 1.0), w=["veall"])
        p.run()
        if b.cfg.get("stop") == "attw":
            return

        xg = sb("xg", [128, 4, 1024])
        scb = sb("scb", [128, 1024])
        shb = sb("shb", [128, 1024])
        tmp = [sb("tmp0", [128, 1024]), sb("tmp1", [128, 1024])]
        hb = sb("hb", [128, 4, 1024], BF16)
        hT = sb("hT", [128, 8, 512], BF16)
        ropec = sb("ropec", [128, 512])
        ropes = sb("ropes", [128, 512])
        t1 = sb("t1", [128, 512])
        t2 = sb("t2", [128, 512])
        fmall = sb("fmall", [128, 13, 512], BF16)
        psT = [ps("psT0", [128, 8, 128], BF16), ps("psT1", [128, 8, 128], BF16)]
        pf = [ps(f"pf{k}", [128, 512]) for k in range(3)]
        pva = ps("pva", [128, 512])
        pvb = ps("pvb", [128, 512])

        NG = NT // 512
        p = Phase(kb, f"A1_{l}", n_iter=NG)
        dma(p, nc, "sp", "xg", xg[:], lambda i: X[bass.ds(i * 512, 512), :].rearrange("(t p) d -> p t d", p=128), w=["xg"])
        dma(p, nc, "act", "scb", scb[:], lambda i: MODT[bass.ds(i * 4, 1), 1024:2048].partition_broadcast(128), w=["scb"])
        dma(p, nc, "act", "shb", shb[:], lambda i: MODT[bass.ds(i * 4, 1), 0:1024].partition_broadcast(128), w=["shb"])
        dma(p, nc, "pool", "ropec", ropec[:], lambda i: cn["rope_cos"][:, bass.ds(i * 512, 512)], w=["ropec"])
        dma(p, nc, "pool", "ropes", ropes[:], lambda i: cn["rope_sin"][:, bass.ds(i * 512, 512)], w=["ropes"])
        ts(p, nc, "dve", scb[:], scb[:], 1.0, None, ALU.add, r=["scb"], w=["scb"])
        for t in range(4):
            tt(p, nc, "dve", tmp[t % 2][:], xg[:, t, :], scb[:], ALU.mult, r=["xg", "scb"], w=[f"tmp{t % 2}"])
            tt(p, nc, "pool", hb[:, t, :], tmp[t % 2][:], shb[:], ALU.add, r=[f"tmp{t % 2}", "shb"], w=[f"hb{t}"])
            for kc in range(8):
                tr(p, nc, psT[t % 2][:, kc, :], hb[:, t, kc * 128:(kc + 1) * 128], identb[:], r=[f"hb{t}"], w=[f"psT{t % 2}"])
            cp(p, nc, "act", hT[:, :, t * 128:(t + 1) * 128], psT[t % 2][:], r=[f"psT{t % 2}"], w=[f"hT{t}"])
        hTall = [f"hT{t}" for t in range(4)]
        pfi = 0
        fk = 0
        for (ca, cb_, dst) in [(0, 4, 0), (1, 5, 1), (2, 6, 2), (3, 7, 3), (8, 9, 4)]:
            ia, ib = pfi % 3, (pfi + 1) % 3
            pfi += 2
            for kc in range(8):
                mm(p, nc, pf[ia][:], wfm[:, kc, ca * 128:(ca + 1) * 128], hT[:, kc, :], kc == 0, kc == 7, r=hTall, w=[f"pf{ia}"])
            for kc in range(8):
                mm(p, nc, pf[ib][:], wfm[:, kc, cb_ * 128:(cb_ + 1) * 128], hT[:, kc, :], kc == 0, kc == 7, r=hTall, w=[f"pf{ib}"])
            tt(p, nc, "dve", t1[:], pf[ia][:], ropec[:], ALU.mult, r=[f"pf{ia}", "ropec"], w=["t1"])
            tt(p, nc, "dve", t2[:], pf[ib][:], ropes[:], ALU.mult, r=[f"pf{ib}", "ropes"], w=["t2"])
            tt(p, nc, "pool", fmall[:, dst, :], t1[:], t2[:], ALU.add, r=["t1", "t2"], w=[f"fm{dst}"])
        for c in range(10, 18):
            dst = 5 + (c - 10)
            ia = pfi % 3
            pfi += 1
            for kc in range(8):
                mm(p, nc, pf[ia][:], wfm[:, kc, c * 128:(c + 1) * 128], hT[:, kc, :], kc == 0, kc == 7, r=hTall, w=[f"pf{ia}"])
            cp(p, nc, "act", fmall[:, dst, :], pf[ia][:], r=[f"pf{ia}"], w=[f"fm{dst}"])
        for t in range(4):
            for kc in range(8):
                mm(p, nc, pva[:], hT[:, kc, t * 128:(t + 1) * 128], wv[:, kc, 128:640], kc == 0, kc == 7, r=[f"hT{t}"], w=["pva"])
            for kc in range(8):
                mm(p, nc, pvb[:, 0:128], hT[:, kc, t * 128:(t + 1) * 128], wv[:, kc, 0:128], kc == 0, kc == 7, r=[f"hT{t}"], w=["pvb"])
            cp(p, nc, "dve", veall[:, t, 2:10, 0:64], pva[:].rearrange("p (h d) -> p h d", d=64), r=["pva"], w=[f"ve{t}"])
            cp(p, nc, "act", veall[:, t, 0:2, 0:64], pvb[:, 0:128].rearrange("p (h d) -> p h d", d=64), r=["pvb"], w=[f"ve{t}"])
        dma(p, nc, "sp", "fmall", lambda i: FM[:, :, bass.ds(i * 512, 512)].rearrange("f p t -> p f t"), fmall[:],
            r=[f"fm{d_}" for d_ in range(13)])
        dma(p, nc, "pool", "veall", lambda i: VE[bass.ds(i * 512, 512), :].rearrange("(t p) c -> p t c", p=128),
            veall[:].rearrange("p t h d -> p t (h d)"), r=[f"ve{t}" for t in range(4)])
        p.run()
    if b.cfg.get("stop") == "A1":
        return

    with ExitStack() as st:
        def sb(name, shape, dt=F32):
            return st.enter_context(nc.sbuf_tensor(un(b, name), list(shape), dt))

        def ps(name, shape, dt=F32):
            return st.enter_context(nc.psum_tensor(un(b, name), list(shape), dt))

        wout = sb("wout", [128, 8, 1024], BF16)
        EB = sb("EB", [128, 9, 8, 128], BF16)
        skx = sb("skx", [128, 8])
        wm = sb("wm", [128, 2, 128], BF16)
        lng = sb("lng", [128, 1024])
        lnb = sb("lnb", [128, 1024])
        with ExitStack() as st2:
            stg = [st2.enter_context(nc.sbuf_tensor(un(b, "stg0"), [128, 8, 512], F32)),
                   st2.enter_context(nc.sbuf_tensor(un(b, "stg1"), [128, 8, 512], F32))]
            nam = st2.enter_context(nc.sbuf_tensor(un(b, "nam"), [128, 9, 128], F32))
            rbs = [st2.enter_context(nc.sbuf_tensor(un(b, "rbs0"), [128, 8, 128], F32)),
                   st2.enter_context(nc.sbuf_tensor(un(b, "rbs1"), [128, 8, 128], F32))]
            p = Phase(kb, f"attpre{l}")
            load_weight_bf16(b, p, wout, g["a_wout"][la].rearrange("(kc p) n -> p kc n", p=128), 1024, "wout", stg)
            dma(p, nc, "pool", "nam", nam[:], cn["namask"][:, :, :], w=["nam"])
            dma(p, nc, "pool", "wm", wm[:], cn["wmask"][:, :, :], w=["wm"])
            dma(p, nc, "pool", "skx", skx[:], g["a_sink"][la:la + 1, :].partition_broadcast(128), w=["skx"])
            dma(p, nc, "pool", "lng", lng[:], g["ln_g"][l, 0:1, :].partition_broadcast(128), w=["lng"])
            dma(p, nc, "pool", "lnb", lnb[:], g["ln_b"][l, 0:1, :].partition_broadcast(128), w=["lnb"])
            act(p, nc, skx[:], skx[:], AF.Exp, r=["skx"], w=["skx"])
            for t in range(9):
                rb = rbs[t % 2]
                dma(p, nc, "sp", f"rbs{t % 2}", rb[:], g["a_rb"][la, :, t, :, :], w=[f"rbs{t % 2}"])
                act(p, nc, rb[:], rb[:], AF.Exp, r=[f"rbs{t % 2}"], w=[f"rbs{t % 2}"])
                tt(p, nc, "dve", EB[:, t, :, :], rb[:], nam[:, t, :].unsqueeze(1).to_broadcast([128, 8, 128]), ALU.mult,
                   r=[f"rbs{t % 2}", "nam"], w=[f"EB{t}"])
            p.run()
        if b.cfg.get("stop") == "attpre":
            return

        qa = sb("qa", [128, 4, 128], BF16)
        ka = sb("ka", [128, 3, 128], BF16)
        va = sb("va", [128, 3, 2, 72], BF16)
        qb = sb("qb", [64, 8, 128], BF16)
        kbt = sb("kbt", [64, 8, 5, 128], BF16)
        vb = sb("vb", [128, 5, 8, 72], BF16)
        xt = sb("xt", [128, 1024])
        g1b = sb("g1b", [128, 1024])
        PT = [sb("PT0", [128, 5, 4, 128], BF16), sb("PT1", [128, 5, 4, 128], BF16)]
        o = sb("o", [128, 1024], BF16)
        oT = sb("oT", [128, 8, 128], BF16)
        t1 = sb("t1", [128, 1024])
        z = sb("z", [128, 1024])
        sq = sb("sq", [128, 1024])
        xo = sb("xo", [128, 1024])
        stt = sb("stt", [128, 8])
        den = sb("den", [128, 4, 1])
        pS = [ps("pS0", [128, 4, 128]), ps("pS1", [128, 4, 128])]
        accb = [ps("acc0", [128, 512]), ps("acc1", [128, 512])]
        acc = [a[:].rearrange("p (h d) -> p h d", d=128) for a in accb]
        psOT = ps("psOT", [128, 8, 128], BF16)
        psY = [ps("psY0", [128, 512]), ps("psY1", [128, 512])]
        o3 = o[:].rearrange("p (h d) -> p h d", d=64)

        def block(p, tok0, tile, wl, nl):
            def T(off):
                return (lambda i: (tok0(i) if callable(tok0) else tok0) + off)

            def TL(i):
                return tile(i) if callable(tile) else tile
            dma(p, nc, "sp", "qa", qa[:], lambda i: FM[0:4, :, bass.ds(T(0)(i), 128)].rearrange("f p t -> p f t"), w=["qa"])
            w0, w1 = wl[0], wl[-1]
            nw = w1 - w0 + 1
            dma(p, nc, "sp", "ka", ka[:, w0 + 1:w1 + 2, :].rearrange("p b t -> p (b t)"),
                lambda i: FM[4, :, bass.ds(T(w0 * 128)(i), nw * 128)], w=["ka"])
            dma(p, nc, "pool", "va", va[:, w0 + 1:w1 + 2, :, :].rearrange("p b h d -> p b (h d)"),
                lambda i: VE[bass.ds(T(w0 * 128)(i), nw * 128), 0:144].rearrange("(b p) c -> p b c", p=128), w=["va"])
            dma(p, nc, "sp", "qb", qb[:], lambda i: FM[5:9, :, bass.ds(T(0)(i), 128)].rearrange("f (two d) t -> d (f two) t", two=2), w=["qb"])
            n0 = nl[0][0]
            nn = len(nl)
            dma(p, nc, "act", "kbt", kbt[:, :, 0:nn, :].rearrange("p f b t -> p f (b t)"),
                lambda i: FM[9:13, :, bass.ds(T(n0 * 128)(i), nn * 128)].rearrange("f (two d) t -> d (f two) t", two=2), w=["kbt"])
            dma(p, nc, "pool", "vb", vb[:, 0:nn, :, :].rearrange("p b h d -> p b (h d)"),
                lambda i: VE[bass.ds(T(n0 * 128)(i), nn * 128), 144:720].rearrange("(b p) c -> p b c", p=128), w=["vb"])
            dma(p, nc, "sp", "xt", xt[:], lambda i: X[bass.ds(T(0)(i), 128), :], w=["xt"])
            dma(p, nc, "act", "g1b", g1b[:], lambda i: MODT[bass.ds(TL(i), 1), 2048:3072].partition_broadcast(128), w=["g1b"])
            lvl = b.cfg.get("a2lvl", 9)
            if lvl < 2:
                return
            u = 0
            for gk in range(2):
                a = acc[gk % 2]
                an = f"acc{gk % 2}"
                Pu = PT[gk % 2]
                pn = f"PT{gk % 2}_"
                for di, dl in enumerate(wl):
                    k = u % 2
                    u += 1
                    mm(p, nc, pS[k][:], ka[64 * gk:64 * gk + 64, dl + 1, :], qa[64 * gk:64 * gk + 64, :, :], True, True,
                       r=["ka", "qa"], w=[f"pS{k}"])
                    act(p, nc, Pu[:, di, :, :], pS[k][:], AF.Exp, scale=0.125, r=[f"pS{k}"], w=[pn + str(di)])
                    if dl != 0:
                        tt(p, nc, "dve", Pu[:, di, :, :], Pu[:, di, :, :],
                           wm[:, 0 if dl == -1 else 1, :].unsqueeze(1).to_broadcast([128, 4, 128]),
                           ALU.mult, r=[pn + str(di)], w=[pn + str(di)])
                for h in range(4):
                    for di, dl in enumerate(wl):
                        mm(p, nc, a[:, h, 0:66], Pu[:, di, h, :], va[:, dl + 1, gk, 0:66], di == 0, di == len(wl) - 1,
                           r=[pn + str(di), "va"], w=[an])
                tt(p, nc, "dve", den[:], a[:, :, 64:65], skx[:, 4 * gk:4 * gk + 4].unsqueeze(2), ALU.add, r=[an], w=["den"])
                p.op("dve", lambda i: nc.vector.reciprocal(den[:], den[:]), r=["den"], w=["den"])
                tt(p, nc, "dve", o3[:, 4 * gk:4 * gk + 4, :], a[:, :, 0:64], den[:].to_broadcast([128, 4, 64]), ALU.mult,
                   r=[an, "den"], w=["o"])
            if lvl < 3:
                return
            for hg in range(2):
                a = acc[hg % 2]
                an = f"acc{hg % 2}"
                Pu = PT[hg % 2]
                pn = f"PT{hg % 2}_"
                for di, (dl, tb) in enumerate(nl):
                    k = u % 2
                    u += 1
                    for uu in range(4):
                        h = 4 * hg + uu
                        mm(p, nc, pS[k][:, uu, :], kbt[:, h, di, :], qb[:, h, :],
                           True, True, r=["kbt", "qb"], w=[f"pS{k}"])
                    act(p, nc, Pu[:, di, :, :], pS[k][:], AF.Exp, scale=0.125, r=[f"pS{k}"], w=[pn + str(di)])
                    tt(p, nc, "dve" if di % 2 == 0 else "pool", Pu[:, di, :, :], Pu[:, di, :, :], EB[:, tb, 4 * hg:4 * hg + 4, :], ALU.mult,
                       r=[pn + str(di)], w=[pn + str(di)])
                for uu in range(4):
                    for di, (dl, tb) in enumerate(nl):
                        mm(p, nc, a[:, uu, 0:66], Pu[:, di, uu, :], vb[:, di, 4 * hg + uu, 0:66], di == 0, di == len(nl) - 1,
                           r=[pn + str(di), "vb"], w=[an])
                cp(p, nc, "dve", den[:], a[:, :, 64:65], r=[an], w=["den"])
                p.op("dve", lambda i: nc.vector.reciprocal(den[:], den[:]), r=["den"], w=["den"])
                tt(p, nc, "dve", o3[:, 8 + 4 * hg:8 + 4 * hg + 4, :], a[:, :, 0:64], den[:].to_broadcast([128, 4, 64]), ALU.mult,
                   r=[an, "den"], w=["o"])
            if b.cfg.get("debug"):
                dma(p, nc, "pool", "odbg", lambda i: g["ODBG"][bass.ds(T(0)(i), 128), :], o[:], r=["o"])
            if lvl < 4:
                return
            for kc in range(8):
                tr(p, nc, psOT[:, kc, :], o[:, kc * 128:(kc + 1) * 128], identb[:], r=["o"], w=["psOT"])
            cp(p, nc, "act", oT[:], psOT[:], r=["psOT"], w=["oT"])
            for n in range(2):
                for kc in range(8):
                    mm(p, nc, psY[n][:], oT[:, kc, :], wout[:, kc, n * 512:(n + 1) * 512], kc == 0, kc == 7, r=["oT"], w=[f"psY{n}"])
            if lvl < 5:
                return
            ts(p, nc, "pool", g1b[:], g1b[:], 1.0, None, ALU.add, r=["g1b"], w=["g1b"])
            for n in range(2):
                tt(p, nc, "dve", t1[:, n * 512:(n + 1) * 512], psY[n][:], g1b[:, n * 512:(n + 1) * 512], ALU.mult,
                   r=[f"psY{n}", "g1b"], w=["t1"])
            p.op("dve", lambda i: nc.vector.scalar_tensor_tensor(z[:], xt[:], alpha, t1[:], ALU.mult, ALU.add), r=["xt", "t1"], w=["z"])
            layer_norm_tail(b, p, z, "z", lng, lnb, xo, "xo", dict(sq=sq, st=stt), "ln")
            dma(p, nc, "sp", "xo", lambda i: X[bass.ds(T(0)(i), 128), :], xo[:], r=["xo"])

        FULL = lambda dls: [(d_, d_ + 3) for d_ in dls]
        for s, T_s in enumerate(seqs):
            nb = T_s // 128
            tok_s = seq_tile0[s] * 128
            p = Phase(kb, f"A2e_{l}_{s}")
            for j in sorted(set([0, 1, nb - 2, nb - 1]))[:b.cfg.get("a2nblk", 4)]:
                wl = [d_ for d_ in (-1, 0, 1) if 0 <= j + d_ < nb]
                if j == 0:
                    nl = FULL([0, 1, 2, 3])
                elif j == 1:
                    nl = FULL([-1, 0, 1, 2])
                elif j == nb - 2:
                    nl = FULL([-2, -1, 0, 1])
                else:
                    nl = FULL([-3, -2, -1, 0])
                block(p, tok_s + j * 128, seq_tile0[s] + j, wl, nl)
            p.run()
            if b.cfg.get("stop") == "A2e":
                return
            if nb > 4:
                p = Phase(kb, f"A2i_{l}_{s}", n_iter=nb - 4)
                block(p, lambda i, tok_s=tok_s: (i + 2) * 128 + tok_s, lambda i, s=s: i + (2 + seq_tile0[s]),
                      [-1, 0, 1], [(-2, 7), (-1, 2), (0, 3), (1, 4), (2, 8)])
                p.run()


def expert_layout(w, kdim):
    L, E, K, n = w.shape
    return np.ascontiguousarray(w.reshape(L, E, kdim, 128, n).transpose(0, 1, 3, 2, 4)).reshape(L * E * 128, kdim * n)


def prepare_shared(inp, cfg):
    f = lambda k: np.ascontiguousarray(np.asarray(inp[k], dtype=np.float32))
    sh = {}
    for k in ["ada_w", "ada_b", "ln_g", "ln_b", "attn_sink", "attn_w_out", "rec_w_in", "rec_lb", "rec_gnorm", "rec_w_out"]:
        sh[k] = f(k)
    wfm, wv = attn_w_layout(f("attn_w_in"))
    sh["attn_wfm"] = wfm
    sh["attn_wv"] = wv
    sh["na_rb"] = na_bias_tables(f("nat_rel_bias"))
    L = sh["ada_w"].shape[0]
    sh["router_w"] = np.ascontiguousarray(np.concatenate([f("router_w_group"), f("router_w_expert")], axis=2))
    sh["router_b"] = np.ascontiguousarray(np.concatenate([f("router_b_group"), f("router_b_expert").reshape(L, 32)], axis=1))
    sh["e_wg"] = expert_layout(f("expert_w_gate"), 8)
    sh["e_wu"] = expert_layout(f("expert_w_up"), 8)
    sh["e_wd"] = expert_layout(f("expert_w_down"), 4)
    sh.update(host_constants(cfg))
    return sh


def run_cores(inp, cfg, n_cores, pps):
    xp = np.asarray(inp["x_prompt"], dtype=np.float32)
    xs = np.asarray(inp["x_sample"], dtype=np.float32)
    cp_ = np.asarray(inp["c_prompt"], dtype=np.float32)
    cs_ = np.asarray(inp["c_sample"], dtype=np.float32)
    sh = prepare_shared(inp, cfg)
    nc = build_program(cfg)
    in_maps = []
    for c in range(n_cores):
        m = dict(sh)
        xc = np.concatenate([xp[pps * c + k] for k in range(pps)] + [xs[c]], axis=0)
        cc = np.stack([cp_[pps * c + k] for k in range(pps)] + [cs_[c]], axis=0)
        m["x"] = np.ascontiguousarray(xc)
        m["cT"] = np.ascontiguousarray(cc.reshape(3, 8, 128).transpose(2, 0, 1))
        in_maps.append(m)
    res = run_bass_kernel_spmd(nc, in_maps, core_ids=list(range(n_cores)))
    if cfg.get("debug"):
        cfg["_dbg"] = res.results
    TP = cfg["seqs"][0]
    yp = np.zeros_like(xp)
    ys = np.zeros_like(xs)
    for c in range(n_cores):
        y = np.asarray(res.results[c]["y"], dtype=np.float32)
        for k in range(pps):
            yp[pps * c + k] = y[k * TP:(k + 1) * TP]
        ys[c] = y[pps * TP:]
    return yp, ys


def kernel(**inputs):
    cfg = dict(seqs=[2048, 2048, 8192], depth=4)
    yp, ys = run_cores(inputs, cfg, 8, 2)
    return (yp, ys)


def build_moe_layer(b, l, g):
    nc, kb = b.nc, b.kb
    seqs = b.cfg["seqs"]
    NT = sum(seqs)
    NTILES = NT // 128
    NBLK = 2 * NTILES + NEXP
    X, MODT, cn = g["X"], g["MODT"], g["cn"]
    identf = g["identf"]
    alpha = g["alpha"]
    BIG = 1.0e9
    if "HF" not in b.dram:
        b.dscr("HF", [NT, D])
        b.dscr("RTD", [128, NTILES, 8])
        b.dscr("DESTD", [128, NTILES, 2], I32)
        b.dscr("IDXD", [128, NBLK], I32)
        b.dscr("XBUF", [NBLK * 128, D])
        b.dscr("YBUF", [NBLK * 128, D])
    HF, RTD, DESTD, IDXD, XBUF, YBUF = [b.dram[k] for k in ["HF", "RTD", "DESTD", "IDXD", "XBUF", "YBUF"]]

    with ExitStack() as st:
        def sb(name, shape, dt=F32):
            return st.enter_context(nc.sbuf_tensor(un(b, name), list(shape), dt))

        def ps(name, shape, dt=F32):
            return st.enter_context(nc.psum_tensor(un(b, name), list(shape), dt))

        rw = sb("rw", [128, 8, 36])
        rbb = sb("rbb", [128, 36])
        io32 = sb("io32", [128, 32])
        trib = sb("trib", [128, 128], BF16)
        oneb = sb("oneb", [128, 128], BF16)
        carry = sb("carry", [128, 32])
        zt = sb("zt", [128, 1024])
        rt = sb("rt", [128, 8])
        p = Phase(kb, f"moepre{l}")
        p.op("pool", lambda i: nc.gpsimd.memset(rt[:], 0.0), w=["rt"])
        dma(p, nc, "sp", "m0", rw[:], g["ro_w"][l].rearrange("(kc p) n -> p kc n", p=128), w=["rw"])
        dma(p, nc, "sp", "m1", rbb[:], g["ro_b"][l:l + 1, :].partition_broadcast(128), w=["rbb"])
        dma(p, nc, "sp", "m2", io32[:], cn["iota32"][:, :], w=["io32"])
        dma(p, nc, "sp", "m3", trib[:], cn["tri_su"][:, :], w=["trib"])
        dma(p, nc, "sp", "m4", oneb[:], cn["ones_b"][:, :], w=["oneb"])
        p.op("pool", lambda i: nc.gpsimd.memset(carry[:], 0.0), w=["carry"])
        p.op("pool", lambda i: nc.gpsimd.memset(zt[:], 0.0), w=["zt"])
        p.run()
        p = Phase(kb, f"moez{l}", n_iter=NBLK)
        dma(p, nc, "sp", "zf", lambda i: XBUF[bass.ds(i * 128, 128), :], zt[:])
        p.run()

        xt = sb("xt", [128, 1024])
        scb = sb("scb", [128, 1024])
        shb = sb("shb", [128, 1024])
        hf = sb("hf", [128, 1024])
        hfT = sb("hfT", [128, 8, 128])
        lg = sb("lg", [128, 36])
        sm = sb("sm", [128, 16])
        ge = sb("ge", [128, 4])
        gmask = sb("gmask", [128, 4])
        lem = sb("lem", [128, 4, 8])
        lem2 = sb("lem2", [128, 32])
        oh1 = sb("oh1", [128, 32])
        oh2 = sb("oh2", [128, 32])
        ohb = sb("ohb", [128, 32], BF16)
        tmp32 = sb("tmp32", [128, 32])
        tot = sb("tot", [128, 32])
        psA = [ps("psA0", [128, 4, 128]), ps("psA1", [128, 4, 128])]
        pl = ps("pl", [128, 512])
        pr = ps("pr", [128, 512])
        pc = ps("pc", [128, 512])
        lemf = lem[:].rearrange("p a b -> p (a b)")

        p = Phase(kb, f"moeR{l}", n_iter=NTILES)
        dma(p, nc, "sp", "xt", xt[:], lambda i: X[bass.ds(i * 128, 128), :], w=["xt"])
        dma(p, nc, "act", "scb", scb[:], lambda i: MODT[bass.ds(i, 1), 4096:5120].partition_broadcast(128), w=["scb"])
        dma(p, nc, "act", "shb", shb[:], lambda i: MODT[bass.ds(i, 1), 3072:4096].partition_broadcast(128), w=["shb"])
        ts(p, nc, "pool", scb[:], scb[:], 1.0, None, ALU.add, r=["scb"], w=["scb"])
        tt(p, nc, "dve", hf[:], xt[:], scb[:], ALU.mult, r=["xt", "scb"], w=["hf"])
        tt(p, nc, "pool", hf[:], hf[:], shb[:], ALU.add, r=["hf", "shb"], w=["hf"])
        dma(p, nc, "sp", "hfo", lambda i: HF[bass.ds(i * 128, 128), :], hf[:], r=["hf"])
        for kc in range(8):
            tr(p, nc, psA[kc // 4][:, kc % 4, :], hf[:, kc * 128:(kc + 1) * 128], identf[:], r=["hf"], w=[f"psA{kc // 4}"])
        cp(p, nc, "act", hfT[:, 0:4, :], psA[0][:], r=["psA0"], w=["hfT0"])
        cp(p, nc, "dve", hfT[:, 4:8, :], psA[1][:], r=["psA1"], w=["hfT1"])
        for kc in range(8):
            mm(p, nc, pl[:, 0:36], hfT[:, kc, :], rw[:, kc, :], kc == 0, kc == 7, r=["hfT0", "hfT1"], w=["pl"])
        tt(p, nc, "dve", lg[:], pl[:, 0:36], rbb[:], ALU.add, r=["pl"], w=["lg"])
        p.op("dve", lambda i: nc.vector.tensor_reduce(sm[:, 0:1], lg[:, 0:4], AX.X, ALU.max), r=["lg"], w=["sm0"])
        ts(p, nc, "dve", sm[:, 1:2], sm[:, 0:1], -1.0, None, ALU.mult, r=["sm0"], w=["sm1"])
        act(p, nc, ge[:], lg[:, 0:4], AF.Exp, bias=sm[:, 1:2], r=["lg", "sm1"], w=["ge"])
        p.op("dve", lambda i: nc.vector.tensor_reduce(sm[:, 2:3], ge[:], AX.X, ALU.add), r=["ge"], w=["sm2"])
        p.op("dve", lambda i: nc.vector.reciprocal(sm[:, 3:4], sm[:, 2:3]), r=["sm2"], w=["sm3"])
        tt(p, nc, "dve", gmask[:], lg[:, 0:4], sm[:, 0:1].to_broadcast([128, 4]), ALU.is_ge, r=["lg", "sm0"], w=["gmask"])
        ts(p, nc, "dve", gmask[:], gmask[:], -1.0, BIG, ALU.add, ALU.mult, r=["gmask"], w=["gmask"])
        tt(p, nc, "dve", lem[:], lg[:, 4:36].rearrange("p (a b) -> p a b", b=8), gmask[:].unsqueeze(2).to_broadcast([128, 4, 8]),
           ALU.add, r=["lg", "gmask"], w=["lem"])
        p.op("dve", lambda i: nc.vector.tensor_reduce(sm[:, 4:5], lemf, AX.X, ALU.max), r=["lem"], w=["sm4"])
        tt(p, nc, "dve", oh1[:], lemf, sm[:, 4:5].to_broadcast([128, 32]), ALU.is_ge, r=["lem", "sm4"], w=["oh1"])
        p.op("dve", lambda i: nc.vector.scalar_tensor_tensor(lem2[:], oh1[:], -BIG, lemf, ALU.mult, ALU.add), r=["oh1", "lem"], w=["lem2"])
        p.op("dve", lambda i: nc.vector.tensor_reduce(sm[:, 5:6], lem2[:], AX.X, ALU.max), r=["lem2"], w=["sm5"])
        tt(p, nc, "dve", oh2[:], lem2[:], sm[:, 5:6].to_broadcast([128, 32]), ALU.is_ge, r=["lem2", "sm5"], w=["oh2"])
        tt(p, nc, "dve", sm[:, 6:7], sm[:, 5:6], sm[:, 4:5], ALU.subtract, r=["sm4", "sm5"], w=["sm6"])
        act(p, nc, sm[:, 7:8], sm[:, 6:7], AF.Exp, r=["sm6"], w=["sm7"])
        ts(p, nc, "dve", sm[:, 8:9], sm[:, 7:8], 1.0, None, ALU.add, r=["sm7"], w=["sm8"])
        p.op("dve", lambda i: nc.vector.reciprocal(sm[:, 8:9], sm[:, 8:9]), r=["sm8"], w=["sm8"])
        tt(p, nc, "dve", rt[:, 2:3], sm[:, 8:9], sm[:, 3:4], ALU.mult, r=["sm8", "sm3"], w=["rt2"])
        tt(p, nc, "dve", rt[:, 3:4], rt[:, 2:3], sm[:, 7:8], ALU.mult, r=["rt2", "sm7"], w=["rt3"])
        tt(p, nc, "dve", tmp32[:], oh1[:], io32[:], ALU.mult, r=["oh1"], w=["tmp32"])
        p.op("dve", lambda i: nc.vector.tensor_reduce(rt[:, 0:1], tmp32[:], AX.X, ALU.add), r=["tmp32"], w=["rt0"])
        tt(p, nc, "dve", tmp32[:], oh2[:], io32[:], ALU.mult, r=["oh2", "rt0"], w=["tmp32"])
        p.op("dve", lambda i: nc.vector.tensor_reduce(rt[:, 1:2], tmp32[:], AX.X, ALU.add), r=["tmp32"], w=["rt1"])
        tt(p, nc, "pool", ohb[:], oh1[:], oh2[:], ALU.add, r=["oh1", "oh2"], w=["ohb"])
        mm(p, nc, pr[:, 0:32], trib[:], ohb[:], True, True, r=["ohb"], w=["pr"])
        mm(p, nc, pc[:, 0:32], oneb[:], ohb[:], True, True, r=["ohb"], w=["pc"])
        tt(p, nc, "dve", tot[:], pr[:, 0:32], carry[:], ALU.add, r=["pr", "carry"], w=["tot"])
        tt(p, nc, "dve", tmp32[:], oh1[:], tot[:], ALU.mult, r=["oh1", "tot", "rt1"], w=["tmp32"])
        p.op("dve", lambda i: nc.vector.tensor_reduce(rt[:, 4:5], tmp32[:], AX.X, ALU.add), r=["tmp32"], w=["rt4"])
        tt(p, nc, "dve", tmp32[:], oh2[:], tot[:], ALU.mult, r=["oh2", "tot", "rt4"], w=["tmp32"])
        p.op("dve", lambda i: nc.vector.tensor_reduce(rt[:, 5:6], tmp32[:], AX.X, ALU.add), r=["tmp32"], w=["rt5"])
        tt(p, nc, "dve", carry[:], carry[:], pc[:, 0:32], ALU.add, r=["carry", "pc", "tot"], w=["carry"])
        dma(p, nc, "act", "rto", lambda i: RTD[:, bass.ds(i, 1), :].rearrange("p o c -> p (o c)"), rt[:],
            r=["rt0", "rt1", "rt2", "rt3", "rt4", "rt5"])
        p.run()

        rta = sb("rta", [128, NTILES, 8])
        cnt = sb("cnt", [128, 32])
        cnti = sb("cnti", [128, 32], I32)
        pend = sb("pend", [128, 32])
        pst = sb("pst", [128, 32])
        onesf = sb("onesf", [128, 32])
        dacc = sb("dacc", [128, NTILES, 2])
        dtmp = sb("dtmp", [128, NTILES, 2])
        dint = sb("dint", [128, NTILES, 2], I32)
        ioblk = sb("ioblk", [128, NBLK])
        bacc = sb("bacc", [128, NBLK])
        btmp = sb("btmp", [128, NBLK])
        iop = sb("iop", [128, 1])
        bint = sb("bint", [128, NBLK], I32)
        p = Phase(kb, f"moeD{l}")
        dma(p, nc, "sp", "d0", rta[:], RTD[:, :, :], w=["rta"])
        dma(p, nc, "sp", "d1", ioblk[:], cn["iota_blk"][:, 0:NBLK], w=["ioblk"])
        dma(p, nc, "sp", "d2", iop[:], cn["iota_p"][:, :], w=["iop"])
        p.op("pool", lambda i: nc.gpsimd.memset(onesf[:], 1.0), w=["onesf"])
        ts(p, nc, "dve", cnt[:], carry[:], 1.0 / 128.0, 0.49609375, ALU.mult, ALU.add, w=["cnt"])
        cp(p, nc, "dve", cnti[:], cnt[:], r=["cnt"], w=["cnti"])
        cp(p, nc, "dve", cnt[:], cnti[:], r=["cnti"], w=["cnt"])
        ts(p, nc, "dve", cnt[:], cnt[:], 128.0, None, ALU.mult, r=["cnt"], w=["cnt"])
        p.op("dve", lambda i: nc.vector.tensor_tensor_scan(pend[:], onesf[:], cnt[:], 0.0, ALU.mult, ALU.add), r=["onesf", "cnt"], w=["pend"])
        tt(p, nc, "dve", pst[:], pend[:], cnt[:], ALU.subtract, r=["pend", "cnt"], w=["pst"])
        cp(p, nc, "dve", dacc[:], rta[:, :, 4:6], r=["rta"], w=["dacc"])
        for e in range(NEXP):
            ts(p, nc, "dve", dtmp[:], rta[:, :, 0:2], float(e), None, ALU.is_equal, r=["rta", "dacc"], w=["dtmp"])
            p.op("dve", lambda i, e=e: nc.vector.scalar_tensor_tensor(dacc[:], dtmp[:], pst[:, e:e + 1], dacc[:], ALU.mult, ALU.add),
                 r=["dtmp", "pst", "dacc"], w=["dacc"])
        cp(p, nc, "dve", dint[:], dacc[:], r=["dacc"], w=["dint"])
        dma(p, nc, "sp", "d3", DESTD[:, :, :], dint[:], r=["dint"])
        p.op("pool", lambda i: nc.gpsimd.memset(bacc[:], 0.0), w=["bacc"])
        for e in range(NEXP):
            ts(p, nc, "dve", btmp[:], ioblk[:], pend[:, e:e + 1], None, ALU.is_ge, r=["ioblk", "pend", "bacc"], w=["btmp"])
            tt(p, nc, "dve", bacc[:], bacc[:], btmp[:], ALU.add, r=["bacc", "btmp"], w=["bacc"])
        ts(p, nc, "dve", bacc[:], bacc[:], float(NEXP - 1), None, ALU.min, r=["bacc"], w=["bacc"])
        ts(p, nc, "dve", bacc[:], bacc[:], 128.0, float(l * NEXP * 128), ALU.mult, ALU.add, r=["bacc"], w=["bacc"])
        tt(p, nc, "dve", bacc[:], bacc[:], iop[:].to_broadcast([128, NBLK]), ALU.add, r=["bacc", "iop"], w=["bacc"])
        cp(p, nc, "dve", bint[:], bacc[:], r=["bacc"], w=["bint"])
        dma(p, nc, "sp", "d4", IDXD[:, :], bint[:], r=["bint"])
        p.run()
    if b.cfg.get("stop") == "moeD":
        return

    with ExitStack() as st:
        def sb(name, shape, dt=F32):
            return st.enter_context(nc.sbuf_tensor(un(b, name), list(shape), dt))
        hf = sb("hf", [128, 1024])
        di = sb("di", [128, 2], I32)
        p = Phase(kb, f"moeS{l}", n_iter=NTILES)
        dma(p, nc, "sp", "hf", hf[:], lambda i: HF[bass.ds(i * 128, 128), :], w=["hf"])
        dma(p, nc, "act", "di", di[:], lambda i: DESTD[:, bass.ds(i, 1), :].rearrange("p o c -> p (o c)"), w=["di"])
        for k in range(2):
            p.dma("pool", f"sc{k}", lambda i, k=k: nc.gpsimd.indirect_dma_start(
                out=XBUF[:, :], out_offset=bass.IndirectOffsetOnAxis(ap=di[:, k:k + 1], axis=0), in_=hf[:], in_offset=None),
                r=["hf", "di"])
        p.run()
    if b.cfg.get("stop") == "moeS":
        return

    with ExitStack() as st:
        def sb(name, shape, dt=F32):
            return st.enter_context(nc.sbuf_tensor(un(b, name), list(shape), dt))

        def ps(name, shape, dt=F32):
            return st.enter_context(nc.psum_tensor(un(b, name), list(shape), dt))
        ix = sb("ix", [128, 1], I32)
        wgf = sb("wgf", [128, 4096])
        wuf = sb("wuf", [128, 4096])
        wdf = sb("wdf", [128, 4096])
        wgb = sb("wgb", [128, 8, 512], BF16)
        wub = sb("wub", [128, 8, 512], BF16)
        wdb = sb("wdb", [128, 4, 1024], BF16)
        xb = sb("xb", [128, 1024])
        xT = sb("xT", [128, 8, 128], BF16)
        sg = sb("sg", [128, 4, 128])
        hT = sb("hT", [128, 4, 128], BF16)
        yb = sb("yb", [128, 1024])
        psA = [ps("psA0", [128, 4, 128]), ps("psA1", [128, 4, 128])]
        psG = ps("psG", [128, 4, 128])
        psU = ps("psU", [128, 4, 128])
        psY = [ps("psY0", [128, 512]), ps("psY1", [128, 512])]
        p = Phase(kb, f"moeB{l}", n_iter=NBLK)
        dma(p, nc, "sp", "ix", ix[:], lambda i: IDXD[:, bass.ds(i, 1)], w=["ix"], slow=True)
        dma(p, nc, "sp", "xb", xb[:], lambda i: XBUF[bass.ds(i * 128, 128), :], w=["xb"])
        for nm, dst, src in [("wgf", wgf, g["e_wg"]), ("wuf", wuf, g["e_wu"]), ("wdf", wdf, g["e_wd"])]:
            p.dma("pool", nm, lambda i, dst=dst, src=src: nc.gpsimd.indirect_dma_start(
                out=dst[:], out_offset=None, in_=src[:, :], in_offset=bass.IndirectOffsetOnAxis(ap=ix[:, 0:1], axis=0)),
                r=["ix"], w=[nm])
        cp(p, nc, "dve", wgb[:].rearrange("p k n -> p (k n)"), wgf[:], r=["wgf"], w=["wgb"])
        cp(p, nc, "act", wub[:].rearrange("p k n -> p (k n)"), wuf[:], r=["wuf"], w=["wub"])
        cp(p, nc, "pool", wdb[:].rearrange("p k n -> p (k n)"), wdf[:], r=["wdf"], w=["wdb"])
        for kc in range(8):
            tr(p, nc, psA[kc // 4][:, kc % 4, :], xb[:, kc * 128:(kc + 1) * 128], identf[:], r=["xb"], w=[f"psA{kc // 4}"])
        cp(p, nc, "act", xT[:, 0:4, :], psA[0][:], r=["psA0"], w=["xT0"])
        cp(p, nc, "dve", xT[:, 4:8, :], psA[1][:], r=["psA1"], w=["xT1"])
        for c in range(4):
            for kc in range(8):
                mm(p, nc, psG[:, c, :], wgb[:, kc, c * 128:(c + 1) * 128], xT[:, kc, :], kc == 0, kc == 7, r=["wgb", "xT0", "xT1"], w=["psG"])
        for c in range(4):
            for kc in range(8):
                mm(p, nc, psU[:, c, :], wub[:, kc, c * 128:(c + 1) * 128], xT[:, kc, :], kc == 0, kc == 7, r=["wub", "xT0", "xT1"], w=["psU"])
        act(p, nc, sg[:], psG[:], AF.Silu, r=["psG"], w=["sg"])
        tt(p, nc, "dve", hT[:], sg[:], psU[:], ALU.mult, r=["sg", "psU"], w=["hT"])
        for n in range(2):
            for c in range(4):
                mm(p, nc, psY[n][:], hT[:, c, :], wdb[:, c, n * 512:(n + 1) * 512], c == 0, c == 3, r=["hT", "wdb"], w=[f"psY{n}"])
        cp(p, nc, "act", yb[:, 0:512], psY[0][:], r=["psY0"], w=["yb0"])
        cp(p, nc, "dve", yb[:, 512:1024], psY[1][:], r=["psY1"], w=["yb1"])
        dma(p, nc, "sp", "yb", lambda i: YBUF[bass.ds(i * 128, 128), :], yb[:], r=["yb0", "yb1"])
        p.run()
    if b.cfg.get("stop") == "moeB":
        return

    with ExitStack() as st:
        def sb(name, shape, dt=F32):
            return st.enter_context(nc.sbuf_tensor(un(b, name), list(shape), dt))
        lng = sb("lng", [128, 1024])
        lnb = sb("lnb", [128, 1024])
        p = Phase(kb, f"moeCpre{l}")
        dma(p, nc, "sp", "c0", lng[:], g["ln_g"][l, 1:2, :].partition_broadcast(128), w=["lng"])
        dma(p, nc, "sp", "c1", lnb[:], g["ln_b"][l, 1:2, :].partition_broadcast(128), w=["lnb"])
        p.run()
        xt = sb("xt", [128, 1024])
        g2b = sb("g2b", [128, 1024])
        rt = sb("rt", [128, 8])
        di = sb("di", [128, 2], I32)
        y0 = sb("y0", [128, 1024])
        y1 = sb("y1", [128, 1024])
        z = sb("z", [128, 1024])
        sq = sb("sq", [128, 1024])
        xo = sb("xo", [128, 1024])
        stt = sb("stt", [128, 8])
        p = Phase(kb, f"moeC{l}", n_iter=NTILES)
        dma(p, nc, "sp", "xt", xt[:], lambda i: X[bass.ds(i * 128, 128), :], w=["xt"])
        dma(p, nc, "act", "g2b", g2b[:], lambda i: MODT[bass.ds(i, 1), 5120:6144].partition_broadcast(128), w=["g2b"])
        dma(p, nc, "act", "rt", rt[:], lambda i: RTD[:, bass.ds(i, 1), :].rearrange("p o c -> p (o c)"), w=["rt"])
        dma(p, nc, "act", "di", di[:], lambda i: DESTD[:, bass.ds(i, 1), :].rearrange("p o c -> p (o c)"), w=["di"])
        for k, yk in enumerate([y0, y1]):
            p.dma("pool", f"ga{k}", lambda i, k=k, yk=yk: nc.gpsimd.indirect_dma_start(
                out=yk[:], out_offset=None, in_=YBUF[:, :], in_offset=bass.IndirectOffsetOnAxis(ap=di[:, k:k + 1], axis=0)),
                r=["di"], w=[f"y{k}"])
        ts(p, nc, "dve", y0[:], y0[:], rt[:, 2:3], None, ALU.mult, r=["y0", "rt"], w=["y0"])
        p.op("dve", lambda i: nc.vector.scalar_tensor_tensor(y0[:], y1[:], rt[:, 3:4], y0[:], ALU.mult, ALU.add), r=["y0", "y1", "rt"], w=["y0"])
        ts(p, nc, "pool", g2b[:], g2b[:], 1.0, None, ALU.add, r=["g2b"], w=["g2b"])
        tt(p, nc, "pool", y0[:], y0[:], g2b[:], ALU.mult, r=["y0", "g2b"], w=["y0"])
        p.op("dve", lambda i: nc.vector.scalar_tensor_tensor(z[:], xt[:], alpha, y0[:], ALU.mult, ALU.add), r=["xt", "y0"], w=["z"])
        layer_norm_tail(b, p, z, "z", lng, lnb, xo, "xo", dict(sq=sq, st=stt), "ln")
        dma(p, nc, "sp", "xo", lambda i: X[bass.ds(i * 128, 128), :], xo[:], r=["xo"])
        p.run()


def build_hgrn_layer(b, l, lr, g):
    nc, kb = b.nc, b.kb
    seqs = b.cfg["seqs"]
    NT = sum(seqs)
    NTILES = NT // 128
    depth = b.cfg["depth"]
    LR = depth // 2
    X, MODT, cn = g["X"], g["MODT"], g["cn"]
    identb = g["identb"]
    alpha = g["alpha"]
    seq_tile0 = g["seq_tile0"]
    if "QZ" not in b.dram:
        b.dscr("QZ", [24, 128, NT])
        b.dscr("VT", [NT, D], BF16)
        b.dscr("GT", [NT, D])
        b.dscr("OF", [NT, D])
        b.dscr("OB", [NT, D])
    QZ, VT, GT, OF, OB = [b.dram[k] for k in ["QZ", "VT", "GT", "OF", "OB"]]

    with ExitStack() as st:
        def sb(name, shape, dt=F32):
            return st.enter_context(nc.sbuf_tensor(un(b, name), list(shape), dt))

        def ps(name, shape, dt=F32):
            return st.enter_context(nc.psum_tensor(un(b, name), list(shape), dt))

        win = sb("win", [128, 8, 5120], BF16)
        with ExitStack() as st2:
            stg = [st2.enter_context(nc.sbuf_tensor(un(b, "stg0"), [128, 8, 512], F32)),
                   st2.enter_context(nc.sbuf_tensor(un(b, "stg1"), [128, 8, 512], F32))]
            p = Phase(kb, f"recw{l}")
            load_weight_bf16(b, p, win, g["r_win"][lr].rearrange("(kc p) n -> p kc n", p=128), 5120, "win", stg)
            p.run()
        xg = sb("xg", [128, 4, 1024])
        scb = sb("scb", [128, 1024])
        shb = sb("shb", [128, 1024])
        tmp = [sb("tmp0", [128, 1024]), sb("tmp1", [128, 1024])]
        hb = sb("hb", [128, 4, 1024], BF16)
        hT = sb("hT", [128, 8, 512], BF16)
        fq = sb("fq", [128, 8, 512])
        vo = sb("vo", [128, 4, 1024], BF16)
        go = sb("go", [128, 4, 1024])
        psT = [ps("psT0", [128, 8, 128], BF16), ps("psT1", [128, 8, 128], BF16)]
        pf = [ps(f"pf{k}", [128, 512]) for k in range(4)]
        NG = NT // 512
        p = Phase(kb, f"H1_{l}", n_iter=NG)
        dma(p, nc, "sp", "xg", xg[:], lambda i: X[bass.ds(i * 512, 512), :].rearrange("(t p) d -> p t d", p=128), w=["xg"])
        dma(p, nc, "act", "scb", scb[:], lambda i: MODT[bass.ds(i * 4, 1), 1024:2048].partition_broadcast(128), w=["scb"])
        dma(p, nc, "act", "shb", shb[:], lambda i: MODT[bass.ds(i * 4, 1), 0:1024].partition_broadcast(128), w=["shb"])
        ts(p, nc, "dve", scb[:], scb[:], 1.0, None, ALU.add, r=["scb"], w=["scb"])
        for t in range(4):
            tt(p, nc, "dve", tmp[t % 2][:], xg[:, t, :], scb[:], ALU.mult, r=["xg", "scb"], w=[f"tmp{t % 2}"])
            tt(p, nc, "pool", hb[:, t, :], tmp[t % 2][:], shb[:], ALU.add, r=[f"tmp{t % 2}", "shb"], w=[f"hb{t}"])
            for kc in range(8):
                tr(p, nc, psT[t % 2][:, kc, :], hb[:, t, kc * 128:(kc + 1) * 128], identb[:], r=[f"hb{t}"], w=[f"psT{t % 2}"])
            cp(p, nc, "act", hT[:, :, t * 128:(t + 1) * 128], psT[t % 2][:], r=[f"psT{t % 2}"], w=[f"hT{t}"])
        hTall = [f"hT{t}" for t in range(4)]
        pfi = 0
        for grp in range(3):
            for hh in range(8):
                c = grp * 8 + hh
                ia = pfi % 4
                pfi += 1
                for kc in range(8):
                    mm(p, nc, pf[ia][:], win[:, kc, c * 128:(c + 1) * 128], hT[:, kc, :], kc == 0, kc == 7, r=hTall, w=[f"pf{ia}"])
                cp(p, nc, "act" if hh % 2 == 0 else "dve", fq[:, hh, :], pf[ia][:], r=[f"pf{ia}"], w=[f"fq{hh}"])
            dma(p, nc, ["sp", "act", "pool"][grp], f"fq{grp}",
                lambda i, grp=grp: QZ[grp * 8:grp * 8 + 8, :, bass.ds(i * 512, 512)].rearrange("f p t -> p f t"), fq[:],
                r=[f"fq{hh}" for hh in range(8)])
        for t in range(4):
            for n in range(4):
                ia = pfi % 4
                pfi += 1
                col0 = 3072 + n * 512
                for kc in range(8):
                    mm(p, nc, pf[ia][:], hT[:, kc, t * 128:(t + 1) * 128], win[:, kc, col0:col0 + 512], kc == 0, kc == 7,
                       r=[f"hT{t}"], w=[f"pf{ia}"])
                if n < 2:
                    cp(p, nc, "act", vo[:, t, n * 512:(n + 1) * 512], pf[ia][:], r=[f"pf{ia}"], w=[f"vo{t}"])
                else:
                    cp(p, nc, "dve", go[:, t, (n - 2) * 512:(n - 1) * 512], pf[ia][:], r=[f"pf{ia}"], w=[f"go{t}"])
        dma(p, nc, "sp", "vo", lambda i: VT[bass.ds(i * 512, 512), :].rearrange("(t p) e -> p t e", p=128), vo[:],
            r=[f"vo{t}" for t in range(4)])
        dma(p, nc, "pool", "go", lambda i: GT[bass.ds(i * 512, 512), :].rearrange("(t p) e -> p t e", p=128), go[:],
            r=[f"go{t}" for t in range(4)])
        p.run()
    if b.cfg.get("stop") == "H1":
        return

    with ExitStack() as st:
        def sb(name, shape, dt=F32):
            return st.enter_context(nc.sbuf_tensor(un(b, name), list(shape), dt))

        def ps(name, shape, dt=F32):
            return st.enter_context(nc.psum_tensor(un(b, name), list(shape), dt))

        lbT = sb("lbT", [128, 8])
        omlT = sb("omlT", [128, 8])
        lbe = sb("lbe", [128, max(LR, 1), 8])
        lsum = sb("lsum", [128, 8])
        hm = sb("hm", [64, 2, 64], BF16)
        smask = sb("smask", [128, 1024])
        S = sb("S", [128, 8, 128])
        p = Phase(kb, f"recpre{l}")
        for j in range(LR):
            dma(p, nc, "sp", f"lb{j}", lbe[:, j, :], g["r_lb"][j].rearrange("(h k) -> k h", k=128), w=[f"lbe{j}"], slow=True)
            act(p, nc, lbe[:, j, :], lbe[:, j, :], AF.Exp, r=[f"lbe{j}"], w=[f"lbe{j}"])
        cp(p, nc, "dve", lsum[:], lbe[:, 0, :], r=["lbe0"], w=["lsum"])
        for j in range(1, LR):
            tt(p, nc, "dve", lsum[:], lsum[:], lbe[:, j, :], ALU.add, r=["lsum", f"lbe{j}"], w=["lsum"])
        p.op("dve", lambda i: nc.vector.reciprocal(lsum[:], lsum[:]), r=["lsum"], w=["lsum"])
        p.op("pool", lambda i: nc.gpsimd.memset(lbT[:], 0.0), w=["lbT"])
        for j in range(1, lr + 1):
            tt(p, nc, "dve", lbT[:], lbT[:], lbe[:, j, :], ALU.add, r=["lbT", f"lbe{j}"], w=["lbT"])
        tt(p, nc, "dve", lbT[:], lbT[:], lsum[:], ALU.mult, r=["lbT", "lsum"], w=["lbT"])
        ts(p, nc, "dve", omlT[:], lbT[:], -1.0, 1.0, ALU.mult, ALU.add, r=["lbT"], w=["omlT"])
        dma(p, nc, "sp", "hm", hm[:], cn["hmask"][:, :, :], w=["hm"])
        dma(p, nc, "sp", "smask", smask[:], cn["scanmask"][:, :], w=["smask"])
        p.run()

        qt = sb("qt", [128, 8, 128])
        zt = sb("zt", [128, 8, 128])
        vt = sb("vt", [64, 2, 1024], BF16)
        u = sb("u", [128, 8, 128])
        w_ = sb("w_", [128, 8, 128])
        f_ = sb("f_", [128, 8, 128])
        lf = sb("lf", [128, 8, 128])
        km = sb("km", [128, 8, 128])
        bb = sb("bb", [128, 8, 128])
        bm = sb("bm", [128, 8, 128])
        Ee = sb("Ee", [128, 8, 128])
        Ei = sb("Ei", [128, 8, 128])
        qe = sb("qe", [128, 8, 128], BF16)
        ke = sb("ke", [128, 8, 128], BF16)
        sc = sb("sc", [128, 8, 2, 4])
        Sc = sb("Sc", [128, 8, 128], BF16)
        AT = sb("AT", [64, 8, 64], BF16)
        keT = sb("keT", [64, 8, 128], BF16)
        oc = sb("oc", [64, 1024])
        psA = ps("psA", [64, 8, 64])
        psKT = ps("psKT", [64, 8, 128], BF16)
        pso = [ps("pso0", [64, 512]), ps("pso1", [64, 512])]
        psdS = [ps("psdS0", [128, 4, 128]), ps("psdS1", [128, 4, 128])]
        lb_bc = lbT[:].unsqueeze(2).to_broadcast([128, 8, 128])
        oml_bc = omlT[:].unsqueeze(2).to_broadcast([128, 8, 128])
        fl = lambda t_: t_[:].rearrange("p h t -> p (h t)")
        c4 = lambda t_: t_[:].rearrange("p h (c t) -> p h c t", t=64)

        def rec_body(p, tok0, dirn, OUT):
            zbase = 8 if dirn == 0 else 16
            dma(p, nc, "sp", "qt", qt[:], lambda i: QZ[0:8, :, bass.ds(tok0(i), 128)].rearrange("f p t -> p f t"), w=["qt"])
            dma(p, nc, "act", "zt", zt[:], lambda i: QZ[zbase:zbase + 8, :, bass.ds(tok0(i), 128)].rearrange("f p t -> p f t"), w=["zt"])
            dma(p, nc, "pool", "vt", vt[:], lambda i: VT[bass.ds(tok0(i), 128), :].rearrange("(c p) e -> p c e", p=64), w=["vt"])
            act(p, nc, u[:], zt[:], AF.Exp, scale=-1.0, r=["zt"], w=["u"])
            ts(p, nc, "dve", w_[:], u[:], 1.0, None, ALU.add, r=["u"], w=["w_"])
            p.op("dve", lambda i: nc.vector.reciprocal(w_[:], w_[:]), r=["w_"], w=["w_"])
            tt(p, nc, "dve", f_[:], w_[:], oml_bc, ALU.mult, r=["w_"], w=["f_"])
            tt(p, nc, "dve", f_[:], f_[:], lb_bc, ALU.add, r=["f_"], w=["f_"])
            act(p, nc, lf[:], f_[:], AF.Ln, r=["f_"], w=["lf"])
            tt(p, nc, "pool", km[:], u[:], w_[:], ALU.mult, r=["u", "w_"], w=["km"])
            tt(p, nc, "pool", km[:], km[:], oml_bc, ALU.mult, r=["km"], w=["km"])
            p.op("dve", lambda i: nc.vector.tensor_tensor_scan(fl(bb), smask[:], fl(lf), 0.0, ALU.mult, ALU.add), r=["lf"], w=["bb"])
            if dirn == 1:
                tt(p, nc, "pool", bm[:], lf[:], bb[:], ALU.subtract, r=["lf", "bb"], w=["bm"])
                tt(p, nc, "pool", c4(bb), c4(bm), c4(bb)[:, :, :, 63:64].to_broadcast([128, 8, 2, 64]), ALU.add, r=["bm", "bb"], w=["bb"])
            endidx = 63 if dirn == 0 else 0
            cref = c4(bb)[:, :, :, 31:32]
            bend = c4(bb)[:, :, :, endidx:endidx + 1]
            act(p, nc, sc[:, :, :, 0:1], cref, AF.Exp, r=["bb"], w=["sc0"])
            act(p, nc, sc[:, :, :, 1:2], bend, AF.Exp, r=["bb"], w=["sc1"])
            tt(p, nc, "dve", sc[:, :, :, 3:4], bend, cref, ALU.subtract, r=["bb"], w=["sc3"])
            act(p, nc, sc[:, :, :, 2:3], sc[:, :, :, 3:4], AF.Exp, r=["sc3"], w=["sc2"])
            tt(p, nc, "pool", c4(bm), c4(bb), cref.to_broadcast([128, 8, 2, 64]), ALU.subtract, r=["bb", "bm"], w=["bm"])
            act(p, nc, Ee[:], bm[:], AF.Exp, r=["bm"], w=["Ee"])
            act(p, nc, Ei[:], bm[:], AF.Exp, scale=-1.0, r=["bm"], w=["Ei"])
            tt(p, nc, "dve", qe[:], qt[:], Ee[:], ALU.mult, r=["qt", "Ee"], w=["qe"])
            tt(p, nc, "pool", ke[:], km[:], Ei[:], ALU.mult, r=["km", "Ei"], w=["ke"])
            order = [0, 1] if dirn == 0 else [1, 0]
            for c in order:
                cs_ = slice(c * 64, (c + 1) * 64)
                for h in range(8):
                    e1 = "act" if h % 2 == 0 else "pool"
                    p.op("act", lambda i, h=h, c=c: nc.scalar.activation(Sc[:, h, :], S[:, h, :], AF.Copy, scale=sc[:, h, c, 0:1]),
                         r=[f"S{h}", "sc0"], w=[f"Sc{h}"])
                    mm(p, nc, psA[:, h, :], ke[:, h, cs_], qe[:, h, cs_], True, True, r=["ke", "qe"], w=["psA"])
                    tt(p, nc, "dve", AT[:, h, :], psA[:, h, :], hm[:, dirn, :], ALU.mult, r=["psA"], w=[f"AT{h}"])
                    tr(p, nc, psKT[:, h, :], ke[:, h, cs_], identb[:], r=["ke"], w=["psKT"])
                    cp(p, nc, "dve" if h % 2 == 0 else "act", keT[:, h, :], psKT[:, h, :], r=["psKT"], w=[f"keT{h}"])
                    po = pso[h // 4][:, (h % 4) * 128:(h % 4 + 1) * 128]
                    mm(p, nc, po, AT[:, h, :], vt[:, c, h * 128:(h + 1) * 128], True, False, r=[f"AT{h}", "vt"], w=[f"pso{h // 4}"])
                    mm(p, nc, po, qe[:, h, cs_], Sc[:, h, :], False, True, r=["qe", f"Sc{h}"], w=[f"pso{h // 4}"])
                    mm(p, nc, psdS[h // 4][:, h % 4, :], keT[:, h, :], vt[:, c, h * 128:(h + 1) * 128], True, True,
                       r=[f"keT{h}", "vt"], w=[f"psdS{h // 4}"])
                    ts(p, nc, "dve", S[:, h, :], S[:, h, :], sc[:, h, c, 1:2], None, ALU.mult, r=[f"S{h}", "sc1"], w=[f"S{h}"])
                    p.op("dve", lambda i, h=h, c=c: nc.vector.scalar_tensor_tensor(S[:, h, :], psdS[h // 4][:, h % 4, :], sc[:, h, c, 2:3],
                                                                                    S[:, h, :], ALU.mult, ALU.add),
                         r=[f"psdS{h // 4}", f"S{h}", "sc2"], w=[f"S{h}"])
                cp(p, nc, "act", oc[:, 0:512], pso[0][:], r=["pso0"], w=["oc0"])
                cp(p, nc, "dve", oc[:, 512:1024], pso[1][:], r=["pso1"], w=["oc1"])
                dma(p, nc, "sp", f"oc", lambda i, c=c: OUT[bass.ds(tok0(i) + c * 64, 64), :], oc[:], r=["oc0", "oc1"])

        for s, T_s in enumerate(seqs):
            nb = T_s // 128
            tok_s = seq_tile0[s] * 128
            for dirn in range(2):
                p = Phase(kb, f"recz{l}_{s}_{dirn}")
                p.op("pool", lambda i: nc.gpsimd.memset(S[:], 0.0), w=["S"])
                p.run()
                p = Phase(kb, f"H2_{l}_{s}_{dirn}", n_iter=nb)
                if dirn == 0:
                    rec_body(p, lambda i, tok_s=tok_s: i * 128 + tok_s, 0, OF)
                else:
                    rec_body(p, lambda i, tok_s=tok_s, nb=nb: (tok_s + (nb - 1) * 128) - i * 128, 1, OB)
                p.run()
    if b.cfg.get("stop") == "H2":
        return

    with ExitStack() as st:
        def sb(name, shape, dt=F32):
            return st.enter_context(nc.sbuf_tensor(un(b, name), list(shape), dt))

        def ps(name, shape, dt=F32):
            return st.enter_context(nc.psum_tensor(un(b, name), list(shape), dt))
        wout = sb("wout", [128, 8, 1024], BF16)
        gnb = sb("gnb", [128, 1024])
        lng = sb("lng", [128, 1024])
        lnb = sb("lnb", [128, 1024])
        with ExitStack() as st2:
            stg = [st2.enter_context(nc.sbuf_tensor(un(b, "stg0"), [128, 8, 512], F32)),
                   st2.enter_context(nc.sbuf_tensor(un(b, "stg1"), [128, 8, 512], F32))]
            p = Phase(kb, f"recw2{l}")
            load_weight_bf16(b, p, wout, g["r_wout"][lr].rearrange("(kc p) n -> p kc n", p=128), 1024, "wout", stg)
            dma(p, nc, "pool", "gnb", gnb[:], g["r_gn"][lr:lr + 1, :].partition_broadcast(128), w=["gnb"])
            dma(p, nc, "pool", "lng", lng[:], g["ln_g"][l, 0:1, :].partition_broadcast(128), w=["lng"])
            dma(p, nc, "pool", "lnb", lnb[:], g["ln_b"][l, 0:1, :].partition_broadcast(128), w=["lnb"])
            p.run()
        of = sb("of", [128, 1024])
        ob = sb("ob", [128, 1024])
        gt = sb("gt", [128, 1024])
        xt = sb("xt", [128, 1024])
        g1b = sb("g1b", [128, 1024])
        sq = sb("sq", [128, 1024])
        ms = sb("ms", [128, 8])
        on = sb("on", [128, 1024], BF16)
        onT = sb("onT", [128, 8, 128], BF16)
        t1 = sb("t1", [128, 1024])
        z = sb("z", [128, 1024])
        xo = sb("xo", [128, 1024])
        stt = sb("stt", [128, 8])
        psOT = ps("psOT", [128, 8, 128], BF16)
        psY = [ps("psY0", [128, 512]), ps("psY1", [128, 512])]
        p = Phase(kb, f"H3_{l}", n_iter=NTILES)
        dma(p, nc, "sp", "of", of[:], lambda i: OF[bass.ds(i * 128, 128), :], w=["of"])
        dma(p, nc, "act", "ob", ob[:], lambda i: OB[bass.ds(i * 128, 128), :], w=["ob"])
        dma(p, nc, "pool", "gt", gt[:], lambda i: GT[bass.ds(i * 128, 128), :], w=["gt"])
        dma(p, nc, "sp", "xt", xt[:], lambda i: X[bass.ds(i * 128, 128), :], w=["xt"])
        dma(p, nc, "act", "g1b", g1b[:], lambda i: MODT[bass.ds(i, 1), 2048:3072].partition_broadcast(128), w=["g1b"])
        tt(p, nc, "dve", of[:], of[:], ob[:], ALU.add, r=["of", "ob"], w=["of"])
        tt(p, nc, "pool", sq[:], of[:], of[:], ALU.mult, r=["of"], w=["sq"])
        p.op("dve", lambda i: nc.vector.tensor_reduce(ms[:], sq[:].rearrange("p (h v) -> p h v", v=128), AX.X, ALU.add), r=["sq"], w=["ms"])
        ts(p, nc, "dve", ms[:], ms[:], 1.0 / 128.0, None, ALU.mult, r=["ms"], w=["ms"])
        act(p, nc, ms[:], ms[:], AF.Ln, bias=b.eps_ln[:, 1:2], r=["ms"], w=["ms"])
        act(p, nc, ms[:], ms[:], AF.Exp, scale=-0.5, r=["ms"], w=["ms"])
        tt(p, nc, "dve", sq[:].rearrange("p (h v) -> p h v", v=128), of[:].rearrange("p (h v) -> p h v", v=128),
           ms[:].unsqueeze(2).to_broadcast([128, 8, 128]), ALU.mult, r=["of", "ms", "sq"], w=["sq"])
        tt(p, nc, "pool", sq[:], sq[:], gnb[:], ALU.mult, r=["sq"], w=["sq"])
        act(p, nc, gt[:], gt[:], AF.Silu, r=["gt"], w=["gt"])
        tt(p, nc, "dve", on[:], sq[:], gt[:], ALU.mult, r=["sq", "gt"], w=["on"])
        for kc in range(8):
            tr(p, nc, psOT[:, kc, :], on[:, kc * 128:(kc + 1) * 128], identb[:], r=["on"], w=["psOT"])
        cp(p, nc, "act", onT[:], psOT[:], r=["psOT"], w=["onT"])
        for n in range(2):
            for kc in range(8):
                mm(p, nc, psY[n][:], onT[:, kc, :], wout[:, kc, n * 512:(n + 1) * 512], kc == 0, kc == 7, r=["onT"], w=[f"psY{n}"])
        ts(p, nc, "pool", g1b[:], g1b[:], 1.0, None, ALU.add, r=["g1b"], w=["g1b"])
        for n in range(2):
            tt(p, nc, "dve", t1[:, n * 512:(n + 1) * 512], psY[n][:], g1b[:, n * 512:(n + 1) * 512], ALU.mult,
               r=[f"psY{n}", "g1b"], w=["t1"])
        p.op("dve", lambda i: nc.vector.scalar_tensor_tensor(z[:], xt[:], alpha, t1[:], ALU.mult, ALU.add), r=["xt", "t1"], w=["z"])
        layer_norm_tail(b, p, z, "z", lng, lnb, xo, "xo", dict(sq=sq, st=stt), "ln")
        dma(p, nc, "sp", "xo", lambda i: X[bass.ds(i * 128, 128), :], xo[:], r=["xo"])
        p.run()
```

```python
import math
from collections import Counter
from contextlib import ExitStack
import numpy as np
import ml_dtypes
import concourse.bass as bass
import concourse.mybir as mybir
from concourse.bass_utils import run_bass_kernel_spmd

F32 = mybir.dt.float32
BF16 = mybir.dt.bfloat16
I32 = mybir.dt.int32
AF = mybir.ActivationFunctionType
ALU = mybir.AluOpType
AX = mybir.AxisListType

D = 1024
HD = 64
N_GROUPS = 4
EPG = 8
NEXP = 32
DEXP = 512
LN_EPS = 1e-5
RMS_EPS = 1e-6
ROPE_THETA = 10000.0


class KB:
    def __init__(self, nc):
        self.nc = nc
        self.engs = {"pe": nc.tensor, "act": nc.scalar, "dve": nc.vector, "pool": nc.gpsimd, "sp": nc.sync}
        self.sems = {}
        self.base = {}
        for e in self.engs:
            self.sems[("e", e)] = nc.alloc_semaphore(name="S_" + e)
            self.base[("e", e)] = 0
        self.chan_owner = {}
        self.nphase = 0

    def sig(self, key):
        if key not in self.sems:
            self.sems[key] = self.nc.alloc_semaphore(name="C_" + str(key[1]))
            self.base[key] = 0
        return self.sems[key]


class Phase:
    def __init__(self, kb, name, n_iter=None):
        self.kb = kb
        self.name = name
        self.n_iter = n_iter
        self.ops = []

    ns = ""

    def _n(self, name):
        return name[1:] if name.startswith("!") else name + self.ns

    def op(self, eng, fn, r=(), w=(), chan=None):
        self.ops.append(dict(eng=eng, fn=fn, r=tuple(self._n(x) for x in r), w=tuple(self._n(x) for x in w),
                             chan=(chan + self.ns) if chan else None))

    def dma(self, eng, chan, fn, r=(), w=()):
        self.op(eng, fn, r, w, chan=chan)

    def run(self):
        kb = self.kb
        nc = kb.nc
        ops = self.ops
        m = len(ops)
        if m == 0:
            return
        loop = self.n_iter is not None
        N = self.n_iter if loop else 1
        sig_of = [("c", o["chan"]) if o["chan"] else ("e", o["eng"]) for o in ops]
        for j, o in enumerate(ops):
            if o["chan"]:
                own = kb.chan_owner.setdefault(o["chan"], o["eng"])
                assert own == o["eng"], f"chan {o['chan']} issued from two engines"
        phys = {}
        kcnt = {"sw": 0, "hw": 0}
        for j, o in enumerate(ops):
            sg_ = sig_of[j]
            if sg_ in phys:
                continue
            if sg_[0] == "e":
                phys[sg_] = sg_
            else:
                kind = "sw" if o["eng"] == "pool" else "hw"
                phys[sg_] = ("c", f"{kind}{kcnt[kind]}")
                kcnt[kind] += 1
        per_iter = Counter(sig_of)
        kidx = []
        cnt = Counter()
        for j in range(m):
            kidx.append(cnt[sig_of[j]])
            cnt[sig_of[j]] += 1
        last_w = {}
        readers = {}
        deps = [set() for _ in range(m)]
        for it in ((0, 1) if loop else (0,)):
            for j, o in enumerate(ops):
                d = set()
                for b in o["r"]:
                    if b in last_w:
                        d.add(last_w[b] + ("raw",))
                for b in o["w"]:
                    if b in last_w:
                        d.add(last_w[b] + ("waw",))
                    for rd in readers.get(b, []):
                        d.add(rd + ("war",))
                if it == (1 if loop else 0):
                    deps[j] = {(jj, itt - it, ty) for (itt, jj, ty) in d if (itt, jj) != (it, j)}
                for b in o["r"]:
                    readers.setdefault(b, []).append((it, j))
                for b in o["w"]:
                    last_w[b] = (it, j)
                    readers[b] = []
        waits = []
        for j, o in enumerate(ops):
            need = {}
            for (jj, off, ty) in deps[j]:
                p = ops[jj]
                s = sig_of[jj]
                if s[0] == "e" and sig_of[j][0] == "e" and p["eng"] == o["eng"]:
                    if o["eng"] == "pe" or ty != "raw":
                        continue
                n_s = per_iter[s]
                if s[0] == "c":
                    if off == 0:
                        q = sum(1 for x in range(j) if sig_of[x] == s)
                        const = q * 16
                    else:
                        const = 0
                else:
                    const = (kidx[jj] + 1) if off == 0 else (kidx[jj] + 1 - n_s)
                if s not in need or const > need[s]:
                    need[s] = const
            waits.append(need)
        known = {}
        for j, o in enumerate(ops):
            kn = known.setdefault(o["eng"], {})
            for s in list(waits[j]):
                if s in kn and kn[s] >= waits[j][s]:
                    del waits[j][s]
                else:
                    kn[s] = waits[j][s]
        T0 = {}
        for s, n_s in per_iter.items():
            kb.sig(phys[s])
            inc = 16 if s[0] == "c" else 1
            T0[s] = kb.base[phys[s]] + (n_s * inc if (loop and s[0] == "e") else 0)
        engines_used = []
        for o in ops:
            if o["eng"] not in engines_used:
                engines_used.append(o["eng"])

        def free_ids(E):
            hs, ids = [], []
            while True:
                try:
                    kb.probe_cnt = getattr(kb, "probe_cnt", 0) + 1
                    h = E.alloc_register(f"probe{kb.probe_cnt}")
                except Exception:
                    break
                hs.append(h)
                ids.append(nc.lookup_reg(h).reg_id)
            for h in hs:
                E.free_register(h)
            return set(ids)

        def emit_engine(ename):
            E = kb.engs[ename]
            before = free_ids(E)
            emit_engine_inner(ename)
            leaked = sorted(before - free_ids(E))
            for rid in leaked:
                kb.probe_cnt += 1
                h = nc.add_register(E.engine, f"leak{kb.probe_cnt}", rid)
                E.free_register(h)

        def emit_engine_inner(ename):
            E = kb.engs[ename]
            mine = [j for j, o in enumerate(ops) if o["eng"] == ename]
            wsigs = []
            for j in mine:
                for s in waits[j]:
                    if s not in wsigs:
                        wsigs.append(s)

            def body(i, regs, tmp):
                for j in mine:
                    o = ops[j]
                    for s, const in waits[j].items():
                        if i is None:
                            E.wait_ge(kb.sig(phys[s]), T0[s] + const)
                        elif const == 0:
                            E.wait_ge(kb.sig(phys[s]), regs[s])
                        else:
                            E.reg_add(tmp, regs[s], const)
                            E.wait_ge(kb.sig(phys[s]), tmp)
                    ins = o["fn"](i)
                    s = sig_of[j]
                    ins.then_inc(kb.sig(phys[s]), 16 if s[0] == "c" else 1)

            if loop:
                if ("e", ename) in per_iter:
                    E.sem_inc(kb.sig(("e", ename)), per_iter[("e", ename)])
                regs = {}
                for k, s in enumerate(wsigs):
                    regs[s] = E.alloc_register(f"w{kb.nphase}_{ename}_{k}")
                    E.reg_mov(regs[s], T0[s])
                tmp = E.alloc_register(f"wt{kb.nphase}_{ename}")
                with E.Fori(0, N) as i:
                    body(i, regs, tmp)
                    for s in wsigs:
                        E.reg_add(regs[s], regs[s], per_iter[s] * (16 if s[0] == "c" else 1))
                for s in wsigs:
                    E.free_register(regs[s])
                E.free_register(tmp)
            else:
                body(None, None, None)
            for s, n_s in per_iter.items():
                owner = kb.chan_owner[s[1]] if s[0] == "c" else s[1]
                if owner == ename:
                    inc = 16 if s[0] == "c" else 1
                    E.wait_ge(kb.sig(phys[s]), T0[s] + N * n_s * inc)

        with nc.Block() as blk:
            reg = {"pe": blk.tensor, "act": blk.scalar, "dve": blk.vector, "pool": blk.gpsimd, "sp": blk.sync}
            for ename in engines_used:
                reg[ename](lambda eng, ename=ename: emit_engine(ename))
        for s, n_s in per_iter.items():
            inc = 16 if s[0] == "c" else 1
            kb.base[phys[s]] = T0[s] + N * n_s * inc
        nc.all_engine_barrier()
        kb.nphase += 1


def _bf(a):
    return np.ascontiguousarray(a).astype(ml_dtypes.bfloat16)


def host_constants(cfg):
    seqs = cfg["seqs"]
    NT = sum(seqs)
    c = {}
    c["ident_f"] = np.eye(128, dtype=np.float32)
    c["ident_b"] = _bf(np.eye(128, dtype=np.float32))
    pos = np.concatenate([np.arange(t) for t in seqs]).astype(np.float32)
    inv = np.power(ROPE_THETA, -np.arange(0, HD, 2, dtype=np.float32) / HD).astype(np.float32)
    d = np.arange(128) % 64
    ang = pos[None, :] * inv[d % 32][:, None]
    c["rope_cos"] = np.cos(ang).astype(np.float32)
    sgn = np.where(d < 32, -1.0, 1.0).astype(np.float32)
    c["rope_sin"] = (np.sin(ang) * sgn[:, None]).astype(np.float32)
    j = np.arange(128)[:, None]
    i = np.arange(128)[None, :]
    c["wmask"] = _bf(np.stack([(j >= i), (j <= i)], axis=1).astype(np.float32))
    kc = np.arange(64)
    qc = np.arange(64)
    qwin = np.clip(qc - 8, 0, 48)
    col_ok = (kc[:, None] >= qwin[None, :]) & (kc[:, None] < qwin[None, :] + 16)
    m = np.zeros((128, 9, 128), np.float32)
    for t in range(9):
        for b in range(2):
            for a in range(2):
                ok = col_ok.astype(np.float32)
                if t == 7 and (a == 1 and b == 0):
                    ok = ok * 0
                if t == 8 and not (a == 1 and b == 0):
                    ok = ok * 0
                m[b * 64:(b + 1) * 64, t, a * 64:(a + 1) * 64] = ok
    c["namask"] = m
    s = np.arange(64)[:, None]
    t = np.arange(64)[None, :]
    c["hmask"] = _bf(np.stack([(s <= t), (s >= t)], axis=1).astype(np.float32))
    sm = np.ones((128, 8, 128), np.float32)
    sm[:, :, 0] = 0
    sm[:, :, 64] = 0
    c["scanmask"] = sm.reshape(128, 1024)
    c["iota32"] = np.tile(np.arange(32, dtype=np.float32)[None, :], (128, 1))
    c["iota_blk"] = np.tile((np.arange(256, dtype=np.float32) * 128.0)[None, :], (128, 1))
    c["iota_p"] = np.arange(128, dtype=np.float32).reshape(128, 1)
    c["tri_su"] = _bf((np.arange(128)[:, None] < np.arange(128)[None, :]).astype(np.float32))
    c["ones_b"] = _bf(np.ones((128, 128), np.float32))
    return c


NA_TABLE_DELTA = [-3, -2, -1, 0, 1, 2, 3, -2, 2]


def na_bias_tables(rel_bias):
    L = rel_bias.shape[0]
    kc = np.arange(64)[:, None]
    qc = np.arange(64)[None, :]
    dc = np.clip(kc - qc, -15, 15) + 15
    out = np.zeros((L, 128, 9, 8, 128), np.float32)
    for t, dl in enumerate(NA_TABLE_DELTA):
        for b in range(2):
            for a in range(2):
                dr = 2 * dl + b - a + 7
                g = rel_bias[:, :, dr, :][:, :, dc]
                out[:, b * 64:(b + 1) * 64, t, :, a * 64:(a + 1) * 64] = g.transpose(0, 2, 1, 3)
    return out


def attn_w_layout(w_in):
    L = w_in.shape[0]
    qa0, ka0, va0, qb0, kb0, vb0 = 0, 512, 640, 768, 1280, 1792
    sw = (np.arange(64) + 32) % 64
    cols = []
    for jc in range(4):
        for h in (jc, 4 + jc):
            cols += list(qa0 + h * 64 + np.arange(64))
    for jc in range(4):
        for h in (jc, 4 + jc):
            cols += list(qa0 + h * 64 + sw)
    for h in range(2):
        cols += list(ka0 + h * 64 + np.arange(64))
    for h in range(2):
        cols += list(ka0 + h * 64 + sw)
    cols += list(qb0 + np.arange(512))
    cols += list(kb0 + np.arange(512))
    cols = np.array(cols)
    vcols = np.concatenate([va0 + np.arange(128), vb0 + np.arange(512)])
    return np.ascontiguousarray(w_in[:, :, cols]), np.ascontiguousarray(w_in[:, :, vcols])


class B:
    def __init__(self, cfg):
        self.cfg = cfg
        self.nc = bass.Bass("TRN2", target_bir_lowering=False)
        self.kb = KB(self.nc)
        self.dram = {}
        self.uid = 0

    def din(self, name, shape, dt=F32):
        t = self.nc.dram_tensor(name, list(shape), dt, kind="ExternalInput")
        self.dram[name] = t
        return t

    def dout(self, name, shape, dt=F32):
        t = self.nc.dram_tensor(name, list(shape), dt, kind="ExternalOutput")
        self.dram[name] = t
        return t

    def dscr(self, name, shape, dt=F32):
        if self.cfg.get("debug"):
            return self.dout(name, shape, dt)
        t = self.nc.dram_tensor(name, list(shape), dt)
        self.dram[name] = t
        return t


def un(b, name):
    b.uid += 1
    return f"{name}_{b.uid}"


def _ap(x, i):
    return x(i) if callable(x) else x


def dma(p, nc, eng, chan, out, in_, r=(), w=(), slow=False):
    E = {"sp": nc.sync, "act": nc.scalar, "pool": nc.gpsimd}[eng]
    if slow:
        p.dma(eng, chan, lambda i: E.dma_start(out=_ap(out, i), in_=_ap(in_, i), allow_slow_non_contiguous=True), r=r, w=w)
    else:
        p.dma(eng, chan, lambda i: E.dma_start(out=_ap(out, i), in_=_ap(in_, i)), r=r, w=w)


def eng_of(nc, e):
    return {"dve": nc.vector, "pool": nc.gpsimd, "act": nc.scalar}[e]


def tt(p, nc, e, out, in0, in1, op, r=(), w=()):
    E = eng_of(nc, e)
    p.op(e, lambda i: E.tensor_tensor(_ap(out, i), _ap(in0, i), _ap(in1, i), op), r=r, w=w)


def ts(p, nc, e, out, in0, s1, s2, op0, op1=None, r=(), w=()):
    E = eng_of(nc, e)
    if op1 is None:
        p.op(e, lambda i: E.tensor_scalar(_ap(out, i), _ap(in0, i), s1, None, op0), r=r, w=w)
    else:
        p.op(e, lambda i: E.tensor_scalar(_ap(out, i), _ap(in0, i), s1, s2, op0, op1), r=r, w=w)


def cp(p, nc, e, out, in_, r=(), w=()):
    if e == "act":
        p.op(e, lambda i: nc.scalar.copy(_ap(out, i), _ap(in_, i)), r=r, w=w)
    else:
        E = eng_of(nc, e)
        p.op(e, lambda i: E.tensor_copy(_ap(out, i), _ap(in_, i)), r=r, w=w)


def act(p, nc, out, in_, func, bias=None, scale=None, r=(), w=()):
    kw = {}
    if bias is not None:
        kw["bias"] = bias
    if scale is not None:
        kw["scale"] = scale
    p.op("act", lambda i: nc.scalar.activation(_ap(out, i), _ap(in_, i), func, **kw), r=r, w=w)


def mm(p, nc, out, lhsT, rhs, start, stop, r=(), w=()):
    p.op("pe", lambda i: nc.tensor.matmul(_ap(out, i), lhsT=_ap(lhsT, i), rhs=_ap(rhs, i), start=start, stop=stop), r=r, w=w)


def tr(p, nc, out, in_, ident, r=(), w=()):
    p.op("pe", lambda i: nc.tensor.transpose(_ap(out, i), _ap(in_, i), ident), r=r, w=w)


def load_weight_bf16(b, p, dst, src, ncols, tag, stg, kdim=8):
    nc = b.nc
    engs = ["dve", "act", "pool"]
    c0 = 0
    k = 0
    while c0 < ncols:
        cw = min(512, ncols - c0)
        st = stg[k % 2]
        sname = f"stg{k % 2}"
        dma(p, nc, "sp" if k % 2 == 0 else "act", f"wstg{k % 2}", st[:, :, 0:cw], src[:, :, c0:c0 + cw], w=[sname])
        cp(p, nc, engs[k % 3], dst[:, :, c0:c0 + cw], st[:, :, 0:cw], r=[sname], w=[tag + str(k)])
        c0 += cw
        k += 1


def layer_norm_tail(b, p, z, zname, lng, lnb, out, outname, scr, tg):
    nc = b.nc
    sq, st = scr["sq"], scr["st"]
    p.op("dve", lambda i: nc.vector.tensor_reduce(st[:, 0:1], z[:], AX.X, ALU.add), r=[zname], w=[tg + "s0"])
    tt(p, nc, "pool", sq[:], z[:], z[:], ALU.mult, r=[zname], w=[tg + "sq"])
    p.op("dve", lambda i: nc.vector.tensor_reduce(st[:, 1:2], sq[:], AX.X, ALU.add), r=[tg + "sq"], w=[tg + "s1"])
    ts(p, nc, "dve", st[:, 2:3], st[:, 0:1], 1.0 / D, None, ALU.mult, r=[tg + "s0"], w=[tg + "s2"])
    ts(p, nc, "dve", st[:, 3:4], st[:, 1:2], 1.0 / D, None, ALU.mult, r=[tg + "s1"], w=[tg + "s3"])
    tt(p, nc, "dve", st[:, 4:5], st[:, 2:3], st[:, 2:3], ALU.mult, r=[tg + "s2"], w=[tg + "s4"])
    tt(p, nc, "dve", st[:, 5:6], st[:, 3:4], st[:, 4:5], ALU.subtract, r=[tg + "s3", tg + "s4"], w=[tg + "s5"])
    act(p, nc, st[:, 6:7], st[:, 5:6], AF.Ln, bias=b.eps_ln[:, 0:1], r=[tg + "s5"], w=[tg + "s6"])
    act(p, nc, st[:, 6:7], st[:, 6:7], AF.Exp, scale=-0.5, r=[tg + "s6"], w=[tg + "s6"])
    p.op("dve", lambda i: nc.vector.scalar_tensor_tensor(st[:, 7:8], st[:, 2:3], -1.0, st[:, 6:7], ALU.mult, ALU.mult),
         r=[tg + "s2", tg + "s6"], w=[tg + "s7"])
    act(p, nc, sq[:], z[:], AF.Identity, bias=st[:, 7:8], scale=st[:, 6:7], r=[zname, tg + "s6", tg + "s7", tg + "sq"], w=[tg + "sq"])
    tt(p, nc, "dve", sq[:], sq[:], lng[:], ALU.mult, r=[tg + "sq"], w=[tg + "sq"])
    tt(p, nc, "pool", out[:], sq[:], lnb[:], ALU.add, r=[tg + "sq"], w=[outname])


def build_program(cfg):
    seqs = cfg["seqs"]
    NT = sum(seqs)
    NTILES = NT // 128
    depth = cfg["depth"]
    LA = (depth + 1) // 2
    LR = depth // 2
    stop = cfg.get("stop")
    alpha = float((2 * cfg.get("dn_depth", depth)) ** 0.25)
    b = B(cfg)
    nc = b.nc
    kb = b.kb

    x_in = b.din("x", [NT, D])
    cT_in = b.din("cT", [128, 3, 8])
    ada_w = b.din("ada_w", [depth, D, 6 * D])
    ada_b = b.din("ada_b", [depth, 6 * D])
    ln_g = b.din("ln_g", [depth, 2, D])
    ln_b = b.din("ln_b", [depth, 2, D])
    a_wfm = b.din("attn_wfm", [LA, D, 2304])
    a_wv = b.din("attn_wv", [LA, D, 640])
    a_sink = b.din("attn_sink", [LA, 8])
    a_rb = b.din("na_rb", [LA, 128, 9, 8, 128])
    a_wout = b.din("attn_w_out", [LA, D, D])
    r_win = b.din("rec_w_in", [max(LR, 1), D, 5 * D])
    r_lb = b.din("rec_lb", [max(LR, 1), D])
    r_gn = b.din("rec_gnorm", [max(LR, 1), D])
    r_wout = b.din("rec_w_out", [max(LR, 1), D, D])
    ro_w = b.din("router_w", [depth, D, 36])
    ro_b = b.din("router_b", [depth, 36])
    e_wg = b.din("e_wg", [depth * NEXP * 128, 4096])
    e_wu = b.din("e_wu", [depth * NEXP * 128, 4096])
    e_wd = b.din("e_wd", [depth * NEXP * 128, 4096])
    cn = {}
    for name, shape, dt in [("ident_f", [128, 128], F32), ("ident_b", [128, 128], BF16), ("rope_cos", [128, NT], F32),
                            ("rope_sin", [128, NT], F32), ("wmask", [128, 2, 128], BF16), ("namask", [128, 9, 128], F32),
                            ("hmask", [64, 2, 64], BF16), ("scanmask", [128, 1024], F32), ("iota32", [128, 32], F32),
                            ("iota_blk", [128, 256], F32), ("iota_p", [128, 1], F32), ("tri_su", [128, 128], BF16),
                            ("ones_b", [128, 128], BF16)]:
        cn[name] = b.din(name, shape, dt)
    y_out = b.dout("y", [NT, D])
    X = b.dscr("X", [NT, D])
    MODT = b.dscr("MODT", [NTILES, 6 * D])
    FM = b.dscr("FM", [13, 128, NT], BF16)
    VE = b.dscr("VE", [NT, 720], BF16)

    ODBG = b.dout("ODBG", [NT, D], BF16) if cfg.get("debug") else None
    seq_tile0 = [0]
    for t in seqs[:-1]:
        seq_tile0.append(seq_tile0[-1] + t // 128)

    with ExitStack() as gs:
        def sb(name, shape, dt=F32, stack=gs):
            return stack.enter_context(nc.sbuf_tensor(un(b, name), list(shape), dt))

        def ps(name, shape, dt=F32, stack=gs):
            return stack.enter_context(nc.psum_tensor(un(b, name), list(shape), dt))

        identf = sb("identf", [128, 128])
        identb = sb("identb", [128, 128], BF16)
        CS = sb("CS", [128, 24, 128])
        epst = sb("epst", [128, 2])
        b.eps_ln = epst
        csb = sb("csb", [128, 24])
        p = Phase(kb, "setup")
        p.op("pool", lambda i: nc.gpsimd.memset(epst[:, 0:1], LN_EPS), w=["epst0"])
        p.op("pool", lambda i: nc.gpsimd.memset(epst[:, 1:2], RMS_EPS), w=["epst1"])
        dma(p, nc, "sp", "k0", identf[:], cn["ident_f"][:, :], w=["identf"])
        dma(p, nc, "sp", "k1", identb[:], cn["ident_b"][:, :], w=["identb"])
        dma(p, nc, "sp", "k2", csb[:], cT_in.ap().rearrange("p s k -> p (s k)"), w=["csb"])
        act(p, nc, csb[:], csb[:], AF.Silu, r=["csb"], w=["csb"])
        cp(p, nc, "dve", CS[:], csb[:].unsqueeze(2).to_broadcast([128, 24, 128]), r=["csb"], w=["CS"])
        p.run()

        p = Phase(kb, "xcopy")
        nchunk = max(1, NT // 1024)
        rows = NT // nchunk
        for k in range(nchunk):
            dma(p, nc, ["sp", "act", "pool"][k % 3], f"xc{k % 3}", X[k * rows:(k + 1) * rows, :], x_in[k * rows:(k + 1) * rows, :])
        p.run()

        for l in range(depth):
            with ExitStack() as st:
                aw = sb("aw", [128, 8, 512], stack=st)
                ab = sb("ab", [128, 512], stack=st)
                mo = [sb(f"mo{s}", [128, 512], stack=st) for s in range(3)]
                pm = [ps(f"pm{s}", [128, 512], stack=st) for s in range(3)]
                p = Phase(kb, f"mod{l}", n_iter=12)
                dma(p, nc, "sp", "aw", aw[:], lambda i: ada_w[l].rearrange("(kc p) n -> p kc n", p=128)[:, :, bass.ds(i * 512, 512)], w=["aw"])
                dma(p, nc, "act", "ab", ab[:], lambda i: ada_b[l:l + 1, bass.ds(i * 512, 512)].partition_broadcast(128), w=["ab"])
                for s in range(3):
                    for kc in range(8):
                        mm(p, nc, pm[s][:], CS[:, s * 8 + kc, :], aw[:, kc, :], kc == 0, kc == 7, r=["aw"], w=[f"pm{s}"])
                    tt(p, nc, "dve", mo[s][:], pm[s][:], ab[:], ALU.add, r=[f"pm{s}", "ab"], w=[f"mo{s}"])
                    nts = seqs[s] // 128
                    dma(p, nc, "pool", f"mo{s}", lambda i, s=s, nts=nts: MODT[seq_tile0[s]:seq_tile0[s] + nts, bass.ds(i * 512, 512)],
                        mo[s][0:nts, :], r=[f"mo{s}"])
                p.run()
            if stop == "mod":
                break
            if l % 2 == 0:
                build_attention_layer(b, l, l // 2, dict(X=X, MODT=MODT, FM=FM, VE=VE, a_wfm=a_wfm, a_wv=a_wv, a_sink=a_sink,
                                                         a_rb=a_rb, a_wout=a_wout, ln_g=ln_g, ln_b=ln_b, cn=cn, identb=identb,
                                                         identf=identf, alpha=alpha, seq_tile0=seq_tile0, ODBG=ODBG))
            else:
                build_hgrn_layer(b, l, l // 2, dict(X=X, MODT=MODT, r_win=r_win, r_lb=r_lb, r_gn=r_gn, r_wout=r_wout,
                                                    ln_g=ln_g, ln_b=ln_b, cn=cn, identb=identb, identf=identf, alpha=alpha,
                                                    seq_tile0=seq_tile0))
            if stop in (f"mix{l}", "attw", "A1", "attpre", "A2e", "H1", "H2"):
                break
            build_moe_layer(b, l, dict(X=X, MODT=MODT, ro_w=ro_w, ro_b=ro_b, e_wg=e_wg, e_wu=e_wu, e_wd=e_wd, ln_g=ln_g,
                                       ln_b=ln_b, cn=cn, identb=identb, identf=identf, alpha=alpha))
            if stop in (f"moe{l}", "moeD", "moeS", "moeB"):
                break

        p = Phase(kb, "ycopy")
        for k in range(nchunk):
            dma(p, nc, ["sp", "act", "pool"][k % 3], f"yc{k % 3}", y_out[k * rows:(k + 1) * rows, :], X[k * rows:(k + 1) * rows, :])
        p.run()
    return nc


def build_attention_layer(b, l, la, g):
    nc, kb = b.nc, b.kb
    seqs = b.cfg["seqs"]
    NT = sum(seqs)
    X, MODT, FM, VE, cn = g["X"], g["MODT"], g["FM"], g["VE"], g["cn"]
    identb = g["identb"]
    alpha = g["alpha"]
    seq_tile0 = g["seq_tile0"]

    with ExitStack() as st:
        def sb(name, shape, dt=F32):
            return st.enter_context(nc.sbuf_tensor(un(b, name), list(shape), dt))

        def ps(name, shape, dt=F32):
            return st.enter_context(nc.psum_tensor(un(b, name), list(shape), dt))

        wfm = sb("wfm", [128, 8, 2304], BF16)
        wv = sb("wv", [128, 8, 640], BF16)
        stg = [sb("stg0", [128, 8, 512]), sb("stg1", [128, 8, 512])]
        veall = sb("veall", [128, 4, 10, 72], BF16)
        p = Phase(kb, f"attw{l}")
        load_weight_bf16(b, p, wfm, g["a_wfm"][la].rearrange("(kc p) n -> p kc n", p=128), 2304, "wfm", stg)
        load_weight_bf16(b, p, wv, g["a_wv"][la].rearrange("(kc p) n -> p kc n", p=128), 640, "wv", stg)
        p.op("pool", lambda i: nc.gpsimd.memset(veall[:], 1.0), w=["veall"])
        p.run()
        if b.cfg.get("stop") == "attw":
            return

        xg = sb("xg", [128, 4, 1024])
        scb = sb("scb", [128, 1024])
        shb = sb("shb", [128, 1024])
        tmp = [sb("tmp0", [128, 1024]), sb("tmp1", [128, 1024])]
        hb = sb("hb", [128, 4, 1024], BF16)
        hT = sb("hT", [128, 8, 512], BF16)
        ropec = sb("ropec", [128, 512])
        ropes = sb("ropes", [128, 512])
        t1 = sb("t1", [128, 512])
        t2 = sb("t2", [128, 512])
        fmall = sb("fmall", [128, 13, 512], BF16)
        psT = [ps("psT0", [128, 8, 128], BF16), ps("psT1", [128, 8, 128], BF16)]
        pf = [ps(f"pf{k}", [128, 512]) for k in range(3)]
        pva = ps("pva", [128, 512])
        pvb = ps("pvb", [128, 512])

        NG = NT // 512
        p = Phase(kb, f"A1_{l}", n_iter=NG)
        dma(p, nc, "sp", "xg", xg[:], lambda i: X[bass.ds(i * 512, 512), :].rearrange("(t p) d -> p t d", p=128), w=["xg"])
        dma(p, nc, "act", "scb", scb[:], lambda i: MODT[bass.ds(i * 4, 1), 1024:2048].partition_broadcast(128), w=["scb"])
        dma(p, nc, "act", "shb", shb[:], lambda i: MODT[bass.ds(i * 4, 1), 0:1024].partition_broadcast(128), w=["shb"])
        dma(p, nc, "pool", "ropec", ropec[:], lambda i: cn["rope_cos"][:, bass.ds(i * 512, 512)], w=["ropec"])
        dma(p, nc, "pool", "ropes", ropes[:], lambda i: cn["rope_sin"][:, bass.ds(i * 512, 512)], w=["ropes"])
        ts(p, nc, "dve", scb[:], scb[:], 1.0, None, ALU.add, r=["scb"], w=["scb"])
        for t in range(4):
            tt(p, nc, "dve", tmp[t % 2][:], xg[:, t, :], scb[:], ALU.mult, r=["xg", "scb"], w=[f"tmp{t % 2}"])
            tt(p, nc, "pool", hb[:, t, :], tmp[t % 2][:], shb[:], ALU.add, r=[f"tmp{t % 2}", "shb"], w=[f"hb{t}"])
            for kc in range(8):
                tr(p, nc, psT[t % 2][:, kc, :], hb[:, t, kc * 128:(kc + 1) * 128], identb[:], r=[f"hb{t}"], w=[f"psT{t % 2}"])
            cp(p, nc, "act", hT[:, :, t * 128:(t + 1) * 128], psT[t % 2][:], r=[f"psT{t % 2}"], w=[f"hT{t}"])
        hTall = [f"hT{t}" for t in range(4)]
        pfi = 0
        fk = 0
        for (ca, cb_, dst) in [(0, 4, 0), (1, 5, 1), (2, 6, 2), (3, 7, 3), (8, 9, 4)]:
            ia, ib = pfi % 3, (pfi + 1) % 3
            pfi += 2
            for kc in range(8):
                mm(p, nc, pf[ia][:], wfm[:, kc, ca * 128:(ca + 1) * 128], hT[:, kc, :], kc == 0, kc == 7, r=hTall, w=[f"pf{ia}"])
            for kc in range(8):
                mm(p, nc, pf[ib][:], wfm[:, kc, cb_ * 128:(cb_ + 1) * 128], hT[:, kc, :], kc == 0, kc == 7, r=hTall, w=[f"pf{ib}"])
            tt(p, nc, "dve", t1[:], pf[ia][:], ropec[:], ALU.mult, r=[f"pf{ia}", "ropec"], w=["t1"])
            tt(p, nc, "dve", t2[:], pf[ib][:], ropes[:], ALU.mult, r=[f"pf{ib}", "ropes"], w=["t2"])
            tt(p, nc, "pool", fmall[:, dst, :], t1[:], t2[:], ALU.add, r=["t1", "t2"], w=[f"fm{dst}"])
        for c in range(10, 18):
            dst = 5 + (c - 10)
            ia = pfi % 3
            pfi += 1
            for kc in range(8):
                mm(p, nc, pf[ia][:], wfm[:, kc, c * 128:(c + 1) * 128], hT[:, kc, :], kc == 0, kc == 7, r=hTall, w=[f"pf{ia}"])
            cp(p, nc, "act", fmall[:, dst, :], pf[ia][:], r=[f"pf{ia}"], w=[f"fm{dst}"])
        for t in range(4):
            for kc in range(8):
                mm(p, nc, pva[:], hT[:, kc, t * 128:(t + 1) * 128], wv[:, kc, 128:640], kc == 0, kc == 7, r=[f"hT{t}"], w=["pva"])
            for kc in range(8):
                mm(p, nc, pvb[:, 0:128], hT[:, kc, t * 128:(t + 1) * 128], wv[:, kc, 0:128], kc == 0, kc == 7, r=[f"hT{t}"], w=["pvb"])
            cp(p, nc, "dve", veall[:, t, 2:10, 0:64], pva[:].rearrange("p (h d) -> p h d", d=64), r=["pva"], w=[f"ve{t}"])
            cp(p, nc, "act", veall[:, t, 0:2, 0:64], pvb[:, 0:128].rearrange("p (h d) -> p h d", d=64), r=["pvb"], w=[f"ve{t}"])
        dma(p, nc, "sp", "fmall", lambda i: FM[:, :, bass.ds(i * 512, 512)].rearrange("f p t -> p f t"), fmall[:],
            r=[f"fm{d_}" for d_ in range(13)])
        dma(p, nc, "pool", "veall", lambda i: VE[bass.ds(i * 512, 512), :].rearrange("(t p) c -> p t c", p=128),
            veall[:].rearrange("p t h d -> p t (h d)"), r=[f"ve{t}" for t in range(4)])
        p.run()
    if b.cfg.get("stop") == "A1":
        return

    with ExitStack() as st:
        def sb(name, shape, dt=F32):
            return st.enter_context(nc.sbuf_tensor(un(b, name), list(shape), dt))

        def ps(name, shape, dt=F32):
            return st.enter_context(nc.psum_tensor(un(b, name), list(shape), dt))

        wout = sb("wout", [128, 8, 1024], BF16)
        EB = sb("EB", [128, 9, 8, 128], BF16)
        skx = sb("skx", [128, 8])
        wm = sb("wm", [128, 2, 128], BF16)
        lng = sb("lng", [128, 1024])
        lnb = sb("lnb", [128, 1024])
        with ExitStack() as st2:
            stg = [st2.enter_context(nc.sbuf_tensor(un(b, "stg0"), [128, 8, 512], F32)),
                   st2.enter_context(nc.sbuf_tensor(un(b, "stg1"), [128, 8, 512], F32))]
            nam = st2.enter_context(nc.sbuf_tensor(un(b, "nam"), [128, 9, 128], F32))
            rbs = [st2.enter_context(nc.sbuf_tensor(un(b, "rbs0"), [128, 8, 128], F32)),
                   st2.enter_context(nc.sbuf_tensor(un(b, "rbs1"), [128, 8, 128], F32))]
            p = Phase(kb, f"attpre{l}")
            load_weight_bf16(b, p, wout, g["a_wout"][la].rearrange("(kc p) n -> p kc n", p=128), 1024, "wout", stg)
            dma(p, nc, "pool", "nam", nam[:], cn["namask"][:, :, :], w=["nam"])
            dma(p, nc, "pool", "wm", wm[:], cn["wmask"][:, :, :], w=["wm"])
            dma(p, nc, "pool", "skx", skx[:], g["a_sink"][la:la + 1, :].partition_broadcast(128), w=["skx"])
            dma(p, nc, "pool", "lng", lng[:], g["ln_g"][l, 0:1, :].partition_broadcast(128), w=["lng"])
            dma(p, nc, "pool", "lnb", lnb[:], g["ln_b"][l, 0:1, :].partition_broadcast(128), w=["lnb"])
            act(p, nc, skx[:], skx[:], AF.Exp, r=["skx"], w=["skx"])
            for t in range(9):
                rb = rbs[t % 2]
                dma(p, nc, "sp", f"rbs{t % 2}", rb[:], g["a_rb"][la, :, t, :, :], w=[f"rbs{t % 2}"])
                act(p, nc, rb[:], rb[:], AF.Exp, r=[f"rbs{t % 2}"], w=[f"rbs{t % 2}"])
                tt(p, nc, "dve", EB[:, t, :, :], rb[:], nam[:, t, :].unsqueeze(1).to_broadcast([128, 8, 128]), ALU.mult,
                   r=[f"rbs{t % 2}", "nam"], w=[f"EB{t}"])
            p.run()
        if b.cfg.get("stop") == "attpre":
            return

        qa = sb("qa", [128, 4, 128], BF16)
        ka = sb("ka", [128, 3, 128], BF16)
        va = sb("va", [128, 3, 2, 72], BF16)
        qb = sb("qb", [64, 8, 128], BF16)
        kbt = sb("kbt", [64, 8, 5, 128], BF16)
        vb = sb("vb", [128, 5, 8, 72], BF16)
        xt = sb("xt", [128, 1024])
        g1b = sb("g1b", [128, 1024])
        PT = [sb("PT0", [128, 5, 4, 128], BF16), sb("PT1", [128, 5, 4, 128], BF16)]
        o = sb("o", [128, 1024], BF16)
        oT = sb("oT", [128, 8, 128], BF16)
        t1 = sb("t1", [128, 1024])
        z = sb("z", [128, 1024])
        sq = sb("sq", [128, 1024])
        xo = sb("xo", [128, 1024])
        stt = sb("stt", [128, 8])
        den = sb("den", [128, 4, 1])
        pS = [ps("pS0", [128, 4, 128]), ps("pS1", [128, 4, 128])]
        accb = [ps("acc0", [128, 512]), ps("acc1", [128, 512])]
        acc = [a[:].rearrange("p (h d) -> p h d", d=128) for a in accb]
        psOT = ps("psOT", [128, 8, 128], BF16)
        psY = [ps("psY0", [128, 512]), ps("psY1", [128, 512])]
        o3 = o[:].rearrange("p (h d) -> p h d", d=64)

        def block(p, tok0, tile, wl, nl):
            def T(off):
                return (lambda i: (tok0(i) if callable(tok0) else tok0) + off)

            def TL(i):
                return tile(i) if callable(tile) else tile
            dma(p, nc, "sp", "qa", qa[:], lambda i: FM[0:4, :, bass.ds(T(0)(i), 128)].rearrange("f p t -> p f t"), w=["qa"])
            w0, w1 = wl[0], wl[-1]
            nw = w1 - w0 + 1
            dma(p, nc, "sp", "ka", ka[:, w0 + 1:w1 + 2, :].rearrange("p b t -> p (b t)"),
                lambda i: FM[4, :, bass.ds(T(w0 * 128)(i), nw * 128)], w=["ka"])
            dma(p, nc, "pool", "va", va[:, w0 + 1:w1 + 2, :, :].rearrange("p b h d -> p b (h d)"),
                lambda i: VE[bass.ds(T(w0 * 128)(i), nw * 128), 0:144].rearrange("(b p) c -> p b c", p=128), w=["va"])
            dma(p, nc, "sp", "qb", qb[:], lambda i: FM[5:9, :, bass.ds(T(0)(i), 128)].rearrange("f (two d) t -> d (f two) t", two=2), w=["qb"])
            n0 = nl[0][0]
            nn = len(nl)
            dma(p, nc, "act", "kbt", kbt[:, :, 0:nn, :].rearrange("p f b t -> p f (b t)"),
                lambda i: FM[9:13, :, bass.ds(T(n0 * 128)(i), nn * 128)].rearrange("f (two d) t -> d (f two) t", two=2), w=["kbt"])
            dma(p, nc, "pool", "vb", vb[:, 0:nn, :, :].rearrange("p b h d -> p b (h d)"),
                lambda i: VE[bass.ds(T(n0 * 128)(i), nn * 128), 144:720].rearrange("(b p) c -> p b c", p=128), w=["vb"])
            dma(p, nc, "sp", "xt", xt[:], lambda i: X[bass.ds(T(0)(i), 128), :], w=["xt"])
            dma(p, nc, "act", "g1b", g1b[:], lambda i: MODT[bass.ds(TL(i), 1), 2048:3072].partition_broadcast(128), w=["g1b"])
            lvl = b.cfg.get("a2lvl", 9)
            if lvl < 2:
                return
            u = 0
            for gk in range(2):
                a = acc[gk % 2]
                an = f"acc{gk % 2}"
                Pu = PT[gk % 2]
                pn = f"PT{gk % 2}_"
                for di, dl in enumerate(wl):
                    k = u % 2
                    u += 1
                    mm(p, nc, pS[k][:], ka[64 * gk:64 * gk + 64, dl + 1, :], qa[64 * gk:64 * gk + 64, :, :], True, True,
                       r=["ka", "qa"], w=[f"pS{k}"])
                    act(p, nc, Pu[:, di, :, :], pS[k][:], AF.Exp, scale=0.125, r=[f"pS{k}"], w=[pn + str(di)])
                    if dl != 0:
                        tt(p, nc, "dve", Pu[:, di, :, :], Pu[:, di, :, :],
                           wm[:, 0 if dl == -1 else 1, :].unsqueeze(1).to_broadcast([128, 4, 128]),
                           ALU.mult, r=[pn + str(di)], w=[pn + str(di)])
                for h in range(4):
                    for di, dl in enumerate(wl):
                        mm(p, nc, a[:, h, 0:66], Pu[:, di, h, :], va[:, dl + 1, gk, 0:66], di == 0, di == len(wl) - 1,
                           r=[pn + str(di), "va"], w=[an])
                tt(p, nc, "dve", den[:], a[:, :, 64:65], skx[:, 4 * gk:4 * gk + 4].unsqueeze(2), ALU.add, r=[an], w=["den"])
                p.op("dve", lambda i: nc.vector.reciprocal(den[:], den[:]), r=["den"], w=["den"])
                tt(p, nc, "dve", o3[:, 4 * gk:4 * gk + 4, :], a[:, :, 0:64], den[:].to_broadcast([128, 4, 64]), ALU.mult,
                   r=[an, "den"], w=["o"])
            if lvl < 3:
                return
            for hg in range(2):
                a = acc[hg % 2]
                an = f"acc{hg % 2}"
                Pu = PT[hg % 2]
                pn = f"PT{hg % 2}_"
                for di, (dl, tb) in enumerate(nl):
                    k = u % 2
                    u += 1
                    for uu in range(4):
                        h = 4 * hg + uu
                        mm(p, nc, pS[k][:, uu, :], kbt[:, h, di, :], qb[:, h, :],
                           True, True, r=["kbt", "qb"], w=[f"pS{k}"])
                    act(p, nc, Pu[:, di, :, :], pS[k][:], AF.Exp, scale=0.125, r=[f"pS{k}"], w=[pn + str(di)])
                    tt(p, nc, "dve" if di % 2 == 0 else "pool", Pu[:, di, :, :], Pu[:, di, :, :], EB[:, tb, 4 * hg:4 * hg + 4, :], ALU.mult,
                       r=[pn + str(di)], w=[pn + str(di)])
                for uu in range(4):
                    for di, (dl, tb) in enumerate(nl):
                        mm(p, nc, a[:, uu, 0:66], Pu[:, di, uu, :], vb[:, di, 4 * hg + uu, 0:66], di == 0, di == len(nl) - 1,
                           r=[pn + str(di), "vb"], w=[an])
                cp(p, nc, "dve", den[:], a[:, :, 64:65], r=[an], w=["den"])
                p.op("dve", lambda i: nc.vector.reciprocal(den[:], den[:]), r=["den"], w=["den"])
                tt(p, nc, "dve", o3[:, 8 + 4 * hg:8 + 4 * hg + 4, :], a[:, :, 0:64], den[:].to_broadcast([128, 4, 64]), ALU.mult,
                   r=[an, "den"], w=["o"])
            if b.cfg.get("debug"):
                dma(p, nc, "pool", "odbg", lambda i: g["ODBG"][bass.ds(T(0)(i), 128), :], o[:], r=["o"])
            if lvl < 4:
                return
            for kc in range(8):
                tr(p, nc, psOT[:, kc, :], o[:, kc * 128:(kc + 1) * 128], identb[:], r=["o"], w=["psOT"])
            cp(p, nc, "act", oT[:], psOT[:], r=["psOT"], w=["oT"])
            for n in range(2):
                for kc in range(8):
                    mm(p, nc, psY[n][:], oT[:, kc, :], wout[:, kc, n * 512:(n + 1) * 512], kc == 0, kc == 7, r=["oT"], w=[f"psY{n}"])
            if lvl < 5:
                return
            ts(p, nc, "pool", g1b[:], g1b[:], 1.0, None, ALU.add, r=["g1b"], w=["g1b"])
            for n in range(2):
                tt(p, nc, "dve", t1[:, n * 512:(n + 1) * 512], psY[n][:], g1b[:, n * 512:(n + 1) * 512], ALU.mult,
                   r=[f"psY{n}", "g1b"], w=["t1"])
            p.op("dve", lambda i: nc.vector.scalar_tensor_tensor(z[:], xt[:], alpha, t1[:], ALU.mult, ALU.add), r=["xt", "t1"], w=["z"])
            layer_norm_tail(b, p, z, "z", lng, lnb, xo, "xo", dict(sq=sq, st=stt), "ln")
            dma(p, nc, "sp", "xo", lambda i: X[bass.ds(T(0)(i), 128), :], xo[:], r=["xo"])

        FULL = lambda dls: [(d_, d_ + 3) for d_ in dls]
        for s, T_s in enumerate(seqs):
            nb = T_s // 128
            tok_s = seq_tile0[s] * 128
            p = Phase(kb, f"A2e_{l}_{s}")
            for j in sorted(set([0, 1, nb - 2, nb - 1]))[:b.cfg.get("a2nblk", 4)]:
                wl = [d_ for d_ in (-1, 0, 1) if 0 <= j + d_ < nb]
                if j == 0:
                    nl = FULL([0, 1, 2, 3])
                elif j == 1:
                    nl = FULL([-1, 0, 1, 2])
                elif j == nb - 2:
                    nl = FULL([-2, -1, 0, 1])
                else:
                    nl = FULL([-3, -2, -1, 0])
                block(p, tok_s + j * 128, seq_tile0[s] + j, wl, nl)
            p.run()
            if b.cfg.get("stop") == "A2e":
                return
            if nb > 4:
                p = Phase(kb, f"A2i_{l}_{s}", n_iter=nb - 4)
                block(p, lambda i, tok_s=tok_s: (i + 2) * 128 + tok_s, lambda i, s=s: i + (2 + seq_tile0[s]),
                      [-1, 0, 1], [(-2, 7), (-1, 2), (0, 3), (1, 4), (2, 8)])
                p.run()


def expert_layout(w, kdim):
    L, E, K, n = w.shape
    return np.ascontiguousarray(w.reshape(L, E, kdim, 128, n).transpose(0, 1, 3, 2, 4)).reshape(L * E * 128, kdim * n)


def prepare_shared(inp, cfg):
    f = lambda k: np.ascontiguousarray(np.asarray(inp[k], dtype=np.float32))
    sh = {}
    for k in ["ada_w", "ada_b", "ln_g", "ln_b", "attn_sink", "attn_w_out", "rec_w_in", "rec_lb", "rec_gnorm", "rec_w_out"]:
        sh[k] = f(k)
    wfm, wv = attn_w_layout(f("attn_w_in"))
    sh["attn_wfm"] = wfm
    sh["attn_wv"] = wv
    sh["na_rb"] = na_bias_tables(f("nat_rel_bias"))
    L = sh["ada_w"].shape[0]
    sh["router_w"] = np.ascontiguousarray(np.concatenate([f("router_w_group"), f("router_w_expert")], axis=2))
    sh["router_b"] = np.ascontiguousarray(np.concatenate([f("router_b_group"), f("router_b_expert").reshape(L, 32)], axis=1))
    sh["e_wg"] = expert_layout(f("expert_w_gate"), 8)
    sh["e_wu"] = expert_layout(f("expert_w_up"), 8)
    sh["e_wd"] = expert_layout(f("expert_w_down"), 4)
    sh.update(host_constants(cfg))
    return sh


def run_cores(inp, cfg, n_cores, pps):
    xp = np.asarray(inp["x_prompt"], dtype=np.float32)
    xs = np.asarray(inp["x_sample"], dtype=np.float32)
    cp_ = np.asarray(inp["c_prompt"], dtype=np.float32)
    cs_ = np.asarray(inp["c_sample"], dtype=np.float32)
    sh = prepare_shared(inp, cfg)
    nc = build_program(cfg)
    in_maps = []
    for c in range(n_cores):
        m = dict(sh)
        xc = np.concatenate([xp[pps * c + k] for k in range(pps)] + [xs[c]], axis=0)
        cc = np.stack([cp_[pps * c + k] for k in range(pps)] + [cs_[c]], axis=0)
        m["x"] = np.ascontiguousarray(xc)
        m["cT"] = np.ascontiguousarray(cc.reshape(3, 8, 128).transpose(2, 0, 1))
        in_maps.append(m)
    res = run_bass_kernel_spmd(nc, in_maps, core_ids=list(range(n_cores)))
    if cfg.get("debug"):
        cfg["_dbg"] = res.results
    TP = cfg["seqs"][0]
    yp = np.zeros_like(xp)
    ys = np.zeros_like(xs)
    for c in range(n_cores):
        y = np.asarray(res.results[c]["y"], dtype=np.float32)
        for k in range(pps):
            yp[pps * c + k] = y[k * TP:(k + 1) * TP]
        ys[c] = y[pps * TP:]
    return yp, ys


def kernel(**inputs):
    cfg = dict(seqs=[2048, 2048, 8192], depth=4)
    yp, ys = run_cores(inputs, cfg, 8, 2)
    return (yp, ys)


def build_moe_layer(b, l, g):
    nc, kb = b.nc, b.kb
    seqs = b.cfg["seqs"]
    NT = sum(seqs)
    NTILES = NT // 128
    NBLK = 2 * NTILES + NEXP
    X, MODT, cn = g["X"], g["MODT"], g["cn"]
    identf = g["identf"]
    alpha = g["alpha"]
    BIG = 1.0e9
    if "HF" not in b.dram:
        b.dscr("HF", [NT, D])
        b.dscr("RTD", [128, NTILES, 8])
        b.dscr("DESTD", [128, NTILES, 2], I32)
        b.dscr("IDXD", [128, NBLK], I32)
        b.dscr("XBUF", [NBLK * 128, D])
        b.dscr("YBUF", [NBLK * 128, D])
    HF, RTD, DESTD, IDXD, XBUF, YBUF = [b.dram[k] for k in ["HF", "RTD", "DESTD", "IDXD", "XBUF", "YBUF"]]

    with ExitStack() as st:
        def sb(name, shape, dt=F32):
            return st.enter_context(nc.sbuf_tensor(un(b, name), list(shape), dt))

        def ps(name, shape, dt=F32):
            return st.enter_context(nc.psum_tensor(un(b, name), list(shape), dt))

        rw = sb("rw", [128, 8, 36])
        rbb = sb("rbb", [128, 36])
        io32 = sb("io32", [128, 32])
        trib = sb("trib", [128, 128], BF16)
        oneb = sb("oneb", [128, 128], BF16)
        carry = sb("carry", [128, 32])
        zt = sb("zt", [128, 1024])
        rt = sb("rt", [128, 8])
        p = Phase(kb, f"moepre{l}")
        p.op("pool", lambda i: nc.gpsimd.memset(rt[:], 0.0), w=["rt"])
        dma(p, nc, "sp", "m0", rw[:], g["ro_w"][l].rearrange("(kc p) n -> p kc n", p=128), w=["rw"])
        dma(p, nc, "sp", "m1", rbb[:], g["ro_b"][l:l + 1, :].partition_broadcast(128), w=["rbb"])
        dma(p, nc, "sp", "m2", io32[:], cn["iota32"][:, :], w=["io32"])
        dma(p, nc, "sp", "m3", trib[:], cn["tri_su"][:, :], w=["trib"])
        dma(p, nc, "sp", "m4", oneb[:], cn["ones_b"][:, :], w=["oneb"])
        p.op("pool", lambda i: nc.gpsimd.memset(carry[:], 0.0), w=["carry"])
        p.op("pool", lambda i: nc.gpsimd.memset(zt[:], 0.0), w=["zt"])
        p.run()
        p = Phase(kb, f"moez{l}", n_iter=NBLK)
        dma(p, nc, "sp", "zf", lambda i: XBUF[bass.ds(i * 128, 128), :], zt[:])
        p.run()

        from types import SimpleNamespace
        sets = []
        for k in range(2):
            T = SimpleNamespace(xt=sb(f"xt{k}", [128, 1024]), scb=sb(f"scb{k}", [128, 1024]), shb=sb(f"shb{k}", [128, 1024]),
                                hf=sb(f"hf{k}", [128, 1024]), hfT=sb(f"hfT{k}", [128, 8, 128]), lg=sb(f"lg{k}", [128, 36]),
                                sm=sb(f"sm{k}", [128, 16]), ge=sb(f"ge{k}", [128, 4]), gmask=sb(f"gmask{k}", [128, 4]),
                                lem=sb(f"lem{k}", [128, 4, 8]), lem2=sb(f"lem2{k}", [128, 32]), oh1=sb(f"oh1{k}", [128, 32]),
                                oh2=sb(f"oh2{k}", [128, 32]), ohb=sb(f"ohb{k}", [128, 32], BF16), tmp32=sb(f"tmp32{k}", [128, 32]),
                                tot=sb(f"tot{k}", [128, 32]), rt=sb(f"rtt{k}", [128, 8]))
            T.lemf = T.lem[:].rearrange("p a b -> p (a b)")
            sets.append(T)
        psA = [ps("psA0", [128, 4, 128]), ps("psA1", [128, 4, 128])]
        pl = ps("pl", [128, 512])
        pr = ps("pr", [128, 512])
        pc = ps("pc", [128, 512])
        p = Phase(kb, f"moeRz{l}")
        for k in range(2):
            p.op("pool", lambda i, k=k: nc.gpsimd.memset(sets[k].rt[:], 0.0), w=[f"rtz{k}"])
        p.run()

        p = Phase(kb, f"moeR{l}", n_iter=NTILES // 2)
        for k in range(2):
            p.ns = f"#{k}"
            T = sets[k]
            dma(p, nc, "sp", "xt", T.xt[:], lambda i, k=k, T=T: X[bass.ds(i * 256 + k * 128, 128), :], w=["xt"])
            dma(p, nc, "act", "scb", T.scb[:], lambda i, k=k, T=T: MODT[bass.ds(i * 2 + k, 1), 4096:5120].partition_broadcast(128), w=["scb"])
            dma(p, nc, "act", "shb", T.shb[:], lambda i, k=k, T=T: MODT[bass.ds(i * 2 + k, 1), 3072:4096].partition_broadcast(128), w=["shb"])
            ts(p, nc, "pool", T.scb[:], T.scb[:], 1.0, None, ALU.add, r=["scb"], w=["scb"])
            tt(p, nc, "dve", T.hf[:], T.xt[:], T.scb[:], ALU.mult, r=["xt", "scb"], w=["hf"])
            tt(p, nc, "pool", T.hf[:], T.hf[:], T.shb[:], ALU.add, r=["hf", "shb"], w=["hf"])
            dma(p, nc, "sp", "hfo", lambda i, k=k, T=T: HF[bass.ds(i * 256 + k * 128, 128), :], T.hf[:], r=["hf"])
            for kc in range(8):
                tr(p, nc, psA[kc // 4][:, kc % 4, :], T.hf[:, kc * 128:(kc + 1) * 128], identf[:], r=["hf"], w=[f"!psA{kc // 4}"])
            cp(p, nc, "act", T.hfT[:, 0:4, :], psA[0][:], r=["!psA0"], w=["hfT0"])
            cp(p, nc, "dve", T.hfT[:, 4:8, :], psA[1][:], r=["!psA1"], w=["hfT1"])
            for kc in range(8):
                mm(p, nc, pl[:, 0:36], T.hfT[:, kc, :], rw[:, kc, :], kc == 0, kc == 7, r=["hfT0", "hfT1"], w=["!pl"])
            tt(p, nc, "dve", T.lg[:], pl[:, 0:36], rbb[:], ALU.add, r=["!pl"], w=["lg"])
            p.op("dve", lambda i, k=k, T=T: nc.vector.tensor_reduce(T.sm[:, 0:1], T.lg[:, 0:4], AX.X, ALU.max), r=["lg"], w=["sm0"])
            ts(p, nc, "dve", T.sm[:, 1:2], T.sm[:, 0:1], -1.0, None, ALU.mult, r=["sm0"], w=["sm1"])
            act(p, nc, T.ge[:], T.lg[:, 0:4], AF.Exp, bias=T.sm[:, 1:2], r=["lg", "sm1"], w=["ge"])
            p.op("dve", lambda i, k=k, T=T: nc.vector.tensor_reduce(T.sm[:, 2:3], T.ge[:], AX.X, ALU.add), r=["ge"], w=["sm2"])
            p.op("dve", lambda i, k=k, T=T: nc.vector.reciprocal(T.sm[:, 3:4], T.sm[:, 2:3]), r=["sm2"], w=["sm3"])
            tt(p, nc, "dve", T.gmask[:], T.lg[:, 0:4], T.sm[:, 0:1].to_broadcast([128, 4]), ALU.is_ge, r=["lg", "sm0"], w=["gmask"])
            ts(p, nc, "dve", T.gmask[:], T.gmask[:], -1.0, BIG, ALU.add, ALU.mult, r=["gmask"], w=["gmask"])
            tt(p, nc, "dve", T.lem[:], T.lg[:, 4:36].rearrange("p (a b) -> p a b", b=8), T.gmask[:].unsqueeze(2).to_broadcast([128, 4, 8]),
               ALU.add, r=["lg", "gmask"], w=["lem"])
            p.op("dve", lambda i, k=k, T=T: nc.vector.tensor_reduce(T.sm[:, 4:5], T.lemf, AX.X, ALU.max), r=["lem"], w=["sm4"])
            tt(p, nc, "dve", T.oh1[:], T.lemf, T.sm[:, 4:5].to_broadcast([128, 32]), ALU.is_ge, r=["lem", "sm4"], w=["oh1"])
            p.op("dve", lambda i, k=k, T=T: nc.vector.scalar_tensor_tensor(T.lem2[:], T.oh1[:], -BIG, T.lemf, ALU.mult, ALU.add), r=["oh1", "lem"], w=["lem2"])
            p.op("dve", lambda i, k=k, T=T: nc.vector.tensor_reduce(T.sm[:, 5:6], T.lem2[:], AX.X, ALU.max), r=["lem2"], w=["sm5"])
            tt(p, nc, "dve", T.oh2[:], T.lem2[:], T.sm[:, 5:6].to_broadcast([128, 32]), ALU.is_ge, r=["lem2", "sm5"], w=["oh2"])
            tt(p, nc, "dve", T.sm[:, 6:7], T.sm[:, 5:6], T.sm[:, 4:5], ALU.subtract, r=["sm4", "sm5"], w=["sm6"])
            act(p, nc, T.sm[:, 7:8], T.sm[:, 6:7], AF.Exp, r=["sm6"], w=["sm7"])
            ts(p, nc, "dve", T.sm[:, 8:9], T.sm[:, 7:8], 1.0, None, ALU.add, r=["sm7"], w=["sm8"])
            p.op("dve", lambda i, k=k, T=T: nc.vector.reciprocal(T.sm[:, 8:9], T.sm[:, 8:9]), r=["sm8"], w=["sm8"])
            tt(p, nc, "dve", T.rt[:, 2:3], T.sm[:, 8:9], T.sm[:, 3:4], ALU.mult, r=["sm8", "sm3"], w=["rt2"])
            tt(p, nc, "dve", T.rt[:, 3:4], T.rt[:, 2:3], T.sm[:, 7:8], ALU.mult, r=["rt2", "sm7"], w=["rt3"])
            tt(p, nc, "dve", T.tmp32[:], T.oh1[:], io32[:], ALU.mult, r=["oh1"], w=["tmp32"])
            p.op("dve", lambda i, k=k, T=T: nc.vector.tensor_reduce(T.rt[:, 0:1], T.tmp32[:], AX.X, ALU.add), r=["tmp32"], w=["rt0"])
            tt(p, nc, "dve", T.tmp32[:], T.oh2[:], io32[:], ALU.mult, r=["oh2", "rt0"], w=["tmp32"])
            p.op("dve", lambda i, k=k, T=T: nc.vector.tensor_reduce(T.rt[:, 1:2], T.tmp32[:], AX.X, ALU.add), r=["tmp32"], w=["rt1"])
            tt(p, nc, "pool", T.ohb[:], T.oh1[:], T.oh2[:], ALU.add, r=["oh1", "oh2"], w=["ohb"])
            mm(p, nc, pr[:, 0:32], trib[:], T.ohb[:], True, True, r=["ohb"], w=["!pr"])
            mm(p, nc, pc[:, 0:32], oneb[:], T.ohb[:], True, True, r=["ohb"], w=["!pc"])
            tt(p, nc, "dve", T.tot[:], pr[:, 0:32], carry[:], ALU.add, r=["!pr", "!carry"], w=["tot"])
            tt(p, nc, "dve", T.tmp32[:], T.oh1[:], T.tot[:], ALU.mult, r=["oh1", "tot", "rt1"], w=["tmp32"])
            p.op("dve", lambda i, k=k, T=T: nc.vector.tensor_reduce(T.rt[:, 4:5], T.tmp32[:], AX.X, ALU.add), r=["tmp32"], w=["rt4"])
            tt(p, nc, "dve", T.tmp32[:], T.oh2[:], T.tot[:], ALU.mult, r=["oh2", "tot", "rt4"], w=["tmp32"])
            p.op("dve", lambda i, k=k, T=T: nc.vector.tensor_reduce(T.rt[:, 5:6], T.tmp32[:], AX.X, ALU.add), r=["tmp32"], w=["rt5"])
            tt(p, nc, "dve", carry[:], carry[:], pc[:, 0:32], ALU.add, r=["!carry", "!pc", "tot"], w=["!carry"])
            dma(p, nc, "act", "rto", lambda i, k=k, T=T: RTD[:, bass.ds(i * 2 + k, 1), :].rearrange("p o c -> p (o c)"), T.rt[:],
                r=["rt0", "rt1", "rt2", "rt3", "rt4", "rt5"])
        p.ns = ""
        p.run()

        rta = sb("rta", [128, NTILES, 8])
        cnt = sb("cnt", [128, 32])
        cnti = sb("cnti", [128, 32], I32)
        pend = sb("pend", [128, 32])
        pst = sb("pst", [128, 32])
        onesf = sb("onesf", [128, 32])
        dacc = sb("dacc", [128, NTILES, 2])
        dtmp = sb("dtmp", [128, NTILES, 2])
        dint = sb("dint", [128, NTILES, 2], I32)
        ioblk = sb("ioblk", [128, NBLK])
        bacc = sb("bacc", [128, NBLK])
        btmp = sb("btmp", [128, NBLK])
        iop = sb("iop", [128, 1])
        bint = sb("bint", [128, NBLK], I32)
        p = Phase(kb, f"moeD{l}")
        dma(p, nc, "sp", "d0", rta[:], RTD[:, :, :], w=["rta"])
        dma(p, nc, "sp", "d1", ioblk[:], cn["iota_blk"][:, 0:NBLK], w=["ioblk"])
        dma(p, nc, "sp", "d2", iop[:], cn["iota_p"][:, :], w=["iop"])
        p.op("pool", lambda i: nc.gpsimd.memset(onesf[:], 1.0), w=["onesf"])
        ts(p, nc, "dve", cnt[:], carry[:], 1.0 / 128.0, 0.49609375, ALU.mult, ALU.add, w=["cnt"])
        cp(p, nc, "dve", cnti[:], cnt[:], r=["cnt"], w=["cnti"])
        cp(p, nc, "dve", cnt[:], cnti[:], r=["cnti"], w=["cnt"])
        ts(p, nc, "dve", cnt[:], cnt[:], 128.0, None, ALU.mult, r=["cnt"], w=["cnt"])
        p.op("dve", lambda i: nc.vector.tensor_tensor_scan(pend[:], onesf[:], cnt[:], 0.0, ALU.mult, ALU.add), r=["onesf", "cnt"], w=["pend"])
        tt(p, nc, "dve", pst[:], pend[:], cnt[:], ALU.subtract, r=["pend", "cnt"], w=["pst"])
        cp(p, nc, "dve", dacc[:], rta[:, :, 4:6], r=["rta"], w=["dacc"])
        for e in range(NEXP):
            ts(p, nc, "dve", dtmp[:], rta[:, :, 0:2], float(e), None, ALU.is_equal, r=["rta", "dacc"], w=["dtmp"])
            p.op("dve", lambda i, e=e: nc.vector.scalar_tensor_tensor(dacc[:], dtmp[:], pst[:, e:e + 1], dacc[:], ALU.mult, ALU.add),
                 r=["dtmp", "pst", "dacc"], w=["dacc"])
        cp(p, nc, "dve", dint[:], dacc[:], r=["dacc"], w=["dint"])
        dma(p, nc, "sp", "d3", DESTD[:, :, :], dint[:], r=["dint"])
        p.op("pool", lambda i: nc.gpsimd.memset(bacc[:], 0.0), w=["bacc"])
        for e in range(NEXP):
            ts(p, nc, "dve", btmp[:], ioblk[:], pend[:, e:e + 1], None, ALU.is_ge, r=["ioblk", "pend", "bacc"], w=["btmp"])
            tt(p, nc, "dve", bacc[:], bacc[:], btmp[:], ALU.add, r=["bacc", "btmp"], w=["bacc"])
        ts(p, nc, "dve", bacc[:], bacc[:], float(NEXP - 1), None, ALU.min, r=["bacc"], w=["bacc"])
        OOBV = float(b.cfg["depth"] * NEXP * 128)
        p.op("pool", lambda i: nc.gpsimd.memset(btmp[:, 0:1], 1.0), r=["btmp"], w=["btmp"])
        tt(p, nc, "dve", btmp[:, 1:NBLK], bacc[:, 1:NBLK], bacc[:, 0:NBLK - 1], ALU.not_equal, r=["bacc", "btmp"], w=["btmp"])
        ts(p, nc, "dve", bacc[:], bacc[:], 128.0, float(l * NEXP * 128), ALU.mult, ALU.add, r=["bacc", "btmp"], w=["bacc"])
        tt(p, nc, "dve", bacc[:], bacc[:], iop[:].to_broadcast([128, NBLK]), ALU.add, r=["bacc", "iop"], w=["bacc"])
        if b.cfg.get("oobskip", True):
            ts(p, nc, "dve", bacc[:], bacc[:], -OOBV, None, ALU.add, r=["bacc"], w=["bacc"])
            tt(p, nc, "dve", bacc[:], bacc[:], btmp[:], ALU.mult, r=["bacc", "btmp"], w=["bacc"])
            ts(p, nc, "dve", bacc[:], bacc[:], OOBV, None, ALU.add, r=["bacc"], w=["bacc"])
        cp(p, nc, "dve", bint[:], bacc[:], r=["bacc"], w=["bint"])
        dma(p, nc, "sp", "d4", IDXD[:, :], bint[:], r=["bint"])
        p.run()
    if b.cfg.get("stop") == "moeD":
        return

    with ExitStack() as st:
        def sb(name, shape, dt=F32):
            return st.enter_context(nc.sbuf_tensor(un(b, name), list(shape), dt))
        hf = sb("hf", [128, 1024])
        di = sb("di", [128, 2], I32)
        p = Phase(kb, f"moeS{l}", n_iter=NTILES)
        dma(p, nc, "sp", "hf", hf[:], lambda i: HF[bass.ds(i * 128, 128), :], w=["hf"])
        dma(p, nc, "act", "di", di[:], lambda i: DESTD[:, bass.ds(i, 1), :].rearrange("p o c -> p (o c)"), w=["di"])
        for k in range(2):
            p.dma("pool", f"sc{k}", lambda i, k=k: nc.gpsimd.indirect_dma_start(
                out=XBUF[:, :], out_offset=bass.IndirectOffsetOnAxis(ap=di[:, k:k + 1], axis=0), in_=hf[:], in_offset=None),
                r=["hf", "di"])
        p.run()
    if b.cfg.get("stop") == "moeS":
        return

    NROWS = b.cfg["depth"] * NEXP * 128
    with ExitStack() as st:
        def sb(name, shape, dt=F32):
            return st.enter_context(nc.sbuf_tensor(un(b, name), list(shape), dt))

        def ps(name, shape, dt=F32):
            return st.enter_context(nc.psum_tensor(un(b, name), list(shape), dt))
        wgf = sb("wgf", [128, 4096])
        wuf = sb("wuf", [128, 4096])
        wdf = sb("wdf", [128, 4096])
        psA = [ps("psA0", [128, 4, 128]), ps("psA1", [128, 4, 128])]
        psG = ps("psG", [128, 4, 128])
        psU = ps("psU", [128, 4, 128])
        psY = [ps("psY0", [128, 512]), ps("psY1", [128, 512])]
        sets = []
        for k in range(2):
            sets.append(dict(ix=sb(f"ix{k}", [128, 1], I32), wgb=sb(f"wgb{k}", [128, 8, 512], BF16), wub=sb(f"wub{k}", [128, 8, 512], BF16),
                             wdb=sb(f"wdb{k}", [128, 4, 1024], BF16), xb=sb(f"xb{k}", [128, 1024]), xT=sb(f"xT{k}", [128, 8, 128], BF16),
                             sg=sb(f"sg{k}", [128, 4, 128]), hT=sb(f"hT{k}", [128, 4, 128], BF16), yb=sb(f"yb{k}", [128, 1024])))
        p = Phase(kb, f"moeB{l}", n_iter=NBLK // 2)
        for k in range(2):
            p.ns = f"#{k}"
            T = sets[k]
            ix, wgb, wub, wdb, xb, xT, sg, hT, yb = [T[n_] for n_ in ["ix", "wgb", "wub", "wdb", "xb", "xT", "sg", "hT", "yb"]]
            dma(p, nc, "sp", "ix", ix[:], lambda i, k=k: IDXD[:, bass.ds(i * 2 + k, 1)], w=["ix"], slow=True)
            dma(p, nc, "act", "xb", xb[:], lambda i, k=k: XBUF[bass.ds(i * 256 + k * 128, 128), :], w=["xb"])
            for nm, dst, src in [("wgf", wgf, g["e_wg"]), ("wuf", wuf, g["e_wu"]), ("wdf", wdf, g["e_wd"])]:
                p.dma("pool", nm, lambda i, dst=dst, src=src, ix=ix: nc.gpsimd.indirect_dma_start(
                    out=dst[:], out_offset=None, in_=src[:, :], in_offset=bass.IndirectOffsetOnAxis(ap=ix[:, 0:1], axis=0),
                    bounds_check=NROWS - 1, oob_is_err=False),
                    r=["ix"], w=["!" + nm])
            cp(p, nc, "dve", wgb[:].rearrange("p k n -> p (k n)"), wgf[:], r=["!wgf"], w=["wgb"])
            cp(p, nc, "act", wub[:].rearrange("p k n -> p (k n)"), wuf[:], r=["!wuf"], w=["wub"])
            cp(p, nc, "pool", wdb[:].rearrange("p k n -> p (k n)"), wdf[:], r=["!wdf"], w=["wdb"])
            for kc in range(8):
                tr(p, nc, psA[kc // 4][:, kc % 4, :], xb[:, kc * 128:(kc + 1) * 128], identf[:], r=["xb"], w=[f"!psA{kc // 4}"])
            cp(p, nc, "act", xT[:, 0:4, :], psA[0][:], r=["!psA0"], w=["xT0"])
            cp(p, nc, "dve", xT[:, 4:8, :], psA[1][:], r=["!psA1"], w=["xT1"])
            for c in range(4):
                for kc in range(8):
                    mm(p, nc, psG[:, c, :], wgb[:, kc, c * 128:(c + 1) * 128], xT[:, kc, :], kc == 0, kc == 7, r=["wgb", "xT0", "xT1"], w=["!psG"])
            for c in range(4):
                for kc in range(8):
                    mm(p, nc, psU[:, c, :], wub[:, kc, c * 128:(c + 1) * 128], xT[:, kc, :], kc == 0, kc == 7, r=["wub", "xT0", "xT1"], w=["!psU"])
            act(p, nc, sg[:], psG[:], AF.Silu, r=["!psG"], w=["sg"])
            tt(p, nc, "dve", hT[:], sg[:], psU[:], ALU.mult, r=["sg", "!psU"], w=["hT"])
            for n in range(2):
                for c in range(4):
                    mm(p, nc, psY[n][:], hT[:, c, :], wdb[:, c, n * 512:(n + 1) * 512], c == 0, c == 3, r=["hT", "wdb"], w=[f"!psY{n}"])
            cp(p, nc, "act", yb[:, 0:512], psY[0][:], r=["!psY0"], w=["yb0"])
            cp(p, nc, "dve", yb[:, 512:1024], psY[1][:], r=["!psY1"], w=["yb1"])
            dma(p, nc, "sp", "yb", lambda i, k=k: YBUF[bass.ds(i * 256 + k * 128, 128), :], yb[:], r=["yb0", "yb1"])
        p.ns = ""
        p.run()
    if b.cfg.get("stop") == "moeB":
        return

    with ExitStack() as st:
        def sb(name, shape, dt=F32):
            return st.enter_context(nc.sbuf_tensor(un(b, name), list(shape), dt))
        lng = sb("lng", [128, 1024])
        lnb = sb("lnb", [128, 1024])
        p = Phase(kb, f"moeCpre{l}")
        dma(p, nc, "sp", "c0", lng[:], g["ln_g"][l, 1:2, :].partition_broadcast(128), w=["lng"])
        dma(p, nc, "sp", "c1", lnb[:], g["ln_b"][l, 1:2, :].partition_broadcast(128), w=["lnb"])
        p.run()
        sets = []
        for k in range(2):
            sets.append(dict(xt=sb(f"xt{k}", [128, 1024]), g2b=sb(f"g2b{k}", [128, 1024]), rt=sb(f"rt{k}", [128, 8]),
                             di=sb(f"di{k}", [128, 2], I32), y0=sb(f"y0{k}", [128, 1024]), y1=sb(f"y1{k}", [128, 1024]),
                             z=sb(f"z{k}", [128, 1024]), sq=sb(f"sq{k}", [128, 1024]), xo=sb(f"xo{k}", [128, 1024]),
                             stt=sb(f"stt{k}", [128, 8])))
        p = Phase(kb, f"moeC{l}", n_iter=NTILES // 2)
        for k in range(2):
            p.ns = f"#{k}"
            T = sets[k]
            xt, g2b, rt, di, y0, y1, z, sq, xo, stt = [T[n_] for n_ in ["xt", "g2b", "rt", "di", "y0", "y1", "z", "sq", "xo", "stt"]]
            dma(p, nc, "sp", "xt", xt[:], lambda i, k=k: X[bass.ds(i * 256 + k * 128, 128), :], w=["xt"])
            dma(p, nc, "act", "g2b", g2b[:], lambda i, k=k: MODT[bass.ds(i * 2 + k, 1), 5120:6144].partition_broadcast(128), w=["g2b"])
            dma(p, nc, "act", "rt", rt[:], lambda i, k=k: RTD[:, bass.ds(i * 2 + k, 1), :].rearrange("p o c -> p (o c)"), w=["rt"])
            dma(p, nc, "sp", "di", di[:], lambda i, k=k: DESTD[:, bass.ds(i * 2 + k, 1), :].rearrange("p o c -> p (o c)"), w=["di"])
            for kk, yk in enumerate([y0, y1]):
                p.dma("pool", f"ga{kk}", lambda i, kk=kk, yk=yk, di=di: nc.gpsimd.indirect_dma_start(
                    out=yk[:], out_offset=None, in_=YBUF[:, :], in_offset=bass.IndirectOffsetOnAxis(ap=di[:, kk:kk + 1], axis=0)),
                    r=["di"], w=[f"y{kk}"])
            ts(p, nc, "dve", y0[:], y0[:], rt[:, 2:3], None, ALU.mult, r=["y0", "rt"], w=["y0"])
            p.op("dve", lambda i, y0=y0, y1=y1, rt=rt: nc.vector.scalar_tensor_tensor(y0[:], y1[:], rt[:, 3:4], y0[:], ALU.mult, ALU.add),
                 r=["y0", "y1", "rt"], w=["y0"])
            ts(p, nc, "pool", g2b[:], g2b[:], 1.0, None, ALU.add, r=["g2b"], w=["g2b"])
            tt(p, nc, "pool", y0[:], y0[:], g2b[:], ALU.mult, r=["y0", "g2b"], w=["y0"])
            p.op("dve", lambda i, z=z, xt=xt, y0=y0: nc.vector.scalar_tensor_tensor(z[:], xt[:], alpha, y0[:], ALU.mult, ALU.add),
                 r=["xt", "y0"], w=["z"])
            layer_norm_tail(b, p, z, "z", lng, lnb, xo, "xo", dict(sq=sq, st=stt), "ln")
            dma(p, nc, "sp", "xo", lambda i, k=k: X[bass.ds(i * 256 + k * 128, 128), :], xo[:], r=["xo"])
        p.ns = ""
        p.run()


def build_hgrn_layer(b, l, lr, g):
    nc, kb = b.nc, b.kb
    seqs = b.cfg["seqs"]
    NT = sum(seqs)
    NTILES = NT // 128
    depth = b.cfg["depth"]
    LR = depth // 2
    X, MODT, cn = g["X"], g["MODT"], g["cn"]
    identb = g["identb"]
    alpha = g["alpha"]
    seq_tile0 = g["seq_tile0"]
    if "QZ" not in b.dram:
        b.dscr("QZ", [24, 128, NT])
        b.dscr("VT", [NT, D], BF16)
        b.dscr("GT", [NT, D])
        b.dscr("OF", [NT, D])
        b.dscr("OB", [NT, D])
    QZ, VT, GT, OF, OB = [b.dram[k] for k in ["QZ", "VT", "GT", "OF", "OB"]]

    with ExitStack() as st:
        def sb(name, shape, dt=F32):
            return st.enter_context(nc.sbuf_tensor(un(b, name), list(shape), dt))

        def ps(name, shape, dt=F32):
            return st.enter_context(nc.psum_tensor(un(b, name), list(shape), dt))

        win = sb("win", [128, 8, 5120], BF16)
        with ExitStack() as st2:
            stg = [st2.enter_context(nc.sbuf_tensor(un(b, "stg0"), [128, 8, 512], F32)),
                   st2.enter_context(nc.sbuf_tensor(un(b, "stg1"), [128, 8, 512], F32))]
            p = Phase(kb, f"recw{l}")
            load_weight_bf16(b, p, win, g["r_win"][lr].rearrange("(kc p) n -> p kc n", p=128), 5120, "win", stg)
            p.run()
        xg = sb("xg", [128, 4, 1024])
        scb = sb("scb", [128, 1024])
        shb = sb("shb", [128, 1024])
        tmp = [sb("tmp0", [128, 1024]), sb("tmp1", [128, 1024])]
        hb = sb("hb", [128, 4, 1024], BF16)
        hT = sb("hT", [128, 8, 512], BF16)
        fq = sb("fq", [128, 8, 512])
        vo = sb("vo", [128, 4, 1024], BF16)
        go = sb("go", [128, 4, 1024])
        psT = [ps("psT0", [128, 8, 128], BF16), ps("psT1", [128, 8, 128], BF16)]
        pf = [ps(f"pf{k}", [128, 512]) for k in range(4)]
        NG = NT // 512
        p = Phase(kb, f"H1_{l}", n_iter=NG)
        dma(p, nc, "sp", "xg", xg[:], lambda i: X[bass.ds(i * 512, 512), :].rearrange("(t p) d -> p t d", p=128), w=["xg"])
        dma(p, nc, "act", "scb", scb[:], lambda i: MODT[bass.ds(i * 4, 1), 1024:2048].partition_broadcast(128), w=["scb"])
        dma(p, nc, "act", "shb", shb[:], lambda i: MODT[bass.ds(i * 4, 1), 0:1024].partition_broadcast(128), w=["shb"])
        ts(p, nc, "dve", scb[:], scb[:], 1.0, None, ALU.add, r=["scb"], w=["scb"])
        for t in range(4):
            tt(p, nc, "dve", tmp[t % 2][:], xg[:, t, :], scb[:], ALU.mult, r=["xg", "scb"], w=[f"tmp{t % 2}"])
            tt(p, nc, "pool", hb[:, t, :], tmp[t % 2][:], shb[:], ALU.add, r=[f"tmp{t % 2}", "shb"], w=[f"hb{t}"])
            for kc in range(8):
                tr(p, nc, psT[t % 2][:, kc, :], hb[:, t, kc * 128:(kc + 1) * 128], identb[:], r=[f"hb{t}"], w=[f"psT{t % 2}"])
            cp(p, nc, "act", hT[:, :, t * 128:(t + 1) * 128], psT[t % 2][:], r=[f"psT{t % 2}"], w=[f"hT{t}"])
        hTall = [f"hT{t}" for t in range(4)]
        pfi = 0
        for grp in range(3):
            for hh in range(8):
                c = grp * 8 + hh
                ia = pfi % 4
                pfi += 1
                for kc in range(8):
                    mm(p, nc, pf[ia][:], win[:, kc, c * 128:(c + 1) * 128], hT[:, kc, :], kc == 0, kc == 7, r=hTall, w=[f"pf{ia}"])
                cp(p, nc, "act" if hh % 2 == 0 else "dve", fq[:, hh, :], pf[ia][:], r=[f"pf{ia}"], w=[f"fq{hh}"])
            dma(p, nc, ["sp", "act", "pool"][grp], f"fq{grp}",
                lambda i, grp=grp: QZ[grp * 8:grp * 8 + 8, :, bass.ds(i * 512, 512)].rearrange("f p t -> p f t"), fq[:],
                r=[f"fq{hh}" for hh in range(8)])
        for t in range(4):
            for n in range(4):
                ia = pfi % 4
                pfi += 1
                col0 = 3072 + n * 512
                for kc in range(8):
                    mm(p, nc, pf[ia][:], hT[:, kc, t * 128:(t + 1) * 128], win[:, kc, col0:col0 + 512], kc == 0, kc == 7,
                       r=[f"hT{t}"], w=[f"pf{ia}"])
                if n < 2:
                    cp(p, nc, "act", vo[:, t, n * 512:(n + 1) * 512], pf[ia][:], r=[f"pf{ia}"], w=[f"vo{t}"])
                else:
                    cp(p, nc, "dve", go[:, t, (n - 2) * 512:(n - 1) * 512], pf[ia][:], r=[f"pf{ia}"], w=[f"go{t}"])
        dma(p, nc, "sp", "vo", lambda i: VT[bass.ds(i * 512, 512), :].rearrange("(t p) e -> p t e", p=128), vo[:],
            r=[f"vo{t}" for t in range(4)])
        dma(p, nc, "pool", "go", lambda i: GT[bass.ds(i * 512, 512), :].rearrange("(t p) e -> p t e", p=128), go[:],
            r=[f"go{t}" for t in range(4)])
        p.run()
    if b.cfg.get("stop") == "H1":
        return

    with ExitStack() as st:
        def sb(name, shape, dt=F32):
            return st.enter_context(nc.sbuf_tensor(un(b, name), list(shape), dt))

        def ps(name, shape, dt=F32):
            return st.enter_context(nc.psum_tensor(un(b, name), list(shape), dt))

        lbT = sb("lbT", [128, 8])
        omlT = sb("omlT", [128, 8])
        lbe = sb("lbe", [128, max(LR, 1), 8])
        lsum = sb("lsum", [128, 8])
        hm = sb("hm", [64, 2, 64], BF16)
        smask = sb("smask", [128, 1024])
        S = sb("S", [128, 8, 128])
        p = Phase(kb, f"recpre{l}")
        for j in range(LR):
            dma(p, nc, "sp", f"lb{j}", lbe[:, j, :], g["r_lb"][j].rearrange("(h k) -> k h", k=128), w=[f"lbe{j}"], slow=True)
            act(p, nc, lbe[:, j, :], lbe[:, j, :], AF.Exp, r=[f"lbe{j}"], w=[f"lbe{j}"])
        cp(p, nc, "dve", lsum[:], lbe[:, 0, :], r=["lbe0"], w=["lsum"])
        for j in range(1, LR):
            tt(p, nc, "dve", lsum[:], lsum[:], lbe[:, j, :], ALU.add, r=["lsum", f"lbe{j}"], w=["lsum"])
        p.op("dve", lambda i: nc.vector.reciprocal(lsum[:], lsum[:]), r=["lsum"], w=["lsum"])
        p.op("pool", lambda i: nc.gpsimd.memset(lbT[:], 0.0), w=["lbT"])
        for j in range(1, lr + 1):
            tt(p, nc, "dve", lbT[:], lbT[:], lbe[:, j, :], ALU.add, r=["lbT", f"lbe{j}"], w=["lbT"])
        tt(p, nc, "dve", lbT[:], lbT[:], lsum[:], ALU.mult, r=["lbT", "lsum"], w=["lbT"])
        ts(p, nc, "dve", omlT[:], lbT[:], -1.0, 1.0, ALU.mult, ALU.add, r=["lbT"], w=["omlT"])
        dma(p, nc, "sp", "hm", hm[:], cn["hmask"][:, :, :], w=["hm"])
        dma(p, nc, "sp", "smask", smask[:], cn["scanmask"][:, :], w=["smask"])
        p.run()

        from types import SimpleNamespace
        sets = []
        for k in range(2):
            T = SimpleNamespace(qt=sb(f"qt{k}", [128, 8, 128]), zt=sb(f"zt{k}", [128, 8, 128]), vt=sb(f"vt{k}", [64, 2, 1024], BF16),
                                u=sb(f"u{k}", [128, 8, 128]), w_=sb(f"w_{k}", [128, 8, 128]), f_=sb(f"f_{k}", [128, 8, 128]),
                                lf=sb(f"lf{k}", [128, 8, 128]), km=sb(f"km{k}", [128, 8, 128]), bb=sb(f"bb{k}", [128, 8, 128]),
                                bm=sb(f"bm{k}", [128, 8, 128]), Ee=sb(f"Ee{k}", [128, 8, 128]), Ei=sb(f"Ei{k}", [128, 8, 128]),
                                qe=sb(f"qe{k}", [128, 8, 128], BF16), ke=sb(f"ke{k}", [128, 8, 128], BF16), sc=sb(f"sc{k}", [128, 8, 2, 4]),
                                Sc=sb(f"Sc{k}", [128, 8, 128], BF16), AT=sb(f"AT{k}", [64, 8, 64], BF16), keT=sb(f"keT{k}", [64, 8, 128], BF16),
                                oc=sb(f"oc{k}", [64, 1024]))
            sets.append(T)
        psA = ps("psA", [64, 8, 64])
        psKT = ps("psKT", [64, 8, 128], BF16)
        pso = [ps("pso0", [64, 512]), ps("pso1", [64, 512])]
        psdS = [ps("psdS0", [128, 4, 128]), ps("psdS1", [128, 4, 128])]
        lb_bc = lbT[:].unsqueeze(2).to_broadcast([128, 8, 128])
        oml_bc = omlT[:].unsqueeze(2).to_broadcast([128, 8, 128])
        fl = lambda t_: t_[:].rearrange("p h t -> p (h t)")
        c4 = lambda t_: t_[:].rearrange("p h (c t) -> p h c t", t=64)

        def rec_body(p, tok0, dirn, OUT, T):
            zbase = 8 if dirn == 0 else 16
            dma(p, nc, "sp", "qt", T.qt[:], lambda i: QZ[0:8, :, bass.ds(tok0(i), 128)].rearrange("f p t -> p f t"), w=["qt"])
            dma(p, nc, "act", "zt", T.zt[:], lambda i: QZ[zbase:zbase + 8, :, bass.ds(tok0(i), 128)].rearrange("f p t -> p f t"), w=["zt"])
            dma(p, nc, "pool", "vt", T.vt[:], lambda i: VT[bass.ds(tok0(i), 128), :].rearrange("(c p) e -> p c e", p=64), w=["vt"])
            act(p, nc, T.u[:], T.zt[:], AF.Exp, scale=-1.0, r=["zt"], w=["u"])
            ts(p, nc, "dve", T.w_[:], T.u[:], 1.0, None, ALU.add, r=["u"], w=["w_"])
            p.op("dve", lambda i: nc.vector.reciprocal(T.w_[:], T.w_[:]), r=["w_"], w=["w_"])
            tt(p, nc, "dve", T.f_[:], T.w_[:], oml_bc, ALU.mult, r=["w_"], w=["f_"])
            tt(p, nc, "dve", T.f_[:], T.f_[:], lb_bc, ALU.add, r=["f_"], w=["f_"])
            act(p, nc, T.lf[:], T.f_[:], AF.Ln, r=["f_"], w=["lf"])
            tt(p, nc, "pool", T.km[:], T.u[:], T.w_[:], ALU.mult, r=["u", "w_"], w=["km"])
            tt(p, nc, "pool", T.km[:], T.km[:], oml_bc, ALU.mult, r=["km"], w=["km"])
            p.op("dve", lambda i: nc.vector.tensor_tensor_scan(fl(T.bb), smask[:], fl(T.lf), 0.0, ALU.mult, ALU.add), r=["lf"], w=["bb"])
            if dirn == 1:
                tt(p, nc, "pool", T.bm[:], T.lf[:], T.bb[:], ALU.subtract, r=["lf", "bb"], w=["bm"])
                tt(p, nc, "pool", c4(T.bb), c4(T.bm), c4(T.bb)[:, :, :, 63:64].to_broadcast([128, 8, 2, 64]), ALU.add, r=["bm", "bb"], w=["bb"])
            endidx = 63 if dirn == 0 else 0
            cref = c4(T.bb)[:, :, :, 31:32]
            bend = c4(T.bb)[:, :, :, endidx:endidx + 1]
            act(p, nc, T.sc[:, :, :, 0:1], cref, AF.Exp, r=["bb"], w=["sc0"])
            act(p, nc, T.sc[:, :, :, 1:2], bend, AF.Exp, r=["bb"], w=["sc1"])
            tt(p, nc, "dve", T.sc[:, :, :, 3:4], bend, cref, ALU.subtract, r=["bb"], w=["sc3"])
            act(p, nc, T.sc[:, :, :, 2:3], T.sc[:, :, :, 3:4], AF.Exp, r=["sc3"], w=["sc2"])
            tt(p, nc, "pool", c4(T.bm), c4(T.bb), cref.to_broadcast([128, 8, 2, 64]), ALU.subtract, r=["bb", "bm"], w=["bm"])
            act(p, nc, T.Ee[:], T.bm[:], AF.Exp, r=["bm"], w=["Ee"])
            act(p, nc, T.Ei[:], T.bm[:], AF.Exp, scale=-1.0, r=["bm"], w=["Ei"])
            tt(p, nc, "dve", T.qe[:], T.qt[:], T.Ee[:], ALU.mult, r=["qt", "Ee"], w=["qe"])
            tt(p, nc, "pool", T.ke[:], T.km[:], T.Ei[:], ALU.mult, r=["km", "Ei"], w=["ke"])
            order = [0, 1] if dirn == 0 else [1, 0]
            for c in order:
                cs_ = slice(c * 64, (c + 1) * 64)
                for h in range(8):
                    e1 = "act" if h % 2 == 0 else "pool"
                    p.op("act", lambda i, h=h, c=c, T=T: nc.scalar.activation(T.Sc[:, h, :], S[:, h, :], AF.Copy, scale=T.sc[:, h, c, 0:1]),
                         r=[f"!S{h}", "sc0"], w=[f"Sc{h}"])
                    mm(p, nc, psA[:, h, :], T.ke[:, h, cs_], T.qe[:, h, cs_], True, True, r=["ke", "qe"], w=["!psA"])
                    tt(p, nc, "dve", T.AT[:, h, :], psA[:, h, :], hm[:, dirn, :], ALU.mult, r=["!psA"], w=[f"AT{h}"])
                    tr(p, nc, psKT[:, h, :], T.ke[:, h, cs_], identb[:], r=["ke"], w=["!psKT"])
                    cp(p, nc, "dve" if h % 2 == 0 else "act", T.keT[:, h, :], psKT[:, h, :], r=["!psKT"], w=[f"keT{h}"])
                    po = pso[h // 4][:, (h % 4) * 128:(h % 4 + 1) * 128]
                    mm(p, nc, po, T.AT[:, h, :], T.vt[:, c, h * 128:(h + 1) * 128], True, False, r=[f"AT{h}", "vt"], w=[f"!pso{h // 4}"])
                    mm(p, nc, po, T.qe[:, h, cs_], T.Sc[:, h, :], False, True, r=["qe", f"Sc{h}"], w=[f"!pso{h // 4}"])
                    mm(p, nc, psdS[h // 4][:, h % 4, :], T.keT[:, h, :], T.vt[:, c, h * 128:(h + 1) * 128], True, True,
                       r=[f"keT{h}", "vt"], w=[f"!psdS{h // 4}"])
                    ts(p, nc, "pool", S[:, h, :], S[:, h, :], T.sc[:, h, c, 1:2], None, ALU.mult, r=[f"!S{h}", "sc1"], w=[f"!S{h}"])
                    p.op("dve", lambda i, h=h, c=c, T=T: nc.vector.scalar_tensor_tensor(S[:, h, :], psdS[h // 4][:, h % 4, :], T.sc[:, h, c, 2:3],
                                                                                    S[:, h, :], ALU.mult, ALU.add),
                         r=[f"!psdS{h // 4}", f"!S{h}", "sc2"], w=[f"!S{h}"])
                cp(p, nc, "act", T.oc[:, 0:512], pso[0][:], r=["!pso0"], w=["oc0"])
                cp(p, nc, "dve", T.oc[:, 512:1024], pso[1][:], r=["!pso1"], w=["oc1"])
                dma(p, nc, "sp", f"oc", lambda i, c=c: OUT[bass.ds(tok0(i) + c * 64, 64), :], T.oc[:], r=["oc0", "oc1"])

        for s, T_s in enumerate(seqs):
            nb = T_s // 128
            tok_s = seq_tile0[s] * 128
            for dirn in range(2):
                p = Phase(kb, f"recz{l}_{s}_{dirn}")
                p.op("pool", lambda i: nc.gpsimd.memset(S[:], 0.0), w=["S"])
                p.run()
                p = Phase(kb, f"H2_{l}_{s}_{dirn}", n_iter=nb // 2)
                for k in range(2):
                    p.ns = f"#{k}"
                    if dirn == 0:
                        rec_body(p, lambda i, tok_s=tok_s, k=k: i * 256 + (tok_s + k * 128), 0, OF, sets[k])
                    else:
                        rec_body(p, lambda i, tok_s=tok_s, nb=nb, k=k: (tok_s + (nb - 1 - k) * 128) - i * 256, 1, OB, sets[k])
                p.ns = ""
                p.run()
    if b.cfg.get("stop") == "H2":
        return

    with ExitStack() as st:
        def sb(name, shape, dt=F32):
            return st.enter_context(nc.sbuf_tensor(un(b, name), list(shape), dt))

        def ps(name, shape, dt=F32):
            return st.enter_context(nc.psum_tensor(un(b, name), list(shape), dt))
        wout = sb("wout", [128, 8, 1024], BF16)
        gnb = sb("gnb", [128, 1024])
        lng = sb("lng", [128, 1024])
        lnb = sb("lnb", [128, 1024])
        with ExitStack() as st2:
            stg = [st2.enter_context(nc.sbuf_tensor(un(b, "stg0"), [128, 8, 512], F32)),
                   st2.enter_context(nc.sbuf_tensor(un(b, "stg1"), [128, 8, 512], F32))]
            p = Phase(kb, f"recw2{l}")
            load_weight_bf16(b, p, wout, g["r_wout"][lr].rearrange("(kc p) n -> p kc n", p=128), 1024, "wout", stg)
            dma(p, nc, "pool", "gnb", gnb[:], g["r_gn"][lr:lr + 1, :].partition_broadcast(128), w=["gnb"])
            dma(p, nc, "pool", "lng", lng[:], g["ln_g"][l, 0:1, :].partition_broadcast(128), w=["lng"])
            dma(p, nc, "pool", "lnb", lnb[:], g["ln_b"][l, 0:1, :].partition_broadcast(128), w=["lnb"])
            p.run()
        of = sb("of", [128, 1024])
        ob = sb("ob", [128, 1024])
        gt = sb("gt", [128, 1024])
        xt = sb("xt", [128, 1024])
        g1b = sb("g1b", [128, 1024])
        sq = sb("sq", [128, 1024])
        ms = sb("ms", [128, 8])
        on = sb("on", [128, 1024], BF16)
        onT = sb("onT", [128, 8, 128], BF16)
        t1 = sb("t1", [128, 1024])
        z = sb("z", [128, 1024])
        xo = sb("xo", [128, 1024])
        stt = sb("stt", [128, 8])
        psOT = ps("psOT", [128, 8, 128], BF16)
        psY = [ps("psY0", [128, 512]), ps("psY1", [128, 512])]
        p = Phase(kb, f"H3_{l}", n_iter=NTILES)
        dma(p, nc, "sp", "of", of[:], lambda i: OF[bass.ds(i * 128, 128), :], w=["of"])
        dma(p, nc, "act", "ob", ob[:], lambda i: OB[bass.ds(i * 128, 128), :], w=["ob"])
        dma(p, nc, "pool", "gt", gt[:], lambda i: GT[bass.ds(i * 128, 128), :], w=["gt"])
        dma(p, nc, "sp", "xt", xt[:], lambda i: X[bass.ds(i * 128, 128), :], w=["xt"])
        dma(p, nc, "act", "g1b", g1b[:], lambda i: MODT[bass.ds(i, 1), 2048:3072].partition_broadcast(128), w=["g1b"])
        tt(p, nc, "dve", of[:], of[:], ob[:], ALU.add, r=["of", "ob"], w=["of"])
        tt(p, nc, "pool", sq[:], of[:], of[:], ALU.mult, r=["of"], w=["sq"])
        p.op("dve", lambda i: nc.vector.tensor_reduce(ms[:], sq[:].rearrange("p (h v) -> p h v", v=128), AX.X, ALU.add), r=["sq"], w=["ms"])
        ts(p, nc, "dve", ms[:], ms[:], 1.0 / 128.0, None, ALU.mult, r=["ms"], w=["ms"])
        act(p, nc, ms[:], ms[:], AF.Ln, bias=b.eps_ln[:, 1:2], r=["ms"], w=["ms"])
        act(p, nc, ms[:], ms[:], AF.Exp, scale=-0.5, r=["ms"], w=["ms"])
        tt(p, nc, "dve", sq[:].rearrange("p (h v) -> p h v", v=128), of[:].rearrange("p (h v) -> p h v", v=128),
           ms[:].unsqueeze(2).to_broadcast([128, 8, 128]), ALU.mult, r=["of", "ms", "sq"], w=["sq"])
        tt(p, nc, "pool", sq[:], sq[:], gnb[:], ALU.mult, r=["sq"], w=["sq"])
        act(p, nc, gt[:], gt[:], AF.Silu, r=["gt"], w=["gt"])
        tt(p, nc, "dve", on[:], sq[:], gt[:], ALU.mult, r=["sq", "gt"], w=["on"])
        for kc in range(8):
            tr(p, nc, psOT[:, kc, :], on[:, kc * 128:(kc + 1) * 128], identb[:], r=["on"], w=["psOT"])
        cp(p, nc, "act", onT[:], psOT[:], r=["psOT"], w=["onT"])
        for n in range(2):
            for kc in range(8):
                mm(p, nc, psY[n][:], onT[:, kc, :], wout[:, kc, n * 512:(n + 1) * 512], kc == 0, kc == 7, r=["onT"], w=[f"psY{n}"])
        ts(p, nc, "pool", g1b[:], g1b[:], 1.0, None, ALU.add, r=["g1b"], w=["g1b"])
        for n in range(2):
            tt(p, nc, "dve", t1[:, n * 512:(n + 1) * 512], psY[n][:], g1b[:, n * 512:(n + 1) * 512], ALU.mult,
               r=[f"psY{n}", "g1b"], w=["t1"])
        p.op("dve", lambda i: nc.vector.scalar_tensor_tensor(z[:], xt[:], alpha, t1[:], ALU.mult, ALU.add), r=["xt", "t1"], w=["z"])
        layer_norm_tail(b, p, z, "z", lng, lnb, xo, "xo", dict(sq=sq, st=stt), "ln")
        dma(p, nc, "sp", "xo", lambda i: X[bass.ds(i * 128, 128), :], xo[:], r=["xo"])
        p.run()
```

```python
import math
from collections import Counter
from contextlib import ExitStack
import numpy as np
import ml_dtypes
import concourse.bass as bass
import concourse.mybir as mybir
from concourse.bass_utils import run_bass_kernel_spmd

F32 = mybir.dt.float32
BF16 = mybir.dt.bfloat16
I32 = mybir.dt.int32
AF = mybir.ActivationFunctionType
ALU = mybir.AluOpType
AX = mybir.AxisListType

D = 1024
HD = 64
N_GROUPS = 4
EPG = 8
NEXP = 32
DEXP = 512
LN_EPS = 1e-5
RMS_EPS = 1e-6
ROPE_THETA = 10000.0


class KB:
    def __init__(self, nc):
        self.nc = nc
        self.engs = {"pe": nc.tensor, "act": nc.scalar, "dve": nc.vector, "pool": nc.gpsimd, "sp": nc.sync}
        self.sems = {}
        self.base = {}
        for e in self.engs:
            self.sems[("e", e)] = nc.alloc_semaphore(name="S_" + e)
            self.base[("e", e)] = 0
        self.chan_owner = {}
        self.nphase = 0

    def sig(self, key):
        if key not in self.sems:
            self.sems[key] = self.nc.alloc_semaphore(name="C_" + str(key[1]))
            self.base[key] = 0
        return self.sems[key]


class Phase:
    def __init__(self, kb, name, n_iter=None):
        self.kb = kb
        self.name = name
        self.n_iter = n_iter
        self.ops = []

    ns = ""

    def _n(self, name):
        return name[1:] if name.startswith("!") else name + self.ns

    def op(self, eng, fn, r=(), w=(), chan=None):
        self.ops.append(dict(eng=eng, fn=fn, r=tuple(self._n(x) for x in r), w=tuple(self._n(x) for x in w),
                             chan=(chan + self.ns) if chan else None))

    def dma(self, eng, chan, fn, r=(), w=()):
        self.op(eng, fn, r, w, chan=chan)

    def interleave(self, a, b_, c):
        A, Bq = self.ops[a:b_], self.ops[b_:c]
        out = []
        for k in range(max(len(A), len(Bq))):
            if k < len(A):
                out.append(A[k])
            if k < len(Bq):
                out.append(Bq[k])
        self.ops[a:c] = out

    def run(self):
        kb = self.kb
        nc = kb.nc
        ops = self.ops
        m = len(ops)
        if m == 0:
            return
        loop = self.n_iter is not None
        N = self.n_iter if loop else 1
        sig_of = [("c", o["chan"]) if o["chan"] else ("e", o["eng"]) for o in ops]
        for j, o in enumerate(ops):
            if o["chan"]:
                own = kb.chan_owner.setdefault(o["chan"], o["eng"])
                assert own == o["eng"], f"chan {o['chan']} issued from two engines"
        phys = {}
        kcnt = {"sw": 0, "hw": 0}
        for j, o in enumerate(ops):
            sg_ = sig_of[j]
            if sg_ in phys:
                continue
            if sg_[0] == "e":
                phys[sg_] = sg_
            else:
                kind = "sw" if o["eng"] == "pool" else "hw"
                phys[sg_] = ("c", f"{kind}{kcnt[kind]}")
                kcnt[kind] += 1
        per_iter = Counter(sig_of)
        kidx = []
        cnt = Counter()
        for j in range(m):
            kidx.append(cnt[sig_of[j]])
            cnt[sig_of[j]] += 1
        last_w = {}
        readers = {}
        deps = [set() for _ in range(m)]
        for it in ((0, 1) if loop else (0,)):
            for j, o in enumerate(ops):
                d = set()
                for b in o["r"]:
                    if b in last_w:
                        d.add(last_w[b] + ("raw",))
                for b in o["w"]:
                    if b in last_w:
                        d.add(last_w[b] + ("waw",))
                    for rd in readers.get(b, []):
                        d.add(rd + ("war",))
                if it == (1 if loop else 0):
                    deps[j] = {(jj, itt - it, ty) for (itt, jj, ty) in d if (itt, jj) != (it, j)}
                for b in o["r"]:
                    readers.setdefault(b, []).append((it, j))
                for b in o["w"]:
                    last_w[b] = (it, j)
                    readers[b] = []
        waits = []
        for j, o in enumerate(ops):
            need = {}
            for (jj, off, ty) in deps[j]:
                p = ops[jj]
                s = sig_of[jj]
                if s[0] == "e" and sig_of[j][0] == "e" and p["eng"] == o["eng"]:
                    if o["eng"] == "pe" or ty != "raw":
                        continue
                n_s = per_iter[s]
                if s[0] == "c":
                    if off == 0:
                        q = sum(1 for x in range(j) if sig_of[x] == s)
                        const = q * 16
                    else:
                        const = 0
                else:
                    const = (kidx[jj] + 1) if off == 0 else (kidx[jj] + 1 - n_s)
                if s not in need or const > need[s]:
                    need[s] = const
            waits.append(need)
        known = {}
        for j, o in enumerate(ops):
            kn = known.setdefault(o["eng"], {})
            for s in list(waits[j]):
                if s in kn and kn[s] >= waits[j][s]:
                    del waits[j][s]
                else:
                    kn[s] = waits[j][s]
        T0 = {}
        for s, n_s in per_iter.items():
            kb.sig(phys[s])
            inc = 16 if s[0] == "c" else 1
            T0[s] = kb.base[phys[s]] + (n_s * inc if (loop and s[0] == "e") else 0)
        engines_used = []
        for o in ops:
            if o["eng"] not in engines_used:
                engines_used.append(o["eng"])

        def free_ids(E):
            hs, ids = [], []
            while True:
                try:
                    kb.probe_cnt = getattr(kb, "probe_cnt", 0) + 1
                    h = E.alloc_register(f"probe{kb.probe_cnt}")
                except Exception:
                    break
                hs.append(h)
                ids.append(nc.lookup_reg(h).reg_id)
            for h in hs:
                E.free_register(h)
            return set(ids)

        def emit_engine(ename):
            E = kb.engs[ename]
            before = free_ids(E)
            emit_engine_inner(ename)
            leaked = sorted(before - free_ids(E))
            for rid in leaked:
                kb.probe_cnt += 1
                h = nc.add_register(E.engine, f"leak{kb.probe_cnt}", rid)
                E.free_register(h)

        def emit_engine_inner(ename):
            E = kb.engs[ename]
            mine = [j for j, o in enumerate(ops) if o["eng"] == ename]
            wsigs = []
            for j in mine:
                for s in waits[j]:
                    if s not in wsigs:
                        wsigs.append(s)

            def body(i, regs, tmp):
                for j in mine:
                    o = ops[j]
                    for s, const in waits[j].items():
                        if i is None:
                            E.wait_ge(kb.sig(phys[s]), T0[s] + const)
                        elif const == 0:
                            E.wait_ge(kb.sig(phys[s]), regs[s])
                        else:
                            E.reg_add(tmp, regs[s], const)
                            E.wait_ge(kb.sig(phys[s]), tmp)
                    ins = o["fn"](i)
                    s = sig_of[j]
                    ins.then_inc(kb.sig(phys[s]), 16 if s[0] == "c" else 1)

            if loop:
                if ("e", ename) in per_iter:
                    E.sem_inc(kb.sig(("e", ename)), per_iter[("e", ename)])
                regs = {}
                for k, s in enumerate(wsigs):
                    regs[s] = E.alloc_register(f"w{kb.nphase}_{ename}_{k}")
                    E.reg_mov(regs[s], T0[s])
                tmp = E.alloc_register(f"wt{kb.nphase}_{ename}")
                with E.Fori(0, N) as i:
                    body(i, regs, tmp)
                    for s in wsigs:
                        E.reg_add(regs[s], regs[s], per_iter[s] * (16 if s[0] == "c" else 1))
                for s in wsigs:
                    E.free_register(regs[s])
                E.free_register(tmp)
            else:
                body(None, None, None)
            for s, n_s in per_iter.items():
                owner = kb.chan_owner[s[1]] if s[0] == "c" else s[1]
                if owner == ename:
                    inc = 16 if s[0] == "c" else 1
                    E.wait_ge(kb.sig(phys[s]), T0[s] + N * n_s * inc)

        with nc.Block() as blk:
            reg = {"pe": blk.tensor, "act": blk.scalar, "dve": blk.vector, "pool": blk.gpsimd, "sp": blk.sync}
            for ename in engines_used:
                reg[ename](lambda eng, ename=ename: emit_engine(ename))
        for s, n_s in per_iter.items():
            inc = 16 if s[0] == "c" else 1
            kb.base[phys[s]] = T0[s] + N * n_s * inc
        nc.all_engine_barrier()
        kb.nphase += 1


def _bf(a):
    return np.ascontiguousarray(a).astype(ml_dtypes.bfloat16)


def host_constants(cfg):
    seqs = cfg["seqs"]
    NT = sum(seqs)
    c = {}
    c["ident_f"] = np.eye(128, dtype=np.float32)
    c["ident_b"] = _bf(np.eye(128, dtype=np.float32))
    pos = np.concatenate([np.arange(t) for t in seqs]).astype(np.float32)
    inv = np.power(ROPE_THETA, -np.arange(0, HD, 2, dtype=np.float32) / HD).astype(np.float32)
    d = np.arange(128) % 64
    ang = pos[None, :] * inv[d % 32][:, None]
    c["rope_cos"] = np.cos(ang).astype(np.float32)
    sgn = np.where(d < 32, -1.0, 1.0).astype(np.float32)
    c["rope_sin"] = (np.sin(ang) * sgn[:, None]).astype(np.float32)
    j = np.arange(128)[:, None]
    i = np.arange(128)[None, :]
    c["wmask"] = _bf(np.stack([(j >= i), (j <= i)], axis=1).astype(np.float32))
    kc = np.arange(64)
    qc = np.arange(64)
    qwin = np.clip(qc - 8, 0, 48)
    col_ok = (kc[:, None] >= qwin[None, :]) & (kc[:, None] < qwin[None, :] + 16)
    m = np.zeros((128, 9, 128), np.float32)
    for t in range(9):
        for b in range(2):
            for a in range(2):
                ok = col_ok.astype(np.float32)
                if t == 7 and (a == 1 and b == 0):
                    ok = ok * 0
                if t == 8 and not (a == 1 and b == 0):
                    ok = ok * 0
                m[b * 64:(b + 1) * 64, t, a * 64:(a + 1) * 64] = ok
    c["namask"] = m
    s = np.arange(64)[:, None]
    t = np.arange(64)[None, :]
    c["hmask"] = _bf(np.stack([(s <= t), (s >= t)], axis=1).astype(np.float32))
    sm = np.ones((128, 8, 128), np.float32)
    sm[:, :, 0] = 0
    sm[:, :, 64] = 0
    c["scanmask"] = sm.reshape(128, 1024)
    c["iota32"] = np.tile(np.arange(32, dtype=np.float32)[None, :], (128, 1))
    c["iota_blk"] = np.tile((np.arange(256, dtype=np.float32) * 128.0)[None, :], (128, 1))
    c["iota_p"] = np.arange(128, dtype=np.float32).reshape(128, 1)
    c["tri_su"] = _bf((np.arange(128)[:, None] < np.arange(128)[None, :]).astype(np.float32))
    c["ones_b"] = _bf(np.ones((128, 128), np.float32))
    return c


NA_TABLE_DELTA = [-3, -2, -1, 0, 1, 2, 3, -2, 2]


def na_bias_tables(rel_bias):
    L = rel_bias.shape[0]
    kc = np.arange(64)[:, None]
    qc = np.arange(64)[None, :]
    dc = np.clip(kc - qc, -15, 15) + 15
    out = np.zeros((L, 128, 9, 8, 128), np.float32)
    for t, dl in enumerate(NA_TABLE_DELTA):
        for b in range(2):
            for a in range(2):
                dr = 2 * dl + b - a + 7
                g = rel_bias[:, :, dr, :][:, :, dc]
                out[:, b * 64:(b + 1) * 64, t, :, a * 64:(a + 1) * 64] = g.transpose(0, 2, 1, 3)
    return out


def attn_w_layout(w_in):
    L = w_in.shape[0]
    qa0, ka0, va0, qb0, kb0, vb0 = 0, 512, 640, 768, 1280, 1792
    sw = (np.arange(64) + 32) % 64
    cols = []
    for jc in range(4):
        for h in (jc, 4 + jc):
            cols += list(qa0 + h * 64 + np.arange(64))
    for jc in range(4):
        for h in (jc, 4 + jc):
            cols += list(qa0 + h * 64 + sw)
    for h in range(2):
        cols += list(ka0 + h * 64 + np.arange(64))
    for h in range(2):
        cols += list(ka0 + h * 64 + sw)
    cols += list(qb0 + np.arange(512))
    cols += list(kb0 + np.arange(512))
    cols = np.array(cols)
    vcols = np.concatenate([va0 + np.arange(128), vb0 + np.arange(512)])
    return np.ascontiguousarray(w_in[:, :, cols]), np.ascontiguousarray(w_in[:, :, vcols])


class B:
    def __init__(self, cfg):
        self.cfg = cfg
        self.nc = bass.Bass("TRN2", target_bir_lowering=False)
        self.kb = KB(self.nc)
        self.dram = {}
        self.uid = 0

    def din(self, name, shape, dt=F32):
        t = self.nc.dram_tensor(name, list(shape), dt, kind="ExternalInput")
        self.dram[name] = t
        return t

    def dout(self, name, shape, dt=F32):
        t = self.nc.dram_tensor(name, list(shape), dt, kind="ExternalOutput")
        self.dram[name] = t
        return t

    def dscr(self, name, shape, dt=F32):
        if self.cfg.get("debug"):
            return self.dout(name, shape, dt)
        t = self.nc.dram_tensor(name, list(shape), dt)
        self.dram[name] = t
        return t


def un(b, name):
    b.uid += 1
    return f"{name}_{b.uid}"


def _ap(x, i):
    return x(i) if callable(x) else x


def dma(p, nc, eng, chan, out, in_, r=(), w=(), slow=False):
    E = {"sp": nc.sync, "act": nc.scalar, "pool": nc.gpsimd}[eng]
    if slow:
        p.dma(eng, chan, lambda i: E.dma_start(out=_ap(out, i), in_=_ap(in_, i), allow_slow_non_contiguous=True), r=r, w=w)
    else:
        p.dma(eng, chan, lambda i: E.dma_start(out=_ap(out, i), in_=_ap(in_, i)), r=r, w=w)


def eng_of(nc, e):
    return {"dve": nc.vector, "pool": nc.gpsimd, "act": nc.scalar}[e]


def tt(p, nc, e, out, in0, in1, op, r=(), w=()):
    E = eng_of(nc, e)
    p.op(e, lambda i: E.tensor_tensor(_ap(out, i), _ap(in0, i), _ap(in1, i), op), r=r, w=w)


def ts(p, nc, e, out, in0, s1, s2, op0, op1=None, r=(), w=()):
    E = eng_of(nc, e)
    if op1 is None:
        p.op(e, lambda i: E.tensor_scalar(_ap(out, i), _ap(in0, i), s1, None, op0), r=r, w=w)
    else:
        p.op(e, lambda i: E.tensor_scalar(_ap(out, i), _ap(in0, i), s1, s2, op0, op1), r=r, w=w)


def cp(p, nc, e, out, in_, r=(), w=()):
    if e == "act":
        p.op(e, lambda i: nc.scalar.copy(_ap(out, i), _ap(in_, i)), r=r, w=w)
    else:
        E = eng_of(nc, e)
        p.op(e, lambda i: E.tensor_copy(_ap(out, i), _ap(in_, i)), r=r, w=w)


def act(p, nc, out, in_, func, bias=None, scale=None, r=(), w=()):
    kw = {}
    if bias is not None:
        kw["bias"] = bias
    if scale is not None:
        kw["scale"] = scale
    p.op("act", lambda i: nc.scalar.activation(_ap(out, i), _ap(in_, i), func, **kw), r=r, w=w)


def mm(p, nc, out, lhsT, rhs, start, stop, r=(), w=()):
    p.op("pe", lambda i: nc.tensor.matmul(_ap(out, i), lhsT=_ap(lhsT, i), rhs=_ap(rhs, i), start=start, stop=stop), r=r, w=w)


def tr(p, nc, out, in_, ident, r=(), w=()):
    p.op("pe", lambda i: nc.tensor.transpose(_ap(out, i), _ap(in_, i), ident), r=r, w=w)


def load_weight_bf16(b, p, dst, src, ncols, tag, stg, kdim=8):
    nc = b.nc
    engs = ["dve", "act", "pool"]
    c0 = 0
    k = 0
    while c0 < ncols:
        cw = min(512, ncols - c0)
        st = stg[k % 2]
        sname = f"stg{k % 2}"
        dma(p, nc, "sp" if k % 2 == 0 else "act", f"wstg{k % 2}", st[:, :, 0:cw], src[:, :, c0:c0 + cw], w=[sname])
        cp(p, nc, engs[k % 3], dst[:, :, c0:c0 + cw], st[:, :, 0:cw], r=[sname], w=[tag + str(k)])
        c0 += cw
        k += 1


def layer_norm_tail(b, p, z, zname, lng, lnb, out, outname, scr, tg):
    nc = b.nc
    sq, st = scr["sq"], scr["st"]
    p.op("dve", lambda i: nc.vector.tensor_reduce(st[:, 0:1], z[:], AX.X, ALU.add), r=[zname], w=[tg + "s0"])
    tt(p, nc, "pool", sq[:], z[:], z[:], ALU.mult, r=[zname], w=[tg + "sq"])
    p.op("dve", lambda i: nc.vector.tensor_reduce(st[:, 1:2], sq[:], AX.X, ALU.add), r=[tg + "sq"], w=[tg + "s1"])
    ts(p, nc, "dve", st[:, 2:3], st[:, 0:1], 1.0 / D, None, ALU.mult, r=[tg + "s0"], w=[tg + "s2"])
    ts(p, nc, "dve", st[:, 3:4], st[:, 1:2], 1.0 / D, None, ALU.mult, r=[tg + "s1"], w=[tg + "s3"])
    tt(p, nc, "dve", st[:, 4:5], st[:, 2:3], st[:, 2:3], ALU.mult, r=[tg + "s2"], w=[tg + "s4"])
    tt(p, nc, "dve", st[:, 5:6], st[:, 3:4], st[:, 4:5], ALU.subtract, r=[tg + "s3", tg + "s4"], w=[tg + "s5"])
    act(p, nc, st[:, 6:7], st[:, 5:6], AF.Ln, bias=b.eps_ln[:, 0:1], r=[tg + "s5"], w=[tg + "s6"])
    act(p, nc, st[:, 6:7], st[:, 6:7], AF.Exp, scale=-0.5, r=[tg + "s6"], w=[tg + "s6"])
    p.op("dve", lambda i: nc.vector.scalar_tensor_tensor(st[:, 7:8], st[:, 2:3], -1.0, st[:, 6:7], ALU.mult, ALU.mult),
         r=[tg + "s2", tg + "s6"], w=[tg + "s7"])
    act(p, nc, sq[:], z[:], AF.Identity, bias=st[:, 7:8], scale=st[:, 6:7], r=[zname, tg + "s6", tg + "s7", tg + "sq"], w=[tg + "sq"])
    tt(p, nc, "dve", sq[:], sq[:], lng[:], ALU.mult, r=[tg + "sq"], w=[tg + "sq"])
    tt(p, nc, "pool", out[:], sq[:], lnb[:], ALU.add, r=[tg + "sq"], w=[outname])


def build_program(cfg):
    seqs = cfg["seqs"]
    NT = sum(seqs)
    NTILES = NT // 128
    depth = cfg["depth"]
    LA = (depth + 1) // 2
    LR = depth // 2
    stop = cfg.get("stop")
    alpha = float((2 * cfg.get("dn_depth", depth)) ** 0.25)
    b = B(cfg)
    nc = b.nc
    kb = b.kb

    x_in = b.din("x", [NT, D])
    cT_in = b.din("cT", [128, 3, 8])
    ada_w = b.din("ada_w", [depth, D, 6 * D])
    ada_b = b.din("ada_b", [depth, 6 * D])
    ln_g = b.din("ln_g", [depth, 2, D])
    ln_b = b.din("ln_b", [depth, 2, D])
    a_wfm = b.din("attn_wfm", [LA, D, 2304])
    a_wv = b.din("attn_wv", [LA, D, 640])
    a_sink = b.din("attn_sink", [LA, 8])
    a_rb = b.din("na_rb", [LA, 128, 9, 8, 128])
    a_wout = b.din("attn_w_out", [LA, D, D])
    r_win = b.din("rec_w_in", [max(LR, 1), D, 5 * D])
    r_lb = b.din("rec_lb", [max(LR, 1), D])
    r_gn = b.din("rec_gnorm", [max(LR, 1), D])
    r_wout = b.din("rec_w_out", [max(LR, 1), D, D])
    ro_w = b.din("router_w", [depth, D, 36])
    ro_b = b.din("router_b", [depth, 36])
    e_wg = b.din("e_wg", [depth * NEXP * 128, 4096])
    e_wu = b.din("e_wu", [depth * NEXP * 128, 4096])
    e_wd = b.din("e_wd", [depth * NEXP * 128, 4096])
    cn = {}
    for name, shape, dt in [("ident_f", [128, 128], F32), ("ident_b", [128, 128], BF16), ("rope_cos", [128, NT], F32),
                            ("rope_sin", [128, NT], F32), ("wmask", [128, 2, 128], BF16), ("namask", [128, 9, 128], F32),
                            ("hmask", [64, 2, 64], BF16), ("scanmask", [128, 1024], F32), ("iota32", [128, 32], F32),
                            ("iota_blk", [128, 256], F32), ("iota_p", [128, 1], F32), ("tri_su", [128, 128], BF16),
                            ("ones_b", [128, 128], BF16)]:
        cn[name] = b.din(name, shape, dt)
    y_out = b.dout("y", [NT, D])
    X = b.dscr("X", [NT, D])
    MODT = b.dscr("MODT", [NTILES, 6 * D])
    FM = b.dscr("FM", [13, 128, NT], BF16)
    VE = b.dscr("VE", [NT, 720], BF16)

    ODBG = b.dout("ODBG", [NT, D], BF16) if cfg.get("debug") else None
    seq_tile0 = [0]
    for t in seqs[:-1]:
        seq_tile0.append(seq_tile0[-1] + t // 128)

    with ExitStack() as gs:
        def sb(name, shape, dt=F32, stack=gs):
            return stack.enter_context(nc.sbuf_tensor(un(b, name), list(shape), dt))

        def ps(name, shape, dt=F32, stack=gs):
            return stack.enter_context(nc.psum_tensor(un(b, name), list(shape), dt))

        identf = sb("identf", [128, 128])
        identb = sb("identb", [128, 128], BF16)
        CS = sb("CS", [128, 24, 128])
        epst = sb("epst", [128, 2])
        b.eps_ln = epst
        csb = sb("csb", [128, 24])
        p = Phase(kb, "setup")
        p.op("pool", lambda i: nc.gpsimd.memset(epst[:, 0:1], LN_EPS), w=["epst0"])
        p.op("pool", lambda i: nc.gpsimd.memset(epst[:, 1:2], RMS_EPS), w=["epst1"])
        dma(p, nc, "sp", "k0", identf[:], cn["ident_f"][:, :], w=["identf"])
        dma(p, nc, "sp", "k1", identb[:], cn["ident_b"][:, :], w=["identb"])
        dma(p, nc, "sp", "k2", csb[:], cT_in.ap().rearrange("p s k -> p (s k)"), w=["csb"])
        act(p, nc, csb[:], csb[:], AF.Silu, r=["csb"], w=["csb"])
        cp(p, nc, "dve", CS[:], csb[:].unsqueeze(2).to_broadcast([128, 24, 128]), r=["csb"], w=["CS"])
        p.run()

        p = Phase(kb, "xcopy")
        nchunk = max(1, NT // 1024)
        rows = NT // nchunk
        for k in range(nchunk):
            dma(p, nc, ["sp", "act", "pool"][k % 3], f"xc{k % 3}", X[k * rows:(k + 1) * rows, :], x_in[k * rows:(k + 1) * rows, :])
        p.run()

        for l in range(depth):
            with ExitStack() as st:
                aw = sb("aw", [128, 8, 512], stack=st)
                ab = sb("ab", [128, 512], stack=st)
                mo = [sb(f"mo{s}", [128, 512], stack=st) for s in range(3)]
                pm = [ps(f"pm{s}", [128, 512], stack=st) for s in range(3)]
                p = Phase(kb, f"mod{l}", n_iter=12)
                dma(p, nc, "sp", "aw", aw[:], lambda i: ada_w[l].rearrange("(kc p) n -> p kc n", p=128)[:, :, bass.ds(i * 512, 512)], w=["aw"])
                dma(p, nc, "act", "ab", ab[:], lambda i: ada_b[l:l + 1, bass.ds(i * 512, 512)].partition_broadcast(128), w=["ab"])
                for s in range(3):
                    for kc in range(8):
                        mm(p, nc, pm[s][:], CS[:, s * 8 + kc, :], aw[:, kc, :], kc == 0, kc == 7, r=["aw"], w=[f"pm{s}"])
                    tt(p, nc, "dve", mo[s][:], pm[s][:], ab[:], ALU.add, r=[f"pm{s}", "ab"], w=[f"mo{s}"])
                    nts = seqs[s] // 128
                    dma(p, nc, "pool", f"mo{s}", lambda i, s=s, nts=nts: MODT[seq_tile0[s]:seq_tile0[s] + nts, bass.ds(i * 512, 512)],
                        mo[s][0:nts, :], r=[f"mo{s}"])
                p.run()
            if stop == "mod":
                break
            if l % 2 == 0:
                build_attention_layer(b, l, l // 2, dict(X=X, MODT=MODT, FM=FM, VE=VE, a_wfm=a_wfm, a_wv=a_wv, a_sink=a_sink,
                                                         a_rb=a_rb, a_wout=a_wout, ln_g=ln_g, ln_b=ln_b, cn=cn, identb=identb,
                                                         identf=identf, alpha=alpha, seq_tile0=seq_tile0, ODBG=ODBG))
            else:
                build_hgrn_layer(b, l, l // 2, dict(X=X, MODT=MODT, r_win=r_win, r_lb=r_lb, r_gn=r_gn, r_wout=r_wout,
                                                    ln_g=ln_g, ln_b=ln_b, cn=cn, identb=identb, identf=identf, alpha=alpha,
                                                    seq_tile0=seq_tile0))
            if stop in (f"mix{l}", "attw", "A1", "attpre", "A2e", "H1", "H2"):
                break
            build_moe_layer(b, l, dict(X=X, MODT=MODT, ro_w=ro_w, ro_b=ro_b, e_wg=e_wg, e_wu=e_wu, e_wd=e_wd, ln_g=ln_g,
                                       ln_b=ln_b, cn=cn, identb=identb, identf=identf, alpha=alpha))
            if stop in (f"moe{l}", "moeD", "moeS", "moeB"):
                break

        p = Phase(kb, "ycopy")
        for k in range(nchunk):
            dma(p, nc, ["sp", "act", "pool"][k % 3], f"yc{k % 3}", y_out[k * rows:(k + 1) * rows, :], X[k * rows:(k + 1) * rows, :])
        p.run()
    return nc


def build_attention_layer(b, l, la, g):
    nc, kb = b.nc, b.kb
    seqs = b.cfg["seqs"]
    NT = sum(seqs)
    X, MODT, FM, VE, cn = g["X"], g["MODT"], g["FM"], g["VE"], g["cn"]
    identb = g["identb"]
    alpha = g["alpha"]
    seq_tile0 = g["seq_tile0"]

    with ExitStack() as st:
        def sb(name, shape, dt=F32):
            return st.enter_context(nc.sbuf_tensor(un(b, name), list(shape), dt))

        def ps(name, shape, dt=F32):
            return st.enter_context(nc.psum_tensor(un(b, name), list(shape), dt))

        wfm = sb("wfm", [128, 8, 2304], BF16)
        wv = sb("wv", [128, 8, 640], BF16)
        stg = [sb("stg0", [128, 8, 512]), sb("stg1", [128, 8, 512])]
        veall = sb("veall", [128, 4, 10, 72], BF16)
        p = Phase(kb, f"attw{l}")
        load_weight_bf16(b, p, wfm, g["a_wfm"][la].rearrange("(kc p) n -> p kc n", p=128), 2304, "wfm", stg)
        load_weight_bf16(b, p, wv, g["a_wv"][la].rearrange("(kc p) n -> p kc n", p=128), 640, "wv", stg)
        p.op("pool", lambda i: nc.gpsimd.memset(veall[:], 1.0), w=["veall"])
        p.run()
        if b.cfg.get("stop") == "attw":
            return

        xg = sb("xg", [128, 4, 1024])
        scb = sb("scb", [128, 1024])
        shb = sb("shb", [128, 1024])
        tmp = [sb("tmp0", [128, 1024]), sb("tmp1", [128, 1024])]
        hb = sb("hb", [128, 4, 1024], BF16)
        hT = sb("hT", [128, 8, 512], BF16)
        ropec = sb("ropec", [128, 512])
        ropes = sb("ropes", [128, 512])
        t1 = sb("t1", [128, 512])
        t2 = sb("t2", [128, 512])
        fmall = sb("fmall", [128, 13, 512], BF16)
        psT = [ps("psT0", [128, 8, 128], BF16), ps("psT1", [128, 8, 128], BF16)]
        pf = [ps(f"pf{k}", [128, 512]) for k in range(3)]
        pva = ps("pva", [128, 512])
        pvb = ps("pvb", [128, 512])

        NG = NT // 512
        p = Phase(kb, f"A1_{l}", n_iter=NG)
        dma(p, nc, "sp", "xg", xg[:], lambda i: X[bass.ds(i * 512, 512), :].rearrange("(t p) d -> p t d", p=128), w=["xg"])
        dma(p, nc, "act", "scb", scb[:], lambda i: MODT[bass.ds(i * 4, 1), 1024:2048].partition_broadcast(128), w=["scb"])
        dma(p, nc, "act", "shb", shb[:], lambda i: MODT[bass.ds(i * 4, 1), 0:1024].partition_broadcast(128), w=["shb"])
        dma(p, nc, "pool", "ropec", ropec[:], lambda i: cn["rope_cos"][:, bass.ds(i * 512, 512)], w=["ropec"])
        dma(p, nc, "pool", "ropes", ropes[:], lambda i: cn["rope_sin"][:, bass.ds(i * 512, 512)], w=["ropes"])
        ts(p, nc, "dve", scb[:], scb[:], 1.0, None, ALU.add, r=["scb"], w=["scb"])
        for t in range(4):
            tt(p, nc, "dve", tmp[t % 2][:], xg[:, t, :], scb[:], ALU.mult, r=["xg", "scb"], w=[f"tmp{t % 2}"])
            tt(p, nc, "pool", hb[:, t, :], tmp[t % 2][:], shb[:], ALU.add, r=[f"tmp{t % 2}", "shb"], w=[f"hb{t}"])
            for kc in range(8):
                tr(p, nc, psT[t % 2][:, kc, :], hb[:, t, kc * 128:(kc + 1) * 128], identb[:], r=[f"hb{t}"], w=[f"psT{t % 2}"])
            cp(p, nc, "act", hT[:, :, t * 128:(t + 1) * 128], psT[t % 2][:], r=[f"psT{t % 2}"], w=[f"hT{t}"])
        hTall = [f"hT{t}" for t in range(4)]
        pfi = 0
        fk = 0
        for (ca, cb_, dst) in [(0, 4, 0), (1, 5, 1), (2, 6, 2), (3, 7, 3), (8, 9, 4)]:
            ia, ib = pfi % 3, (pfi + 1) % 3
            pfi += 2
            for kc in range(8):
                mm(p, nc, pf[ia][:], wfm[:, kc, ca * 128:(ca + 1) * 128], hT[:, kc, :], kc == 0, kc == 7, r=hTall, w=[f"pf{ia}"])
            for kc in range(8):
                mm(p, nc, pf[ib][:], wfm[:, kc, cb_ * 128:(cb_ + 1) * 128], hT[:, kc, :], kc == 0, kc == 7, r=hTall, w=[f"pf{ib}"])
            tt(p, nc, "dve", t1[:], pf[ia][:], ropec[:], ALU.mult, r=[f"pf{ia}", "ropec"], w=["t1"])
            tt(p, nc, "dve", t2[:], pf[ib][:], ropes[:], ALU.mult, r=[f"pf{ib}", "ropes"], w=["t2"])
            tt(p, nc, "pool", fmall[:, dst, :], t1[:], t2[:], ALU.add, r=["t1", "t2"], w=[f"fm{dst}"])
        for c in range(10, 18):
            dst = 5 + (c - 10)
            ia = pfi % 3
            pfi += 1
            for kc in range(8):
                mm(p, nc, pf[ia][:], wfm[:, kc, c * 128:(c + 1) * 128], hT[:, kc, :], kc == 0, kc == 7, r=hTall, w=[f"pf{ia}"])
            cp(p, nc, "act", fmall[:, dst, :], pf[ia][:], r=[f"pf{ia}"], w=[f"fm{dst}"])
        for t in range(4):
            for kc in range(8):
                mm(p, nc, pva[:], hT[:, kc, t * 128:(t + 1) * 128], wv[:, kc, 128:640], kc == 0, kc == 7, r=[f"hT{t}"], w=["pva"])
            for kc in range(8):
                mm(p, nc, pvb[:, 0:128], hT[:, kc, t * 128:(t + 1) * 128], wv[:, kc, 0:128], kc == 0, kc == 7, r=[f"hT{t}"], w=["pvb"])
            cp(p, nc, "dve", veall[:, t, 2:10, 0:64], pva[:].rearrange("p (h d) -> p h d", d=64), r=["pva"], w=[f"ve{t}"])
            cp(p, nc, "act", veall[:, t, 0:2, 0:64], pvb[:, 0:128].rearrange("p (h d) -> p h d", d=64), r=["pvb"], w=[f"ve{t}"])
        dma(p, nc, "sp", "fmall", lambda i: FM[:, :, bass.ds(i * 512, 512)].rearrange("f p t -> p f t"), fmall[:],
            r=[f"fm{d_}" for d_ in range(13)])
        dma(p, nc, "pool", "veall", lambda i: VE[bass.ds(i * 512, 512), :].rearrange("(t p) c -> p t c", p=128),
            veall[:].rearrange("p t h d -> p t (h d)"), r=[f"ve{t}" for t in range(4)])
        p.run()
    if b.cfg.get("stop") == "A1":
        return

    with ExitStack() as st:
        def sb(name, shape, dt=F32):
            return st.enter_context(nc.sbuf_tensor(un(b, name), list(shape), dt))

        def ps(name, shape, dt=F32):
            return st.enter_context(nc.psum_tensor(un(b, name), list(shape), dt))

        wout = sb("wout", [128, 8, 1024], BF16)
        EB = sb("EB", [128, 9, 8, 128], BF16)
        skx = sb("skx", [128, 8])
        wm = sb("wm", [128, 2, 128], BF16)
        lng = sb("lng", [128, 1024])
        lnb = sb("lnb", [128, 1024])
        with ExitStack() as st2:
            stg = [st2.enter_context(nc.sbuf_tensor(un(b, "stg0"), [128, 8, 512], F32)),
                   st2.enter_context(nc.sbuf_tensor(un(b, "stg1"), [128, 8, 512], F32))]
            nam = st2.enter_context(nc.sbuf_tensor(un(b, "nam"), [128, 9, 128], F32))
            rbs = [st2.enter_context(nc.sbuf_tensor(un(b, "rbs0"), [128, 8, 128], F32)),
                   st2.enter_context(nc.sbuf_tensor(un(b, "rbs1"), [128, 8, 128], F32))]
            p = Phase(kb, f"attpre{l}")
            load_weight_bf16(b, p, wout, g["a_wout"][la].rearrange("(kc p) n -> p kc n", p=128), 1024, "wout", stg)
            dma(p, nc, "pool", "nam", nam[:], cn["namask"][:, :, :], w=["nam"])
            dma(p, nc, "pool", "wm", wm[:], cn["wmask"][:, :, :], w=["wm"])
            dma(p, nc, "pool", "skx", skx[:], g["a_sink"][la:la + 1, :].partition_broadcast(128), w=["skx"])
            dma(p, nc, "pool", "lng", lng[:], g["ln_g"][l, 0:1, :].partition_broadcast(128), w=["lng"])
            dma(p, nc, "pool", "lnb", lnb[:], g["ln_b"][l, 0:1, :].partition_broadcast(128), w=["lnb"])
            act(p, nc, skx[:], skx[:], AF.Exp, r=["skx"], w=["skx"])
            for t in range(9):
                rb = rbs[t % 2]
                dma(p, nc, "sp", f"rbs{t % 2}", rb[:], g["a_rb"][la, :, t, :, :], w=[f"rbs{t % 2}"])
                act(p, nc, rb[:], rb[:], AF.Exp, r=[f"rbs{t % 2}"], w=[f"rbs{t % 2}"])
                tt(p, nc, "dve", EB[:, t, :, :], rb[:], nam[:, t, :].unsqueeze(1).to_broadcast([128, 8, 128]), ALU.mult,
                   r=[f"rbs{t % 2}", "nam"], w=[f"EB{t}"])
            p.run()
        if b.cfg.get("stop") == "attpre":
            return

        qa = sb("qa", [128, 4, 128], BF16)
        ka = sb("ka", [128, 3, 128], BF16)
        va = sb("va", [128, 3, 2, 72], BF16)
        qb = sb("qb", [64, 8, 128], BF16)
        kbt = sb("kbt", [64, 8, 5, 128], BF16)
        vb = sb("vb", [128, 5, 8, 72], BF16)
        xt = sb("xt", [128, 1024])
        g1b = sb("g1b", [128, 1024])
        PT = [sb("PT0", [128, 5, 4, 128], BF16), sb("PT1", [128, 5, 4, 128], BF16)]
        o = sb("o", [128, 1024], BF16)
        oT = sb("oT", [128, 8, 128], BF16)
        t1 = sb("t1", [128, 1024])
        z = sb("z", [128, 1024])
        sq = sb("sq", [128, 1024])
        xo = sb("xo", [128, 1024])
        stt = sb("stt", [128, 8])
        den = sb("den", [128, 4, 1])
        pS = [ps("pS0", [128, 4, 128]), ps("pS1", [128, 4, 128])]
        accb = [ps("acc0", [128, 512]), ps("acc1", [128, 512])]
        acc = [a[:].rearrange("p (h d) -> p h d", d=128) for a in accb]
        psOT = ps("psOT", [128, 8, 128], BF16)
        psY = [ps("psY0", [128, 512]), ps("psY1", [128, 512])]
        o3 = o[:].rearrange("p (h d) -> p h d", d=64)

        def block(p, tok0, tile, wl, nl):
            def T(off):
                return (lambda i: (tok0(i) if callable(tok0) else tok0) + off)

            def TL(i):
                return tile(i) if callable(tile) else tile
            dma(p, nc, "sp", "qa", qa[:], lambda i: FM[0:4, :, bass.ds(T(0)(i), 128)].rearrange("f p t -> p f t"), w=["qa"])
            w0, w1 = wl[0], wl[-1]
            nw = w1 - w0 + 1
            dma(p, nc, "sp", "ka", ka[:, w0 + 1:w1 + 2, :].rearrange("p b t -> p (b t)"),
                lambda i: FM[4, :, bass.ds(T(w0 * 128)(i), nw * 128)], w=["ka"])
            dma(p, nc, "pool", "va", va[:, w0 + 1:w1 + 2, :, :].rearrange("p b h d -> p b (h d)"),
                lambda i: VE[bass.ds(T(w0 * 128)(i), nw * 128), 0:144].rearrange("(b p) c -> p b c", p=128), w=["va"])
            dma(p, nc, "sp", "qb", qb[:], lambda i: FM[5:9, :, bass.ds(T(0)(i), 128)].rearrange("f (two d) t -> d (f two) t", two=2), w=["qb"])
            n0 = nl[0][0]
            nn = len(nl)
            dma(p, nc, "act", "kbt", kbt[:, :, 0:nn, :].rearrange("p f b t -> p f (b t)"),
                lambda i: FM[9:13, :, bass.ds(T(n0 * 128)(i), nn * 128)].rearrange("f (two d) t -> d (f two) t", two=2), w=["kbt"])
            dma(p, nc, "pool", "vb", vb[:, 0:nn, :, :].rearrange("p b h d -> p b (h d)"),
                lambda i: VE[bass.ds(T(n0 * 128)(i), nn * 128), 144:720].rearrange("(b p) c -> p b c", p=128), w=["vb"])
            dma(p, nc, "sp", "xt", xt[:], lambda i: X[bass.ds(T(0)(i), 128), :], w=["xt"])
            dma(p, nc, "act", "g1b", g1b[:], lambda i: MODT[bass.ds(TL(i), 1), 2048:3072].partition_broadcast(128), w=["g1b"])
            lvl = b.cfg.get("a2lvl", 9)
            if lvl < 2:
                return
            u = 0
            for gk in range(2):
                a = acc[gk % 2]
                an = f"acc{gk % 2}"
                Pu = PT[gk % 2]
                pn = f"PT{gk % 2}_"
                for di, dl in enumerate(wl):
                    k = u % 2
                    u += 1
                    mm(p, nc, pS[k][:], ka[64 * gk:64 * gk + 64, dl + 1, :], qa[64 * gk:64 * gk + 64, :, :], True, True,
                       r=["ka", "qa"], w=[f"pS{k}"])
                    act(p, nc, Pu[:, di, :, :], pS[k][:], AF.Exp, scale=0.125, r=[f"pS{k}"], w=[pn + str(di)])
                    if dl != 0:
                        tt(p, nc, "dve", Pu[:, di, :, :], Pu[:, di, :, :],
                           wm[:, 0 if dl == -1 else 1, :].unsqueeze(1).to_broadcast([128, 4, 128]),
                           ALU.mult, r=[pn + str(di)], w=[pn + str(di)])
                for h in range(4):
                    for di, dl in enumerate(wl):
                        mm(p, nc, a[:, h, 0:66], Pu[:, di, h, :], va[:, dl + 1, gk, 0:66], di == 0, di == len(wl) - 1,
                           r=[pn + str(di), "va"], w=[an])
                tt(p, nc, "dve", den[:], a[:, :, 64:65], skx[:, 4 * gk:4 * gk + 4].unsqueeze(2), ALU.add, r=[an], w=["den"])
                p.op("dve", lambda i: nc.vector.reciprocal(den[:], den[:]), r=["den"], w=["den"])
                tt(p, nc, "dve", o3[:, 4 * gk:4 * gk + 4, :], a[:, :, 0:64], den[:].to_broadcast([128, 4, 64]), ALU.mult,
                   r=[an, "den"], w=["o"])
            if lvl < 3:
                return
            for hg in range(2):
                a = acc[hg % 2]
                an = f"acc{hg % 2}"
                Pu = PT[hg % 2]
                pn = f"PT{hg % 2}_"
                for di, (dl, tb) in enumerate(nl):
                    k = u % 2
                    u += 1
                    for uu in range(4):
                        h = 4 * hg + uu
                        mm(p, nc, pS[k][:, uu, :], kbt[:, h, di, :], qb[:, h, :],
                           True, True, r=["kbt", "qb"], w=[f"pS{k}"])
                    act(p, nc, Pu[:, di, :, :], pS[k][:], AF.Exp, scale=0.125, r=[f"pS{k}"], w=[pn + str(di)])
                    tt(p, nc, "dve" if di % 2 == 0 else "pool", Pu[:, di, :, :], Pu[:, di, :, :], EB[:, tb, 4 * hg:4 * hg + 4, :], ALU.mult,
                       r=[pn + str(di)], w=[pn + str(di)])
                for uu in range(4):
                    for di, (dl, tb) in enumerate(nl):
                        mm(p, nc, a[:, uu, 0:66], Pu[:, di, uu, :], vb[:, di, 4 * hg + uu, 0:66], di == 0, di == len(nl) - 1,
                           r=[pn + str(di), "vb"], w=[an])
                cp(p, nc, "dve", den[:], a[:, :, 64:65], r=[an], w=["den"])
                p.op("dve", lambda i: nc.vector.reciprocal(den[:], den[:]), r=["den"], w=["den"])
                tt(p, nc, "dve", o3[:, 8 + 4 * hg:8 + 4 * hg + 4, :], a[:, :, 0:64], den[:].to_broadcast([128, 4, 64]), ALU.mult,
                   r=[an, "den"], w=["o"])
            if b.cfg.get("debug"):
                dma(p, nc, "pool", "odbg", lambda i: g["ODBG"][bass.ds(T(0)(i), 128), :], o[:], r=["o"])
            if lvl < 4:
                return
            for kc in range(8):
                tr(p, nc, psOT[:, kc, :], o[:, kc * 128:(kc + 1) * 128], identb[:], r=["o"], w=["psOT"])
            cp(p, nc, "act", oT[:], psOT[:], r=["psOT"], w=["oT"])
            for n in range(2):
                for kc in range(8):
                    mm(p, nc, psY[n][:], oT[:, kc, :], wout[:, kc, n * 512:(n + 1) * 512], kc == 0, kc == 7, r=["oT"], w=[f"psY{n}"])
            if lvl < 5:
                return
            ts(p, nc, "pool", g1b[:], g1b[:], 1.0, None, ALU.add, r=["g1b"], w=["g1b"])
            for n in range(2):
                tt(p, nc, "dve", t1[:, n * 512:(n + 1) * 512], psY[n][:], g1b[:, n * 512:(n + 1) * 512], ALU.mult,
                   r=[f"psY{n}", "g1b"], w=["t1"])
            p.op("dve", lambda i: nc.vector.scalar_tensor_tensor(z[:], xt[:], alpha, t1[:], ALU.mult, ALU.add), r=["xt", "t1"], w=["z"])
            layer_norm_tail(b, p, z, "z", lng, lnb, xo, "xo", dict(sq=sq, st=stt), "ln")
            dma(p, nc, "sp", "xo", lambda i: X[bass.ds(T(0)(i), 128), :], xo[:], r=["xo"])

        FULL = lambda dls: [(d_, d_ + 3) for d_ in dls]
        for s, T_s in enumerate(seqs):
            nb = T_s // 128
            tok_s = seq_tile0[s] * 128
            p = Phase(kb, f"A2e_{l}_{s}")
            for j in sorted(set([0, 1, nb - 2, nb - 1]))[:b.cfg.get("a2nblk", 4)]:
                wl = [d_ for d_ in (-1, 0, 1) if 0 <= j + d_ < nb]
                if j == 0:
                    nl = FULL([0, 1, 2, 3])
                elif j == 1:
                    nl = FULL([-1, 0, 1, 2])
                elif j == nb - 2:
                    nl = FULL([-2, -1, 0, 1])
                else:
                    nl = FULL([-3, -2, -1, 0])
                block(p, tok_s + j * 128, seq_tile0[s] + j, wl, nl)
            p.run()
            if b.cfg.get("stop") == "A2e":
                return
            if nb > 4:
                p = Phase(kb, f"A2i_{l}_{s}", n_iter=nb - 4)
                block(p, lambda i, tok_s=tok_s: (i + 2) * 128 + tok_s, lambda i, s=s: i + (2 + seq_tile0[s]),
                      [-1, 0, 1], [(-2, 7), (-1, 2), (0, 3), (1, 4), (2, 8)])
                p.run()


def expert_layout(w, kdim):
    L, E, K, n = w.shape
    return np.ascontiguousarray(w.reshape(L, E, kdim, 128, n).transpose(0, 1, 3, 2, 4)).reshape(L * E * 128, kdim * n)


def prepare_shared(inp, cfg):
    f = lambda k: np.ascontiguousarray(np.asarray(inp[k], dtype=np.float32))
    sh = {}
    for k in ["ada_w", "ada_b", "ln_g", "ln_b", "attn_sink", "attn_w_out", "rec_w_in", "rec_lb", "rec_gnorm", "rec_w_out"]:
        sh[k] = f(k)
    wfm, wv = attn_w_layout(f("attn_w_in"))
    sh["attn_wfm"] = wfm
    sh["attn_wv"] = wv
    sh["na_rb"] = na_bias_tables(f("nat_rel_bias"))
    L = sh["ada_w"].shape[0]
    sh["router_w"] = np.ascontiguousarray(np.concatenate([f("router_w_group"), f("router_w_expert")], axis=2))
    sh["router_b"] = np.ascontiguousarray(np.concatenate([f("router_b_group"), f("router_b_expert").reshape(L, 32)], axis=1))
    sh["e_wg"] = expert_layout(f("expert_w_gate"), 8)
    sh["e_wu"] = expert_layout(f("expert_w_up"), 8)
    sh["e_wd"] = expert_layout(f("expert_w_down"), 4)
    sh.update(host_constants(cfg))
    return sh


def run_cores(inp, cfg, n_cores, pps):
    xp = np.asarray(inp["x_prompt"], dtype=np.float32)
    xs = np.asarray(inp["x_sample"], dtype=np.float32)
    cp_ = np.asarray(inp["c_prompt"], dtype=np.float32)
    cs_ = np.asarray(inp["c_sample"], dtype=np.float32)
    sh = prepare_shared(inp, cfg)
    nc = build_program(cfg)
    in_maps = []
    for c in range(n_cores):
        m = dict(sh)
        xc = np.concatenate([xp[pps * c + k] for k in range(pps)] + [xs[c]], axis=0)
        cc = np.stack([cp_[pps * c + k] for k in range(pps)] + [cs_[c]], axis=0)
        m["x"] = np.ascontiguousarray(xc)
        m["cT"] = np.ascontiguousarray(cc.reshape(3, 8, 128).transpose(2, 0, 1))
        in_maps.append(m)
    res = run_bass_kernel_spmd(nc, in_maps, core_ids=list(range(n_cores)))
    if cfg.get("debug"):
        cfg["_dbg"] = res.results
    TP = cfg["seqs"][0]
    yp = np.zeros_like(xp)
    ys = np.zeros_like(xs)
    for c in range(n_cores):
        y = np.asarray(res.results[c]["y"], dtype=np.float32)
        for k in range(pps):
            yp[pps * c + k] = y[k * TP:(k + 1) * TP]
        ys[c] = y[pps * TP:]
    return yp, ys


def kernel(**inputs):
    cfg = dict(seqs=[2048, 2048, 8192], depth=4)
    yp, ys = run_cores(inputs, cfg, 8, 2)
    return (yp, ys)


def build_moe_layer(b, l, g):
    nc, kb = b.nc, b.kb
    seqs = b.cfg["seqs"]
    NT = sum(seqs)
    NTILES = NT // 128
    NBLK = 2 * NTILES + NEXP
    X, MODT, cn = g["X"], g["MODT"], g["cn"]
    identf = g["identf"]
    alpha = g["alpha"]
    BIG = 1.0e9
    if "HF" not in b.dram:
        b.dscr("HF", [NT, D])
        b.dscr("RTD", [128, NTILES, 8])
        b.dscr("DESTD", [128, NTILES, 2], I32)
        b.dscr("IDXD", [128, NBLK], I32)
        b.dscr("XBUF", [NBLK * 128, D])
        b.dscr("YBUF", [NBLK * 128, D])
    HF, RTD, DESTD, IDXD, XBUF, YBUF = [b.dram[k] for k in ["HF", "RTD", "DESTD", "IDXD", "XBUF", "YBUF"]]

    with ExitStack() as st:
        def sb(name, shape, dt=F32):
            return st.enter_context(nc.sbuf_tensor(un(b, name), list(shape), dt))

        def ps(name, shape, dt=F32):
            return st.enter_context(nc.psum_tensor(un(b, name), list(shape), dt))

        rw = sb("rw", [128, 8, 36])
        rbb = sb("rbb", [128, 36])
        io32 = sb("io32", [128, 32])
        trib = sb("trib", [128, 128], BF16)
        oneb = sb("oneb", [128, 128], BF16)
        carry = sb("carry", [128, 32])
        zt = sb("zt", [128, 1024])
        rt = sb("rt", [128, 8])
        p = Phase(kb, f"moepre{l}")
        p.op("pool", lambda i: nc.gpsimd.memset(rt[:], 0.0), w=["rt"])
        dma(p, nc, "sp", "m0", rw[:], g["ro_w"][l].rearrange("(kc p) n -> p kc n", p=128), w=["rw"])
        dma(p, nc, "sp", "m1", rbb[:], g["ro_b"][l:l + 1, :].partition_broadcast(128), w=["rbb"])
        dma(p, nc, "sp", "m2", io32[:], cn["iota32"][:, :], w=["io32"])
        dma(p, nc, "sp", "m3", trib[:], cn["tri_su"][:, :], w=["trib"])
        dma(p, nc, "sp", "m4", oneb[:], cn["ones_b"][:, :], w=["oneb"])
        p.op("pool", lambda i: nc.gpsimd.memset(carry[:], 0.0), w=["carry"])
        p.op("pool", lambda i: nc.gpsimd.memset(zt[:], 0.0), w=["zt"])
        p.run()
        p = Phase(kb, f"moez{l}", n_iter=NBLK)
        dma(p, nc, "sp", "zf", lambda i: XBUF[bass.ds(i * 128, 128), :], zt[:])
        p.run()

        from types import SimpleNamespace
        sets = []
        for k in range(2):
            T = SimpleNamespace(xt=sb(f"xt{k}", [128, 1024]), scb=sb(f"scb{k}", [128, 1024]), shb=sb(f"shb{k}", [128, 1024]),
                                hf=sb(f"hf{k}", [128, 1024]), hfT=sb(f"hfT{k}", [128, 8, 128]), lg=sb(f"lg{k}", [128, 36]),
                                sm=sb(f"sm{k}", [128, 16]), ge=sb(f"ge{k}", [128, 4]), gmask=sb(f"gmask{k}", [128, 4]),
                                lem=sb(f"lem{k}", [128, 4, 8]), lem2=sb(f"lem2{k}", [128, 32]), oh1=sb(f"oh1{k}", [128, 32]),
                                oh2=sb(f"oh2{k}", [128, 32]), ohb=sb(f"ohb{k}", [128, 32], BF16), tmp32=sb(f"tmp32{k}", [128, 32]),
                                tot=sb(f"tot{k}", [128, 32]), rt=sb(f"rtt{k}", [128, 8]))
            T.lemf = T.lem[:].rearrange("p a b -> p (a b)")
            sets.append(T)
        psA = [ps("psA0", [128, 4, 128]), ps("psA1", [128, 4, 128])]
        pl = ps("pl", [128, 512])
        pr = ps("pr", [128, 512])
        pc = ps("pc", [128, 512])
        p = Phase(kb, f"moeRz{l}")
        for k in range(2):
            p.op("pool", lambda i, k=k: nc.gpsimd.memset(sets[k].rt[:], 0.0), w=[f"rtz{k}"])
        p.run()

        p = Phase(kb, f"moeR{l}", n_iter=NTILES // 2)
        for k in range(2):
            p.ns = f"#{k}"
            T = sets[k]
            dma(p, nc, "sp", "xt", T.xt[:], lambda i, k=k, T=T: X[bass.ds(i * 256 + k * 128, 128), :], w=["xt"])
            dma(p, nc, "act", "scb", T.scb[:], lambda i, k=k, T=T: MODT[bass.ds(i * 2 + k, 1), 4096:5120].partition_broadcast(128), w=["scb"])
            dma(p, nc, "act", "shb", T.shb[:], lambda i, k=k, T=T: MODT[bass.ds(i * 2 + k, 1), 3072:4096].partition_broadcast(128), w=["shb"])
            ts(p, nc, "pool", T.scb[:], T.scb[:], 1.0, None, ALU.add, r=["scb"], w=["scb"])
            tt(p, nc, "dve", T.hf[:], T.xt[:], T.scb[:], ALU.mult, r=["xt", "scb"], w=["hf"])
            tt(p, nc, "pool", T.hf[:], T.hf[:], T.shb[:], ALU.add, r=["hf", "shb"], w=["hf"])
            dma(p, nc, "sp", "hfo", lambda i, k=k, T=T: HF[bass.ds(i * 256 + k * 128, 128), :], T.hf[:], r=["hf"])
            for kc in range(8):
                tr(p, nc, psA[kc // 4][:, kc % 4, :], T.hf[:, kc * 128:(kc + 1) * 128], identf[:], r=["hf"], w=[f"!psA{kc // 4}"])
            cp(p, nc, "act", T.hfT[:, 0:4, :], psA[0][:], r=["!psA0"], w=["hfT0"])
            cp(p, nc, "dve", T.hfT[:, 4:8, :], psA[1][:], r=["!psA1"], w=["hfT1"])
            for kc in range(8):
                mm(p, nc, pl[:, 0:36], T.hfT[:, kc, :], rw[:, kc, :], kc == 0, kc == 7, r=["hfT0", "hfT1"], w=["!pl"])
            tt(p, nc, "dve", T.lg[:], pl[:, 0:36], rbb[:], ALU.add, r=["!pl"], w=["lg"])
            p.op("dve", lambda i, k=k, T=T: nc.vector.tensor_reduce(T.sm[:, 0:1], T.lg[:, 0:4], AX.X, ALU.max), r=["lg"], w=["sm0"])
            ts(p, nc, "dve", T.sm[:, 1:2], T.sm[:, 0:1], -1.0, None, ALU.mult, r=["sm0"], w=["sm1"])
            act(p, nc, T.ge[:], T.lg[:, 0:4], AF.Exp, bias=T.sm[:, 1:2], r=["lg", "sm1"], w=["ge"])
            p.op("dve", lambda i, k=k, T=T: nc.vector.tensor_reduce(T.sm[:, 2:3], T.ge[:], AX.X, ALU.add), r=["ge"], w=["sm2"])
            p.op("dve", lambda i, k=k, T=T: nc.vector.reciprocal(T.sm[:, 3:4], T.sm[:, 2:3]), r=["sm2"], w=["sm3"])
            tt(p, nc, "dve", T.gmask[:], T.lg[:, 0:4], T.sm[:, 0:1].to_broadcast([128, 4]), ALU.is_ge, r=["lg", "sm0"], w=["gmask"])
            ts(p, nc, "dve", T.gmask[:], T.gmask[:], -1.0, BIG, ALU.add, ALU.mult, r=["gmask"], w=["gmask"])
            tt(p, nc, "dve", T.lem[:], T.lg[:, 4:36].rearrange("p (a b) -> p a b", b=8), T.gmask[:].unsqueeze(2).to_broadcast([128, 4, 8]),
               ALU.add, r=["lg", "gmask"], w=["lem"])
            p.op("dve", lambda i, k=k, T=T: nc.vector.tensor_reduce(T.sm[:, 4:5], T.lemf, AX.X, ALU.max), r=["lem"], w=["sm4"])
            tt(p, nc, "dve", T.oh1[:], T.lemf, T.sm[:, 4:5].to_broadcast([128, 32]), ALU.is_ge, r=["lem", "sm4"], w=["oh1"])
            p.op("dve", lambda i, k=k, T=T: nc.vector.scalar_tensor_tensor(T.lem2[:], T.oh1[:], -BIG, T.lemf, ALU.mult, ALU.add), r=["oh1", "lem"], w=["lem2"])
            p.op("dve", lambda i, k=k, T=T: nc.vector.tensor_reduce(T.sm[:, 5:6], T.lem2[:], AX.X, ALU.max), r=["lem2"], w=["sm5"])
            tt(p, nc, "dve", T.oh2[:], T.lem2[:], T.sm[:, 5:6].to_broadcast([128, 32]), ALU.is_ge, r=["lem2", "sm5"], w=["oh2"])
            tt(p, nc, "dve", T.sm[:, 6:7], T.sm[:, 5:6], T.sm[:, 4:5], ALU.subtract, r=["sm4", "sm5"], w=["sm6"])
            act(p, nc, T.sm[:, 7:8], T.sm[:, 6:7], AF.Exp, r=["sm6"], w=["sm7"])
            ts(p, nc, "dve", T.sm[:, 8:9], T.sm[:, 7:8], 1.0, None, ALU.add, r=["sm7"], w=["sm8"])
            p.op("dve", lambda i, k=k, T=T: nc.vector.reciprocal(T.sm[:, 8:9], T.sm[:, 8:9]), r=["sm8"], w=["sm8"])
            tt(p, nc, "dve", T.rt[:, 2:3], T.sm[:, 8:9], T.sm[:, 3:4], ALU.mult, r=["sm8", "sm3"], w=["rt2"])
            tt(p, nc, "dve", T.rt[:, 3:4], T.rt[:, 2:3], T.sm[:, 7:8], ALU.mult, r=["rt2", "sm7"], w=["rt3"])
            tt(p, nc, "dve", T.tmp32[:], T.oh1[:], io32[:], ALU.mult, r=["oh1"], w=["tmp32"])
            p.op("dve", lambda i, k=k, T=T: nc.vector.tensor_reduce(T.rt[:, 0:1], T.tmp32[:], AX.X, ALU.add), r=["tmp32"], w=["rt0"])
            tt(p, nc, "dve", T.tmp32[:], T.oh2[:], io32[:], ALU.mult, r=["oh2", "rt0"], w=["tmp32"])
            p.op("dve", lambda i, k=k, T=T: nc.vector.tensor_reduce(T.rt[:, 1:2], T.tmp32[:], AX.X, ALU.add), r=["tmp32"], w=["rt1"])
            tt(p, nc, "pool", T.ohb[:], T.oh1[:], T.oh2[:], ALU.add, r=["oh1", "oh2"], w=["ohb"])
            mm(p, nc, pr[:, 0:32], trib[:], T.ohb[:], True, True, r=["ohb"], w=["!pr"])
            mm(p, nc, pc[:, 0:32], oneb[:], T.ohb[:], True, True, r=["ohb"], w=["!pc"])
            tt(p, nc, "dve", T.tot[:], pr[:, 0:32], carry[:], ALU.add, r=["!pr", "!carry"], w=["tot"])
            tt(p, nc, "dve", T.tmp32[:], T.oh1[:], T.tot[:], ALU.mult, r=["oh1", "tot", "rt1"], w=["tmp32"])
            p.op("dve", lambda i, k=k, T=T: nc.vector.tensor_reduce(T.rt[:, 4:5], T.tmp32[:], AX.X, ALU.add), r=["tmp32"], w=["rt4"])
            tt(p, nc, "dve", T.tmp32[:], T.oh2[:], T.tot[:], ALU.mult, r=["oh2", "tot", "rt4"], w=["tmp32"])
            p.op("dve", lambda i, k=k, T=T: nc.vector.tensor_reduce(T.rt[:, 5:6], T.tmp32[:], AX.X, ALU.add), r=["tmp32"], w=["rt5"])
            tt(p, nc, "dve", carry[:], carry[:], pc[:, 0:32], ALU.add, r=["!carry", "!pc", "tot"], w=["!carry"])
            dma(p, nc, "act", "rto", lambda i, k=k, T=T: RTD[:, bass.ds(i * 2 + k, 1), :].rearrange("p o c -> p (o c)"), T.rt[:],
                r=["rt0", "rt1", "rt2", "rt3", "rt4", "rt5"])
        p.ns = ""
        p.run()

        rta = sb("rta", [128, NTILES, 8])
        cnt = sb("cnt", [128, 32])
        cnti = sb("cnti", [128, 32], I32)
        pend = sb("pend", [128, 32])
        pst = sb("pst", [128, 32])
        onesf = sb("onesf", [128, 32])
        dacc = sb("dacc", [128, NTILES, 2])
        dtmp = sb("dtmp", [128, NTILES, 2])
        dint = sb("dint", [128, NTILES, 2], I32)
        ioblk = sb("ioblk", [128, NBLK])
        bacc = sb("bacc", [128, NBLK])
        btmp = sb("btmp", [128, NBLK])
        iop = sb("iop", [128, 1])
        bint = sb("bint", [128, NBLK], I32)
        p = Phase(kb, f"moeD{l}")
        dma(p, nc, "sp", "d0", rta[:], RTD[:, :, :], w=["rta"])
        dma(p, nc, "sp", "d1", ioblk[:], cn["iota_blk"][:, 0:NBLK], w=["ioblk"])
        dma(p, nc, "sp", "d2", iop[:], cn["iota_p"][:, :], w=["iop"])
        p.op("pool", lambda i: nc.gpsimd.memset(onesf[:], 1.0), w=["onesf"])
        ts(p, nc, "dve", cnt[:], carry[:], 1.0 / 128.0, 0.49609375, ALU.mult, ALU.add, w=["cnt"])
        cp(p, nc, "dve", cnti[:], cnt[:], r=["cnt"], w=["cnti"])
        cp(p, nc, "dve", cnt[:], cnti[:], r=["cnti"], w=["cnt"])
        ts(p, nc, "dve", cnt[:], cnt[:], 128.0, None, ALU.mult, r=["cnt"], w=["cnt"])
        p.op("dve", lambda i: nc.vector.tensor_tensor_scan(pend[:], onesf[:], cnt[:], 0.0, ALU.mult, ALU.add), r=["onesf", "cnt"], w=["pend"])
        tt(p, nc, "dve", pst[:], pend[:], cnt[:], ALU.subtract, r=["pend", "cnt"], w=["pst"])
        cp(p, nc, "dve", dacc[:], rta[:, :, 4:6], r=["rta"], w=["dacc"])
        for e in range(NEXP):
            ts(p, nc, "dve", dtmp[:], rta[:, :, 0:2], float(e), None, ALU.is_equal, r=["rta", "dacc"], w=["dtmp"])
            p.op("dve", lambda i, e=e: nc.vector.scalar_tensor_tensor(dacc[:], dtmp[:], pst[:, e:e + 1], dacc[:], ALU.mult, ALU.add),
                 r=["dtmp", "pst", "dacc"], w=["dacc"])
        cp(p, nc, "dve", dint[:], dacc[:], r=["dacc"], w=["dint"])
        dma(p, nc, "sp", "d3", DESTD[:, :, :], dint[:], r=["dint"])
        p.op("pool", lambda i: nc.gpsimd.memset(bacc[:], 0.0), w=["bacc"])
        for e in range(NEXP):
            ts(p, nc, "dve", btmp[:], ioblk[:], pend[:, e:e + 1], None, ALU.is_ge, r=["ioblk", "pend", "bacc"], w=["btmp"])
            tt(p, nc, "dve", bacc[:], bacc[:], btmp[:], ALU.add, r=["bacc", "btmp"], w=["bacc"])
        ts(p, nc, "dve", bacc[:], bacc[:], float(NEXP - 1), None, ALU.min, r=["bacc"], w=["bacc"])
        OOBV = float(b.cfg["depth"] * NEXP * 128)
        p.op("pool", lambda i: nc.gpsimd.memset(btmp[:, 0:1], 1.0), r=["btmp"], w=["btmp"])
        tt(p, nc, "dve", btmp[:, 1:NBLK], bacc[:, 1:NBLK], bacc[:, 0:NBLK - 1], ALU.not_equal, r=["bacc", "btmp"], w=["btmp"])
        ts(p, nc, "dve", bacc[:], bacc[:], 128.0, float(l * NEXP * 128), ALU.mult, ALU.add, r=["bacc", "btmp"], w=["bacc"])
        tt(p, nc, "dve", bacc[:], bacc[:], iop[:].to_broadcast([128, NBLK]), ALU.add, r=["bacc", "iop"], w=["bacc"])
        if b.cfg.get("oobskip", True):
            ts(p, nc, "dve", bacc[:], bacc[:], -OOBV, None, ALU.add, r=["bacc"], w=["bacc"])
            tt(p, nc, "dve", bacc[:], bacc[:], btmp[:], ALU.mult, r=["bacc", "btmp"], w=["bacc"])
            ts(p, nc, "dve", bacc[:], bacc[:], OOBV, None, ALU.add, r=["bacc"], w=["bacc"])
        cp(p, nc, "dve", bint[:], bacc[:], r=["bacc"], w=["bint"])
        dma(p, nc, "sp", "d4", IDXD[:, :], bint[:], r=["bint"])
        p.run()
    if b.cfg.get("stop") == "moeD":
        return

    with ExitStack() as st:
        def sb(name, shape, dt=F32):
            return st.enter_context(nc.sbuf_tensor(un(b, name), list(shape), dt))
        hf = sb("hf", [128, 1024])
        di = sb("di", [128, 2], I32)
        p = Phase(kb, f"moeS{l}", n_iter=NTILES)
        dma(p, nc, "sp", "hf", hf[:], lambda i: HF[bass.ds(i * 128, 128), :], w=["hf"])
        dma(p, nc, "act", "di", di[:], lambda i: DESTD[:, bass.ds(i, 1), :].rearrange("p o c -> p (o c)"), w=["di"])
        for k in range(2):
            p.dma("pool", f"sc{k}", lambda i, k=k: nc.gpsimd.indirect_dma_start(
                out=XBUF[:, :], out_offset=bass.IndirectOffsetOnAxis(ap=di[:, k:k + 1], axis=0), in_=hf[:], in_offset=None),
                r=["hf", "di"])
        p.run()
    if b.cfg.get("stop") == "moeS":
        return

    NROWS = b.cfg["depth"] * NEXP * 128
    with ExitStack() as st:
        def sb(name, shape, dt=F32):
            return st.enter_context(nc.sbuf_tensor(un(b, name), list(shape), dt))

        def ps(name, shape, dt=F32):
            return st.enter_context(nc.psum_tensor(un(b, name), list(shape), dt))
        wgf = sb("wgf", [128, 4096])
        wuf = sb("wuf", [128, 4096])
        wdf = sb("wdf", [128, 4096])
        psA = [ps("psA0", [128, 4, 128]), ps("psA1", [128, 4, 128])]
        psG = ps("psG", [128, 4, 128])
        psU = ps("psU", [128, 4, 128])
        psY = [ps("psY0", [128, 512]), ps("psY1", [128, 512])]
        sets = []
        for k in range(2):
            sets.append(dict(ix=sb(f"ix{k}", [128, 1], I32), wgb=sb(f"wgb{k}", [128, 8, 512], BF16), wub=sb(f"wub{k}", [128, 8, 512], BF16),
                             wdb=sb(f"wdb{k}", [128, 4, 1024], BF16), xb=sb(f"xb{k}", [128, 1024]), xT=sb(f"xT{k}", [128, 8, 128], BF16),
                             sg=sb(f"sg{k}", [128, 4, 128]), hT=sb(f"hT{k}", [128, 4, 128], BF16), yb=sb(f"yb{k}", [128, 1024])))
        p = Phase(kb, f"moeB{l}", n_iter=NBLK // 2)
        for k in range(2):
            p.ns = f"#{k}"
            T = sets[k]
            ix, wgb, wub, wdb, xb, xT, sg, hT, yb = [T[n_] for n_ in ["ix", "wgb", "wub", "wdb", "xb", "xT", "sg", "hT", "yb"]]
            dma(p, nc, "sp", "ix", ix[:], lambda i, k=k: IDXD[:, bass.ds(i * 2 + k, 1)], w=["ix"], slow=True)
            dma(p, nc, "act", "xb", xb[:], lambda i, k=k: XBUF[bass.ds(i * 256 + k * 128, 128), :], w=["xb"])
            for nm, dst, src in [("wgf", wgf, g["e_wg"]), ("wuf", wuf, g["e_wu"]), ("wdf", wdf, g["e_wd"])]:
                p.dma("pool", nm, lambda i, dst=dst, src=src, ix=ix: nc.gpsimd.indirect_dma_start(
                    out=dst[:], out_offset=None, in_=src[:, :], in_offset=bass.IndirectOffsetOnAxis(ap=ix[:, 0:1], axis=0),
                    bounds_check=NROWS - 1, oob_is_err=False),
                    r=["ix"], w=["!" + nm])
            cp(p, nc, "dve", wgb[:].rearrange("p k n -> p (k n)"), wgf[:], r=["!wgf"], w=["wgb"])
            cp(p, nc, "act", wub[:].rearrange("p k n -> p (k n)"), wuf[:], r=["!wuf"], w=["wub"])
            cp(p, nc, "pool", wdb[:].rearrange("p k n -> p (k n)"), wdf[:], r=["!wdf"], w=["wdb"])
            for kc in range(8):
                tr(p, nc, psA[kc // 4][:, kc % 4, :], xb[:, kc * 128:(kc + 1) * 128], identf[:], r=["xb"], w=[f"!psA{kc // 4}"])
            cp(p, nc, "act", xT[:, 0:4, :], psA[0][:], r=["!psA0"], w=["xT0"])
            cp(p, nc, "dve", xT[:, 4:8, :], psA[1][:], r=["!psA1"], w=["xT1"])
            for c in range(4):
                for kc in range(8):
                    mm(p, nc, psG[:, c, :], wgb[:, kc, c * 128:(c + 1) * 128], xT[:, kc, :], kc == 0, kc == 7, r=["wgb", "xT0", "xT1"], w=["!psG"])
            for c in range(4):
                for kc in range(8):
                    mm(p, nc, psU[:, c, :], wub[:, kc, c * 128:(c + 1) * 128], xT[:, kc, :], kc == 0, kc == 7, r=["wub", "xT0", "xT1"], w=["!psU"])
            act(p, nc, sg[:], psG[:], AF.Silu, r=["!psG"], w=["sg"])
            tt(p, nc, "dve", hT[:], sg[:], psU[:], ALU.mult, r=["sg", "!psU"], w=["hT"])
            for n in range(2):
                for c in range(4):
                    mm(p, nc, psY[n][:], hT[:, c, :], wdb[:, c, n * 512:(n + 1) * 512], c == 0, c == 3, r=["hT", "wdb"], w=[f"!psY{n}"])
            cp(p, nc, "act", yb[:, 0:512], psY[0][:], r=["!psY0"], w=["yb0"])
            cp(p, nc, "dve", yb[:, 512:1024], psY[1][:], r=["!psY1"], w=["yb1"])
            dma(p, nc, "sp", "yb", lambda i, k=k: YBUF[bass.ds(i * 256 + k * 128, 128), :], yb[:], r=["yb0", "yb1"])
        p.ns = ""
        p.run()
    if b.cfg.get("stop") == "moeB":
        return

    with ExitStack() as st:
        def sb(name, shape, dt=F32):
            return st.enter_context(nc.sbuf_tensor(un(b, name), list(shape), dt))
        lng = sb("lng", [128, 1024])
        lnb = sb("lnb", [128, 1024])
        p = Phase(kb, f"moeCpre{l}")
        dma(p, nc, "sp", "c0", lng[:], g["ln_g"][l, 1:2, :].partition_broadcast(128), w=["lng"])
        dma(p, nc, "sp", "c1", lnb[:], g["ln_b"][l, 1:2, :].partition_broadcast(128), w=["lnb"])
        p.run()
        sets = []
        for k in range(2):
            sets.append(dict(xt=sb(f"xt{k}", [128, 1024]), g2b=sb(f"g2b{k}", [128, 1024]), rt=sb(f"rt{k}", [128, 8]),
                             di=sb(f"di{k}", [128, 2], I32), y0=sb(f"y0{k}", [128, 1024]), y1=sb(f"y1{k}", [128, 1024]),
                             z=sb(f"z{k}", [128, 1024]), sq=sb(f"sq{k}", [128, 1024]), xo=sb(f"xo{k}", [128, 1024]),
                             stt=sb(f"stt{k}", [128, 8])))
        p = Phase(kb, f"moeC{l}", n_iter=NTILES // 2)
        for k in range(2):
            p.ns = f"#{k}"
            T = sets[k]
            xt, g2b, rt, di, y0, y1, z, sq, xo, stt = [T[n_] for n_ in ["xt", "g2b", "rt", "di", "y0", "y1", "z", "sq", "xo", "stt"]]
            dma(p, nc, "sp", "xt", xt[:], lambda i, k=k: X[bass.ds(i * 256 + k * 128, 128), :], w=["xt"])
            dma(p, nc, "act", "g2b", g2b[:], lambda i, k=k: MODT[bass.ds(i * 2 + k, 1), 5120:6144].partition_broadcast(128), w=["g2b"])
            dma(p, nc, "act", "rt", rt[:], lambda i, k=k: RTD[:, bass.ds(i * 2 + k, 1), :].rearrange("p o c -> p (o c)"), w=["rt"])
            dma(p, nc, "sp", "di", di[:], lambda i, k=k: DESTD[:, bass.ds(i * 2 + k, 1), :].rearrange("p o c -> p (o c)"), w=["di"])
            for kk, yk in enumerate([y0, y1]):
                p.dma("pool", f"ga{kk}", lambda i, kk=kk, yk=yk, di=di: nc.gpsimd.indirect_dma_start(
                    out=yk[:], out_offset=None, in_=YBUF[:, :], in_offset=bass.IndirectOffsetOnAxis(ap=di[:, kk:kk + 1], axis=0)),
                    r=["di"], w=[f"y{kk}"])
            ts(p, nc, "dve", y0[:], y0[:], rt[:, 2:3], None, ALU.mult, r=["y0", "rt"], w=["y0"])
            p.op("dve", lambda i, y0=y0, y1=y1, rt=rt: nc.vector.scalar_tensor_tensor(y0[:], y1[:], rt[:, 3:4], y0[:], ALU.mult, ALU.add),
                 r=["y0", "y1", "rt"], w=["y0"])
            ts(p, nc, "pool", g2b[:], g2b[:], 1.0, None, ALU.add, r=["g2b"], w=["g2b"])
            tt(p, nc, "pool", y0[:], y0[:], g2b[:], ALU.mult, r=["y0", "g2b"], w=["y0"])
            p.op("dve", lambda i, z=z, xt=xt, y0=y0: nc.vector.scalar_tensor_tensor(z[:], xt[:], alpha, y0[:], ALU.mult, ALU.add),
                 r=["xt", "y0"], w=["z"])
            layer_norm_tail(b, p, z, "z", lng, lnb, xo, "xo", dict(sq=sq, st=stt), "ln")
            dma(p, nc, "sp", "xo", lambda i, k=k: X[bass.ds(i * 256 + k * 128, 128), :], xo[:], r=["xo"])
            if k == 0:
                n_half = len(p.ops)
        p.ns = ""
        p.interleave(0, n_half, len(p.ops))
        p.run()


def build_hgrn_layer(b, l, lr, g):
    nc, kb = b.nc, b.kb
    seqs = b.cfg["seqs"]
    NT = sum(seqs)
    NTILES = NT // 128
    depth = b.cfg["depth"]
    LR = depth // 2
    X, MODT, cn = g["X"], g["MODT"], g["cn"]
    identb = g["identb"]
    alpha = g["alpha"]
    seq_tile0 = g["seq_tile0"]
    if "QZ" not in b.dram:
        b.dscr("QZ", [24, 128, NT])
        b.dscr("VT", [NT, D], BF16)
        b.dscr("GT", [NT, D])
        b.dscr("OF", [NT, D])
        b.dscr("OB", [NT, D])
    QZ, VT, GT, OF, OB = [b.dram[k] for k in ["QZ", "VT", "GT", "OF", "OB"]]

    with ExitStack() as st:
        def sb(name, shape, dt=F32):
            return st.enter_context(nc.sbuf_tensor(un(b, name), list(shape), dt))

        def ps(name, shape, dt=F32):
            return st.enter_context(nc.psum_tensor(un(b, name), list(shape), dt))

        win = sb("win", [128, 8, 5120], BF16)
        with ExitStack() as st2:
            stg = [st2.enter_context(nc.sbuf_tensor(un(b, "stg0"), [128, 8, 512], F32)),
                   st2.enter_context(nc.sbuf_tensor(un(b, "stg1"), [128, 8, 512], F32))]
            p = Phase(kb, f"recw{l}")
            load_weight_bf16(b, p, win, g["r_win"][lr].rearrange("(kc p) n -> p kc n", p=128), 5120, "win", stg)
            p.run()
        xg = sb("xg", [128, 4, 1024])
        scb = sb("scb", [128, 1024])
        shb = sb("shb", [128, 1024])
        tmp = [sb("tmp0", [128, 1024]), sb("tmp1", [128, 1024])]
        hb = sb("hb", [128, 4, 1024], BF16)
        hT = sb("hT", [128, 8, 512], BF16)
        fq = sb("fq", [128, 8, 512])
        vo = sb("vo", [128, 4, 1024], BF16)
        go = sb("go", [128, 4, 1024])
        psT = [ps("psT0", [128, 8, 128], BF16), ps("psT1", [128, 8, 128], BF16)]
        pf = [ps(f"pf{k}", [128, 512]) for k in range(4)]
        NG = NT // 512
        p = Phase(kb, f"H1_{l}", n_iter=NG)
        dma(p, nc, "sp", "xg", xg[:], lambda i: X[bass.ds(i * 512, 512), :].rearrange("(t p) d -> p t d", p=128), w=["xg"])
        dma(p, nc, "act", "scb", scb[:], lambda i: MODT[bass.ds(i * 4, 1), 1024:2048].partition_broadcast(128), w=["scb"])
        dma(p, nc, "act", "shb", shb[:], lambda i: MODT[bass.ds(i * 4, 1), 0:1024].partition_broadcast(128), w=["shb"])
        ts(p, nc, "dve", scb[:], scb[:], 1.0, None, ALU.add, r=["scb"], w=["scb"])
        for t in range(4):
            tt(p, nc, "dve", tmp[t % 2][:], xg[:, t, :], scb[:], ALU.mult, r=["xg", "scb"], w=[f"tmp{t % 2}"])
            tt(p, nc, "pool", hb[:, t, :], tmp[t % 2][:], shb[:], ALU.add, r=[f"tmp{t % 2}", "shb"], w=[f"hb{t}"])
            for kc in range(8):
                tr(p, nc, psT[t % 2][:, kc, :], hb[:, t, kc * 128:(kc + 1) * 128], identb[:], r=[f"hb{t}"], w=[f"psT{t % 2}"])
            cp(p, nc, "act", hT[:, :, t * 128:(t + 1) * 128], psT[t % 2][:], r=[f"psT{t % 2}"], w=[f"hT{t}"])
        hTall = [f"hT{t}" for t in range(4)]
        pfi = 0
        for grp in range(3):
            for hh in range(8):
                c = grp * 8 + hh
                ia = pfi % 4
                pfi += 1
                for kc in range(8):
                    mm(p, nc, pf[ia][:], win[:, kc, c * 128:(c + 1) * 128], hT[:, kc, :], kc == 0, kc == 7, r=hTall, w=[f"pf{ia}"])
                cp(p, nc, "act" if hh % 2 == 0 else "dve", fq[:, hh, :], pf[ia][:], r=[f"pf{ia}"], w=[f"fq{hh}"])
            dma(p, nc, ["sp", "act", "pool"][grp], f"fq{grp}",
                lambda i, grp=grp: QZ[grp * 8:grp * 8 + 8, :, bass.ds(i * 512, 512)].rearrange("f p t -> p f t"), fq[:],
                r=[f"fq{hh}" for hh in range(8)])
        for t in range(4):
            for n in range(4):
                ia = pfi % 4
                pfi += 1
                col0 = 3072 + n * 512
                for kc in range(8):
                    mm(p, nc, pf[ia][:], hT[:, kc, t * 128:(t + 1) * 128], win[:, kc, col0:col0 + 512], kc == 0, kc == 7,
                       r=[f"hT{t}"], w=[f"pf{ia}"])
                if n < 2:
                    cp(p, nc, "act", vo[:, t, n * 512:(n + 1) * 512], pf[ia][:], r=[f"pf{ia}"], w=[f"vo{t}"])
                else:
                    cp(p, nc, "dve", go[:, t, (n - 2) * 512:(n - 1) * 512], pf[ia][:], r=[f"pf{ia}"], w=[f"go{t}"])
        dma(p, nc, "sp", "vo", lambda i: VT[bass.ds(i * 512, 512), :].rearrange("(t p) e -> p t e", p=128), vo[:],
            r=[f"vo{t}" for t in range(4)])
        dma(p, nc, "pool", "go", lambda i: GT[bass.ds(i * 512, 512), :].rearrange("(t p) e -> p t e", p=128), go[:],
            r=[f"go{t}" for t in range(4)])
        p.run()
    if b.cfg.get("stop") == "H1":
        return

    with ExitStack() as st:
        def sb(name, shape, dt=F32):
            return st.enter_context(nc.sbuf_tensor(un(b, name), list(shape), dt))

        def ps(name, shape, dt=F32):
            return st.enter_context(nc.psum_tensor(un(b, name), list(shape), dt))

        lbT = sb("lbT", [128, 8])
        omlT = sb("omlT", [128, 8])
        lbe = sb("lbe", [128, max(LR, 1), 8])
        lsum = sb("lsum", [128, 8])
        hm = sb("hm", [64, 2, 64], BF16)
        smask = sb("smask", [128, 1024])
        S = sb("S", [128, 8, 128])
        p = Phase(kb, f"recpre{l}")
        for j in range(LR):
            dma(p, nc, "sp", f"lb{j}", lbe[:, j, :], g["r_lb"][j].rearrange("(h k) -> k h", k=128), w=[f"lbe{j}"], slow=True)
            act(p, nc, lbe[:, j, :], lbe[:, j, :], AF.Exp, r=[f"lbe{j}"], w=[f"lbe{j}"])
        cp(p, nc, "dve", lsum[:], lbe[:, 0, :], r=["lbe0"], w=["lsum"])
        for j in range(1, LR):
            tt(p, nc, "dve", lsum[:], lsum[:], lbe[:, j, :], ALU.add, r=["lsum", f"lbe{j}"], w=["lsum"])
        p.op("dve", lambda i: nc.vector.reciprocal(lsum[:], lsum[:]), r=["lsum"], w=["lsum"])
        p.op("pool", lambda i: nc.gpsimd.memset(lbT[:], 0.0), w=["lbT"])
        for j in range(1, lr + 1):
            tt(p, nc, "dve", lbT[:], lbT[:], lbe[:, j, :], ALU.add, r=["lbT", f"lbe{j}"], w=["lbT"])
        tt(p, nc, "dve", lbT[:], lbT[:], lsum[:], ALU.mult, r=["lbT", "lsum"], w=["lbT"])
        ts(p, nc, "dve", omlT[:], lbT[:], -1.0, 1.0, ALU.mult, ALU.add, r=["lbT"], w=["omlT"])
        dma(p, nc, "sp", "hm", hm[:], cn["hmask"][:, :, :], w=["hm"])
        dma(p, nc, "sp", "smask", smask[:], cn["scanmask"][:, :], w=["smask"])
        p.run()

        from types import SimpleNamespace
        sets = []
        for k in range(2):
            T = SimpleNamespace(qt=sb(f"qt{k}", [128, 8, 128]), zt=sb(f"zt{k}", [128, 8, 128]), vt=sb(f"vt{k}", [64, 2, 1024], BF16),
                                u=sb(f"u{k}", [128, 8, 128]), w_=sb(f"w_{k}", [128, 8, 128]), f_=sb(f"f_{k}", [128, 8, 128]),
                                lf=sb(f"lf{k}", [128, 8, 128]), km=sb(f"km{k}", [128, 8, 128]), bb=sb(f"bb{k}", [128, 8, 128]),
                                bm=sb(f"bm{k}", [128, 8, 128]), Ee=sb(f"Ee{k}", [128, 8, 128]), Ei=sb(f"Ei{k}", [128, 8, 128]),
                                qe=sb(f"qe{k}", [128, 8, 128], BF16), ke=sb(f"ke{k}", [128, 8, 128], BF16), sc=sb(f"sc{k}", [128, 8, 2, 4]),
                                Sc=sb(f"Sc{k}", [128, 8, 128], BF16), AT=sb(f"AT{k}", [64, 8, 64], BF16), keT=sb(f"keT{k}", [64, 8, 128], BF16),
                                oc=sb(f"oc{k}", [64, 1024]))
            sets.append(T)
        psA = ps("psA", [64, 8, 64])
        psKT2 = [ps("psKT0", [64, 8, 128], BF16), ps("psKT1", [64, 8, 128], BF16)]
        pso = [ps("pso0", [64, 512]), ps("pso1", [64, 512])]
        psdS = [ps("psdS0", [128, 4, 128]), ps("psdS1", [128, 4, 128])]
        lb_bc = lbT[:].unsqueeze(2).to_broadcast([128, 8, 128])
        oml_bc = omlT[:].unsqueeze(2).to_broadcast([128, 8, 128])
        fl = lambda t_: t_[:].rearrange("p h t -> p (h t)")
        c4 = lambda t_: t_[:].rearrange("p h (c t) -> p h c t", t=64)

        def rec_body(p, tok0, dirn, OUT, T):
            zbase = 8 if dirn == 0 else 16
            dma(p, nc, "sp", "qt", T.qt[:], lambda i: QZ[0:8, :, bass.ds(tok0(i), 128)].rearrange("f p t -> p f t"), w=["qt"])
            dma(p, nc, "act", "zt", T.zt[:], lambda i: QZ[zbase:zbase + 8, :, bass.ds(tok0(i), 128)].rearrange("f p t -> p f t"), w=["zt"])
            dma(p, nc, "pool", "vt", T.vt[:], lambda i: VT[bass.ds(tok0(i), 128), :].rearrange("(c p) e -> p c e", p=64), w=["vt"])
            act(p, nc, T.u[:], T.zt[:], AF.Exp, scale=-1.0, r=["zt"], w=["u"])
            ts(p, nc, "dve", T.w_[:], T.u[:], 1.0, None, ALU.add, r=["u"], w=["w_"])
            p.op("dve", lambda i: nc.vector.reciprocal(T.w_[:], T.w_[:]), r=["w_"], w=["w_"])
            tt(p, nc, "dve", T.f_[:], T.w_[:], oml_bc, ALU.mult, r=["w_"], w=["f_"])
            tt(p, nc, "dve", T.f_[:], T.f_[:], lb_bc, ALU.add, r=["f_"], w=["f_"])
            act(p, nc, T.lf[:], T.f_[:], AF.Ln, r=["f_"], w=["lf"])
            tt(p, nc, "pool", T.km[:], T.u[:], T.w_[:], ALU.mult, r=["u", "w_"], w=["km"])
            tt(p, nc, "pool", T.km[:], T.km[:], oml_bc, ALU.mult, r=["km"], w=["km"])
            p.op("dve", lambda i: nc.vector.tensor_tensor_scan(fl(T.bb), smask[:], fl(T.lf), 0.0, ALU.mult, ALU.add), r=["lf"], w=["bb"])
            if dirn == 1:
                tt(p, nc, "pool", T.bm[:], T.lf[:], T.bb[:], ALU.subtract, r=["lf", "bb"], w=["bm"])
                tt(p, nc, "pool", c4(T.bb), c4(T.bm), c4(T.bb)[:, :, :, 63:64].to_broadcast([128, 8, 2, 64]), ALU.add, r=["bm", "bb"], w=["bb"])
            endidx = 63 if dirn == 0 else 0
            cref = c4(T.bb)[:, :, :, 31:32]
            bend = c4(T.bb)[:, :, :, endidx:endidx + 1]
            act(p, nc, T.sc[:, :, :, 0:1], cref, AF.Exp, r=["bb"], w=["sc0"])
            act(p, nc, T.sc[:, :, :, 1:2], bend, AF.Exp, r=["bb"], w=["sc1"])
            tt(p, nc, "dve", T.sc[:, :, :, 3:4], bend, cref, ALU.subtract, r=["bb"], w=["sc3"])
            act(p, nc, T.sc[:, :, :, 2:3], T.sc[:, :, :, 3:4], AF.Exp, r=["sc3"], w=["sc2"])
            tt(p, nc, "pool", c4(T.bm), c4(T.bb), cref.to_broadcast([128, 8, 2, 64]), ALU.subtract, r=["bb", "bm"], w=["bm"])
            act(p, nc, T.Ee[:], T.bm[:], AF.Exp, r=["bm"], w=["Ee"])
            act(p, nc, T.Ei[:], T.bm[:], AF.Exp, scale=-1.0, r=["bm"], w=["Ei"])
            tt(p, nc, "dve", T.qe[:], T.qt[:], T.Ee[:], ALU.mult, r=["qt", "Ee"], w=["qe"])
            tt(p, nc, "pool", T.ke[:], T.km[:], T.Ei[:], ALU.mult, r=["km", "Ei"], w=["ke"])
            order = [0, 1] if dirn == 0 else [1, 0]
            for c in order:
                cs_ = slice(c * 64, (c + 1) * 64)
                for h in range(8):
                    p.op("act", lambda i, h=h, c=c, T=T: nc.scalar.activation(T.Sc[:, h, :], S[:, h, :], AF.Copy, scale=T.sc[:, h, c, 0:1]),
                         r=[f"!S{h}", "sc0"], w=[f"Sc{h}"])
                for h in range(8):
                    mm(p, nc, psA[:, h, :], T.ke[:, h, cs_], T.qe[:, h, cs_], True, True, r=["ke", "qe"], w=["!psA"])
                for h in range(8):
                    tt(p, nc, "dve", T.AT[:, h, :], psA[:, h, :], hm[:, dirn, :], ALU.mult, r=["!psA"], w=[f"AT{h}"])
                for h in range(8):
                    tr(p, nc, psKT2[h // 4][:, h % 4, :], T.ke[:, h, cs_], identb[:], r=["ke"], w=[f"!psKT{h // 4}"])
                for h in range(8):
                    cp(p, nc, "dve" if h < 4 else "act", T.keT[:, h, :], psKT2[h // 4][:, h % 4, :], r=[f"!psKT{h // 4}"], w=[f"keT{h}"])
                for h in range(8):
                    po = pso[h // 4][:, (h % 4) * 128:(h % 4 + 1) * 128]
                    mm(p, nc, po, T.AT[:, h, :], T.vt[:, c, h * 128:(h + 1) * 128], True, False, r=[f"AT{h}", "vt"], w=[f"!pso{h // 4}"])
                    mm(p, nc, po, T.qe[:, h, cs_], T.Sc[:, h, :], False, True, r=["qe", f"Sc{h}"], w=[f"!pso{h // 4}"])
                for h in range(8):
                    mm(p, nc, psdS[h // 4][:, h % 4, :], T.keT[:, h, :], T.vt[:, c, h * 128:(h + 1) * 128], True, True,
                       r=[f"keT{h}", "vt"], w=[f"!psdS{h // 4}"])
                for h in range(8):
                    ts(p, nc, "pool", S[:, h, :], S[:, h, :], T.sc[:, h, c, 1:2], None, ALU.mult, r=[f"!S{h}", "sc1"], w=[f"!S{h}"])
                for h in range(8):
                    p.op("dve", lambda i, h=h, c=c, T=T: nc.vector.scalar_tensor_tensor(S[:, h, :], psdS[h // 4][:, h % 4, :], T.sc[:, h, c, 2:3],
                                                                                         S[:, h, :], ALU.mult, ALU.add),
                         r=[f"!psdS{h // 4}", f"!S{h}", "sc2"], w=[f"!S{h}"])
                cp(p, nc, "act", T.oc[:, 0:512], pso[0][:], r=["!pso0"], w=["oc0"])
                cp(p, nc, "dve", T.oc[:, 512:1024], pso[1][:], r=["!pso1"], w=["oc1"])
                dma(p, nc, "sp", f"oc", lambda i, c=c: OUT[bass.ds(tok0(i) + c * 64, 64), :], T.oc[:], r=["oc0", "oc1"])

        for s, T_s in enumerate(seqs):
            nb = T_s // 128
            tok_s = seq_tile0[s] * 128
            for dirn in range(2):
                p = Phase(kb, f"recz{l}_{s}_{dirn}")
                p.op("pool", lambda i: nc.gpsimd.memset(S[:], 0.0), w=["S"])
                p.run()
                p = Phase(kb, f"H2_{l}_{s}_{dirn}", n_iter=nb // 2)
                for k in range(2):
                    p.ns = f"#{k}"
                    if dirn == 0:
                        rec_body(p, lambda i, tok_s=tok_s, k=k: i * 256 + (tok_s + k * 128), 0, OF, sets[k])
                    else:
                        rec_body(p, lambda i, tok_s=tok_s, nb=nb, k=k: (tok_s + (nb - 1 - k) * 128) - i * 256, 1, OB, sets[k])
                p.ns = ""
                p.run()
    if b.cfg.get("stop") == "H2":
        return

    with ExitStack() as st:
        def sb(name, shape, dt=F32):
            return st.enter_context(nc.sbuf_tensor(un(b, name), list(shape), dt))

        def ps(name, shape, dt=F32):
            return st.enter_context(nc.psum_tensor(un(b, name), list(shape), dt))
        wout = sb("wout", [128, 8, 1024], BF16)
        gnb = sb("gnb", [128, 1024])
        lng = sb("lng", [128, 1024])
        lnb = sb("lnb", [128, 1024])
        with ExitStack() as st2:
            stg = [st2.enter_context(nc.sbuf_tensor(un(b, "stg0"), [128, 8, 512], F32)),
                   st2.enter_context(nc.sbuf_tensor(un(b, "stg1"), [128, 8, 512], F32))]
            p = Phase(kb, f"recw2{l}")
            load_weight_bf16(b, p, wout, g["r_wout"][lr].rearrange("(kc p) n -> p kc n", p=128), 1024, "wout", stg)
            dma(p, nc, "pool", "gnb", gnb[:], g["r_gn"][lr:lr + 1, :].partition_broadcast(128), w=["gnb"])
            dma(p, nc, "pool", "lng", lng[:], g["ln_g"][l, 0:1, :].partition_broadcast(128), w=["lng"])
            dma(p, nc, "pool", "lnb", lnb[:], g["ln_b"][l, 0:1, :].partition_broadcast(128), w=["lnb"])
            p.run()
        of = sb("of", [128, 1024])
        ob = sb("ob", [128, 1024])
        gt = sb("gt", [128, 1024])
        xt = sb("xt", [128, 1024])
        g1b = sb("g1b", [128, 1024])
        sq = sb("sq", [128, 1024])
        ms = sb("ms", [128, 8])
        on = sb("on", [128, 1024], BF16)
        onT = sb("onT", [128, 8, 128], BF16)
        t1 = sb("t1", [128, 1024])
        z = sb("z", [128, 1024])
        xo = sb("xo", [128, 1024])
        stt = sb("stt", [128, 8])
        psOT = ps("psOT", [128, 8, 128], BF16)
        psY = [ps("psY0", [128, 512]), ps("psY1", [128, 512])]
        p = Phase(kb, f"H3_{l}", n_iter=NTILES)
        dma(p, nc, "sp", "of", of[:], lambda i: OF[bass.ds(i * 128, 128), :], w=["of"])
        dma(p, nc, "act", "ob", ob[:], lambda i: OB[bass.ds(i * 128, 128), :], w=["ob"])
        dma(p, nc, "pool", "gt", gt[:], lambda i: GT[bass.ds(i * 128, 128), :], w=["gt"])
        dma(p, nc, "sp", "xt", xt[:], lambda i: X[bass.ds(i * 128, 128), :], w=["xt"])
        dma(p, nc, "act", "g1b", g1b[:], lambda i: MODT[bass.ds(i, 1), 2048:3072].partition_broadcast(128), w=["g1b"])
        tt(p, nc, "dve", of[:], of[:], ob[:], ALU.add, r=["of", "ob"], w=["of"])
        tt(p, nc, "pool", sq[:], of[:], of[:], ALU.mult, r=["of"], w=["sq"])
        p.op("dve", lambda i: nc.vector.tensor_reduce(ms[:], sq[:].rearrange("p (h v) -> p h v", v=128), AX.X, ALU.add), r=["sq"], w=["ms"])
        ts(p, nc, "dve", ms[:], ms[:], 1.0 / 128.0, None, ALU.mult, r=["ms"], w=["ms"])
        act(p, nc, ms[:], ms[:], AF.Ln, bias=b.eps_ln[:, 1:2], r=["ms"], w=["ms"])
        act(p, nc, ms[:], ms[:], AF.Exp, scale=-0.5, r=["ms"], w=["ms"])
        tt(p, nc, "dve", sq[:].rearrange("p (h v) -> p h v", v=128), of[:].rearrange("p (h v) -> p h v", v=128),
           ms[:].unsqueeze(2).to_broadcast([128, 8, 128]), ALU.mult, r=["of", "ms", "sq"], w=["sq"])
        tt(p, nc, "pool", sq[:], sq[:], gnb[:], ALU.mult, r=["sq"], w=["sq"])
        act(p, nc, gt[:], gt[:], AF.Silu, r=["gt"], w=["gt"])
        tt(p, nc, "dve", on[:], sq[:], gt[:], ALU.mult, r=["sq", "gt"], w=["on"])
        for kc in range(8):
            tr(p, nc, psOT[:, kc, :], on[:, kc * 128:(kc + 1) * 128], identb[:], r=["on"], w=["psOT"])
        cp(p, nc, "act", onT[:], psOT[:], r=["psOT"], w=["onT"])
        for n in range(2):
            for kc in range(8):
                mm(p, nc, psY[n][:], onT[:, kc, :], wout[:, kc, n * 512:(n + 1) * 512], kc == 0, kc == 7, r=["onT"], w=[f"psY{n}"])
        ts(p, nc, "pool", g1b[:], g1b[:], 1.0, None, ALU.add, r=["g1b"], w=["g1b"])
        for n in range(2):
            tt(p, nc, "dve", t1[:, n * 512:(n + 1) * 512], psY[n][:], g1b[:, n * 512:(n + 1) * 512], ALU.mult,
               r=[f"psY{n}", "g1b"], w=["t1"])
        p.op("dve", lambda i: nc.vector.scalar_tensor_tensor(z[:], xt[:], alpha, t1[:], ALU.mult, ALU.add), r=["xt", "t1"], w=["z"])
        layer_norm_tail(b, p, z, "z", lng, lnb, xo, "xo", dict(sq=sq, st=stt), "ln")
        dma(p, nc, "sp", "xo", lambda i: X[bass.ds(i * 128, 128), :], xo[:], r=["xo"])
        p.run()
```

```python
import math
from collections import Counter
from contextlib import ExitStack
import numpy as np
import ml_dtypes
import concourse.bass as bass
import concourse.mybir as mybir
from concourse.bass_utils import run_bass_kernel_spmd

F32 = mybir.dt.float32
BF16 = mybir.dt.bfloat16
I32 = mybir.dt.int32
AF = mybir.ActivationFunctionType
ALU = mybir.AluOpType
AX = mybir.AxisListType

D = 1024
HD = 64
N_GROUPS = 4
EPG = 8
NEXP = 32
DEXP = 512
LN_EPS = 1e-5
RMS_EPS = 1e-6
ROPE_THETA = 10000.0


class KB:
    def __init__(self, nc):
        self.nc = nc
        self.engs = {"pe": nc.tensor, "act": nc.scalar, "dve": nc.vector, "pool": nc.gpsimd, "sp": nc.sync}
        self.sems = {}
        self.base = {}
        for e in self.engs:
            self.sems[("e", e)] = nc.alloc_semaphore(name="S_" + e)
            self.base[("e", e)] = 0
        self.chan_owner = {}
        self.nphase = 0

    def sig(self, key):
        if key not in self.sems:
            self.sems[key] = self.nc.alloc_semaphore(name="C_" + str(key[1]))
            self.base[key] = 0
        return self.sems[key]


class Phase:
    def __init__(self, kb, name, n_iter=None):
        self.kb = kb
        self.name = name
        self.n_iter = n_iter
        self.ops = []

    ns = ""

    def _n(self, name):
        return name[1:] if name.startswith("!") else name + self.ns

    def op(self, eng, fn, r=(), w=(), chan=None):
        self.ops.append(dict(eng=eng, fn=fn, r=tuple(self._n(x) for x in r), w=tuple(self._n(x) for x in w),
                             chan=(chan + self.ns) if chan else None))

    def dma(self, eng, chan, fn, r=(), w=()):
        self.op(eng, fn, r, w, chan=chan)

    def interleave(self, a, b_, c):
        A, Bq = self.ops[a:b_], self.ops[b_:c]
        out = []
        for k in range(max(len(A), len(Bq))):
            if k < len(A):
                out.append(A[k])
            if k < len(Bq):
                out.append(Bq[k])
        self.ops[a:c] = out

    def run(self):
        kb = self.kb
        nc = kb.nc
        ops = self.ops
        m = len(ops)
        if m == 0:
            return
        loop = self.n_iter is not None
        N = self.n_iter if loop else 1
        sig_of = [("c", o["chan"]) if o["chan"] else ("e", o["eng"]) for o in ops]
        for j, o in enumerate(ops):
            if o["chan"]:
                own = kb.chan_owner.setdefault(o["chan"], o["eng"])
                assert own == o["eng"], f"chan {o['chan']} issued from two engines"
        phys = {}
        kcnt = {"sw": 0, "hw": 0}
        for j, o in enumerate(ops):
            sg_ = sig_of[j]
            if sg_ in phys:
                continue
            if sg_[0] == "e":
                phys[sg_] = sg_
            else:
                kind = "sw" if o["eng"] == "pool" else "hw"
                phys[sg_] = ("c", f"{kind}{kcnt[kind]}")
                kcnt[kind] += 1
        per_iter = Counter(sig_of)
        kidx = []
        cnt = Counter()
        for j in range(m):
            kidx.append(cnt[sig_of[j]])
            cnt[sig_of[j]] += 1
        last_w = {}
        readers = {}
        deps = [set() for _ in range(m)]
        for it in ((0, 1) if loop else (0,)):
            for j, o in enumerate(ops):
                d = set()
                for b in o["r"]:
                    if b in last_w:
                        d.add(last_w[b] + ("raw",))
                for b in o["w"]:
                    if b in last_w:
                        d.add(last_w[b] + ("waw",))
                    for rd in readers.get(b, []):
                        d.add(rd + ("war",))
                if it == (1 if loop else 0):
                    deps[j] = {(jj, itt - it, ty) for (itt, jj, ty) in d if (itt, jj) != (it, j)}
                for b in o["r"]:
                    readers.setdefault(b, []).append((it, j))
                for b in o["w"]:
                    last_w[b] = (it, j)
                    readers[b] = []
        waits = []
        for j, o in enumerate(ops):
            need = {}
            for (jj, off, ty) in deps[j]:
                p = ops[jj]
                s = sig_of[jj]
                if s[0] == "e" and sig_of[j][0] == "e" and p["eng"] == o["eng"]:
                    if o["eng"] == "pe" or ty != "raw":
                        continue
                n_s = per_iter[s]
                if s[0] == "c":
                    if off == 0:
                        q = sum(1 for x in range(j) if sig_of[x] == s)
                        const = q * 16
                    else:
                        const = 0
                else:
                    const = (kidx[jj] + 1) if off == 0 else (kidx[jj] + 1 - n_s)
                if s not in need or const > need[s]:
                    need[s] = const
            waits.append(need)
        known = {}
        for j, o in enumerate(ops):
            kn = known.setdefault(o["eng"], {})
            for s in list(waits[j]):
                if s in kn and kn[s] >= waits[j][s]:
                    del waits[j][s]
                else:
                    kn[s] = waits[j][s]
        T0 = {}
        for s, n_s in per_iter.items():
            kb.sig(phys[s])
            inc = 16 if s[0] == "c" else 1
            T0[s] = kb.base[phys[s]] + (n_s * inc if (loop and s[0] == "e") else 0)
        engines_used = []
        for o in ops:
            if o["eng"] not in engines_used:
                engines_used.append(o["eng"])

        def free_ids(E):
            hs, ids = [], []
            while True:
                try:
                    kb.probe_cnt = getattr(kb, "probe_cnt", 0) + 1
                    h = E.alloc_register(f"probe{kb.probe_cnt}")
                except Exception:
                    break
                hs.append(h)
                ids.append(nc.lookup_reg(h).reg_id)
            for h in hs:
                E.free_register(h)
            return set(ids)

        def emit_engine(ename):
            E = kb.engs[ename]
            before = free_ids(E)
            emit_engine_inner(ename)
            leaked = sorted(before - free_ids(E))
            for rid in leaked:
                kb.probe_cnt += 1
                h = nc.add_register(E.engine, f"leak{kb.probe_cnt}", rid)
                E.free_register(h)

        def emit_engine_inner(ename):
            E = kb.engs[ename]
            mine = [j for j, o in enumerate(ops) if o["eng"] == ename]
            wsigs = []
            for j in mine:
                for s in waits[j]:
                    if s not in wsigs:
                        wsigs.append(s)

            def body(i, regs, tmp):
                for j in mine:
                    o = ops[j]
                    for s, const in waits[j].items():
                        if i is None:
                            E.wait_ge(kb.sig(phys[s]), T0[s] + const)
                        elif const == 0:
                            E.wait_ge(kb.sig(phys[s]), regs[s])
                        else:
                            E.reg_add(tmp, regs[s], const)
                            E.wait_ge(kb.sig(phys[s]), tmp)
                    ins = o["fn"](i)
                    s = sig_of[j]
                    ins.then_inc(kb.sig(phys[s]), 16 if s[0] == "c" else 1)

            if loop:
                if ("e", ename) in per_iter:
                    E.sem_inc(kb.sig(("e", ename)), per_iter[("e", ename)])
                regs = {}
                for k, s in enumerate(wsigs):
                    regs[s] = E.alloc_register(f"w{kb.nphase}_{ename}_{k}")
                    E.reg_mov(regs[s], T0[s])
                tmp = E.alloc_register(f"wt{kb.nphase}_{ename}")
                with E.Fori(0, N) as i:
                    body(i, regs, tmp)
                    for s in wsigs:
                        E.reg_add(regs[s], regs[s], per_iter[s] * (16 if s[0] == "c" else 1))
                for s in wsigs:
                    E.free_register(regs[s])
                E.free_register(tmp)
            else:
                body(None, None, None)
            for s, n_s in per_iter.items():
                owner = kb.chan_owner[s[1]] if s[0] == "c" else s[1]
                if owner == ename:
                    inc = 16 if s[0] == "c" else 1
                    E.wait_ge(kb.sig(phys[s]), T0[s] + N * n_s * inc)

        with nc.Block() as blk:
            reg = {"pe": blk.tensor, "act": blk.scalar, "dve": blk.vector, "pool": blk.gpsimd, "sp": blk.sync}
            for ename in engines_used:
                reg[ename](lambda eng, ename=ename: emit_engine(ename))
        for s, n_s in per_iter.items():
            inc = 16 if s[0] == "c" else 1
            kb.base[phys[s]] = T0[s] + N * n_s * inc
        nc.all_engine_barrier()
        kb.nphase += 1


def _bf(a):
    return np.ascontiguousarray(a).astype(ml_dtypes.bfloat16)


def host_constants(cfg):
    seqs = cfg["seqs"]
    NT = sum(seqs)
    c = {}
    c["ident_f"] = np.eye(128, dtype=np.float32)
    c["ident_b"] = _bf(np.eye(128, dtype=np.float32))
    pos = np.concatenate([np.arange(t) for t in seqs]).astype(np.float32)
    inv = np.power(ROPE_THETA, -np.arange(0, HD, 2, dtype=np.float32) / HD).astype(np.float32)
    d = np.arange(128) % 64
    ang = pos[None, :] * inv[d % 32][:, None]
    c["rope_cos"] = np.cos(ang).astype(np.float32)
    sgn = np.where(d < 32, -1.0, 1.0).astype(np.float32)
    c["rope_sin"] = (np.sin(ang) * sgn[:, None]).astype(np.float32)
    j = np.arange(128)[:, None]
    i = np.arange(128)[None, :]
    c["wmask"] = _bf(np.stack([(j >= i), (j <= i)], axis=1).astype(np.float32))
    kc = np.arange(64)
    qc = np.arange(64)
    qwin = np.clip(qc - 8, 0, 48)
    col_ok = (kc[:, None] >= qwin[None, :]) & (kc[:, None] < qwin[None, :] + 16)
    m = np.zeros((128, 9, 128), np.float32)
    for t in range(9):
        for b in range(2):
            for a in range(2):
                ok = col_ok.astype(np.float32)
                if t == 7 and (a == 1 and b == 0):
                    ok = ok * 0
                if t == 8 and not (a == 1 and b == 0):
                    ok = ok * 0
                m[b * 64:(b + 1) * 64, t, a * 64:(a + 1) * 64] = ok
    c["namask"] = m
    s = np.arange(64)[:, None]
    t = np.arange(64)[None, :]
    c["hmask"] = _bf(np.stack([(s <= t), (s >= t)], axis=1).astype(np.float32))
    sm = np.ones((128, 8, 128), np.float32)
    sm[:, :, 0] = 0
    sm[:, :, 64] = 0
    c["scanmask"] = sm.reshape(128, 1024)
    c["iota32"] = np.tile(np.arange(32, dtype=np.float32)[None, :], (128, 1))
    c["iota_blk"] = np.tile((np.arange(256, dtype=np.float32) * 128.0)[None, :], (128, 1))
    c["iota_p"] = np.arange(128, dtype=np.float32).reshape(128, 1)
    c["tri_su"] = _bf((np.arange(128)[:, None] < np.arange(128)[None, :]).astype(np.float32))
    c["ones_b"] = _bf(np.ones((128, 128), np.float32))
    return c


NA_TABLE_DELTA = [-3, -2, -1, 0, 1, 2, 3, -2, 2]


def na_bias_tables(rel_bias):
    L = rel_bias.shape[0]
    kc = np.arange(64)[:, None]
    qc = np.arange(64)[None, :]
    dc = np.clip(kc - qc, -15, 15) + 15
    out = np.zeros((L, 128, 9, 8, 128), np.float32)
    for t, dl in enumerate(NA_TABLE_DELTA):
        for b in range(2):
            for a in range(2):
                dr = 2 * dl + b - a + 7
                g = rel_bias[:, :, dr, :][:, :, dc]
                out[:, b * 64:(b + 1) * 64, t, :, a * 64:(a + 1) * 64] = g.transpose(0, 2, 1, 3)
    return out


def attn_w_layout(w_in):
    L = w_in.shape[0]
    qa0, ka0, va0, qb0, kb0, vb0 = 0, 512, 640, 768, 1280, 1792
    sw = (np.arange(64) + 32) % 64
    cols = []
    for jc in range(4):
        for h in (jc, 4 + jc):
            cols += list(qa0 + h * 64 + np.arange(64))
    for jc in range(4):
        for h in (jc, 4 + jc):
            cols += list(qa0 + h * 64 + sw)
    for h in range(2):
        cols += list(ka0 + h * 64 + np.arange(64))
    for h in range(2):
        cols += list(ka0 + h * 64 + sw)
    cols += list(qb0 + np.arange(512))
    cols += list(kb0 + np.arange(512))
    cols = np.array(cols)
    vcols = np.concatenate([va0 + np.arange(128), vb0 + np.arange(512)])
    return np.ascontiguousarray(w_in[:, :, cols]), np.ascontiguousarray(w_in[:, :, vcols])


class B:
    def __init__(self, cfg):
        self.cfg = cfg
        self.nc = bass.Bass("TRN2", target_bir_lowering=False)
        self.kb = KB(self.nc)
        self.dram = {}
        self.uid = 0

    def din(self, name, shape, dt=F32):
        t = self.nc.dram_tensor(name, list(shape), dt, kind="ExternalInput")
        self.dram[name] = t
        return t

    def dout(self, name, shape, dt=F32):
        t = self.nc.dram_tensor(name, list(shape), dt, kind="ExternalOutput")
        self.dram[name] = t
        return t

    def dscr(self, name, shape, dt=F32):
        if self.cfg.get("debug"):
            return self.dout(name, shape, dt)
        t = self.nc.dram_tensor(name, list(shape), dt)
        self.dram[name] = t
        return t


def un(b, name):
    b.uid += 1
    return f"{name}_{b.uid}"


def _ap(x, i):
    return x(i) if callable(x) else x


def dma(p, nc, eng, chan, out, in_, r=(), w=(), slow=False):
    E = {"sp": nc.sync, "act": nc.scalar, "pool": nc.gpsimd}[eng]
    if slow:
        p.dma(eng, chan, lambda i: E.dma_start(out=_ap(out, i), in_=_ap(in_, i), allow_slow_non_contiguous=True), r=r, w=w)
    else:
        p.dma(eng, chan, lambda i: E.dma_start(out=_ap(out, i), in_=_ap(in_, i)), r=r, w=w)


def eng_of(nc, e):
    return {"dve": nc.vector, "pool": nc.gpsimd, "act": nc.scalar}[e]


def tt(p, nc, e, out, in0, in1, op, r=(), w=()):
    E = eng_of(nc, e)
    p.op(e, lambda i: E.tensor_tensor(_ap(out, i), _ap(in0, i), _ap(in1, i), op), r=r, w=w)


def ts(p, nc, e, out, in0, s1, s2, op0, op1=None, r=(), w=()):
    E = eng_of(nc, e)
    if op1 is None:
        p.op(e, lambda i: E.tensor_scalar(_ap(out, i), _ap(in0, i), s1, None, op0), r=r, w=w)
    else:
        p.op(e, lambda i: E.tensor_scalar(_ap(out, i), _ap(in0, i), s1, s2, op0, op1), r=r, w=w)


def cp(p, nc, e, out, in_, r=(), w=()):
    if e == "act":
        p.op(e, lambda i: nc.scalar.copy(_ap(out, i), _ap(in_, i)), r=r, w=w)
    else:
        E = eng_of(nc, e)
        p.op(e, lambda i: E.tensor_copy(_ap(out, i), _ap(in_, i)), r=r, w=w)


def act(p, nc, out, in_, func, bias=None, scale=None, r=(), w=()):
    kw = {}
    if bias is not None:
        kw["bias"] = bias
    if scale is not None:
        kw["scale"] = scale
    p.op("act", lambda i: nc.scalar.activation(_ap(out, i), _ap(in_, i), func, **kw), r=r, w=w)


def mm(p, nc, out, lhsT, rhs, start, stop, r=(), w=()):
    p.op("pe", lambda i: nc.tensor.matmul(_ap(out, i), lhsT=_ap(lhsT, i), rhs=_ap(rhs, i), start=start, stop=stop), r=r, w=w)


def tr(p, nc, out, in_, ident, r=(), w=()):
    p.op("pe", lambda i: nc.tensor.transpose(_ap(out, i), _ap(in_, i), ident), r=r, w=w)


def load_weight_bf16(b, p, dst, src, ncols, tag, stg, kdim=8):
    nc = b.nc
    engs = ["dve", "act", "pool"]
    c0 = 0
    k = 0
    while c0 < ncols:
        cw = min(512, ncols - c0)
        st = stg[k % 2]
        sname = f"stg{k % 2}"
        dma(p, nc, "sp" if k % 2 == 0 else "act", f"wstg{k % 2}", st[:, :, 0:cw], src[:, :, c0:c0 + cw], w=[sname])
        cp(p, nc, engs[k % 3], dst[:, :, c0:c0 + cw], st[:, :, 0:cw], r=[sname], w=[tag + str(k)])
        c0 += cw
        k += 1


def layer_norm_tail(b, p, z, zname, lng, lnb, out, outname, scr, tg):
    nc = b.nc
    sq, st = scr["sq"], scr["st"]
    p.op("dve", lambda i: nc.vector.tensor_reduce(st[:, 0:1], z[:], AX.X, ALU.add), r=[zname], w=[tg + "s0"])
    tt(p, nc, "pool", sq[:], z[:], z[:], ALU.mult, r=[zname], w=[tg + "sq"])
    p.op("dve", lambda i: nc.vector.tensor_reduce(st[:, 1:2], sq[:], AX.X, ALU.add), r=[tg + "sq"], w=[tg + "s1"])
    ts(p, nc, "dve", st[:, 2:3], st[:, 0:1], 1.0 / D, None, ALU.mult, r=[tg + "s0"], w=[tg + "s2"])
    ts(p, nc, "dve", st[:, 3:4], st[:, 1:2], 1.0 / D, None, ALU.mult, r=[tg + "s1"], w=[tg + "s3"])
    tt(p, nc, "dve", st[:, 4:5], st[:, 2:3], st[:, 2:3], ALU.mult, r=[tg + "s2"], w=[tg + "s4"])
    tt(p, nc, "dve", st[:, 5:6], st[:, 3:4], st[:, 4:5], ALU.subtract, r=[tg + "s3", tg + "s4"], w=[tg + "s5"])
    act(p, nc, st[:, 6:7], st[:, 5:6], AF.Ln, bias=b.eps_ln[:, 0:1], r=[tg + "s5"], w=[tg + "s6"])
    act(p, nc, st[:, 6:7], st[:, 6:7], AF.Exp, scale=-0.5, r=[tg + "s6"], w=[tg + "s6"])
    p.op("dve", lambda i: nc.vector.scalar_tensor_tensor(st[:, 7:8], st[:, 2:3], -1.0, st[:, 6:7], ALU.mult, ALU.mult),
         r=[tg + "s2", tg + "s6"], w=[tg + "s7"])
    act(p, nc, sq[:], z[:], AF.Identity, bias=st[:, 7:8], scale=st[:, 6:7], r=[zname, tg + "s6", tg + "s7", tg + "sq"], w=[tg + "sq"])
    tt(p, nc, "dve", sq[:], sq[:], lng[:], ALU.mult, r=[tg + "sq"], w=[tg + "sq"])
    tt(p, nc, "pool", out[:], sq[:], lnb[:], ALU.add, r=[tg + "sq"], w=[outname])


def build_program(cfg):
    seqs = cfg["seqs"]
    NT = sum(seqs)
    NTILES = NT // 128
    depth = cfg["depth"]
    LA = (depth + 1) // 2
    LR = depth // 2
    stop = cfg.get("stop")
    alpha = float((2 * cfg.get("dn_depth", depth)) ** 0.25)
    b = B(cfg)
    nc = b.nc
    kb = b.kb

    x_in = b.din("x", [NT, D])
    cT_in = b.din("cT", [128, 3, 8])
    ada_w = b.din("ada_w", [depth, D, 6 * D])
    ada_b = b.din("ada_b", [depth, 6 * D])
    ln_g = b.din("ln_g", [depth, 2, D])
    ln_b = b.din("ln_b", [depth, 2, D])
    a_wfm = b.din("attn_wfm", [LA, D, 2304])
    a_wv = b.din("attn_wv", [LA, D, 640])
    a_sink = b.din("attn_sink", [LA, 8])
    a_rb = b.din("na_rb", [LA, 128, 9, 8, 128])
    a_wout = b.din("attn_w_out", [LA, D, D])
    r_win = b.din("rec_w_in", [max(LR, 1), D, 5 * D])
    r_lb = b.din("rec_lb", [max(LR, 1), D])
    r_gn = b.din("rec_gnorm", [max(LR, 1), D])
    r_wout = b.din("rec_w_out", [max(LR, 1), D, D])
    ro_w = b.din("router_w", [depth, D, 36])
    ro_b = b.din("router_b", [depth, 36])
    e_w = b.din("e_w", [depth * NEXP * 128, 12288])
    cn = {}
    for name, shape, dt in [("ident_f", [128, 128], F32), ("ident_b", [128, 128], BF16), ("rope_cos", [128, NT], F32),
                            ("rope_sin", [128, NT], F32), ("wmask", [128, 2, 128], BF16), ("namask", [128, 9, 128], F32),
                            ("hmask", [64, 2, 64], BF16), ("scanmask", [128, 1024], F32), ("iota32", [128, 32], F32),
                            ("iota_blk", [128, 256], F32), ("iota_p", [128, 1], F32), ("tri_su", [128, 128], BF16),
                            ("ones_b", [128, 128], BF16)]:
        cn[name] = b.din(name, shape, dt)
    y_out = b.dout("y", [NT, D])
    X = b.dscr("X", [NT, D])
    MODT = b.dscr("MODT", [NTILES, 6 * D])
    FM = b.dscr("FM", [13, 128, NT], BF16)
    VE = b.dscr("VE", [NT, 720], BF16)

    ODBG = b.dout("ODBG", [NT, D], BF16) if cfg.get("debug") else None
    seq_tile0 = [0]
    for t in seqs[:-1]:
        seq_tile0.append(seq_tile0[-1] + t // 128)

    with ExitStack() as gs:
        def sb(name, shape, dt=F32, stack=gs):
            return stack.enter_context(nc.sbuf_tensor(un(b, name), list(shape), dt))

        def ps(name, shape, dt=F32, stack=gs):
            return stack.enter_context(nc.psum_tensor(un(b, name), list(shape), dt))

        identf = sb("identf", [128, 128])
        identb = sb("identb", [128, 128], BF16)
        CS = sb("CS", [128, 24, 128])
        epst = sb("epst", [128, 2])
        b.eps_ln = epst
        csb = sb("csb", [128, 24])
        p = Phase(kb, "setup")
        p.op("pool", lambda i: nc.gpsimd.memset(epst[:, 0:1], LN_EPS), w=["epst0"])
        p.op("pool", lambda i: nc.gpsimd.memset(epst[:, 1:2], RMS_EPS), w=["epst1"])
        dma(p, nc, "sp", "k0", identf[:], cn["ident_f"][:, :], w=["identf"])
        dma(p, nc, "sp", "k1", identb[:], cn["ident_b"][:, :], w=["identb"])
        dma(p, nc, "sp", "k2", csb[:], cT_in.ap().rearrange("p s k -> p (s k)"), w=["csb"])
        act(p, nc, csb[:], csb[:], AF.Silu, r=["csb"], w=["csb"])
        cp(p, nc, "dve", CS[:], csb[:].unsqueeze(2).to_broadcast([128, 24, 128]), r=["csb"], w=["CS"])
        p.run()

        p = Phase(kb, "xcopy")
        nchunk = max(1, NT // 1024)
        rows = NT // nchunk
        for k in range(nchunk):
            dma(p, nc, ["sp", "act", "pool"][k % 3], f"xc{k % 3}", X[k * rows:(k + 1) * rows, :], x_in[k * rows:(k + 1) * rows, :])
        p.run()

        for l in range(depth):
            with ExitStack() as st:
                aw = sb("aw", [128, 8, 512], stack=st)
                ab = sb("ab", [128, 512], stack=st)
                mo = [sb(f"mo{s}", [128, 512], stack=st) for s in range(3)]
                pm = [ps(f"pm{s}", [128, 512], stack=st) for s in range(3)]
                p = Phase(kb, f"mod{l}", n_iter=12)
                dma(p, nc, "sp", "aw", aw[:], lambda i: ada_w[l].rearrange("(kc p) n -> p kc n", p=128)[:, :, bass.ds(i * 512, 512)], w=["aw"])
                dma(p, nc, "act", "ab", ab[:], lambda i: ada_b[l:l + 1, bass.ds(i * 512, 512)].partition_broadcast(128), w=["ab"])
                for s in range(3):
                    for kc in range(8):
                        mm(p, nc, pm[s][:], CS[:, s * 8 + kc, :], aw[:, kc, :], kc == 0, kc == 7, r=["aw"], w=[f"pm{s}"])
                    tt(p, nc, "dve", mo[s][:], pm[s][:], ab[:], ALU.add, r=[f"pm{s}", "ab"], w=[f"mo{s}"])
                    nts = seqs[s] // 128
                    dma(p, nc, "pool", f"mo{s}", lambda i, s=s, nts=nts: MODT[seq_tile0[s]:seq_tile0[s] + nts, bass.ds(i * 512, 512)],
                        mo[s][0:nts, :], r=[f"mo{s}"])
                p.run()
            if stop == "mod":
                break
            if l % 2 == 0:
                build_attention_layer(b, l, l // 2, dict(X=X, MODT=MODT, FM=FM, VE=VE, a_wfm=a_wfm, a_wv=a_wv, a_sink=a_sink,
                                                         a_rb=a_rb, a_wout=a_wout, ln_g=ln_g, ln_b=ln_b, cn=cn, identb=identb,
                                                         identf=identf, alpha=alpha, seq_tile0=seq_tile0, ODBG=ODBG))
            else:
                build_hgrn_layer(b, l, l // 2, dict(X=X, MODT=MODT, r_win=r_win, r_lb=r_lb, r_gn=r_gn, r_wout=r_wout,
                                                    ln_g=ln_g, ln_b=ln_b, cn=cn, identb=identb, identf=identf, alpha=alpha,
                                                    seq_tile0=seq_tile0))
            if stop in (f"mix{l}", "attw", "A1", "attpre", "A2e") or (stop in ("H1", "H2") and l % 2 == 1):
                break
            build_moe_layer(b, l, dict(X=X, MODT=MODT, ro_w=ro_w, ro_b=ro_b, e_w=e_w, ln_g=ln_g,
                                       ln_b=ln_b, cn=cn, identb=identb, identf=identf, alpha=alpha))
            if stop in (f"moe{l}", "moeD", "moeS", "moeB"):
                break

        p = Phase(kb, "ycopy")
        for k in range(nchunk):
            dma(p, nc, ["sp", "act", "pool"][k % 3], f"yc{k % 3}", y_out[k * rows:(k + 1) * rows, :], X[k * rows:(k + 1) * rows, :])
        p.run()
    return nc


def build_attention_layer(b, l, la, g):
    nc, kb = b.nc, b.kb
    seqs = b.cfg["seqs"]
    NT = sum(seqs)
    X, MODT, FM, VE, cn = g["X"], g["MODT"], g["FM"], g["VE"], g["cn"]
    identb = g["identb"]
    alpha = g["alpha"]
    seq_tile0 = g["seq_tile0"]

    with ExitStack() as st:
        def sb(name, shape, dt=F32):
            return st.enter_context(nc.sbuf_tensor(un(b, name), list(shape), dt))

        def ps(name, shape, dt=F32):
            return st.enter_context(nc.psum_tensor(un(b, name), list(shape), dt))

        wfm = sb("wfm", [128, 8, 2304], BF16)
        wv = sb("wv", [128, 8, 640], BF16)
        stg = [sb("stg0", [128, 8, 512]), sb("stg1", [128, 8, 512])]
        veall = sb("veall", [128, 4, 10, 72], BF16)
        p = Phase(kb, f"attw{l}")
        load_weight_bf16(b, p, wfm, g["a_wfm"][la].rearrange("(kc p) n -> p kc n", p=128), 2304, "wfm", stg)
        load_weight_bf16(b, p, wv, g["a_wv"][la].rearrange("(kc p) n -> p kc n", p=128), 640, "wv", stg)
        p.op("pool", lambda i: nc.gpsimd.memset(veall[:], 1.0), w=["veall"])
        p.run()
        if b.cfg.get("stop") == "attw":
            return

        xg = sb("xg", [128, 4, 1024])
        scb = sb("scb", [128, 1024])
        shb = sb("shb", [128, 1024])
        tmp = [sb("tmp0", [128, 1024]), sb("tmp1", [128, 1024])]
        hb = sb("hb", [128, 4, 1024], BF16)
        hT = sb("hT", [128, 8, 512], BF16)
        ropec = sb("ropec", [128, 512])
        ropes = sb("ropes", [128, 512])
        t1 = sb("t1", [128, 512])
        t2 = sb("t2", [128, 512])
        fmall = sb("fmall", [128, 13, 512], BF16)
        psT = [ps("psT0", [128, 8, 128], BF16), ps("psT1", [128, 8, 128], BF16)]
        pf = [ps(f"pf{k}", [128, 512]) for k in range(3)]
        pva = ps("pva", [128, 512])
        pvb = ps("pvb", [128, 512])

        NG = NT // 512
        p = Phase(kb, f"A1_{l}", n_iter=NG)
        dma(p, nc, "sp", "xg", xg[:], lambda i: X[bass.ds(i * 512, 512), :].rearrange("(t p) d -> p t d", p=128), w=["xg"])
        dma(p, nc, "act", "scb", scb[:], lambda i: MODT[bass.ds(i * 4, 1), 1024:2048].partition_broadcast(128), w=["scb"])
        dma(p, nc, "act", "shb", shb[:], lambda i: MODT[bass.ds(i * 4, 1), 0:1024].partition_broadcast(128), w=["shb"])
        dma(p, nc, "pool", "ropec", ropec[:], lambda i: cn["rope_cos"][:, bass.ds(i * 512, 512)], w=["ropec"])
        dma(p, nc, "pool", "ropes", ropes[:], lambda i: cn["rope_sin"][:, bass.ds(i * 512, 512)], w=["ropes"])
        ts(p, nc, "dve", scb[:], scb[:], 1.0, None, ALU.add, r=["scb"], w=["scb"])
        for t in range(4):
            tt(p, nc, "dve", tmp[t % 2][:], xg[:, t, :], scb[:], ALU.mult, r=["xg", "scb"], w=[f"tmp{t % 2}"])
            tt(p, nc, "pool", hb[:, t, :], tmp[t % 2][:], shb[:], ALU.add, r=[f"tmp{t % 2}", "shb"], w=[f"hb{t}"])
            for kc in range(8):
                tr(p, nc, psT[t % 2][:, kc, :], hb[:, t, kc * 128:(kc + 1) * 128], identb[:], r=[f"hb{t}"], w=[f"psT{t % 2}"])
            cp(p, nc, "act", hT[:, :, t * 128:(t + 1) * 128], psT[t % 2][:], r=[f"psT{t % 2}"], w=[f"hT{t}"])
        hTall = [f"hT{t}" for t in range(4)]
        pfi = 0
        fk = 0
        for (ca, cb_, dst) in [(0, 4, 0), (1, 5, 1), (2, 6, 2), (3, 7, 3), (8, 9, 4)]:
            ia, ib = pfi % 3, (pfi + 1) % 3
            pfi += 2
            for kc in range(8):
                mm(p, nc, pf[ia][:], wfm[:, kc, ca * 128:(ca + 1) * 128], hT[:, kc, :], kc == 0, kc == 7, r=hTall, w=[f"pf{ia}"])
            for kc in range(8):
                mm(p, nc, pf[ib][:], wfm[:, kc, cb_ * 128:(cb_ + 1) * 128], hT[:, kc, :], kc == 0, kc == 7, r=hTall, w=[f"pf{ib}"])
            tt(p, nc, "dve", t1[:], pf[ia][:], ropec[:], ALU.mult, r=[f"pf{ia}", "ropec"], w=["t1"])
            tt(p, nc, "dve", t2[:], pf[ib][:], ropes[:], ALU.mult, r=[f"pf{ib}", "ropes"], w=["t2"])
            tt(p, nc, "pool", fmall[:, dst, :], t1[:], t2[:], ALU.add, r=["t1", "t2"], w=[f"fm{dst}"])
        for c in range(10, 18):
            dst = 5 + (c - 10)
            ia = pfi % 3
            pfi += 1
            for kc in range(8):
                mm(p, nc, pf[ia][:], wfm[:, kc, c * 128:(c + 1) * 128], hT[:, kc, :], kc == 0, kc == 7, r=hTall, w=[f"pf{ia}"])
            cp(p, nc, "act", fmall[:, dst, :], pf[ia][:], r=[f"pf{ia}"], w=[f"fm{dst}"])
        for t in range(4):
            for kc in range(8):
                mm(p, nc, pva[:], hT[:, kc, t * 128:(t + 1) * 128], wv[:, kc, 128:640], kc == 0, kc == 7, r=[f"hT{t}"], w=["pva"])
            for kc in range(8):
                mm(p, nc, pvb[:, 0:128], hT[:, kc, t * 128:(t + 1) * 128], wv[:, kc, 0:128], kc == 0, kc == 7, r=[f"hT{t}"], w=["pvb"])
            cp(p, nc, "dve", veall[:, t, 2:10, 0:64], pva[:].rearrange("p (h d) -> p h d", d=64), r=["pva"], w=[f"ve{t}"])
            cp(p, nc, "act", veall[:, t, 0:2, 0:64], pvb[:, 0:128].rearrange("p (h d) -> p h d", d=64), r=["pvb"], w=[f"ve{t}"])
        dma(p, nc, "sp", "fmall", lambda i: FM[:, :, bass.ds(i * 512, 512)].rearrange("f p t -> p f t"), fmall[:],
            r=[f"fm{d_}" for d_ in range(13)])
        dma(p, nc, "pool", "veall", lambda i: VE[bass.ds(i * 512, 512), :].rearrange("(t p) c -> p t c", p=128),
            veall[:].rearrange("p t h d -> p t (h d)"), r=[f"ve{t}" for t in range(4)])
        p.run()
    if b.cfg.get("stop") == "A1":
        return

    with ExitStack() as st:
        def sb(name, shape, dt=F32):
            return st.enter_context(nc.sbuf_tensor(un(b, name), list(shape), dt))

        def ps(name, shape, dt=F32):
            return st.enter_context(nc.psum_tensor(un(b, name), list(shape), dt))

        wout = sb("wout", [128, 8, 1024], BF16)
        EB = sb("EB", [128, 9, 8, 128], BF16)
        skx = sb("skx", [128, 8])
        wm = sb("wm", [128, 2, 128], BF16)
        lng = sb("lng", [128, 1024])
        lnb = sb("lnb", [128, 1024])
        with ExitStack() as st2:
            stg = [st2.enter_context(nc.sbuf_tensor(un(b, "stg0"), [128, 8, 512], F32)),
                   st2.enter_context(nc.sbuf_tensor(un(b, "stg1"), [128, 8, 512], F32))]
            nam = st2.enter_context(nc.sbuf_tensor(un(b, "nam"), [128, 9, 128], F32))
            rbs = [st2.enter_context(nc.sbuf_tensor(un(b, "rbs0"), [128, 8, 128], F32)),
                   st2.enter_context(nc.sbuf_tensor(un(b, "rbs1"), [128, 8, 128], F32))]
            p = Phase(kb, f"attpre{l}")
            load_weight_bf16(b, p, wout, g["a_wout"][la].rearrange("(kc p) n -> p kc n", p=128), 1024, "wout", stg)
            dma(p, nc, "pool", "nam", nam[:], cn["namask"][:, :, :], w=["nam"])
            dma(p, nc, "pool", "wm", wm[:], cn["wmask"][:, :, :], w=["wm"])
            dma(p, nc, "pool", "skx", skx[:], g["a_sink"][la:la + 1, :].partition_broadcast(128), w=["skx"])
            dma(p, nc, "pool", "lng", lng[:], g["ln_g"][l, 0:1, :].partition_broadcast(128), w=["lng"])
            dma(p, nc, "pool", "lnb", lnb[:], g["ln_b"][l, 0:1, :].partition_broadcast(128), w=["lnb"])
            act(p, nc, skx[:], skx[:], AF.Exp, r=["skx"], w=["skx"])
            for t in range(9):
                rb = rbs[t % 2]
                dma(p, nc, "sp", f"rbs{t % 2}", rb[:], g["a_rb"][la, :, t, :, :], w=[f"rbs{t % 2}"])
                act(p, nc, rb[:], rb[:], AF.Exp, r=[f"rbs{t % 2}"], w=[f"rbs{t % 2}"])
                tt(p, nc, "dve", EB[:, t, :, :], rb[:], nam[:, t, :].unsqueeze(1).to_broadcast([128, 8, 128]), ALU.mult,
                   r=[f"rbs{t % 2}", "nam"], w=[f"EB{t}"])
            p.run()
        if b.cfg.get("stop") == "attpre":
            return

        qa = sb("qa", [128, 4, 128], BF16)
        ka = sb("ka", [128, 3, 128], BF16)
        va = sb("va", [128, 3, 2, 72], BF16)
        qb = sb("qb", [64, 8, 128], BF16)
        kbt = sb("kbt", [64, 8, 5, 128], BF16)
        vb = sb("vb", [128, 5, 8, 72], BF16)
        xt = sb("xt", [128, 1024])
        g1b = sb("g1b", [128, 1024])
        PT = [sb("PT0", [128, 5, 4, 128], BF16), sb("PT1", [128, 5, 4, 128], BF16)]
        o = sb("o", [128, 1024], BF16)
        oT = sb("oT", [128, 8, 128], BF16)
        t1 = sb("t1", [128, 1024])
        z = sb("z", [128, 1024])
        sq = sb("sq", [128, 1024])
        xo = sb("xo", [128, 1024])
        stt = sb("stt", [128, 8])
        den = sb("den", [128, 4, 1])
        pS = [ps("pS0", [128, 4, 128]), ps("pS1", [128, 4, 128])]
        accb = [ps("acc0", [128, 512]), ps("acc1", [128, 512])]
        acc = [a[:].rearrange("p (h d) -> p h d", d=128) for a in accb]
        psOT = ps("psOT", [128, 8, 128], BF16)
        psY = [ps("psY0", [128, 512]), ps("psY1", [128, 512])]
        o3 = o[:].rearrange("p (h d) -> p h d", d=64)

        def block(p, tok0, tile, wl, nl):
            def T(off):
                return (lambda i: (tok0(i) if callable(tok0) else tok0) + off)

            def TL(i):
                return tile(i) if callable(tile) else tile
            dma(p, nc, "sp", "qa", qa[:], lambda i: FM[0:4, :, bass.ds(T(0)(i), 128)].rearrange("f p t -> p f t"), w=["qa"])
            w0, w1 = wl[0], wl[-1]
            nw = w1 - w0 + 1
            dma(p, nc, "sp", "ka", ka[:, w0 + 1:w1 + 2, :].rearrange("p b t -> p (b t)"),
                lambda i: FM[4, :, bass.ds(T(w0 * 128)(i), nw * 128)], w=["ka"])
            dma(p, nc, "pool", "va", va[:, w0 + 1:w1 + 2, :, :].rearrange("p b h d -> p b (h d)"),
                lambda i: VE[bass.ds(T(w0 * 128)(i), nw * 128), 0:144].rearrange("(b p) c -> p b c", p=128), w=["va"])
            dma(p, nc, "sp", "qb", qb[:], lambda i: FM[5:9, :, bass.ds(T(0)(i), 128)].rearrange("f (two d) t -> d (f two) t", two=2), w=["qb"])
            n0 = nl[0][0]
            nn = len(nl)
            dma(p, nc, "act", "kbt", kbt[:, :, 0:nn, :].rearrange("p f b t -> p f (b t)"),
                lambda i: FM[9:13, :, bass.ds(T(n0 * 128)(i), nn * 128)].rearrange("f (two d) t -> d (f two) t", two=2), w=["kbt"])
            dma(p, nc, "pool", "vb", vb[:, 0:nn, :, :].rearrange("p b h d -> p b (h d)"),
                lambda i: VE[bass.ds(T(n0 * 128)(i), nn * 128), 144:720].rearrange("(b p) c -> p b c", p=128), w=["vb"])
            dma(p, nc, "sp", "xt", xt[:], lambda i: X[bass.ds(T(0)(i), 128), :], w=["xt"])
            dma(p, nc, "act", "g1b", g1b[:], lambda i: MODT[bass.ds(TL(i), 1), 2048:3072].partition_broadcast(128), w=["g1b"])
            lvl = b.cfg.get("a2lvl", 9)
            if lvl < 2:
                return
            u = 0
            for gk in range(2):
                a = acc[gk % 2]
                an = f"acc{gk % 2}"
                Pu = PT[gk % 2]
                pn = f"PT{gk % 2}_"
                for di, dl in enumerate(wl):
                    k = u % 2
                    u += 1
                    mm(p, nc, pS[k][:], ka[64 * gk:64 * gk + 64, dl + 1, :], qa[64 * gk:64 * gk + 64, :, :], True, True,
                       r=["ka", "qa"], w=[f"pS{k}"])
                    act(p, nc, Pu[:, di, :, :], pS[k][:], AF.Exp, scale=0.125, r=[f"pS{k}"], w=[pn + str(di)])
                    if dl != 0:
                        tt(p, nc, "dve", Pu[:, di, :, :], Pu[:, di, :, :],
                           wm[:, 0 if dl == -1 else 1, :].unsqueeze(1).to_broadcast([128, 4, 128]),
                           ALU.mult, r=[pn + str(di)], w=[pn + str(di)])
                for h in range(4):
                    for di, dl in enumerate(wl):
                        mm(p, nc, a[:, h, 0:66], Pu[:, di, h, :], va[:, dl + 1, gk, 0:66], di == 0, di == len(wl) - 1,
                           r=[pn + str(di), "va"], w=[an])
                tt(p, nc, "dve", den[:], a[:, :, 64:65], skx[:, 4 * gk:4 * gk + 4].unsqueeze(2), ALU.add, r=[an], w=["den"])
                p.op("dve", lambda i: nc.vector.reciprocal(den[:], den[:]), r=["den"], w=["den"])
                tt(p, nc, "dve", o3[:, 4 * gk:4 * gk + 4, :], a[:, :, 0:64], den[:].to_broadcast([128, 4, 64]), ALU.mult,
                   r=[an, "den"], w=["o"])
            if lvl < 3:
                return
            for hg in range(2):
                a = acc[hg % 2]
                an = f"acc{hg % 2}"
                Pu = PT[hg % 2]
                pn = f"PT{hg % 2}_"
                for di, (dl, tb) in enumerate(nl):
                    k = u % 2
                    u += 1
                    for uu in range(4):
                        h = 4 * hg + uu
                        mm(p, nc, pS[k][:, uu, :], kbt[:, h, di, :], qb[:, h, :],
                           True, True, r=["kbt", "qb"], w=[f"pS{k}"])
                    act(p, nc, Pu[:, di, :, :], pS[k][:], AF.Exp, scale=0.125, r=[f"pS{k}"], w=[pn + str(di)])
                    tt(p, nc, "dve" if di % 2 == 0 else "pool", Pu[:, di, :, :], Pu[:, di, :, :], EB[:, tb, 4 * hg:4 * hg + 4, :], ALU.mult,
                       r=[pn + str(di)], w=[pn + str(di)])
                for uu in range(4):
                    for di, (dl, tb) in enumerate(nl):
                        mm(p, nc, a[:, uu, 0:66], Pu[:, di, uu, :], vb[:, di, 4 * hg + uu, 0:66], di == 0, di == len(nl) - 1,
                           r=[pn + str(di), "vb"], w=[an])
                cp(p, nc, "dve", den[:], a[:, :, 64:65], r=[an], w=["den"])
                p.op("dve", lambda i: nc.vector.reciprocal(den[:], den[:]), r=["den"], w=["den"])
                tt(p, nc, "dve", o3[:, 8 + 4 * hg:8 + 4 * hg + 4, :], a[:, :, 0:64], den[:].to_broadcast([128, 4, 64]), ALU.mult,
                   r=[an, "den"], w=["o"])
            if b.cfg.get("debug"):
                dma(p, nc, "pool", "odbg", lambda i: g["ODBG"][bass.ds(T(0)(i), 128), :], o[:], r=["o"])
            if lvl < 4:
                return
            for kc in range(8):
                tr(p, nc, psOT[:, kc, :], o[:, kc * 128:(kc + 1) * 128], identb[:], r=["o"], w=["psOT"])
            cp(p, nc, "act", oT[:], psOT[:], r=["psOT"], w=["oT"])
            for n in range(2):
                for kc in range(8):
                    mm(p, nc, psY[n][:], oT[:, kc, :], wout[:, kc, n * 512:(n + 1) * 512], kc == 0, kc == 7, r=["oT"], w=[f"psY{n}"])
            if lvl < 5:
                return
            ts(p, nc, "pool", g1b[:], g1b[:], 1.0, None, ALU.add, r=["g1b"], w=["g1b"])
            for n in range(2):
                tt(p, nc, "dve", t1[:, n * 512:(n + 1) * 512], psY[n][:], g1b[:, n * 512:(n + 1) * 512], ALU.mult,
                   r=[f"psY{n}", "g1b"], w=["t1"])
            p.op("dve", lambda i: nc.vector.scalar_tensor_tensor(z[:], xt[:], alpha, t1[:], ALU.mult, ALU.add), r=["xt", "t1"], w=["z"])
            layer_norm_tail(b, p, z, "z", lng, lnb, xo, "xo", dict(sq=sq, st=stt), "ln")
            dma(p, nc, "sp", "xo", lambda i: X[bass.ds(T(0)(i), 128), :], xo[:], r=["xo"])

        FULL = lambda dls: [(d_, d_ + 3) for d_ in dls]
        for s, T_s in enumerate(seqs):
            nb = T_s // 128
            tok_s = seq_tile0[s] * 128
            p = Phase(kb, f"A2e_{l}_{s}")
            for j in sorted(set([0, 1, nb - 2, nb - 1]))[:b.cfg.get("a2nblk", 4)]:
                wl = [d_ for d_ in (-1, 0, 1) if 0 <= j + d_ < nb]
                if j == 0:
                    nl = FULL([0, 1, 2, 3])
                elif j == 1:
                    nl = FULL([-1, 0, 1, 2])
                elif j == nb - 2:
                    nl = FULL([-2, -1, 0, 1])
                else:
                    nl = FULL([-3, -2, -1, 0])
                block(p, tok_s + j * 128, seq_tile0[s] + j, wl, nl)
            p.run()
            if b.cfg.get("stop") == "A2e":
                return
            if nb > 4:
                p = Phase(kb, f"A2i_{l}_{s}", n_iter=nb - 4)
                block(p, lambda i, tok_s=tok_s: (i + 2) * 128 + tok_s, lambda i, s=s: i + (2 + seq_tile0[s]),
                      [-1, 0, 1], [(-2, 7), (-1, 2), (0, 3), (1, 4), (2, 8)])
                p.run()


def expert_layout(w, kdim):
    L, E, K, n = w.shape
    return np.ascontiguousarray(w.reshape(L, E, kdim, 128, n).transpose(0, 1, 3, 2, 4)).reshape(L * E * 128, kdim * n)


def prepare_shared(inp, cfg):
    f = lambda k: np.ascontiguousarray(np.asarray(inp[k], dtype=np.float32))
    sh = {}
    for k in ["ada_w", "ada_b", "ln_g", "ln_b", "attn_sink", "attn_w_out", "rec_w_in", "rec_lb", "rec_gnorm", "rec_w_out"]:
        sh[k] = f(k)
    wfm, wv = attn_w_layout(f("attn_w_in"))
    sh["attn_wfm"] = wfm
    sh["attn_wv"] = wv
    sh["na_rb"] = na_bias_tables(f("nat_rel_bias"))
    L = sh["ada_w"].shape[0]
    sh["router_w"] = np.ascontiguousarray(np.concatenate([f("router_w_group"), f("router_w_expert")], axis=2))
    sh["router_b"] = np.ascontiguousarray(np.concatenate([f("router_b_group"), f("router_b_expert").reshape(L, 32)], axis=1))
    sh["e_w"] = np.concatenate([expert_layout(f("expert_w_gate"), 8), expert_layout(f("expert_w_up"), 8),
                                expert_layout(f("expert_w_down"), 4)], axis=1)
    sh.update(host_constants(cfg))
    return sh


def run_cores(inp, cfg, n_cores, pps):
    xp = np.asarray(inp["x_prompt"], dtype=np.float32)
    xs = np.asarray(inp["x_sample"], dtype=np.float32)
    cp_ = np.asarray(inp["c_prompt"], dtype=np.float32)
    cs_ = np.asarray(inp["c_sample"], dtype=np.float32)
    sh = prepare_shared(inp, cfg)
    nc = build_program(cfg)
    in_maps = []
    for c in range(n_cores):
        m = dict(sh)
        xc = np.concatenate([xp[pps * c + k] for k in range(pps)] + [xs[c]], axis=0)
        cc = np.stack([cp_[pps * c + k] for k in range(pps)] + [cs_[c]], axis=0)
        m["x"] = np.ascontiguousarray(xc)
        m["cT"] = np.ascontiguousarray(cc.reshape(3, 8, 128).transpose(2, 0, 1))
        in_maps.append(m)
    res = run_bass_kernel_spmd(nc, in_maps, core_ids=list(range(n_cores)))
    if cfg.get("debug"):
        cfg["_dbg"] = res.results
    TP = cfg["seqs"][0]
    yp = np.zeros_like(xp)
    ys = np.zeros_like(xs)
    for c in range(n_cores):
        y = np.asarray(res.results[c]["y"], dtype=np.float32)
        for k in range(pps):
            yp[pps * c + k] = y[k * TP:(k + 1) * TP]
        ys[c] = y[pps * TP:]
    return yp, ys


def kernel(**inputs):
    cfg = dict(seqs=[2048, 2048, 8192], depth=4)
    yp, ys = run_cores(inputs, cfg, 8, 2)
    return (yp, ys)


def build_moe_layer(b, l, g):
    nc, kb = b.nc, b.kb
    seqs = b.cfg["seqs"]
    NT = sum(seqs)
    NTILES = NT // 128
    NBLK = 2 * NTILES + NEXP
    X, MODT, cn = g["X"], g["MODT"], g["cn"]
    identf = g["identf"]
    alpha = g["alpha"]
    BIG = 1.0e9
    if "HF" not in b.dram:
        b.dscr("HF", [NT, D])
        b.dscr("RTD", [128, NTILES, 8])
        b.dscr("DESTD", [128, NTILES, 2], I32)
        b.dscr("IDXD", [128, NBLK], I32)
        b.dscr("XBUF", [NBLK * 128, D])
        b.dscr("YBUF", [NBLK * 128, D])
    HF, RTD, DESTD, IDXD, XBUF, YBUF = [b.dram[k] for k in ["HF", "RTD", "DESTD", "IDXD", "XBUF", "YBUF"]]

    with ExitStack() as st:
        def sb(name, shape, dt=F32):
            return st.enter_context(nc.sbuf_tensor(un(b, name), list(shape), dt))

        def ps(name, shape, dt=F32):
            return st.enter_context(nc.psum_tensor(un(b, name), list(shape), dt))

        rw = sb("rw", [128, 8, 36])
        rbb = sb("rbb", [128, 36])
        io32 = sb("io32", [128, 32])
        trib = sb("trib", [128, 128], BF16)
        oneb = sb("oneb", [128, 128], BF16)
        carry = sb("carry", [128, 32])
        zt = sb("zt", [128, 1024])
        rt = sb("rt", [128, 8])
        p = Phase(kb, f"moepre{l}")
        p.op("pool", lambda i: nc.gpsimd.memset(rt[:], 0.0), w=["rt"])
        dma(p, nc, "sp", "m0", rw[:], g["ro_w"][l].rearrange("(kc p) n -> p kc n", p=128), w=["rw"])
        dma(p, nc, "sp", "m1", rbb[:], g["ro_b"][l:l + 1, :].partition_broadcast(128), w=["rbb"])
        dma(p, nc, "sp", "m2", io32[:], cn["iota32"][:, :], w=["io32"])
        dma(p, nc, "sp", "m3", trib[:], cn["tri_su"][:, :], w=["trib"])
        dma(p, nc, "sp", "m4", oneb[:], cn["ones_b"][:, :], w=["oneb"])
        p.op("pool", lambda i: nc.gpsimd.memset(carry[:], 0.0), w=["carry"])
        p.op("pool", lambda i: nc.gpsimd.memset(zt[:], 0.0), w=["zt"])
        p.run()
        p = Phase(kb, f"moez{l}", n_iter=NBLK)
        dma(p, nc, "sp", "zf", lambda i: XBUF[bass.ds(i * 128, 128), :], zt[:])
        p.run()

        from types import SimpleNamespace
        sets = []
        for k in range(2):
            T = SimpleNamespace(xt=sb(f"xt{k}", [128, 1024]), scb=sb(f"scb{k}", [128, 1024]), shb=sb(f"shb{k}", [128, 1024]),
                                hf=sb(f"hf{k}", [128, 1024]), hfT=sb(f"hfT{k}", [128, 8, 128]), lg=sb(f"lg{k}", [128, 36]),
                                sm=sb(f"sm{k}", [128, 16]), ge=sb(f"ge{k}", [128, 4]), gmask=sb(f"gmask{k}", [128, 4]),
                                lem=sb(f"lem{k}", [128, 4, 8]), lem2=sb(f"lem2{k}", [128, 32]), oh1=sb(f"oh1{k}", [128, 32]),
                                oh2=sb(f"oh2{k}", [128, 32]), ohb=sb(f"ohb{k}", [128, 32], BF16), tmp32=sb(f"tmp32{k}", [128, 32]),
                                tot=sb(f"tot{k}", [128, 32]), rt=sb(f"rtt{k}", [128, 8]))
            T.lemf = T.lem[:].rearrange("p a b -> p (a b)")
            sets.append(T)
        psA = [ps("psA0", [128, 4, 128]), ps("psA1", [128, 4, 128])]
        pl = ps("pl", [128, 512])
        pr = ps("pr", [128, 512])
        pc = ps("pc", [128, 512])
        p = Phase(kb, f"moeRz{l}")
        for k in range(2):
            p.op("pool", lambda i, k=k: nc.gpsimd.memset(sets[k].rt[:], 0.0), w=[f"rtz{k}"])
        p.run()

        p = Phase(kb, f"moeR{l}", n_iter=NTILES // 2)
        for k in range(2):
            p.ns = f"#{k}"
            T = sets[k]
            dma(p, nc, "sp", "xt", T.xt[:], lambda i, k=k, T=T: X[bass.ds(i * 256 + k * 128, 128), :], w=["xt"])
            dma(p, nc, "act", "scb", T.scb[:], lambda i, k=k, T=T: MODT[bass.ds(i * 2 + k, 1), 4096:5120].partition_broadcast(128), w=["scb"])
            dma(p, nc, "act", "shb", T.shb[:], lambda i, k=k, T=T: MODT[bass.ds(i * 2 + k, 1), 3072:4096].partition_broadcast(128), w=["shb"])
            ts(p, nc, "pool", T.scb[:], T.scb[:], 1.0, None, ALU.add, r=["scb"], w=["scb"])
            tt(p, nc, "dve", T.hf[:], T.xt[:], T.scb[:], ALU.mult, r=["xt", "scb"], w=["hf"])
            tt(p, nc, "pool", T.hf[:], T.hf[:], T.shb[:], ALU.add, r=["hf", "shb"], w=["hf"])
            dma(p, nc, "sp", "hfo", lambda i, k=k, T=T: HF[bass.ds(i * 256 + k * 128, 128), :], T.hf[:], r=["hf"])
            for kc in range(8):
                tr(p, nc, psA[kc // 4][:, kc % 4, :], T.hf[:, kc * 128:(kc + 1) * 128], identf[:], r=["hf"], w=[f"!psA{kc // 4}"])
            cp(p, nc, "act", T.hfT[:, 0:4, :], psA[0][:], r=["!psA0"], w=["hfT0"])
            cp(p, nc, "dve", T.hfT[:, 4:8, :], psA[1][:], r=["!psA1"], w=["hfT1"])
            for kc in range(8):
                mm(p, nc, pl[:, 0:36], T.hfT[:, kc, :], rw[:, kc, :], kc == 0, kc == 7, r=["hfT0", "hfT1"], w=["!pl"])
            tt(p, nc, "dve", T.lg[:], pl[:, 0:36], rbb[:], ALU.add, r=["!pl"], w=["lg"])
            p.op("dve", lambda i, k=k, T=T: nc.vector.tensor_reduce(T.sm[:, 0:1], T.lg[:, 0:4], AX.X, ALU.max), r=["lg"], w=["sm0"])
            ts(p, nc, "dve", T.sm[:, 1:2], T.sm[:, 0:1], -1.0, None, ALU.mult, r=["sm0"], w=["sm1"])
            act(p, nc, T.ge[:], T.lg[:, 0:4], AF.Exp, bias=T.sm[:, 1:2], r=["lg", "sm1"], w=["ge"])
            p.op("dve", lambda i, k=k, T=T: nc.vector.tensor_reduce(T.sm[:, 2:3], T.ge[:], AX.X, ALU.add), r=["ge"], w=["sm2"])
            p.op("dve", lambda i, k=k, T=T: nc.vector.reciprocal(T.sm[:, 3:4], T.sm[:, 2:3]), r=["sm2"], w=["sm3"])
            tt(p, nc, "dve", T.gmask[:], T.lg[:, 0:4], T.sm[:, 0:1].to_broadcast([128, 4]), ALU.is_ge, r=["lg", "sm0"], w=["gmask"])
            ts(p, nc, "dve", T.gmask[:], T.gmask[:], -1.0, BIG, ALU.add, ALU.mult, r=["gmask"], w=["gmask"])
            tt(p, nc, "dve", T.lem[:], T.lg[:, 4:36].rearrange("p (a b) -> p a b", b=8), T.gmask[:].unsqueeze(2).to_broadcast([128, 4, 8]),
               ALU.add, r=["lg", "gmask"], w=["lem"])
            p.op("dve", lambda i, k=k, T=T: nc.vector.tensor_reduce(T.sm[:, 4:5], T.lemf, AX.X, ALU.max), r=["lem"], w=["sm4"])
            tt(p, nc, "dve", T.oh1[:], T.lemf, T.sm[:, 4:5].to_broadcast([128, 32]), ALU.is_ge, r=["lem", "sm4"], w=["oh1"])
            p.op("dve", lambda i, k=k, T=T: nc.vector.scalar_tensor_tensor(T.lem2[:], T.oh1[:], -BIG, T.lemf, ALU.mult, ALU.add), r=["oh1", "lem"], w=["lem2"])
            p.op("dve", lambda i, k=k, T=T: nc.vector.tensor_reduce(T.sm[:, 5:6], T.lem2[:], AX.X, ALU.max), r=["lem2"], w=["sm5"])
            tt(p, nc, "dve", T.oh2[:], T.lem2[:], T.sm[:, 5:6].to_broadcast([128, 32]), ALU.is_ge, r=["lem2", "sm5"], w=["oh2"])
            tt(p, nc, "dve", T.sm[:, 6:7], T.sm[:, 5:6], T.sm[:, 4:5], ALU.subtract, r=["sm4", "sm5"], w=["sm6"])
            act(p, nc, T.sm[:, 7:8], T.sm[:, 6:7], AF.Exp, r=["sm6"], w=["sm7"])
            ts(p, nc, "dve", T.sm[:, 8:9], T.sm[:, 7:8], 1.0, None, ALU.add, r=["sm7"], w=["sm8"])
            p.op("dve", lambda i, k=k, T=T: nc.vector.reciprocal(T.sm[:, 8:9], T.sm[:, 8:9]), r=["sm8"], w=["sm8"])
            tt(p, nc, "dve", T.rt[:, 2:3], T.sm[:, 8:9], T.sm[:, 3:4], ALU.mult, r=["sm8", "sm3"], w=["rt2"])
            tt(p, nc, "dve", T.rt[:, 3:4], T.rt[:, 2:3], T.sm[:, 7:8], ALU.mult, r=["rt2", "sm7"], w=["rt3"])
            tt(p, nc, "dve", T.tmp32[:], T.oh1[:], io32[:], ALU.mult, r=["oh1"], w=["tmp32"])
            p.op("dve", lambda i, k=k, T=T: nc.vector.tensor_reduce(T.rt[:, 0:1], T.tmp32[:], AX.X, ALU.add), r=["tmp32"], w=["rt0"])
            tt(p, nc, "dve", T.tmp32[:], T.oh2[:], io32[:], ALU.mult, r=["oh2", "rt0"], w=["tmp32"])
            p.op("dve", lambda i, k=k, T=T: nc.vector.tensor_reduce(T.rt[:, 1:2], T.tmp32[:], AX.X, ALU.add), r=["tmp32"], w=["rt1"])
            tt(p, nc, "pool", T.ohb[:], T.oh1[:], T.oh2[:], ALU.add, r=["oh1", "oh2"], w=["ohb"])
            mm(p, nc, pr[:, 0:32], trib[:], T.ohb[:], True, True, r=["ohb"], w=["!pr"])
            mm(p, nc, pc[:, 0:32], oneb[:], T.ohb[:], True, True, r=["ohb"], w=["!pc"])
            tt(p, nc, "dve", T.tot[:], pr[:, 0:32], carry[:], ALU.add, r=["!pr", "!carry"], w=["tot"])
            tt(p, nc, "dve", T.tmp32[:], T.oh1[:], T.tot[:], ALU.mult, r=["oh1", "tot", "rt1"], w=["tmp32"])
            p.op("dve", lambda i, k=k, T=T: nc.vector.tensor_reduce(T.rt[:, 4:5], T.tmp32[:], AX.X, ALU.add), r=["tmp32"], w=["rt4"])
            tt(p, nc, "dve", T.tmp32[:], T.oh2[:], T.tot[:], ALU.mult, r=["oh2", "tot", "rt4"], w=["tmp32"])
            p.op("dve", lambda i, k=k, T=T: nc.vector.tensor_reduce(T.rt[:, 5:6], T.tmp32[:], AX.X, ALU.add), r=["tmp32"], w=["rt5"])
            tt(p, nc, "dve", carry[:], carry[:], pc[:, 0:32], ALU.add, r=["!carry", "!pc", "tot"], w=["!carry"])
            dma(p, nc, "act", "rto", lambda i, k=k, T=T: RTD[:, bass.ds(i * 2 + k, 1), :].rearrange("p o c -> p (o c)"), T.rt[:],
                r=["rt0", "rt1", "rt2", "rt3", "rt4", "rt5"])
        p.ns = ""
        p.run()

        rta = sb("rta", [128, NTILES, 8])
        cnt = sb("cnt", [128, 32])
        cnti = sb("cnti", [128, 32], I32)
        pend = sb("pend", [128, 32])
        pst = sb("pst", [128, 32])
        onesf = sb("onesf", [128, 32])
        dacc = sb("dacc", [128, NTILES, 2])
        dtmp = sb("dtmp", [128, NTILES, 2])
        dint = sb("dint", [128, NTILES, 2], I32)
        ioblk = sb("ioblk", [128, NBLK])
        bacc = sb("bacc", [128, NBLK])
        btmp = sb("btmp", [128, NBLK])
        iop = sb("iop", [128, 1])
        bint = sb("bint", [128, NBLK], I32)
        p = Phase(kb, f"moeD{l}")
        dma(p, nc, "sp", "d0", rta[:], RTD[:, :, :], w=["rta"])
        dma(p, nc, "sp", "d1", ioblk[:], cn["iota_blk"][:, 0:NBLK], w=["ioblk"])
        dma(p, nc, "sp", "d2", iop[:], cn["iota_p"][:, :], w=["iop"])
        p.op("pool", lambda i: nc.gpsimd.memset(onesf[:], 1.0), w=["onesf"])
        ts(p, nc, "dve", cnt[:], carry[:], 1.0 / 128.0, 0.49609375, ALU.mult, ALU.add, w=["cnt"])
        cp(p, nc, "dve", cnti[:], cnt[:], r=["cnt"], w=["cnti"])
        cp(p, nc, "dve", cnt[:], cnti[:], r=["cnti"], w=["cnt"])
        ts(p, nc, "dve", cnt[:], cnt[:], 128.0, None, ALU.mult, r=["cnt"], w=["cnt"])
        p.op("dve", lambda i: nc.vector.tensor_tensor_scan(pend[:], onesf[:], cnt[:], 0.0, ALU.mult, ALU.add), r=["onesf", "cnt"], w=["pend"])
        tt(p, nc, "dve", pst[:], pend[:], cnt[:], ALU.subtract, r=["pend", "cnt"], w=["pst"])
        cp(p, nc, "dve", dacc[:], rta[:, :, 4:6], r=["rta"], w=["dacc"])
        for e in range(NEXP):
            ts(p, nc, "dve", dtmp[:], rta[:, :, 0:2], float(e), None, ALU.is_equal, r=["rta", "dacc"], w=["dtmp"])
            p.op("dve", lambda i, e=e: nc.vector.scalar_tensor_tensor(dacc[:], dtmp[:], pst[:, e:e + 1], dacc[:], ALU.mult, ALU.add),
                 r=["dtmp", "pst", "dacc"], w=["dacc"])
        cp(p, nc, "dve", dint[:], dacc[:], r=["dacc"], w=["dint"])
        dma(p, nc, "sp", "d3", DESTD[:, :, :], dint[:], r=["dint"])
        p.op("pool", lambda i: nc.gpsimd.memset(bacc[:], 0.0), w=["bacc"])
        for e in range(NEXP):
            ts(p, nc, "dve", btmp[:], ioblk[:], pend[:, e:e + 1], None, ALU.is_ge, r=["ioblk", "pend", "bacc"], w=["btmp"])
            tt(p, nc, "dve", bacc[:], bacc[:], btmp[:], ALU.add, r=["bacc", "btmp"], w=["bacc"])
        ts(p, nc, "dve", bacc[:], bacc[:], float(NEXP - 1), None, ALU.min, r=["bacc"], w=["bacc"])
        OOBV = float(b.cfg["depth"] * NEXP * 128)
        p.op("pool", lambda i: nc.gpsimd.memset(btmp[:, 0:1], 1.0), r=["btmp"], w=["btmp"])
        p.op("pool", lambda i: nc.gpsimd.memset(btmp[:, 1:2], 1.0), r=["btmp"], w=["btmp"])
        tt(p, nc, "dve", btmp[:, 2:NBLK], bacc[:, 2:NBLK], bacc[:, 0:NBLK - 2], ALU.not_equal, r=["bacc", "btmp"], w=["btmp"])
        ts(p, nc, "dve", bacc[:], bacc[:], 128.0, float(l * NEXP * 128), ALU.mult, ALU.add, r=["bacc", "btmp"], w=["bacc"])
        tt(p, nc, "dve", bacc[:], bacc[:], iop[:].to_broadcast([128, NBLK]), ALU.add, r=["bacc", "iop"], w=["bacc"])
        if b.cfg.get("oobskip", True):
            ts(p, nc, "dve", bacc[:], bacc[:], -OOBV, None, ALU.add, r=["bacc"], w=["bacc"])
            tt(p, nc, "dve", bacc[:], bacc[:], btmp[:], ALU.mult, r=["bacc", "btmp"], w=["bacc"])
            ts(p, nc, "dve", bacc[:], bacc[:], OOBV, None, ALU.add, r=["bacc"], w=["bacc"])
        cp(p, nc, "dve", bint[:], bacc[:], r=["bacc"], w=["bint"])
        dma(p, nc, "sp", "d4", IDXD[:, :], bint[:], r=["bint"])
        p.run()
    if b.cfg.get("stop") == "moeD":
        return

    with ExitStack() as st:
        def sb(name, shape, dt=F32):
            return st.enter_context(nc.sbuf_tensor(un(b, name), list(shape), dt))
        hf = sb("hf", [128, 1024])
        di = sb("di", [128, 2], I32)
        p = Phase(kb, f"moeS{l}", n_iter=NTILES)
        dma(p, nc, "sp", "hf", hf[:], lambda i: HF[bass.ds(i * 128, 128), :], w=["hf"])
        dma(p, nc, "act", "di", di[:], lambda i: DESTD[:, bass.ds(i, 1), :].rearrange("p o c -> p (o c)"), w=["di"])
        for k in range(2):
            p.dma("pool", f"sc{k}", lambda i, k=k: nc.gpsimd.indirect_dma_start(
                out=XBUF[:, :], out_offset=bass.IndirectOffsetOnAxis(ap=di[:, k:k + 1], axis=0), in_=hf[:], in_offset=None),
                r=["hf", "di"])
        p.run()
    if b.cfg.get("stop") == "moeS":
        return

    NROWS = b.cfg["depth"] * NEXP * 128
    with ExitStack() as st:
        def sb(name, shape, dt=F32):
            return st.enter_context(nc.sbuf_tensor(un(b, name), list(shape), dt))

        def ps(name, shape, dt=F32):
            return st.enter_context(nc.psum_tensor(un(b, name), list(shape), dt))
        wfs = [sb("wf0", [128, 12288]), sb("wf1", [128, 12288])]
        psA = [ps("psA0", [128, 4, 128]), ps("psA1", [128, 4, 128])]
        psG = ps("psG", [128, 4, 128])
        psU = ps("psU", [128, 4, 128])
        psY = [ps("psY0", [128, 512]), ps("psY1", [128, 512])]
        sets = []
        for k in range(2):
            sets.append(dict(ix=sb(f"ix{k}", [128, 1], I32), wgb=sb(f"wgb{k}", [128, 8, 512], BF16), wub=sb(f"wub{k}", [128, 8, 512], BF16),
                             wdb=sb(f"wdb{k}", [128, 4, 1024], BF16), xb=sb(f"xb{k}", [128, 1024]), xT=sb(f"xT{k}", [128, 8, 128], BF16),
                             sg=sb(f"sg{k}", [128, 4, 128]), hT=sb(f"hT{k}", [128, 4, 128], BF16), yb=sb(f"yb{k}", [128, 1024])))
        p = Phase(kb, f"moeB{l}", n_iter=NBLK // 2)
        segs = []
        for k in range(2):
            p.ns = f"#{k}"
            T = sets[k]
            segs.append(len(p.ops))
            ix, wgb, wub, wdb, xb, xT, sg, hT, yb = [T[n_] for n_ in ["ix", "wgb", "wub", "wdb", "xb", "xT", "sg", "hT", "yb"]]
            dma(p, nc, "sp", "ix", ix[:], lambda i, k=k: IDXD[:, bass.ds(i * 2 + k, 1)], w=["ix"], slow=True)
            dma(p, nc, "act", "xb", xb[:], lambda i, k=k: XBUF[bass.ds(i * 256 + k * 128, 128), :], w=["xb"])
            wf = wfs[k]
            p.dma("pool", "wf", lambda i, ix=ix, wf=wf: nc.gpsimd.indirect_dma_start(
                out=wf[:], out_offset=None, in_=g["e_w"][:, :], in_offset=bass.IndirectOffsetOnAxis(ap=ix[:, 0:1], axis=0),
                bounds_check=NROWS - 1, oob_is_err=False),
                r=["ix"], w=["wf"])
            segs.append(len(p.ops))
            cp(p, nc, "dve", wgb[:].rearrange("p k n -> p (k n)"), wf[:, 0:4096], r=["wf"], w=["wgb"])
            cp(p, nc, "act", wub[:].rearrange("p k n -> p (k n)"), wf[:, 4096:8192], r=["wf"], w=["wub"])
            cp(p, nc, "dve", wdb[:, 0:2, :].rearrange("p k n -> p (k n)"), wf[:, 8192:10240], r=["wf"], w=["wdb0"])
            cp(p, nc, "act", wdb[:, 2:4, :].rearrange("p k n -> p (k n)"), wf[:, 10240:12288], r=["wf"], w=["wdb1"])
            segs.append(len(p.ops))
            for kc in range(8):
                tr(p, nc, psA[kc // 4][:, kc % 4, :], xb[:, kc * 128:(kc + 1) * 128], identf[:], r=["xb"], w=[f"!psA{kc // 4}"])
            cp(p, nc, "act", xT[:, 0:4, :], psA[0][:], r=["!psA0"], w=["xT0"])
            cp(p, nc, "dve", xT[:, 4:8, :], psA[1][:], r=["!psA1"], w=["xT1"])
            for c in range(4):
                for kc in range(8):
                    mm(p, nc, psG[:, c, :], wgb[:, kc, c * 128:(c + 1) * 128], xT[:, kc, :], kc == 0, kc == 7, r=["wgb", "xT0", "xT1"], w=["!psG"])
            for c in range(4):
                for kc in range(8):
                    mm(p, nc, psU[:, c, :], wub[:, kc, c * 128:(c + 1) * 128], xT[:, kc, :], kc == 0, kc == 7, r=["wub", "xT0", "xT1"], w=["!psU"])
            act(p, nc, sg[:], psG[:], AF.Silu, r=["!psG"], w=["sg"])
            tt(p, nc, "dve", hT[:], sg[:], psU[:], ALU.mult, r=["sg", "!psU"], w=["hT"])
            for n in range(2):
                for c in range(4):
                    mm(p, nc, psY[n][:], hT[:, c, :], wdb[:, c, n * 512:(n + 1) * 512], c == 0, c == 3, r=["hT", "wdb0", "wdb1"], w=[f"!psY{n}"])
            cp(p, nc, "act", yb[:, 0:512], psY[0][:], r=["!psY0"], w=["yb0"])
            cp(p, nc, "dve", yb[:, 512:1024], psY[1][:], r=["!psY1"], w=["yb1"])
            dma(p, nc, "sp", "yb", lambda i, k=k: YBUF[bass.ds(i * 256 + k * 128, 128), :], yb[:], r=["yb0", "yb1"])
        p.ns = ""
        a0, a1, a2, b0, b1, b2 = segs
        o_ = p.ops
        p.ops = o_[a0:a1] + o_[b0:b1] + o_[a1:a2] + o_[b1:b2] + o_[a2:b0] + o_[b2:]
        p.run()
    if b.cfg.get("stop") == "moeB":
        return

    with ExitStack() as st:
        def sb(name, shape, dt=F32):
            return st.enter_context(nc.sbuf_tensor(un(b, name), list(shape), dt))
        lng = sb("lng", [128, 1024])
        lnb = sb("lnb", [128, 1024])
        p = Phase(kb, f"moeCpre{l}")
        dma(p, nc, "sp", "c0", lng[:], g["ln_g"][l, 1:2, :].partition_broadcast(128), w=["lng"])
        dma(p, nc, "sp", "c1", lnb[:], g["ln_b"][l, 1:2, :].partition_broadcast(128), w=["lnb"])
        p.run()
        sets = []
        for k in range(2):
            sets.append(dict(xt=sb(f"xt{k}", [128, 1024]), g2b=sb(f"g2b{k}", [128, 1024]), rt=sb(f"rt{k}", [128, 8]),
                             di=sb(f"di{k}", [128, 2], I32), y0=sb(f"y0{k}", [128, 1024]), y1=sb(f"y1{k}", [128, 1024]),
                             z=sb(f"z{k}", [128, 1024]), sq=sb(f"sq{k}", [128, 1024]), xo=sb(f"xo{k}", [128, 1024]),
                             stt=sb(f"stt{k}", [128, 8])))
        p = Phase(kb, f"moeC{l}", n_iter=NTILES // 2)
        for k in range(2):
            p.ns = f"#{k}"
            T = sets[k]
            xt, g2b, rt, di, y0, y1, z, sq, xo, stt = [T[n_] for n_ in ["xt", "g2b", "rt", "di", "y0", "y1", "z", "sq", "xo", "stt"]]
            dma(p, nc, "sp", "xt", xt[:], lambda i, k=k: X[bass.ds(i * 256 + k * 128, 128), :], w=["xt"])
            dma(p, nc, "act", "g2b", g2b[:], lambda i, k=k: MODT[bass.ds(i * 2 + k, 1), 5120:6144].partition_broadcast(128), w=["g2b"])
            dma(p, nc, "act", "rt", rt[:], lambda i, k=k: RTD[:, bass.ds(i * 2 + k, 1), :].rearrange("p o c -> p (o c)"), w=["rt"])
            dma(p, nc, "sp", "di", di[:], lambda i, k=k: DESTD[:, bass.ds(i * 2 + k, 1), :].rearrange("p o c -> p (o c)"), w=["di"])
            for kk, yk in enumerate([y0, y1]):
                p.dma("pool", f"ga{kk}", lambda i, kk=kk, yk=yk, di=di: nc.gpsimd.indirect_dma_start(
                    out=yk[:], out_offset=None, in_=YBUF[:, :], in_offset=bass.IndirectOffsetOnAxis(ap=di[:, kk:kk + 1], axis=0)),
                    r=["di"], w=[f"y{kk}"])
            ts(p, nc, "dve", y0[:], y0[:], rt[:, 2:3], None, ALU.mult, r=["y0", "rt"], w=["y0"])
            p.op("dve", lambda i, y0=y0, y1=y1, rt=rt: nc.vector.scalar_tensor_tensor(y0[:], y1[:], rt[:, 3:4], y0[:], ALU.mult, ALU.add),
                 r=["y0", "y1", "rt"], w=["y0"])
            ts(p, nc, "pool", g2b[:], g2b[:], 1.0, None, ALU.add, r=["g2b"], w=["g2b"])
            tt(p, nc, "pool", y0[:], y0[:], g2b[:], ALU.mult, r=["y0", "g2b"], w=["y0"])
            p.op("dve", lambda i, z=z, xt=xt, y0=y0: nc.vector.scalar_tensor_tensor(z[:], xt[:], alpha, y0[:], ALU.mult, ALU.add),
                 r=["xt", "y0"], w=["z"])
            layer_norm_tail(b, p, z, "z", lng, lnb, xo, "xo", dict(sq=sq, st=stt), "ln")
            dma(p, nc, "sp", "xo", lambda i, k=k: X[bass.ds(i * 256 + k * 128, 128), :], xo[:], r=["xo"])
            if k == 0:
                n_half = len(p.ops)
        p.ns = ""
        p.interleave(0, n_half, len(p.ops))
        p.run()


def build_hgrn_layer(b, l, lr, g):
    nc, kb = b.nc, b.kb
    seqs = b.cfg["seqs"]
    NT = sum(seqs)
    NTILES = NT // 128
    depth = b.cfg["depth"]
    LR = depth // 2
    X, MODT, cn = g["X"], g["MODT"], g["cn"]
    identb = g["identb"]
    alpha = g["alpha"]
    seq_tile0 = g["seq_tile0"]
    if "QZ" not in b.dram:
        b.dscr("QZ", [24, 128, NT])
        b.dscr("VT", [NT, D], BF16)
        b.dscr("GT", [NT, D])
        b.dscr("OF", [NT, D])
        b.dscr("OB", [NT, D])
    QZ, VT, GT, OF, OB = [b.dram[k] for k in ["QZ", "VT", "GT", "OF", "OB"]]

    with ExitStack() as st:
        def sb(name, shape, dt=F32):
            return st.enter_context(nc.sbuf_tensor(un(b, name), list(shape), dt))

        def ps(name, shape, dt=F32):
            return st.enter_context(nc.psum_tensor(un(b, name), list(shape), dt))

        win = sb("win", [128, 8, 5120], BF16)
        with ExitStack() as st2:
            stg = [st2.enter_context(nc.sbuf_tensor(un(b, "stg0"), [128, 8, 512], F32)),
                   st2.enter_context(nc.sbuf_tensor(un(b, "stg1"), [128, 8, 512], F32))]
            p = Phase(kb, f"recw{l}")
            load_weight_bf16(b, p, win, g["r_win"][lr].rearrange("(kc p) n -> p kc n", p=128), 5120, "win", stg)
            p.run()
        xg = sb("xg", [128, 4, 1024])
        scb = sb("scb", [128, 1024])
        shb = sb("shb", [128, 1024])
        tmp = [sb("tmp0", [128, 1024]), sb("tmp1", [128, 1024])]
        hb = sb("hb", [128, 4, 1024], BF16)
        hT = sb("hT", [128, 8, 512], BF16)
        fq = sb("fq", [128, 8, 512])
        vo = sb("vo", [128, 4, 1024], BF16)
        go = sb("go", [128, 4, 1024])
        psT = [ps("psT0", [128, 8, 128], BF16), ps("psT1", [128, 8, 128], BF16)]
        pf = [ps(f"pf{k}", [128, 512]) for k in range(4)]
        NG = NT // 512
        p = Phase(kb, f"H1_{l}", n_iter=NG)
        dma(p, nc, "sp", "xg", xg[:], lambda i: X[bass.ds(i * 512, 512), :].rearrange("(t p) d -> p t d", p=128), w=["xg"])
        dma(p, nc, "act", "scb", scb[:], lambda i: MODT[bass.ds(i * 4, 1), 1024:2048].partition_broadcast(128), w=["scb"])
        dma(p, nc, "act", "shb", shb[:], lambda i: MODT[bass.ds(i * 4, 1), 0:1024].partition_broadcast(128), w=["shb"])
        ts(p, nc, "dve", scb[:], scb[:], 1.0, None, ALU.add, r=["scb"], w=["scb"])
        for t in range(4):
            tt(p, nc, "dve", tmp[t % 2][:], xg[:, t, :], scb[:], ALU.mult, r=["xg", "scb"], w=[f"tmp{t % 2}"])
            tt(p, nc, "pool", hb[:, t, :], tmp[t % 2][:], shb[:], ALU.add, r=[f"tmp{t % 2}", "shb"], w=[f"hb{t}"])
            for kc in range(8):
                tr(p, nc, psT[t % 2][:, kc, :], hb[:, t, kc * 128:(kc + 1) * 128], identb[:], r=[f"hb{t}"], w=[f"psT{t % 2}"])
            cp(p, nc, "act", hT[:, :, t * 128:(t + 1) * 128], psT[t % 2][:], r=[f"psT{t % 2}"], w=[f"hT{t}"])
        hTall = [f"hT{t}" for t in range(4)]
        pfi = 0
        for grp in range(3):
            for hh in range(8):
                c = grp * 8 + hh
                ia = pfi % 4
                pfi += 1
                for kc in range(8):
                    mm(p, nc, pf[ia][:], win[:, kc, c * 128:(c + 1) * 128], hT[:, kc, :], kc == 0, kc == 7, r=hTall, w=[f"pf{ia}"])
                cp(p, nc, "act" if hh % 2 == 0 else "dve", fq[:, hh, :], pf[ia][:], r=[f"pf{ia}"], w=[f"fq{hh}"])
            dma(p, nc, ["sp", "act", "pool"][grp], f"fq{grp}",
                lambda i, grp=grp: QZ[grp * 8:grp * 8 + 8, :, bass.ds(i * 512, 512)].rearrange("f p t -> p f t"), fq[:],
                r=[f"fq{hh}" for hh in range(8)])
        for t in range(4):
            for n in range(4):
                ia = pfi % 4
                pfi += 1
                col0 = 3072 + n * 512
                for kc in range(8):
                    mm(p, nc, pf[ia][:], hT[:, kc, t * 128:(t + 1) * 128], win[:, kc, col0:col0 + 512], kc == 0, kc == 7,
                       r=[f"hT{t}"], w=[f"pf{ia}"])
                if n < 2:
                    cp(p, nc, "act", vo[:, t, n * 512:(n + 1) * 512], pf[ia][:], r=[f"pf{ia}"], w=[f"vo{t}"])
                else:
                    cp(p, nc, "dve", go[:, t, (n - 2) * 512:(n - 1) * 512], pf[ia][:], r=[f"pf{ia}"], w=[f"go{t}"])
        dma(p, nc, "sp", "vo", lambda i: VT[bass.ds(i * 512, 512), :].rearrange("(t p) e -> p t e", p=128), vo[:],
            r=[f"vo{t}" for t in range(4)])
        dma(p, nc, "pool", "go", lambda i: GT[bass.ds(i * 512, 512), :].rearrange("(t p) e -> p t e", p=128), go[:],
            r=[f"go{t}" for t in range(4)])
        p.run()
    if b.cfg.get("stop") == "H1":
        return

    with ExitStack() as st:
        def sb(name, shape, dt=F32):
            return st.enter_context(nc.sbuf_tensor(un(b, name), list(shape), dt))

        def ps(name, shape, dt=F32):
            return st.enter_context(nc.psum_tensor(un(b, name), list(shape), dt))

        lbT = sb("lbT", [128, 8])
        omlT = sb("omlT", [128, 8])
        lbe = sb("lbe", [128, max(LR, 1), 8])
        lsum = sb("lsum", [128, 8])
        hm = sb("hm", [64, 2, 64], BF16)
        smask = sb("smask", [128, 1024])
        S = sb("S", [128, 8, 128])
        p = Phase(kb, f"recpre{l}")
        for j in range(LR):
            dma(p, nc, "sp", f"lb{j}", lbe[:, j, :], g["r_lb"][j].rearrange("(h k) -> k h", k=128), w=[f"lbe{j}"], slow=True)
            act(p, nc, lbe[:, j, :], lbe[:, j, :], AF.Exp, r=[f"lbe{j}"], w=[f"lbe{j}"])
        cp(p, nc, "dve", lsum[:], lbe[:, 0, :], r=["lbe0"], w=["lsum"])
        for j in range(1, LR):
            tt(p, nc, "dve", lsum[:], lsum[:], lbe[:, j, :], ALU.add, r=["lsum", f"lbe{j}"], w=["lsum"])
        p.op("dve", lambda i: nc.vector.reciprocal(lsum[:], lsum[:]), r=["lsum"], w=["lsum"])
        p.op("pool", lambda i: nc.gpsimd.memset(lbT[:], 0.0), w=["lbT"])
        for j in range(1, lr + 1):
            tt(p, nc, "dve", lbT[:], lbT[:], lbe[:, j, :], ALU.add, r=["lbT", f"lbe{j}"], w=["lbT"])
        tt(p, nc, "dve", lbT[:], lbT[:], lsum[:], ALU.mult, r=["lbT", "lsum"], w=["lbT"])
        ts(p, nc, "dve", omlT[:], lbT[:], -1.0, 1.0, ALU.mult, ALU.add, r=["lbT"], w=["omlT"])
        dma(p, nc, "sp", "hm", hm[:], cn["hmask"][:, :, :], w=["hm"])
        dma(p, nc, "sp", "smask", smask[:], cn["scanmask"][:, :], w=["smask"])
        p.run()

        from types import SimpleNamespace
        sets = []
        for k in range(2):
            T = SimpleNamespace(qt=sb(f"qt{k}", [128, 8, 128]), zt=sb(f"zt{k}", [128, 8, 128]), vt=sb(f"vt{k}", [64, 2, 1024], BF16),
                                u=sb(f"u{k}", [128, 8, 128]), w_=sb(f"w_{k}", [128, 8, 128]), f_=sb(f"f_{k}", [128, 8, 128]),
                                lf=sb(f"lf{k}", [128, 8, 128]), km=sb(f"km{k}", [128, 8, 128]), bb=sb(f"bb{k}", [128, 8, 128]),
                                bm=sb(f"bm{k}", [128, 8, 128]), Ee=sb(f"Ee{k}", [128, 8, 128]), Ei=sb(f"Ei{k}", [128, 8, 128]),
                                qe=sb(f"qe{k}", [128, 8, 128], BF16), ke=sb(f"ke{k}", [128, 8, 128], BF16), sc=sb(f"sc{k}", [128, 8, 2, 4]),
                                Sc=sb(f"Sc{k}", [128, 8, 128], BF16), AT=sb(f"AT{k}", [64, 8, 64], BF16), keT=sb(f"keT{k}", [64, 8, 128], BF16),
                                oc=sb(f"oc{k}", [64, 1024]))
            sets.append(T)
        psA = ps("psA", [64, 8, 64])
        psKT2 = [ps("psKT0", [64, 8, 128], BF16), ps("psKT1", [64, 8, 128], BF16)]
        pso = [ps("pso0", [64, 512]), ps("pso1", [64, 512])]
        psdS = [ps("psdS0", [128, 4, 128]), ps("psdS1", [128, 4, 128])]
        lb_bc = lbT[:].unsqueeze(2).to_broadcast([128, 8, 128])
        oml_bc = omlT[:].unsqueeze(2).to_broadcast([128, 8, 128])
        fl = lambda t_: t_[:].rearrange("p h t -> p (h t)")
        c4 = lambda t_: t_[:].rearrange("p h (c t) -> p h c t", t=64)

        def rec_body(p, tok0, dirn, OUT, T, marks):
            zbase = 8 if dirn == 0 else 16
            dma(p, nc, "sp", "qt", T.qt[:], lambda i: QZ[0:8, :, bass.ds(tok0(i), 128)].rearrange("f p t -> p f t"), w=["qt"])
            dma(p, nc, "act", "zt", T.zt[:], lambda i: QZ[zbase:zbase + 8, :, bass.ds(tok0(i), 128)].rearrange("f p t -> p f t"), w=["zt"])
            dma(p, nc, "pool", "vt", T.vt[:], lambda i: VT[bass.ds(tok0(i), 128), :].rearrange("(c p) e -> p c e", p=64), w=["vt"])
            act(p, nc, T.u[:], T.zt[:], AF.Exp, scale=-1.0, r=["zt"], w=["u"])
            ts(p, nc, "dve", T.w_[:], T.u[:], 1.0, None, ALU.add, r=["u"], w=["w_"])
            p.op("dve", lambda i: nc.vector.reciprocal(T.w_[:], T.w_[:]), r=["w_"], w=["w_"])
            tt(p, nc, "dve", T.f_[:], T.w_[:], oml_bc, ALU.mult, r=["w_"], w=["f_"])
            tt(p, nc, "dve", T.f_[:], T.f_[:], lb_bc, ALU.add, r=["f_"], w=["f_"])
            act(p, nc, T.lf[:], T.f_[:], AF.Ln, r=["f_"], w=["lf"])
            tt(p, nc, "pool", T.km[:], T.u[:], T.w_[:], ALU.mult, r=["u", "w_"], w=["km"])
            tt(p, nc, "pool", T.km[:], T.km[:], oml_bc, ALU.mult, r=["km"], w=["km"])
            p.op("dve", lambda i: nc.vector.tensor_tensor_scan(fl(T.bb), smask[:], fl(T.lf), 0.0, ALU.mult, ALU.add), r=["lf"], w=["bb"])
            if dirn == 1:
                tt(p, nc, "pool", T.bm[:], T.lf[:], T.bb[:], ALU.subtract, r=["lf", "bb"], w=["bm"])
                tt(p, nc, "pool", c4(T.bb), c4(T.bm), c4(T.bb)[:, :, :, 63:64].to_broadcast([128, 8, 2, 64]), ALU.add, r=["bm", "bb"], w=["bb"])
            endidx = 63 if dirn == 0 else 0
            cref = c4(T.bb)[:, :, :, 31:32]
            bend = c4(T.bb)[:, :, :, endidx:endidx + 1]
            act(p, nc, T.sc[:, :, :, 0:1], cref, AF.Exp, r=["bb"], w=["sc0"])
            act(p, nc, T.sc[:, :, :, 1:2], bend, AF.Exp, r=["bb"], w=["sc1"])
            tt(p, nc, "dve", T.sc[:, :, :, 3:4], bend, cref, ALU.subtract, r=["bb"], w=["sc3"])
            act(p, nc, T.sc[:, :, :, 2:3], T.sc[:, :, :, 3:4], AF.Exp, r=["sc3"], w=["sc2"])
            tt(p, nc, "pool", c4(T.bm), c4(T.bb), cref.to_broadcast([128, 8, 2, 64]), ALU.subtract, r=["bb", "bm"], w=["bm"])
            act(p, nc, T.Ee[:], T.bm[:], AF.Exp, r=["bm"], w=["Ee"])
            act(p, nc, T.Ei[:], T.bm[:], AF.Exp, scale=-1.0, r=["bm"], w=["Ei"])
            tt(p, nc, "dve", T.qe[:], T.qt[:], T.Ee[:], ALU.mult, r=["qt", "Ee"], w=["qe"])
            tt(p, nc, "pool", T.ke[:], T.km[:], T.Ei[:], ALU.mult, r=["km", "Ei"], w=["ke"])
            marks.append(len(p.ops))
            order = [0, 1] if dirn == 0 else [1, 0]
            for c in order:
                cs_ = slice(c * 64, (c + 1) * 64)
                for h in range(8):
                    p.op("act", lambda i, h=h, c=c, T=T: nc.scalar.activation(T.Sc[:, h, :], S[:, h, :], AF.Copy, scale=T.sc[:, h, c, 0:1]),
                         r=[f"!S{h}", "sc0"], w=[f"Sc{h}"])
                for h in range(8):
                    mm(p, nc, psA[:, h, :], T.ke[:, h, cs_], T.qe[:, h, cs_], True, True, r=["ke", "qe"], w=["!psA"])
                for h in range(8):
                    tt(p, nc, "dve", T.AT[:, h, :], psA[:, h, :], hm[:, dirn, :], ALU.mult, r=["!psA"], w=[f"AT{h}"])
                for h in range(8):
                    tr(p, nc, psKT2[h // 4][:, h % 4, :], T.ke[:, h, cs_], identb[:], r=["ke"], w=[f"!psKT{h // 4}"])
                for h in range(8):
                    cp(p, nc, "dve" if h < 4 else "act", T.keT[:, h, :], psKT2[h // 4][:, h % 4, :], r=[f"!psKT{h // 4}"], w=[f"keT{h}"])
                for h in range(8):
                    po = pso[h // 4][:, (h % 4) * 128:(h % 4 + 1) * 128]
                    mm(p, nc, po, T.AT[:, h, :], T.vt[:, c, h * 128:(h + 1) * 128], True, False, r=[f"AT{h}", "vt"], w=[f"!pso{h // 4}"])
                    mm(p, nc, po, T.qe[:, h, cs_], T.Sc[:, h, :], False, True, r=["qe", f"Sc{h}"], w=[f"!pso{h // 4}"])
                for h in range(8):
                    mm(p, nc, psdS[h // 4][:, h % 4, :], T.keT[:, h, :], T.vt[:, c, h * 128:(h + 1) * 128], True, True,
                       r=[f"keT{h}", "vt"], w=[f"!psdS{h // 4}"])
                for h in range(8):
                    ts(p, nc, "pool", S[:, h, :], S[:, h, :], T.sc[:, h, c, 1:2], None, ALU.mult, r=[f"!S{h}", "sc1"], w=[f"!S{h}"])
                for h in range(8):
                    p.op("dve", lambda i, h=h, c=c, T=T: nc.vector.scalar_tensor_tensor(S[:, h, :], psdS[h // 4][:, h % 4, :], T.sc[:, h, c, 2:3],
                                                                                         S[:, h, :], ALU.mult, ALU.add),
                         r=[f"!psdS{h // 4}", f"!S{h}", "sc2"], w=[f"!S{h}"])
                cp(p, nc, "act", T.oc[:, 0:512], pso[0][:], r=["!pso0"], w=["oc0"])
                cp(p, nc, "dve", T.oc[:, 512:1024], pso[1][:], r=["!pso1"], w=["oc1"])
                dma(p, nc, "sp", f"oc", lambda i, c=c: OUT[bass.ds(tok0(i) + c * 64, 64), :], T.oc[:], r=["oc0", "oc1"])

        for s, T_s in enumerate(seqs):
            nb = T_s // 128
            tok_s = seq_tile0[s] * 128
            for dirn in range(2):
                p = Phase(kb, f"recz{l}_{s}_{dirn}")
                p.op("pool", lambda i: nc.gpsimd.memset(S[:], 0.0), w=["S"])
                p.run()
                p = Phase(kb, f"H2_{l}_{s}_{dirn}", n_iter=nb // 2)
                marks = []
                for k in range(2):
                    p.ns = f"#{k}"
                    marks.append(len(p.ops))
                    if dirn == 0:
                        rec_body(p, lambda i, tok_s=tok_s, k=k: i * 256 + (tok_s + k * 128), 0, OF, sets[k], marks)
                    else:
                        rec_body(p, lambda i, tok_s=tok_s, nb=nb, k=k: (tok_s + (nb - 1 - k) * 128) - i * 256, 1, OB, sets[k], marks)
                p.ns = ""
                a0, a1, b0, b1 = marks
                o_ = p.ops
                prepA, chA, prepB, chB = o_[a0:a1], o_[a1:b0], o_[b0:b1], o_[b1:]
                z_ = []
                for q_ in range(max(len(prepA), len(prepB))):
                    if q_ < len(prepA):
                        z_.append(prepA[q_])
                    if q_ < len(prepB):
                        z_.append(prepB[q_])
                p.ops = z_ + chA + chB
                p.run()
    if b.cfg.get("stop") == "H2":
        return

    with ExitStack() as st:
        def sb(name, shape, dt=F32):
            return st.enter_context(nc.sbuf_tensor(un(b, name), list(shape), dt))

        def ps(name, shape, dt=F32):
            return st.enter_context(nc.psum_tensor(un(b, name), list(shape), dt))
        wout = sb("wout", [128, 8, 1024], BF16)
        gnb = sb("gnb", [128, 1024])
        lng = sb("lng", [128, 1024])
        lnb = sb("lnb", [128, 1024])
        with ExitStack() as st2:
            stg = [st2.enter_context(nc.sbuf_tensor(un(b, "stg0"), [128, 8, 512], F32)),
                   st2.enter_context(nc.sbuf_tensor(un(b, "stg1"), [128, 8, 512], F32))]
            p = Phase(kb, f"recw2{l}")
            load_weight_bf16(b, p, wout, g["r_wout"][lr].rearrange("(kc p) n -> p kc n", p=128), 1024, "wout", stg)
            dma(p, nc, "pool", "gnb", gnb[:], g["r_gn"][lr:lr + 1, :].partition_broadcast(128), w=["gnb"])
            dma(p, nc, "pool", "lng", lng[:], g["ln_g"][l, 0:1, :].partition_broadcast(128), w=["lng"])
            dma(p, nc, "pool", "lnb", lnb[:], g["ln_b"][l, 0:1, :].partition_broadcast(128), w=["lnb"])
            p.run()
        from types import SimpleNamespace
        sets = []
        for k in range(2):
            T = SimpleNamespace(of=sb(f"of{k}", [128, 1024]), ob=sb(f"ob{k}", [128, 1024]), gt=sb(f"gt{k}", [128, 1024]),
                                xt=sb(f"xt{k}", [128, 1024]), g1b=sb(f"g1b{k}", [128, 1024]), sq=sb(f"sq{k}", [128, 1024]),
                                ms=sb(f"ms{k}", [128, 8]), on=sb(f"on{k}", [128, 1024], BF16), onT=sb(f"onT{k}", [128, 8, 128], BF16),
                                t1=sb(f"t1{k}", [128, 1024]), z=sb(f"z{k}", [128, 1024]), xo=sb(f"xo{k}", [128, 1024]),
                                stt=sb(f"stt{k}", [128, 8]), psOT=ps(f"psOT{k}", [128, 8, 128], BF16),
                                psY=[ps(f"psY0{k}", [128, 512]), ps(f"psY1{k}", [128, 512])])
            sets.append(T)
        p = Phase(kb, f"H3_{l}", n_iter=NTILES // 2)
        for k in range(2):
            p.ns = f"#{k}"
            T = sets[k]
            dma(p, nc, "sp", "of", T.of[:], lambda i, k=k, T=T: OF[bass.ds(i * 256 + k * 128, 128), :], w=["of"])
            dma(p, nc, "act", "ob", T.ob[:], lambda i, k=k, T=T: OB[bass.ds(i * 256 + k * 128, 128), :], w=["ob"])
            dma(p, nc, "pool", "gt", T.gt[:], lambda i, k=k, T=T: GT[bass.ds(i * 256 + k * 128, 128), :], w=["gt"])
            dma(p, nc, "sp", "xt", T.xt[:], lambda i, k=k, T=T: X[bass.ds(i * 256 + k * 128, 128), :], w=["xt"])
            dma(p, nc, "act", "g1b", T.g1b[:], lambda i, k=k, T=T: MODT[bass.ds(i * 2 + k, 1), 2048:3072].partition_broadcast(128), w=["g1b"])
            tt(p, nc, "dve", T.of[:], T.of[:], T.ob[:], ALU.add, r=["of", "ob"], w=["of"])
            tt(p, nc, "pool", T.sq[:], T.of[:], T.of[:], ALU.mult, r=["of"], w=["sq"])
            p.op("dve", lambda i, k=k, T=T: nc.vector.tensor_reduce(T.ms[:], T.sq[:].rearrange("p (h v) -> p h v", v=128), AX.X, ALU.add), r=["sq"], w=["ms"])
            ts(p, nc, "dve", T.ms[:], T.ms[:], 1.0 / 128.0, None, ALU.mult, r=["ms"], w=["ms"])
            act(p, nc, T.ms[:], T.ms[:], AF.Ln, bias=b.eps_ln[:, 1:2], r=["ms"], w=["ms"])
            act(p, nc, T.ms[:], T.ms[:], AF.Exp, scale=-0.5, r=["ms"], w=["ms"])
            tt(p, nc, "dve", T.sq[:].rearrange("p (h v) -> p h v", v=128), T.of[:].rearrange("p (h v) -> p h v", v=128),
               T.ms[:].unsqueeze(2).to_broadcast([128, 8, 128]), ALU.mult, r=["of", "ms", "sq"], w=["sq"])
            tt(p, nc, "pool", T.sq[:], T.sq[:], gnb[:], ALU.mult, r=["sq"], w=["sq"])
            act(p, nc, T.gt[:], T.gt[:], AF.Silu, r=["gt"], w=["gt"])
            tt(p, nc, "dve", T.on[:], T.sq[:], T.gt[:], ALU.mult, r=["sq", "gt"], w=["on"])
            for kc in range(8):
                tr(p, nc, T.psOT[:, kc, :], T.on[:, kc * 128:(kc + 1) * 128], identb[:], r=["on"], w=["psOT"])
            cp(p, nc, "act", T.onT[:], T.psOT[:], r=["psOT"], w=["onT"])
            for n in range(2):
                for kc in range(8):
                    mm(p, nc, T.psY[n][:], T.onT[:, kc, :], wout[:, kc, n * 512:(n + 1) * 512], kc == 0, kc == 7, r=["onT"], w=[f"psY{n}"])
            ts(p, nc, "pool", T.g1b[:], T.g1b[:], 1.0, None, ALU.add, r=["g1b"], w=["g1b"])
            for n in range(2):
                tt(p, nc, "dve", T.t1[:, n * 512:(n + 1) * 512], T.psY[n][:], T.g1b[:, n * 512:(n + 1) * 512], ALU.mult,
                   r=[f"psY{n}", "g1b"], w=["t1"])
            p.op("dve", lambda i, k=k, T=T: nc.vector.scalar_tensor_tensor(T.z[:], T.xt[:], alpha, T.t1[:], ALU.mult, ALU.add), r=["xt", "t1"], w=["z"])
            layer_norm_tail(b, p, T.z, "z", lng, lnb, T.xo, "xo", dict(sq=T.sq, st=T.stt), "ln")
            dma(p, nc, "sp", "xo", lambda i, k=k, T=T: X[bass.ds(i * 256 + k * 128, 128), :], T.xo[:], r=["xo"])
            if k == 0:
                n_half = len(p.ops)
        p.ns = ""
        p.interleave(0, n_half, len(p.ops))
        p.run()
```

```python
import math
from collections import Counter
from contextlib import ExitStack
import numpy as np
import ml_dtypes
import concourse.bass as bass
import concourse.mybir as mybir
from concourse.bass_utils import run_bass_kernel_spmd

F32 = mybir.dt.float32
BF16 = mybir.dt.bfloat16
I32 = mybir.dt.int32
AF = mybir.ActivationFunctionType
ALU = mybir.AluOpType
AX = mybir.AxisListType

D = 1024
HD = 64
N_GROUPS = 4
EPG = 8
NEXP = 32
DEXP = 512
LN_EPS = 1e-5
RMS_EPS = 1e-6
ROPE_THETA = 10000.0


class KB:
    def __init__(self, nc):
        self.nc = nc
        self.engs = {"pe": nc.tensor, "act": nc.scalar, "dve": nc.vector, "pool": nc.gpsimd, "sp": nc.sync}
        self.sems = {}
        self.base = {}
        for e in self.engs:
            self.sems[("e", e)] = nc.alloc_semaphore(name="S_" + e)
            self.base[("e", e)] = 0
        self.chan_owner = {}
        self.nphase = 0

    def sig(self, key):
        if key not in self.sems:
            self.sems[key] = self.nc.alloc_semaphore(name="C_" + str(key[1]))
            self.base[key] = 0
        return self.sems[key]


class Phase:
    def __init__(self, kb, name, n_iter=None):
        self.kb = kb
        self.name = name
        self.n_iter = n_iter
        self.ops = []

    ns = ""

    def _n(self, name):
        return name[1:] if name.startswith("!") else name + self.ns

    def op(self, eng, fn, r=(), w=(), chan=None):
        self.ops.append(dict(eng=eng, fn=fn, r=tuple(self._n(x) for x in r), w=tuple(self._n(x) for x in w),
                             chan=(chan + self.ns) if chan else None))

    def dma(self, eng, chan, fn, r=(), w=()):
        self.op(eng, fn, r, w, chan=chan)

    def interleave(self, a, b_, c):
        A, Bq = self.ops[a:b_], self.ops[b_:c]
        out = []
        for k in range(max(len(A), len(Bq))):
            if k < len(A):
                out.append(A[k])
            if k < len(Bq):
                out.append(Bq[k])
        self.ops[a:c] = out

    def run(self):
        kb = self.kb
        nc = kb.nc
        ops = self.ops
        m = len(ops)
        if m == 0:
            return
        loop = self.n_iter is not None
        N = self.n_iter if loop else 1
        sig_of = [("c", o["chan"]) if o["chan"] else ("e", o["eng"]) for o in ops]
        for j, o in enumerate(ops):
            if o["chan"]:
                own = kb.chan_owner.setdefault(o["chan"], o["eng"])
                assert own == o["eng"], f"chan {o['chan']} issued from two engines"
        phys = {}
        kcnt = {"sw": 0, "hw": 0}
        for j, o in enumerate(ops):
            sg_ = sig_of[j]
            if sg_ in phys:
                continue
            if sg_[0] == "e":
                phys[sg_] = sg_
            else:
                kind = "sw" if o["eng"] == "pool" else "hw"
                phys[sg_] = ("c", f"{kind}{kcnt[kind]}")
                kcnt[kind] += 1
        per_iter = Counter(sig_of)
        kidx = []
        cnt = Counter()
        for j in range(m):
            kidx.append(cnt[sig_of[j]])
            cnt[sig_of[j]] += 1
        last_w = {}
        readers = {}
        deps = [set() for _ in range(m)]
        for it in ((0, 1) if loop else (0,)):
            for j, o in enumerate(ops):
                d = set()
                for b in o["r"]:
                    if b in last_w:
                        d.add(last_w[b] + ("raw",))
                for b in o["w"]:
                    if b in last_w:
                        d.add(last_w[b] + ("waw",))
                    for rd in readers.get(b, []):
                        d.add(rd + ("war",))
                if it == (1 if loop else 0):
                    deps[j] = {(jj, itt - it, ty) for (itt, jj, ty) in d if (itt, jj) != (it, j)}
                for b in o["r"]:
                    readers.setdefault(b, []).append((it, j))
                for b in o["w"]:
                    last_w[b] = (it, j)
                    readers[b] = []
        waits = []
        for j, o in enumerate(ops):
            need = {}
            for (jj, off, ty) in deps[j]:
                p = ops[jj]
                s = sig_of[jj]
                if s[0] == "e" and sig_of[j][0] == "e" and p["eng"] == o["eng"]:
                    if o["eng"] == "pe" or ty != "raw":
                        continue
                n_s = per_iter[s]
                if s[0] == "c":
                    if off == 0:
                        q = sum(1 for x in range(j) if sig_of[x] == s)
                        const = q * 16
                    else:
                        const = 0
                else:
                    const = (kidx[jj] + 1) if off == 0 else (kidx[jj] + 1 - n_s)
                if s not in need or const > need[s]:
                    need[s] = const
            waits.append(need)
        known = {}
        for j, o in enumerate(ops):
            kn = known.setdefault(o["eng"], {})
            for s in list(waits[j]):
                if s in kn and kn[s] >= waits[j][s]:
                    del waits[j][s]
                else:
                    kn[s] = waits[j][s]
        T0 = {}
        for s, n_s in per_iter.items():
            kb.sig(phys[s])
            inc = 16 if s[0] == "c" else 1
            T0[s] = kb.base[phys[s]] + (n_s * inc if (loop and s[0] == "e") else 0)
        engines_used = []
        for o in ops:
            if o["eng"] not in engines_used:
                engines_used.append(o["eng"])

        def free_ids(E):
            hs, ids = [], []
            while True:
                try:
                    kb.probe_cnt = getattr(kb, "probe_cnt", 0) + 1
                    h = E.alloc_register(f"probe{kb.probe_cnt}")
                except Exception:
                    break
                hs.append(h)
                ids.append(nc.lookup_reg(h).reg_id)
            for h in hs:
                E.free_register(h)
            return set(ids)

        def emit_engine(ename):
            E = kb.engs[ename]
            before = free_ids(E)
            emit_engine_inner(ename)
            leaked = sorted(before - free_ids(E))
            for rid in leaked:
                kb.probe_cnt += 1
                h = nc.add_register(E.engine, f"leak{kb.probe_cnt}", rid)
                E.free_register(h)

        def emit_engine_inner(ename):
            E = kb.engs[ename]
            mine = [j for j, o in enumerate(ops) if o["eng"] == ename]
            wsigs = []
            for j in mine:
                for s in waits[j]:
                    if s not in wsigs:
                        wsigs.append(s)

            def body(i, regs, tmp):
                for j in mine:
                    o = ops[j]
                    for s, const in waits[j].items():
                        if i is None:
                            E.wait_ge(kb.sig(phys[s]), T0[s] + const)
                        elif const == 0:
                            E.wait_ge(kb.sig(phys[s]), regs[s])
                        else:
                            E.reg_add(tmp, regs[s], const)
                            E.wait_ge(kb.sig(phys[s]), tmp)
                    ins = o["fn"](i)
                    s = sig_of[j]
                    ins.then_inc(kb.sig(phys[s]), 16 if s[0] == "c" else 1)

            if loop:
                if ("e", ename) in per_iter:
                    E.sem_inc(kb.sig(("e", ename)), per_iter[("e", ename)])
                regs = {}
                for k, s in enumerate(wsigs):
                    regs[s] = E.alloc_register(f"w{kb.nphase}_{ename}_{k}")
                    E.reg_mov(regs[s], T0[s])
                tmp = E.alloc_register(f"wt{kb.nphase}_{ename}")
                with E.Fori(0, N) as i:
                    body(i, regs, tmp)
                    for s in wsigs:
                        E.reg_add(regs[s], regs[s], per_iter[s] * (16 if s[0] == "c" else 1))
                for s in wsigs:
                    E.free_register(regs[s])
                E.free_register(tmp)
            else:
                body(None, None, None)
            for s, n_s in per_iter.items():
                owner = kb.chan_owner[s[1]] if s[0] == "c" else s[1]
                if owner == ename:
                    inc = 16 if s[0] == "c" else 1
                    E.wait_ge(kb.sig(phys[s]), T0[s] + N * n_s * inc)

        with nc.Block() as blk:
            reg = {"pe": blk.tensor, "act": blk.scalar, "dve": blk.vector, "pool": blk.gpsimd, "sp": blk.sync}
            for ename in engines_used:
                reg[ename](lambda eng, ename=ename: emit_engine(ename))
        for s, n_s in per_iter.items():
            inc = 16 if s[0] == "c" else 1
            kb.base[phys[s]] = T0[s] + N * n_s * inc
        nc.all_engine_barrier()
        kb.nphase += 1


def _bf(a):
    return np.ascontiguousarray(a).astype(ml_dtypes.bfloat16)


def host_constants(cfg):
    seqs = cfg["seqs"]
    NT = sum(seqs)
    c = {}
    c["ident_f"] = np.eye(128, dtype=np.float32)
    c["ident_b"] = _bf(np.eye(128, dtype=np.float32))
    pos = np.concatenate([np.arange(t) for t in seqs]).astype(np.float32)
    inv = np.power(ROPE_THETA, -np.arange(0, HD, 2, dtype=np.float32) / HD).astype(np.float32)
    d = np.arange(128) % 64
    ang = pos[None, :] * inv[d % 32][:, None]
    c["rope_cos"] = np.cos(ang).astype(np.float32)
    sgn = np.where(d < 32, -1.0, 1.0).astype(np.float32)
    c["rope_sin"] = (np.sin(ang) * sgn[:, None]).astype(np.float32)
    j = np.arange(128)[:, None]
    i = np.arange(128)[None, :]
    c["wmask"] = _bf(np.stack([(j >= i), (j <= i)], axis=1).astype(np.float32))
    kc = np.arange(64)
    qc = np.arange(64)
    qwin = np.clip(qc - 8, 0, 48)
    col_ok = (kc[:, None] >= qwin[None, :]) & (kc[:, None] < qwin[None, :] + 16)
    m = np.zeros((128, 9, 128), np.float32)
    for t in range(9):
        for b in range(2):
            for a in range(2):
                ok = col_ok.astype(np.float32)
                if t == 7 and (a == 1 and b == 0):
                    ok = ok * 0
                if t == 8 and not (a == 1 and b == 0):
                    ok = ok * 0
                m[b * 64:(b + 1) * 64, t, a * 64:(a + 1) * 64] = ok
    c["namask"] = m
    s = np.arange(64)[:, None]
    t = np.arange(64)[None, :]
    c["hmask"] = _bf(np.stack([(s <= t), (s >= t)], axis=1).astype(np.float32))
    sm = np.ones((128, 8, 128), np.float32)
    sm[:, :, 0] = 0
    sm[:, :, 64] = 0
    c["scanmask"] = sm.reshape(128, 1024)
    c["iota32"] = np.tile(np.arange(32, dtype=np.float32)[None, :], (128, 1))
    c["iota_blk"] = np.tile((np.arange(256, dtype=np.float32) * 128.0)[None, :], (128, 1))
    c["iota_p"] = np.arange(128, dtype=np.float32).reshape(128, 1)
    c["tri_su"] = _bf((np.arange(128)[:, None] < np.arange(128)[None, :]).astype(np.float32))
    c["ones_b"] = _bf(np.ones((128, 128), np.float32))
    return c


NA_TABLE_DELTA = [-3, -2, -1, 0, 1, 2, 3, -2, 2]


def na_bias_tables(rel_bias):
    L = rel_bias.shape[0]
    kc = np.arange(64)[:, None]
    qc = np.arange(64)[None, :]
    dc = np.clip(kc - qc, -15, 15) + 15
    out = np.zeros((L, 128, 9, 8, 128), np.float32)
    for t, dl in enumerate(NA_TABLE_DELTA):
        for b in range(2):
            for a in range(2):
                dr = 2 * dl + b - a + 7
                g = rel_bias[:, :, dr, :][:, :, dc]
                out[:, b * 64:(b + 1) * 64, t, :, a * 64:(a + 1) * 64] = g.transpose(0, 2, 1, 3)
    return out


def attn_w_layout(w_in):
    L = w_in.shape[0]
    qa0, ka0, va0, qb0, kb0, vb0 = 0, 512, 640, 768, 1280, 1792
    sw = (np.arange(64) + 32) % 64
    cols = []
    for jc in range(4):
        for h in (jc, 4 + jc):
            cols += list(qa0 + h * 64 + np.arange(64))
    for jc in range(4):
        for h in (jc, 4 + jc):
            cols += list(qa0 + h * 64 + sw)
    for h in range(2):
        cols += list(ka0 + h * 64 + np.arange(64))
    for h in range(2):
        cols += list(ka0 + h * 64 + sw)
    cols += list(qb0 + np.arange(512))
    cols += list(kb0 + np.arange(512))
    cols = np.array(cols)
    vcols = np.concatenate([va0 + np.arange(128), vb0 + np.arange(512)])
    return np.ascontiguousarray(w_in[:, :, cols]), np.ascontiguousarray(w_in[:, :, vcols])


class B:
    def __init__(self, cfg):
        self.cfg = cfg
        self.nc = bass.Bass("TRN2", target_bir_lowering=False)
        self.kb = KB(self.nc)
        self.dram = {}
        self.uid = 0

    def din(self, name, shape, dt=F32):
        t = self.nc.dram_tensor(name, list(shape), dt, kind="ExternalInput")
        self.dram[name] = t
        return t

    def dout(self, name, shape, dt=F32):
        t = self.nc.dram_tensor(name, list(shape), dt, kind="ExternalOutput")
        self.dram[name] = t
        return t

    def dscr(self, name, shape, dt=F32):
        if self.cfg.get("debug"):
            return self.dout(name, shape, dt)
        t = self.nc.dram_tensor(name, list(shape), dt)
        self.dram[name] = t
        return t


def un(b, name):
    b.uid += 1
    return f"{name}_{b.uid}"


def _ap(x, i):
    return x(i) if callable(x) else x


def dma(p, nc, eng, chan, out, in_, r=(), w=(), slow=False):
    E = {"sp": nc.sync, "act": nc.scalar, "pool": nc.gpsimd}[eng]
    if slow:
        p.dma(eng, chan, lambda i: E.dma_start(out=_ap(out, i), in_=_ap(in_, i), allow_slow_non_contiguous=True), r=r, w=w)
    else:
        p.dma(eng, chan, lambda i: E.dma_start(out=_ap(out, i), in_=_ap(in_, i)), r=r, w=w)


def eng_of(nc, e):
    return {"dve": nc.vector, "pool": nc.gpsimd, "act": nc.scalar}[e]


def tt(p, nc, e, out, in0, in1, op, r=(), w=()):
    E = eng_of(nc, e)
    p.op(e, lambda i: E.tensor_tensor(_ap(out, i), _ap(in0, i), _ap(in1, i), op), r=r, w=w)


def ts(p, nc, e, out, in0, s1, s2, op0, op1=None, r=(), w=()):
    E = eng_of(nc, e)
    if op1 is None:
        p.op(e, lambda i: E.tensor_scalar(_ap(out, i), _ap(in0, i), s1, None, op0), r=r, w=w)
    else:
        p.op(e, lambda i: E.tensor_scalar(_ap(out, i), _ap(in0, i), s1, s2, op0, op1), r=r, w=w)


def cp(p, nc, e, out, in_, r=(), w=()):
    if e == "act":
        p.op(e, lambda i: nc.scalar.copy(_ap(out, i), _ap(in_, i)), r=r, w=w)
    else:
        E = eng_of(nc, e)
        p.op(e, lambda i: E.tensor_copy(_ap(out, i), _ap(in_, i)), r=r, w=w)


def act(p, nc, out, in_, func, bias=None, scale=None, r=(), w=()):
    kw = {}
    if bias is not None:
        kw["bias"] = bias
    if scale is not None:
        kw["scale"] = scale
    p.op("act", lambda i: nc.scalar.activation(_ap(out, i), _ap(in_, i), func, **kw), r=r, w=w)


def mm(p, nc, out, lhsT, rhs, start, stop, r=(), w=()):
    p.op("pe", lambda i: nc.tensor.matmul(_ap(out, i), lhsT=_ap(lhsT, i), rhs=_ap(rhs, i), start=start, stop=stop), r=r, w=w)


def tr(p, nc, out, in_, ident, r=(), w=()):
    p.op("pe", lambda i: nc.tensor.transpose(_ap(out, i), _ap(in_, i), ident), r=r, w=w)


def load_weight_bf16(b, p, dst, src, ncols, tag, stg, kdim=8):
    nc = b.nc
    engs = ["dve", "act", "pool"]
    c0 = 0
    k = 0
    while c0 < ncols:
        cw = min(512, ncols - c0)
        st = stg[k % 2]
        sname = f"stg{k % 2}"
        dma(p, nc, "sp" if k % 2 == 0 else "act", f"wstg{k % 2}", st[:, :, 0:cw], src[:, :, c0:c0 + cw], w=[sname])
        cp(p, nc, engs[k % 3], dst[:, :, c0:c0 + cw], st[:, :, 0:cw], r=[sname], w=[tag + str(k)])
        c0 += cw
        k += 1


def layer_norm_tail(b, p, z, zname, lng, lnb, out, outname, scr, tg):
    nc = b.nc
    sq, st = scr["sq"], scr["st"]
    p.op("dve", lambda i: nc.vector.tensor_reduce(st[:, 0:1], z[:], AX.X, ALU.add), r=[zname], w=[tg + "s0"])
    tt(p, nc, "pool", sq[:], z[:], z[:], ALU.mult, r=[zname], w=[tg + "sq"])
    p.op("dve", lambda i: nc.vector.tensor_reduce(st[:, 1:2], sq[:], AX.X, ALU.add), r=[tg + "sq"], w=[tg + "s1"])
    ts(p, nc, "dve", st[:, 2:3], st[:, 0:1], 1.0 / D, None, ALU.mult, r=[tg + "s0"], w=[tg + "s2"])
    ts(p, nc, "dve", st[:, 3:4], st[:, 1:2], 1.0 / D, None, ALU.mult, r=[tg + "s1"], w=[tg + "s3"])
    tt(p, nc, "dve", st[:, 4:5], st[:, 2:3], st[:, 2:3], ALU.mult, r=[tg + "s2"], w=[tg + "s4"])
    tt(p, nc, "dve", st[:, 5:6], st[:, 3:4], st[:, 4:5], ALU.subtract, r=[tg + "s3", tg + "s4"], w=[tg + "s5"])
    act(p, nc, st[:, 6:7], st[:, 5:6], AF.Ln, bias=b.eps_ln[:, 0:1], r=[tg + "s5"], w=[tg + "s6"])
    act(p, nc, st[:, 6:7], st[:, 6:7], AF.Exp, scale=-0.5, r=[tg + "s6"], w=[tg + "s6"])
    p.op("dve", lambda i: nc.vector.scalar_tensor_tensor(st[:, 7:8], st[:, 2:3], -1.0, st[:, 6:7], ALU.mult, ALU.mult),
         r=[tg + "s2", tg + "s6"], w=[tg + "s7"])
    act(p, nc, sq[:], z[:], AF.Identity, bias=st[:, 7:8], scale=st[:, 6:7], r=[zname, tg + "s6", tg + "s7", tg + "sq"], w=[tg + "sq"])
    tt(p, nc, "dve", sq[:], sq[:], lng[:], ALU.mult, r=[tg + "sq"], w=[tg + "sq"])
    tt(p, nc, "pool", out[:], sq[:], lnb[:], ALU.add, r=[tg + "sq"], w=[outname])


def build_program(cfg):
    seqs = cfg["seqs"]
    NT = sum(seqs)
    NTILES = NT // 128
    depth = cfg["depth"]
    LA = (depth + 1) // 2
    LR = depth // 2
    stop = cfg.get("stop")
    alpha = float((2 * cfg.get("dn_depth", depth)) ** 0.25)
    b = B(cfg)
    nc = b.nc
    kb = b.kb

    x_in = b.din("x", [NT, D])
    cT_in = b.din("cT", [128, 3, 8])
    ada_w = b.din("ada_w", [depth, D, 6 * D])
    ada_b = b.din("ada_b", [depth, 6 * D])
    ln_g = b.din("ln_g", [depth, 2, D])
    ln_b = b.din("ln_b", [depth, 2, D])
    a_wfm = b.din("attn_wfm", [LA, D, 2304])
    a_wv = b.din("attn_wv", [LA, D, 640])
    a_sink = b.din("attn_sink", [LA, 8])
    a_rb = b.din("na_rb", [LA, 128, 9, 8, 128])
    a_wout = b.din("attn_w_out", [LA, D, D])
    r_win = b.din("rec_w_in", [max(LR, 1), D, 5 * D])
    r_lb = b.din("rec_lb", [max(LR, 1), D])
    r_gn = b.din("rec_gnorm", [max(LR, 1), D])
    r_wout = b.din("rec_w_out", [max(LR, 1), D, D])
    ro_w = b.din("router_w", [depth, D, 36])
    ro_b = b.din("router_b", [depth, 36])
    e_w = b.din("e_w", [depth * NEXP * 128, 12288])
    cn = {}
    for name, shape, dt in [("ident_f", [128, 128], F32), ("ident_b", [128, 128], BF16), ("rope_cos", [128, NT], F32),
                            ("rope_sin", [128, NT], F32), ("wmask", [128, 2, 128], BF16), ("namask", [128, 9, 128], F32),
                            ("hmask", [64, 2, 64], BF16), ("scanmask", [128, 1024], F32), ("iota32", [128, 32], F32),
                            ("iota_blk", [128, 256], F32), ("iota_p", [128, 1], F32), ("tri_su", [128, 128], BF16),
                            ("ones_b", [128, 128], BF16)]:
        cn[name] = b.din(name, shape, dt)
    y_out = b.dout("y", [NT, D])
    X = b.dscr("X", [NT, D])
    MODT = b.dscr("MODT", [NTILES, 6 * D])
    FM = b.dscr("FM", [13, 128, NT], BF16)
    VE = b.dscr("VE", [NT, 720], BF16)

    ODBG = b.dout("ODBG", [NT, D], BF16) if cfg.get("debug") else None
    seq_tile0 = [0]
    for t in seqs[:-1]:
        seq_tile0.append(seq_tile0[-1] + t // 128)

    with ExitStack() as gs:
        def sb(name, shape, dt=F32, stack=gs):
            return stack.enter_context(nc.sbuf_tensor(un(b, name), list(shape), dt))

        def ps(name, shape, dt=F32, stack=gs):
            return stack.enter_context(nc.psum_tensor(un(b, name), list(shape), dt))

        identf = sb("identf", [128, 128])
        identb = sb("identb", [128, 128], BF16)
        CS = sb("CS", [128, 24, 128])
        epst = sb("epst", [128, 2])
        b.eps_ln = epst
        csb = sb("csb", [128, 24])
        p = Phase(kb, "setup")
        p.op("pool", lambda i: nc.gpsimd.memset(epst[:, 0:1], LN_EPS), w=["epst0"])
        p.op("pool", lambda i: nc.gpsimd.memset(epst[:, 1:2], RMS_EPS), w=["epst1"])
        dma(p, nc, "sp", "k0", identf[:], cn["ident_f"][:, :], w=["identf"])
        dma(p, nc, "sp", "k1", identb[:], cn["ident_b"][:, :], w=["identb"])
        dma(p, nc, "sp", "k2", csb[:], cT_in.ap().rearrange("p s k -> p (s k)"), w=["csb"])
        act(p, nc, csb[:], csb[:], AF.Silu, r=["csb"], w=["csb"])
        cp(p, nc, "dve", CS[:], csb[:].unsqueeze(2).to_broadcast([128, 24, 128]), r=["csb"], w=["CS"])
        p.run()

        p = Phase(kb, "xcopy")
        nchunk = max(1, NT // 1024)
        rows = NT // nchunk
        for k in range(nchunk):
            dma(p, nc, ["sp", "act", "pool"][k % 3], f"xc{k % 3}", X[k * rows:(k + 1) * rows, :], x_in[k * rows:(k + 1) * rows, :])
        p.run()

        for l in range(depth):
            with ExitStack() as st:
                aw = sb("aw", [128, 8, 512], stack=st)
                ab = sb("ab", [128, 512], stack=st)
                mo = [sb(f"mo{s}", [128, 512], stack=st) for s in range(3)]
                pm = [ps(f"pm{s}", [128, 512], stack=st) for s in range(3)]
                p = Phase(kb, f"mod{l}", n_iter=12)
                dma(p, nc, "sp", "aw", aw[:], lambda i: ada_w[l].rearrange("(kc p) n -> p kc n", p=128)[:, :, bass.ds(i * 512, 512)], w=["aw"])
                dma(p, nc, "act", "ab", ab[:], lambda i: ada_b[l:l + 1, bass.ds(i * 512, 512)].partition_broadcast(128), w=["ab"])
                for s in range(3):
                    for kc in range(8):
                        mm(p, nc, pm[s][:], CS[:, s * 8 + kc, :], aw[:, kc, :], kc == 0, kc == 7, r=["aw"], w=[f"pm{s}"])
                    tt(p, nc, "dve", mo[s][:], pm[s][:], ab[:], ALU.add, r=[f"pm{s}", "ab"], w=[f"mo{s}"])
                    nts = seqs[s] // 128
                    dma(p, nc, "pool", f"mo{s}", lambda i, s=s, nts=nts: MODT[seq_tile0[s]:seq_tile0[s] + nts, bass.ds(i * 512, 512)],
                        mo[s][0:nts, :], r=[f"mo{s}"])
                p.run()
            if stop == "mod":
                break
            if l % 2 == 0:
                build_attention_layer(b, l, l // 2, dict(X=X, MODT=MODT, FM=FM, VE=VE, a_wfm=a_wfm, a_wv=a_wv, a_sink=a_sink,
                                                         a_rb=a_rb, a_wout=a_wout, ln_g=ln_g, ln_b=ln_b, cn=cn, identb=identb,
                                                         identf=identf, alpha=alpha, seq_tile0=seq_tile0, ODBG=ODBG))
            else:
                build_hgrn_layer(b, l, l // 2, dict(X=X, MODT=MODT, r_win=r_win, r_lb=r_lb, r_gn=r_gn, r_wout=r_wout,
                                                    ln_g=ln_g, ln_b=ln_b, cn=cn, identb=identb, identf=identf, alpha=alpha,
                                                    seq_tile0=seq_tile0))
            if stop in (f"mix{l}", "attw", "A1", "attpre", "A2e") or (stop in ("H1", "H2") and l % 2 == 1):
                break
            build_moe_layer(b, l, dict(X=X, MODT=MODT, ro_w=ro_w, ro_b=ro_b, e_w=e_w, ln_g=ln_g,
                                       ln_b=ln_b, cn=cn, identb=identb, identf=identf, alpha=alpha))
            if stop in (f"moe{l}", "moeD", "moeS", "moeB"):
                break

        p = Phase(kb, "ycopy")
        for k in range(nchunk):
            dma(p, nc, ["sp", "act", "pool"][k % 3], f"yc{k % 3}", y_out[k * rows:(k + 1) * rows, :], X[k * rows:(k + 1) * rows, :])
        p.run()
    return nc


def build_attention_layer(b, l, la, g):
    nc, kb = b.nc, b.kb
    seqs = b.cfg["seqs"]
    NT = sum(seqs)
    X, MODT, FM, VE, cn = g["X"], g["MODT"], g["FM"], g["VE"], g["cn"]
    identb = g["identb"]
    alpha = g["alpha"]
    seq_tile0 = g["seq_tile0"]

    with ExitStack() as st:
        def sb(name, shape, dt=F32):
            return st.enter_context(nc.sbuf_tensor(un(b, name), list(shape), dt))

        def ps(name, shape, dt=F32):
            return st.enter_context(nc.psum_tensor(un(b, name), list(shape), dt))

        wfm = sb("wfm", [128, 8, 2304], BF16)
        wv = sb("wv", [128, 8, 640], BF16)
        stg = [sb("stg0", [128, 8, 512]), sb("stg1", [128, 8, 512])]
        veall = sb("veall", [128, 4, 10, 72], BF16)
        p = Phase(kb, f"attw{l}")
        load_weight_bf16(b, p, wfm, g["a_wfm"][la].rearrange("(kc p) n -> p kc n", p=128), 2304, "wfm", stg)
        load_weight_bf16(b, p, wv, g["a_wv"][la].rearrange("(kc p) n -> p kc n", p=128), 640, "wv", stg)
        p.op("pool", lambda i: nc.gpsimd.memset(veall[:], 1.0), w=["veall"])
        p.run()
        if b.cfg.get("stop") == "attw":
            return

        xg = sb("xg", [128, 4, 1024])
        scb = sb("scb", [128, 1024])
        shb = sb("shb", [128, 1024])
        tmp = [sb("tmp0", [128, 1024]), sb("tmp1", [128, 1024])]
        hb = sb("hb", [128, 4, 1024], BF16)
        hT = sb("hT", [128, 8, 512], BF16)
        ropec = sb("ropec", [128, 512])
        ropes = sb("ropes", [128, 512])
        t1 = sb("t1", [128, 512])
        t2 = sb("t2", [128, 512])
        fmall = sb("fmall", [128, 13, 512], BF16)
        psT = [ps("psT0", [128, 8, 128], BF16), ps("psT1", [128, 8, 128], BF16)]
        pf = [ps(f"pf{k}", [128, 512]) for k in range(3)]
        pva = ps("pva", [128, 512])
        pvb = ps("pvb", [128, 512])

        NG = NT // 512
        p = Phase(kb, f"A1_{l}", n_iter=NG)
        dma(p, nc, "sp", "xg", xg[:], lambda i: X[bass.ds(i * 512, 512), :].rearrange("(t p) d -> p t d", p=128), w=["xg"])
        dma(p, nc, "act", "scb", scb[:], lambda i: MODT[bass.ds(i * 4, 1), 1024:2048].partition_broadcast(128), w=["scb"])
        dma(p, nc, "act", "shb", shb[:], lambda i: MODT[bass.ds(i * 4, 1), 0:1024].partition_broadcast(128), w=["shb"])
        dma(p, nc, "pool", "ropec", ropec[:], lambda i: cn["rope_cos"][:, bass.ds(i * 512, 512)], w=["ropec"])
        dma(p, nc, "pool", "ropes", ropes[:], lambda i: cn["rope_sin"][:, bass.ds(i * 512, 512)], w=["ropes"])
        ts(p, nc, "dve", scb[:], scb[:], 1.0, None, ALU.add, r=["scb"], w=["scb"])
        for t in range(4):
            tt(p, nc, "dve", tmp[t % 2][:], xg[:, t, :], scb[:], ALU.mult, r=["xg", "scb"], w=[f"tmp{t % 2}"])
            tt(p, nc, "pool", hb[:, t, :], tmp[t % 2][:], shb[:], ALU.add, r=[f"tmp{t % 2}", "shb"], w=[f"hb{t}"])
            for kc in range(8):
                tr(p, nc, psT[t % 2][:, kc, :], hb[:, t, kc * 128:(kc + 1) * 128], identb[:], r=[f"hb{t}"], w=[f"psT{t % 2}"])
            cp(p, nc, "act", hT[:, :, t * 128:(t + 1) * 128], psT[t % 2][:], r=[f"psT{t % 2}"], w=[f"hT{t}"])
        hTall = [f"hT{t}" for t in range(4)]
        pfi = 0
        fk = 0
        for (ca, cb_, dst) in [(0, 4, 0), (1, 5, 1), (2, 6, 2), (3, 7, 3), (8, 9, 4)]:
            ia, ib = pfi % 3, (pfi + 1) % 3
            pfi += 2
            for kc in range(8):
                mm(p, nc, pf[ia][:], wfm[:, kc, ca * 128:(ca + 1) * 128], hT[:, kc, :], kc == 0, kc == 7, r=hTall, w=[f"pf{ia}"])
            for kc in range(8):
                mm(p, nc, pf[ib][:], wfm[:, kc, cb_ * 128:(cb_ + 1) * 128], hT[:, kc, :], kc == 0, kc == 7, r=hTall, w=[f"pf{ib}"])
            tt(p, nc, "dve", t1[:], pf[ia][:], ropec[:], ALU.mult, r=[f"pf{ia}", "ropec"], w=["t1"])
            tt(p, nc, "dve", t2[:], pf[ib][:], ropes[:], ALU.mult, r=[f"pf{ib}", "ropes"], w=["t2"])
            tt(p, nc, "pool", fmall[:, dst, :], t1[:], t2[:], ALU.add, r=["t1", "t2"], w=[f"fm{dst}"])
        for c in range(10, 18):
            dst = 5 + (c - 10)
            ia = pfi % 3
            pfi += 1
            for kc in range(8):
                mm(p, nc, pf[ia][:], wfm[:, kc, c * 128:(c + 1) * 128], hT[:, kc, :], kc == 0, kc == 7, r=hTall, w=[f"pf{ia}"])
            cp(p, nc, "act", fmall[:, dst, :], pf[ia][:], r=[f"pf{ia}"], w=[f"fm{dst}"])
        for t in range(4):
            for kc in range(8):
                mm(p, nc, pva[:], hT[:, kc, t * 128:(t + 1) * 128], wv[:, kc, 128:640], kc == 0, kc == 7, r=[f"hT{t}"], w=["pva"])
            for kc in range(8):
                mm(p, nc, pvb[:, 0:128], hT[:, kc, t * 128:(t + 1) * 128], wv[:, kc, 0:128], kc == 0, kc == 7, r=[f"hT{t}"], w=["pvb"])
            cp(p, nc, "dve", veall[:, t, 2:10, 0:64], pva[:].rearrange("p (h d) -> p h d", d=64), r=["pva"], w=[f"ve{t}"])
            cp(p, nc, "act", veall[:, t, 0:2, 0:64], pvb[:, 0:128].rearrange("p (h d) -> p h d", d=64), r=["pvb"], w=[f"ve{t}"])
        dma(p, nc, "sp", "fmall", lambda i: FM[:, :, bass.ds(i * 512, 512)].rearrange("f p t -> p f t"), fmall[:],
            r=[f"fm{d_}" for d_ in range(13)])
        dma(p, nc, "pool", "veall", lambda i: VE[bass.ds(i * 512, 512), :].rearrange("(t p) c -> p t c", p=128),
            veall[:].rearrange("p t h d -> p t (h d)"), r=[f"ve{t}" for t in range(4)])
        p.run()
    if b.cfg.get("stop") == "A1":
        return

    with ExitStack() as st:
        def sb(name, shape, dt=F32):
            return st.enter_context(nc.sbuf_tensor(un(b, name), list(shape), dt))

        def ps(name, shape, dt=F32):
            return st.enter_context(nc.psum_tensor(un(b, name), list(shape), dt))

        wout = sb("wout", [128, 8, 1024], BF16)
        EB = sb("EB", [128, 9, 8, 128], BF16)
        skx = sb("skx", [128, 8])
        wm = sb("wm", [128, 2, 128], BF16)
        lng = sb("lng", [128, 1024])
        lnb = sb("lnb", [128, 1024])
        with ExitStack() as st2:
            stg = [st2.enter_context(nc.sbuf_tensor(un(b, "stg0"), [128, 8, 512], F32)),
                   st2.enter_context(nc.sbuf_tensor(un(b, "stg1"), [128, 8, 512], F32))]
            nam = st2.enter_context(nc.sbuf_tensor(un(b, "nam"), [128, 9, 128], F32))
            rbs = [st2.enter_context(nc.sbuf_tensor(un(b, "rbs0"), [128, 8, 128], F32)),
                   st2.enter_context(nc.sbuf_tensor(un(b, "rbs1"), [128, 8, 128], F32))]
            p = Phase(kb, f"attpre{l}")
            load_weight_bf16(b, p, wout, g["a_wout"][la].rearrange("(kc p) n -> p kc n", p=128), 1024, "wout", stg)
            dma(p, nc, "pool", "nam", nam[:], cn["namask"][:, :, :], w=["nam"])
            dma(p, nc, "pool", "wm", wm[:], cn["wmask"][:, :, :], w=["wm"])
            dma(p, nc, "pool", "skx", skx[:], g["a_sink"][la:la + 1, :].partition_broadcast(128), w=["skx"])
            dma(p, nc, "pool", "lng", lng[:], g["ln_g"][l, 0:1, :].partition_broadcast(128), w=["lng"])
            dma(p, nc, "pool", "lnb", lnb[:], g["ln_b"][l, 0:1, :].partition_broadcast(128), w=["lnb"])
            act(p, nc, skx[:], skx[:], AF.Exp, r=["skx"], w=["skx"])
            for t in range(9):
                rb = rbs[t % 2]
                dma(p, nc, "sp", f"rbs{t % 2}", rb[:], g["a_rb"][la, :, t, :, :], w=[f"rbs{t % 2}"])
                act(p, nc, rb[:], rb[:], AF.Exp, r=[f"rbs{t % 2}"], w=[f"rbs{t % 2}"])
                tt(p, nc, "dve", EB[:, t, :, :], rb[:], nam[:, t, :].unsqueeze(1).to_broadcast([128, 8, 128]), ALU.mult,
                   r=[f"rbs{t % 2}", "nam"], w=[f"EB{t}"])
            p.run()
        if b.cfg.get("stop") == "attpre":
            return

        qa = sb("qa", [128, 4, 128], BF16)
        ka = sb("ka", [128, 3, 128], BF16)
        va = sb("va", [128, 3, 2, 72], BF16)
        qb = sb("qb", [64, 8, 128], BF16)
        kbt = sb("kbt", [64, 8, 5, 128], BF16)
        vb = sb("vb", [128, 5, 8, 72], BF16)
        xt = sb("xt", [128, 1024])
        g1b = sb("g1b", [128, 1024])
        PT = [sb("PT0", [128, 5, 4, 128], BF16), sb("PT1", [128, 5, 4, 128], BF16)]
        o = sb("o", [128, 1024], BF16)
        oT = sb("oT", [128, 8, 128], BF16)
        t1 = sb("t1", [128, 1024])
        z = sb("z", [128, 1024])
        sq = sb("sq", [128, 1024])
        xo = sb("xo", [128, 1024])
        stt = sb("stt", [128, 8])
        den = sb("den", [128, 4, 1])
        pS = [ps("pS0", [128, 4, 128]), ps("pS1", [128, 4, 128])]
        accb = [ps("acc0", [128, 512]), ps("acc1", [128, 512])]
        acc = [a[:].rearrange("p (h d) -> p h d", d=128) for a in accb]
        psOT = ps("psOT", [128, 8, 128], BF16)
        psY = [ps("psY0", [128, 512]), ps("psY1", [128, 512])]
        o3 = o[:].rearrange("p (h d) -> p h d", d=64)

        def block(p, tok0, tile, wl, nl):
            def T(off):
                return (lambda i: (tok0(i) if callable(tok0) else tok0) + off)

            def TL(i):
                return tile(i) if callable(tile) else tile
            dma(p, nc, "sp", "qa", qa[:], lambda i: FM[0:4, :, bass.ds(T(0)(i), 128)].rearrange("f p t -> p f t"), w=["qa"])
            w0, w1 = wl[0], wl[-1]
            nw = w1 - w0 + 1
            dma(p, nc, "sp", "ka", ka[:, w0 + 1:w1 + 2, :].rearrange("p b t -> p (b t)"),
                lambda i: FM[4, :, bass.ds(T(w0 * 128)(i), nw * 128)], w=["ka"])
            dma(p, nc, "pool", "va", va[:, w0 + 1:w1 + 2, :, :].rearrange("p b h d -> p b (h d)"),
                lambda i: VE[bass.ds(T(w0 * 128)(i), nw * 128), 0:144].rearrange("(b p) c -> p b c", p=128), w=["va"])
            dma(p, nc, "sp", "qb", qb[:], lambda i: FM[5:9, :, bass.ds(T(0)(i), 128)].rearrange("f (two d) t -> d (f two) t", two=2), w=["qb"])
            n0 = nl[0][0]
            nn = len(nl)
            dma(p, nc, "act", "kbt", kbt[:, :, 0:nn, :].rearrange("p f b t -> p f (b t)"),
                lambda i: FM[9:13, :, bass.ds(T(n0 * 128)(i), nn * 128)].rearrange("f (two d) t -> d (f two) t", two=2), w=["kbt"])
            dma(p, nc, "pool", "vb", vb[:, 0:nn, :, :].rearrange("p b h d -> p b (h d)"),
                lambda i: VE[bass.ds(T(n0 * 128)(i), nn * 128), 144:720].rearrange("(b p) c -> p b c", p=128), w=["vb"])
            dma(p, nc, "sp", "xt", xt[:], lambda i: X[bass.ds(T(0)(i), 128), :], w=["xt"])
            dma(p, nc, "act", "g1b", g1b[:], lambda i: MODT[bass.ds(TL(i), 1), 2048:3072].partition_broadcast(128), w=["g1b"])
            lvl = b.cfg.get("a2lvl", 9)
            if lvl < 2:
                return
            u = 0
            useg = []
            for gk in range(2):
                a = acc[gk % 2]
                an = f"acc{gk % 2}"
                Pu = PT[gk % 2]
                pn = f"PT{gk % 2}_"
                useg.append([len(p.ops)])
                for di, dl in enumerate(wl):
                    k = u % 2
                    u += 1
                    mm(p, nc, pS[k][:], ka[64 * gk:64 * gk + 64, dl + 1, :], qa[64 * gk:64 * gk + 64, :, :], True, True,
                       r=["ka", "qa"], w=[f"pS{k}"])
                    act(p, nc, Pu[:, di, :, :], pS[k][:], AF.Exp, scale=0.125, r=[f"pS{k}"], w=[pn + str(di)])
                    if dl != 0:
                        tt(p, nc, "dve", Pu[:, di, :, :], Pu[:, di, :, :],
                           wm[:, 0 if dl == -1 else 1, :].unsqueeze(1).to_broadcast([128, 4, 128]),
                           ALU.mult, r=[pn + str(di)], w=[pn + str(di)])
                useg[-1].append(len(p.ops))
                for h in range(4):
                    for di, dl in enumerate(wl):
                        mm(p, nc, a[:, h, 0:66], Pu[:, di, h, :], va[:, dl + 1, gk, 0:66], di == 0, di == len(wl) - 1,
                           r=[pn + str(di), "va"], w=[an])
                tt(p, nc, "dve", den[:], a[:, :, 64:65], skx[:, 4 * gk:4 * gk + 4].unsqueeze(2), ALU.add, r=[an], w=["den"])
                p.op("dve", lambda i: nc.vector.reciprocal(den[:], den[:]), r=["den"], w=["den"])
                tt(p, nc, "dve", o3[:, 4 * gk:4 * gk + 4, :], a[:, :, 0:64], den[:].to_broadcast([128, 4, 64]), ALU.mult,
                   r=[an, "den"], w=["o"])
            if lvl < 3:
                return
            for hg in range(2):
                a = acc[hg % 2]
                an = f"acc{hg % 2}"
                Pu = PT[hg % 2]
                pn = f"PT{hg % 2}_"
                useg[-1].append(len(p.ops))
                useg.append([len(p.ops)])
                for di, (dl, tb) in enumerate(nl):
                    k = u % 2
                    u += 1
                    for uu in range(4):
                        h = 4 * hg + uu
                        mm(p, nc, pS[k][:, uu, :], kbt[:, h, di, :], qb[:, h, :],
                           True, True, r=["kbt", "qb"], w=[f"pS{k}"])
                    act(p, nc, Pu[:, di, :, :], pS[k][:], AF.Exp, scale=0.125, r=[f"pS{k}"], w=[pn + str(di)])
                    tt(p, nc, "dve" if di % 2 == 0 else "pool", Pu[:, di, :, :], Pu[:, di, :, :], EB[:, tb, 4 * hg:4 * hg + 4, :], ALU.mult,
                       r=[pn + str(di)], w=[pn + str(di)])
                useg[-1].append(len(p.ops))
                for uu in range(4):
                    for di, (dl, tb) in enumerate(nl):
                        mm(p, nc, a[:, uu, 0:66], Pu[:, di, uu, :], vb[:, di, 4 * hg + uu, 0:66], di == 0, di == len(nl) - 1,
                           r=[pn + str(di), "vb"], w=[an])
                cp(p, nc, "dve", den[:], a[:, :, 64:65], r=[an], w=["den"])
                p.op("dve", lambda i: nc.vector.reciprocal(den[:], den[:]), r=["den"], w=["den"])
                tt(p, nc, "dve", o3[:, 8 + 4 * hg:8 + 4 * hg + 4, :], a[:, :, 0:64], den[:].to_broadcast([128, 4, 64]), ALU.mult,
                   r=[an, "den"], w=["o"])
            useg[-1].append(len(p.ops))
            if len(useg) == 4 and all(len(x) == 3 for x in useg) and b.cfg.get("a2pipe", True):
                o_ = p.ops
                sem = [o_[x[0]:x[1]] for x in useg]
                pv = [o_[x[1]:x[2]] for x in useg]
                neworder = sem[0] + sem[1] + pv[0] + sem[2] + pv[1] + sem[3] + pv[2] + pv[3]
                p.ops = o_[:useg[0][0]] + neworder + o_[useg[3][2]:]
            if b.cfg.get("debug"):
                dma(p, nc, "pool", "odbg", lambda i: g["ODBG"][bass.ds(T(0)(i), 128), :], o[:], r=["o"])
            if lvl < 4:
                return
            for kc in range(8):
                tr(p, nc, psOT[:, kc, :], o[:, kc * 128:(kc + 1) * 128], identb[:], r=["o"], w=["psOT"])
            cp(p, nc, "act", oT[:], psOT[:], r=["psOT"], w=["oT"])
            for n in range(2):
                for kc in range(8):
                    mm(p, nc, psY[n][:], oT[:, kc, :], wout[:, kc, n * 512:(n + 1) * 512], kc == 0, kc == 7, r=["oT"], w=[f"psY{n}"])
            if lvl < 5:
                return
            ts(p, nc, "pool", g1b[:], g1b[:], 1.0, None, ALU.add, r=["g1b"], w=["g1b"])
            for n in range(2):
                tt(p, nc, "dve", t1[:, n * 512:(n + 1) * 512], psY[n][:], g1b[:, n * 512:(n + 1) * 512], ALU.mult,
                   r=[f"psY{n}", "g1b"], w=["t1"])
            p.op("dve", lambda i: nc.vector.scalar_tensor_tensor(z[:], xt[:], alpha, t1[:], ALU.mult, ALU.add), r=["xt", "t1"], w=["z"])
            layer_norm_tail(b, p, z, "z", lng, lnb, xo, "xo", dict(sq=sq, st=stt), "ln")
            dma(p, nc, "sp", "xo", lambda i: X[bass.ds(T(0)(i), 128), :], xo[:], r=["xo"])

        FULL = lambda dls: [(d_, d_ + 3) for d_ in dls]
        for s, T_s in enumerate(seqs):
            nb = T_s // 128
            tok_s = seq_tile0[s] * 128
            p = Phase(kb, f"A2e_{l}_{s}")
            for j in sorted(set([0, 1, nb - 2, nb - 1]))[:b.cfg.get("a2nblk", 4)]:
                wl = [d_ for d_ in (-1, 0, 1) if 0 <= j + d_ < nb]
                if j == 0:
                    nl = FULL([0, 1, 2, 3])
                elif j == 1:
                    nl = FULL([-1, 0, 1, 2])
                elif j == nb - 2:
                    nl = FULL([-2, -1, 0, 1])
                else:
                    nl = FULL([-3, -2, -1, 0])
                block(p, tok_s + j * 128, seq_tile0[s] + j, wl, nl)
            p.run()
            if b.cfg.get("stop") == "A2e":
                return
            if nb > 4:
                p = Phase(kb, f"A2i_{l}_{s}", n_iter=nb - 4)
                block(p, lambda i, tok_s=tok_s: (i + 2) * 128 + tok_s, lambda i, s=s: i + (2 + seq_tile0[s]),
                      [-1, 0, 1], [(-2, 7), (-1, 2), (0, 3), (1, 4), (2, 8)])
                p.run()


def expert_layout(w, kdim):
    L, E, K, n = w.shape
    return np.ascontiguousarray(w.reshape(L, E, kdim, 128, n).transpose(0, 1, 3, 2, 4)).reshape(L * E * 128, kdim * n)


def prepare_shared(inp, cfg):
    f = lambda k: np.ascontiguousarray(np.asarray(inp[k], dtype=np.float32))
    sh = {}
    for k in ["ada_w", "ada_b", "ln_g", "ln_b", "attn_sink", "attn_w_out", "rec_w_in", "rec_lb", "rec_gnorm", "rec_w_out"]:
        sh[k] = f(k)
    wfm, wv = attn_w_layout(f("attn_w_in"))
    sh["attn_wfm"] = wfm
    sh["attn_wv"] = wv
    sh["na_rb"] = na_bias_tables(f("nat_rel_bias"))
    L = sh["ada_w"].shape[0]
    sh["router_w"] = np.ascontiguousarray(np.concatenate([f("router_w_group"), f("router_w_expert")], axis=2))
    sh["router_b"] = np.ascontiguousarray(np.concatenate([f("router_b_group"), f("router_b_expert").reshape(L, 32)], axis=1))
    sh["e_w"] = np.concatenate([expert_layout(f("expert_w_gate"), 8), expert_layout(f("expert_w_up"), 8),
                                expert_layout(f("expert_w_down"), 4)], axis=1)
    sh.update(host_constants(cfg))
    return sh


def run_cores(inp, cfg, n_cores, pps):
    xp = np.asarray(inp["x_prompt"], dtype=np.float32)
    xs = np.asarray(inp["x_sample"], dtype=np.float32)
    cp_ = np.asarray(inp["c_prompt"], dtype=np.float32)
    cs_ = np.asarray(inp["c_sample"], dtype=np.float32)
    sh = prepare_shared(inp, cfg)
    nc = build_program(cfg)
    in_maps = []
    for c in range(n_cores):
        m = dict(sh)
        xc = np.concatenate([xp[pps * c + k] for k in range(pps)] + [xs[c]], axis=0)
        cc = np.stack([cp_[pps * c + k] for k in range(pps)] + [cs_[c]], axis=0)
        m["x"] = np.ascontiguousarray(xc)
        m["cT"] = np.ascontiguousarray(cc.reshape(3, 8, 128).transpose(2, 0, 1))
        in_maps.append(m)
    res = run_bass_kernel_spmd(nc, in_maps, core_ids=list(range(n_cores)))
    if cfg.get("debug"):
        cfg["_dbg"] = res.results
    TP = cfg["seqs"][0]
    yp = np.zeros_like(xp)
    ys = np.zeros_like(xs)
    for c in range(n_cores):
        y = np.asarray(res.results[c]["y"], dtype=np.float32)
        for k in range(pps):
            yp[pps * c + k] = y[k * TP:(k + 1) * TP]
        ys[c] = y[pps * TP:]
    return yp, ys


def kernel(**inputs):
    cfg = dict(seqs=[2048, 2048, 8192], depth=4)
    yp, ys = run_cores(inputs, cfg, 8, 2)
    return (yp, ys)


def build_moe_layer(b, l, g):
    nc, kb = b.nc, b.kb
    seqs = b.cfg["seqs"]
    NT = sum(seqs)
    NTILES = NT // 128
    NBLK = 2 * NTILES + NEXP
    X, MODT, cn = g["X"], g["MODT"], g["cn"]
    identf = g["identf"]
    alpha = g["alpha"]
    BIG = 1.0e9
    if "HF" not in b.dram:
        b.dscr("HF", [NT, D])
        b.dscr("RTD", [128, NTILES, 8])
        b.dscr("DESTD", [128, NTILES, 2], I32)
        b.dscr("IDXD", [128, NBLK], I32)
        b.dscr("XBUF", [NBLK * 128, D])
        b.dscr("YBUF", [NBLK * 128, D])
    HF, RTD, DESTD, IDXD, XBUF, YBUF = [b.dram[k] for k in ["HF", "RTD", "DESTD", "IDXD", "XBUF", "YBUF"]]

    with ExitStack() as st:
        def sb(name, shape, dt=F32):
            return st.enter_context(nc.sbuf_tensor(un(b, name), list(shape), dt))

        def ps(name, shape, dt=F32):
            return st.enter_context(nc.psum_tensor(un(b, name), list(shape), dt))

        rw = sb("rw", [128, 8, 36])
        rbb = sb("rbb", [128, 36])
        io32 = sb("io32", [128, 32])
        trib = sb("trib", [128, 128], BF16)
        oneb = sb("oneb", [128, 128], BF16)
        carry = sb("carry", [128, 32])
        zt = sb("zt", [128, 1024])
        rt = sb("rt", [128, 8])
        p = Phase(kb, f"moepre{l}")
        p.op("pool", lambda i: nc.gpsimd.memset(rt[:], 0.0), w=["rt"])
        dma(p, nc, "sp", "m0", rw[:], g["ro_w"][l].rearrange("(kc p) n -> p kc n", p=128), w=["rw"])
        dma(p, nc, "sp", "m1", rbb[:], g["ro_b"][l:l + 1, :].partition_broadcast(128), w=["rbb"])
        dma(p, nc, "sp", "m2", io32[:], cn["iota32"][:, :], w=["io32"])
        dma(p, nc, "sp", "m3", trib[:], cn["tri_su"][:, :], w=["trib"])
        dma(p, nc, "sp", "m4", oneb[:], cn["ones_b"][:, :], w=["oneb"])
        p.op("pool", lambda i: nc.gpsimd.memset(carry[:], 0.0), w=["carry"])
        p.op("pool", lambda i: nc.gpsimd.memset(zt[:], 0.0), w=["zt"])
        p.run()
        p = Phase(kb, f"moez{l}", n_iter=NBLK)
        dma(p, nc, "sp", "zf", lambda i: XBUF[bass.ds(i * 128, 128), :], zt[:])
        p.run()

        from types import SimpleNamespace
        sets = []
        for k in range(2):
            T = SimpleNamespace(xt=sb(f"xt{k}", [128, 1024]), scb=sb(f"scb{k}", [128, 1024]), shb=sb(f"shb{k}", [128, 1024]),
                                hf=sb(f"hf{k}", [128, 1024]), hfT=sb(f"hfT{k}", [128, 8, 128]), lg=sb(f"lg{k}", [128, 36]),
                                sm=sb(f"sm{k}", [128, 16]), ge=sb(f"ge{k}", [128, 4]), gmask=sb(f"gmask{k}", [128, 4]),
                                lem=sb(f"lem{k}", [128, 4, 8]), lem2=sb(f"lem2{k}", [128, 32]), oh1=sb(f"oh1{k}", [128, 32]),
                                oh2=sb(f"oh2{k}", [128, 32]), ohb=sb(f"ohb{k}", [128, 32], BF16), tmp32=sb(f"tmp32{k}", [128, 32]),
                                tot=sb(f"tot{k}", [128, 32]), rt=sb(f"rtt{k}", [128, 8]))
            T.lemf = T.lem[:].rearrange("p a b -> p (a b)")
            sets.append(T)
        psAs = [[ps(f"psA0_{k}", [128, 4, 128]), ps(f"psA1_{k}", [128, 4, 128])] for k in range(2)]
        pms = [ps(f"pm{k}", [128, 512]) for k in range(2)]
        p = Phase(kb, f"moeRz{l}")
        for k in range(2):
            p.op("pool", lambda i, k=k: nc.gpsimd.memset(sets[k].rt[:], 0.0), w=[f"rtz{k}"])
        p.run()

        p = Phase(kb, f"moeR{l}", n_iter=NTILES // 2)
        for k in range(2):
            p.ns = f"#{k}"
            T = sets[k]
            psA = psAs[k]
            pl = pms[k][:, 0:64]
            pr = pms[k][:, 64:128]
            pc = pms[k][:, 128:192]
            dma(p, nc, "sp", "xt", T.xt[:], lambda i, k=k, T=T: X[bass.ds(i * 256 + k * 128, 128), :], w=["xt"])
            dma(p, nc, "act", "scb", T.scb[:], lambda i, k=k, T=T: MODT[bass.ds(i * 2 + k, 1), 4096:5120].partition_broadcast(128), w=["scb"])
            dma(p, nc, "act", "shb", T.shb[:], lambda i, k=k, T=T: MODT[bass.ds(i * 2 + k, 1), 3072:4096].partition_broadcast(128), w=["shb"])
            ts(p, nc, "pool", T.scb[:], T.scb[:], 1.0, None, ALU.add, r=["scb"], w=["scb"])
            tt(p, nc, "dve", T.hf[:], T.xt[:], T.scb[:], ALU.mult, r=["xt", "scb"], w=["hf"])
            tt(p, nc, "pool", T.hf[:], T.hf[:], T.shb[:], ALU.add, r=["hf", "shb"], w=["hf"])
            dma(p, nc, "sp", "hfo", lambda i, k=k, T=T: HF[bass.ds(i * 256 + k * 128, 128), :], T.hf[:], r=["hf"])
            for kc in range(8):
                tr(p, nc, psA[kc // 4][:, kc % 4, :], T.hf[:, kc * 128:(kc + 1) * 128], identf[:], r=["hf"], w=[f"psA{kc // 4}"])
            cp(p, nc, "act", T.hfT[:, 0:4, :], psA[0][:], r=["psA0"], w=["hfT0"])
            cp(p, nc, "dve", T.hfT[:, 4:8, :], psA[1][:], r=["psA1"], w=["hfT1"])
            for kc in range(8):
                mm(p, nc, pl[:, 0:36], T.hfT[:, kc, :], rw[:, kc, :], kc == 0, kc == 7, r=["hfT0", "hfT1"], w=["pm"])
            tt(p, nc, "dve", T.lg[:], pl[:, 0:36], rbb[:], ALU.add, r=["pm"], w=["lg"])
            p.op("dve", lambda i, k=k, T=T: nc.vector.tensor_reduce(T.sm[:, 0:1], T.lg[:, 0:4], AX.X, ALU.max), r=["lg"], w=["sm0"])
            ts(p, nc, "dve", T.sm[:, 1:2], T.sm[:, 0:1], -1.0, None, ALU.mult, r=["sm0"], w=["sm1"])
            act(p, nc, T.ge[:], T.lg[:, 0:4], AF.Exp, bias=T.sm[:, 1:2], r=["lg", "sm1"], w=["ge"])
            p.op("dve", lambda i, k=k, T=T: nc.vector.tensor_reduce(T.sm[:, 2:3], T.ge[:], AX.X, ALU.add), r=["ge"], w=["sm2"])
            p.op("dve", lambda i, k=k, T=T: nc.vector.reciprocal(T.sm[:, 3:4], T.sm[:, 2:3]), r=["sm2"], w=["sm3"])
            tt(p, nc, "dve", T.gmask[:], T.lg[:, 0:4], T.sm[:, 0:1].to_broadcast([128, 4]), ALU.is_ge, r=["lg", "sm0"], w=["gmask"])
            ts(p, nc, "dve", T.gmask[:], T.gmask[:], -1.0, BIG, ALU.add, ALU.mult, r=["gmask"], w=["gmask"])
            tt(p, nc, "dve", T.lem[:], T.lg[:, 4:36].rearrange("p (a b) -> p a b", b=8), T.gmask[:].unsqueeze(2).to_broadcast([128, 4, 8]),
               ALU.add, r=["lg", "gmask"], w=["lem"])
            p.op("dve", lambda i, k=k, T=T: nc.vector.tensor_reduce(T.sm[:, 4:5], T.lemf, AX.X, ALU.max), r=["lem"], w=["sm4"])
            tt(p, nc, "dve", T.oh1[:], T.lemf, T.sm[:, 4:5].to_broadcast([128, 32]), ALU.is_ge, r=["lem", "sm4"], w=["oh1"])
            p.op("dve", lambda i, k=k, T=T: nc.vector.scalar_tensor_tensor(T.lem2[:], T.oh1[:], -BIG, T.lemf, ALU.mult, ALU.add), r=["oh1", "lem"], w=["lem2"])
            p.op("dve", lambda i, k=k, T=T: nc.vector.tensor_reduce(T.sm[:, 5:6], T.lem2[:], AX.X, ALU.max), r=["lem2"], w=["sm5"])
            tt(p, nc, "dve", T.oh2[:], T.lem2[:], T.sm[:, 5:6].to_broadcast([128, 32]), ALU.is_ge, r=["lem2", "sm5"], w=["oh2"])
            tt(p, nc, "dve", T.sm[:, 6:7], T.sm[:, 5:6], T.sm[:, 4:5], ALU.subtract, r=["sm4", "sm5"], w=["sm6"])
            act(p, nc, T.sm[:, 7:8], T.sm[:, 6:7], AF.Exp, r=["sm6"], w=["sm7"])
            ts(p, nc, "dve", T.sm[:, 8:9], T.sm[:, 7:8], 1.0, None, ALU.add, r=["sm7"], w=["sm8"])
            p.op("dve", lambda i, k=k, T=T: nc.vector.reciprocal(T.sm[:, 8:9], T.sm[:, 8:9]), r=["sm8"], w=["sm8"])
            tt(p, nc, "dve", T.rt[:, 2:3], T.sm[:, 8:9], T.sm[:, 3:4], ALU.mult, r=["sm8", "sm3"], w=["rt2"])
            tt(p, nc, "dve", T.rt[:, 3:4], T.rt[:, 2:3], T.sm[:, 7:8], ALU.mult, r=["rt2", "sm7"], w=["rt3"])
            tt(p, nc, "dve", T.tmp32[:], T.oh1[:], io32[:], ALU.mult, r=["oh1"], w=["tmp32"])
            p.op("dve", lambda i, k=k, T=T: nc.vector.tensor_reduce(T.rt[:, 0:1], T.tmp32[:], AX.X, ALU.add), r=["tmp32"], w=["rt0"])
            tt(p, nc, "dve", T.tmp32[:], T.oh2[:], io32[:], ALU.mult, r=["oh2", "rt0"], w=["tmp32"])
            p.op("dve", lambda i, k=k, T=T: nc.vector.tensor_reduce(T.rt[:, 1:2], T.tmp32[:], AX.X, ALU.add), r=["tmp32"], w=["rt1"])
            tt(p, nc, "pool", T.ohb[:], T.oh1[:], T.oh2[:], ALU.add, r=["oh1", "oh2"], w=["ohb"])
            mm(p, nc, pr[:, 0:32], trib[:], T.ohb[:], True, True, r=["ohb"], w=["pm"])
            mm(p, nc, pc[:, 0:32], oneb[:], T.ohb[:], True, True, r=["ohb"], w=["pm"])
            tt(p, nc, "dve", T.tot[:], pr[:, 0:32], carry[:], ALU.add, r=["pm", "!carry"], w=["tot"])
            if k == 1:
                tt(p, nc, "dve", T.tot[:], T.tot[:], pms[0][:, 128:160], ALU.add, r=["tot", "!pm#0"], w=["tot"])
            tt(p, nc, "dve", T.tmp32[:], T.oh1[:], T.tot[:], ALU.mult, r=["oh1", "tot", "rt1"], w=["tmp32"])
            p.op("dve", lambda i, k=k, T=T: nc.vector.tensor_reduce(T.rt[:, 4:5], T.tmp32[:], AX.X, ALU.add), r=["tmp32"], w=["rt4"])
            tt(p, nc, "dve", T.tmp32[:], T.oh2[:], T.tot[:], ALU.mult, r=["oh2", "tot", "rt4"], w=["tmp32"])
            p.op("dve", lambda i, k=k, T=T: nc.vector.tensor_reduce(T.rt[:, 5:6], T.tmp32[:], AX.X, ALU.add), r=["tmp32"], w=["rt5"])
            if k == 1:
                tt(p, nc, "dve", carry[:], carry[:], pms[0][:, 128:160], ALU.add, r=["!carry", "!pm#0", "!tot#0", "tot"], w=["!carry"])
                tt(p, nc, "dve", carry[:], carry[:], pc[:, 0:32], ALU.add, r=["!carry", "pm", "tot"], w=["!carry"])
            dma(p, nc, "act", "rto", lambda i, k=k, T=T: RTD[:, bass.ds(i * 2 + k, 1), :].rearrange("p o c -> p (o c)"), T.rt[:],
                r=["rt0", "rt1", "rt2", "rt3", "rt4", "rt5"])
            if k == 0:
                n_half = len(p.ops)
        p.ns = ""
        p.interleave(0, n_half, len(p.ops))
        p.run()

        rta = sb("rta", [128, NTILES, 8])
        cnt = sb("cnt", [128, 32])
        cnti = sb("cnti", [128, 32], I32)
        pend = sb("pend", [128, 32])
        pst = sb("pst", [128, 32])
        onesf = sb("onesf", [128, 32])
        dacc = sb("dacc", [128, NTILES, 2])
        dtmp = sb("dtmp", [128, NTILES, 2])
        dint = sb("dint", [128, NTILES, 2], I32)
        ioblk = sb("ioblk", [128, NBLK])
        bacc = sb("bacc", [128, NBLK])
        btmp = sb("btmp", [128, NBLK])
        iop = sb("iop", [128, 1])
        bint = sb("bint", [128, NBLK], I32)
        p = Phase(kb, f"moeD{l}")
        dma(p, nc, "sp", "d0", rta[:], RTD[:, :, :], w=["rta"])
        dma(p, nc, "sp", "d1", ioblk[:], cn["iota_blk"][:, 0:NBLK], w=["ioblk"])
        dma(p, nc, "sp", "d2", iop[:], cn["iota_p"][:, :], w=["iop"])
        p.op("pool", lambda i: nc.gpsimd.memset(onesf[:], 1.0), w=["onesf"])
        ts(p, nc, "dve", cnt[:], carry[:], 1.0 / 128.0, 0.49609375, ALU.mult, ALU.add, w=["cnt"])
        cp(p, nc, "dve", cnti[:], cnt[:], r=["cnt"], w=["cnti"])
        cp(p, nc, "dve", cnt[:], cnti[:], r=["cnti"], w=["cnt"])
        ts(p, nc, "dve", cnt[:], cnt[:], 128.0, None, ALU.mult, r=["cnt"], w=["cnt"])
        p.op("dve", lambda i: nc.vector.tensor_tensor_scan(pend[:], onesf[:], cnt[:], 0.0, ALU.mult, ALU.add), r=["onesf", "cnt"], w=["pend"])
        tt(p, nc, "dve", pst[:], pend[:], cnt[:], ALU.subtract, r=["pend", "cnt"], w=["pst"])
        cp(p, nc, "dve", dacc[:], rta[:, :, 4:6], r=["rta"], w=["dacc"])
        for e in range(NEXP):
            ts(p, nc, "dve", dtmp[:], rta[:, :, 0:2], float(e), None, ALU.is_equal, r=["rta", "dacc"], w=["dtmp"])
            p.op("dve", lambda i, e=e: nc.vector.scalar_tensor_tensor(dacc[:], dtmp[:], pst[:, e:e + 1], dacc[:], ALU.mult, ALU.add),
                 r=["dtmp", "pst", "dacc"], w=["dacc"])
        cp(p, nc, "dve", dint[:], dacc[:], r=["dacc"], w=["dint"])
        dma(p, nc, "sp", "d3", DESTD[:, :, :], dint[:], r=["dint"])
        p.op("pool", lambda i: nc.gpsimd.memset(bacc[:], 0.0), w=["bacc"])
        for e in range(NEXP):
            ts(p, nc, "dve", btmp[:], ioblk[:], pend[:, e:e + 1], None, ALU.is_ge, r=["ioblk", "pend", "bacc"], w=["btmp"])
            tt(p, nc, "dve", bacc[:], bacc[:], btmp[:], ALU.add, r=["bacc", "btmp"], w=["bacc"])
        ts(p, nc, "dve", bacc[:], bacc[:], float(NEXP - 1), None, ALU.min, r=["bacc"], w=["bacc"])
        OOBV = float(b.cfg["depth"] * NEXP * 128)
        p.op("pool", lambda i: nc.gpsimd.memset(btmp[:, 0:1], 1.0), r=["btmp"], w=["btmp"])
        p.op("pool", lambda i: nc.gpsimd.memset(btmp[:, 1:2], 1.0), r=["btmp"], w=["btmp"])
        tt(p, nc, "dve", btmp[:, 2:NBLK], bacc[:, 2:NBLK], bacc[:, 0:NBLK - 2], ALU.not_equal, r=["bacc", "btmp"], w=["btmp"])
        ts(p, nc, "dve", bacc[:], bacc[:], 128.0, float(l * NEXP * 128), ALU.mult, ALU.add, r=["bacc", "btmp"], w=["bacc"])
        tt(p, nc, "dve", bacc[:], bacc[:], iop[:].to_broadcast([128, NBLK]), ALU.add, r=["bacc", "iop"], w=["bacc"])
        if b.cfg.get("oobskip", True):
            ts(p, nc, "dve", bacc[:], bacc[:], -OOBV, None, ALU.add, r=["bacc"], w=["bacc"])
            tt(p, nc, "dve", bacc[:], bacc[:], btmp[:], ALU.mult, r=["bacc", "btmp"], w=["bacc"])
            ts(p, nc, "dve", bacc[:], bacc[:], OOBV, None, ALU.add, r=["bacc"], w=["bacc"])
        cp(p, nc, "dve", bint[:], bacc[:], r=["bacc"], w=["bint"])
        dma(p, nc, "sp", "d4", IDXD[:, :], bint[:], r=["bint"])
        p.run()
    if b.cfg.get("stop") == "moeD":
        return

    with ExitStack() as st:
        def sb(name, shape, dt=F32):
            return st.enter_context(nc.sbuf_tensor(un(b, name), list(shape), dt))
        hf = sb("hf", [128, 1024])
        di = sb("di", [128, 2], I32)
        p = Phase(kb, f"moeS{l}", n_iter=NTILES)
        dma(p, nc, "sp", "hf", hf[:], lambda i: HF[bass.ds(i * 128, 128), :], w=["hf"])
        dma(p, nc, "act", "di", di[:], lambda i: DESTD[:, bass.ds(i, 1), :].rearrange("p o c -> p (o c)"), w=["di"])
        for k in range(2):
            p.dma("pool", f"sc{k}", lambda i, k=k: nc.gpsimd.indirect_dma_start(
                out=XBUF[:, :], out_offset=bass.IndirectOffsetOnAxis(ap=di[:, k:k + 1], axis=0), in_=hf[:], in_offset=None),
                r=["hf", "di"])
        p.run()
    if b.cfg.get("stop") == "moeS":
        return

    NROWS = b.cfg["depth"] * NEXP * 128
    with ExitStack() as st:
        def sb(name, shape, dt=F32):
            return st.enter_context(nc.sbuf_tensor(un(b, name), list(shape), dt))

        def ps(name, shape, dt=F32):
            return st.enter_context(nc.psum_tensor(un(b, name), list(shape), dt))
        wfs = [sb("wf0", [128, 12288]), sb("wf1", [128, 12288])]
        psA = [ps("psA0", [128, 4, 128]), ps("psA1", [128, 4, 128])]
        psG = ps("psG", [128, 4, 128])
        psU = ps("psU", [128, 4, 128])
        psY = [ps("psY0", [128, 512]), ps("psY1", [128, 512])]
        sets = []
        for k in range(2):
            sets.append(dict(ix=sb(f"ix{k}", [128, 1], I32), wgb=sb(f"wgb{k}", [128, 8, 512], BF16), wub=sb(f"wub{k}", [128, 8, 512], BF16),
                             wdb=sb(f"wdb{k}", [128, 4, 1024], BF16), xb=sb(f"xb{k}", [128, 1024]), xT=sb(f"xT{k}", [128, 8, 128], BF16),
                             sg=sb(f"sg{k}", [128, 4, 128]), hT=sb(f"hT{k}", [128, 4, 128], BF16), yb=sb(f"yb{k}", [128, 1024])))
        p = Phase(kb, f"moeB{l}", n_iter=NBLK // 2)
        segs = []
        for k in range(2):
            p.ns = f"#{k}"
            T = sets[k]
            segs.append(len(p.ops))
            ix, wgb, wub, wdb, xb, xT, sg, hT, yb = [T[n_] for n_ in ["ix", "wgb", "wub", "wdb", "xb", "xT", "sg", "hT", "yb"]]
            dma(p, nc, "sp", "ix", ix[:], lambda i, k=k: IDXD[:, bass.ds(i * 2 + k, 1)], w=["ix"], slow=True)
            dma(p, nc, "act", "xb", xb[:], lambda i, k=k: XBUF[bass.ds(i * 256 + k * 128, 128), :], w=["xb"])
            wf = wfs[k]
            p.dma("pool", "wf", lambda i, ix=ix, wf=wf: nc.gpsimd.indirect_dma_start(
                out=wf[:], out_offset=None, in_=g["e_w"][:, :], in_offset=bass.IndirectOffsetOnAxis(ap=ix[:, 0:1], axis=0),
                bounds_check=NROWS - 1, oob_is_err=False),
                r=["ix"], w=["wf"])
            segs.append(len(p.ops))
            cp(p, nc, "dve", wgb[:].rearrange("p k n -> p (k n)"), wf[:, 0:4096], r=["wf"], w=["wgb"])
            cp(p, nc, "act", wub[:].rearrange("p k n -> p (k n)"), wf[:, 4096:8192], r=["wf"], w=["wub"])
            cp(p, nc, "dve", wdb[:, 0:2, :].rearrange("p k n -> p (k n)"), wf[:, 8192:10240], r=["wf"], w=["wdb0"])
            cp(p, nc, "act", wdb[:, 2:4, :].rearrange("p k n -> p (k n)"), wf[:, 10240:12288], r=["wf"], w=["wdb1"])
            segs.append(len(p.ops))
            for kc in range(8):
                tr(p, nc, psA[kc // 4][:, kc % 4, :], xb[:, kc * 128:(kc + 1) * 128], identf[:], r=["xb"], w=[f"!psA{kc // 4}"])
            cp(p, nc, "act", xT[:, 0:4, :], psA[0][:], r=["!psA0"], w=["xT0"])
            cp(p, nc, "dve", xT[:, 4:8, :], psA[1][:], r=["!psA1"], w=["xT1"])
            for c in range(4):
                for kc in range(8):
                    mm(p, nc, psG[:, c, :], wgb[:, kc, c * 128:(c + 1) * 128], xT[:, kc, :], kc == 0, kc == 7, r=["wgb", "xT0", "xT1"], w=["!psG"])
            for c in range(4):
                for kc in range(8):
                    mm(p, nc, psU[:, c, :], wub[:, kc, c * 128:(c + 1) * 128], xT[:, kc, :], kc == 0, kc == 7, r=["wub", "xT0", "xT1"], w=["!psU"])
            act(p, nc, sg[:], psG[:], AF.Silu, r=["!psG"], w=["sg"])
            tt(p, nc, "dve", hT[:], sg[:], psU[:], ALU.mult, r=["sg", "!psU"], w=["hT"])
            for n in range(2):
                for c in range(4):
                    mm(p, nc, psY[n][:], hT[:, c, :], wdb[:, c, n * 512:(n + 1) * 512], c == 0, c == 3, r=["hT", "wdb0", "wdb1"], w=[f"!psY{n}"])
            cp(p, nc, "act", yb[:, 0:512], psY[0][:], r=["!psY0"], w=["yb0"])
            cp(p, nc, "dve", yb[:, 512:1024], psY[1][:], r=["!psY1"], w=["yb1"])
            dma(p, nc, "sp", "yb", lambda i, k=k: YBUF[bass.ds(i * 256 + k * 128, 128), :], yb[:], r=["yb0", "yb1"])
        p.ns = ""
        a0, a1, a2, b0, b1, b2 = segs
        o_ = p.ops
        p.ops = o_[a0:a1] + o_[b0:b1] + o_[a1:a2] + o_[b1:b2] + o_[a2:b0] + o_[b2:]
        p.run()
    if b.cfg.get("stop") == "moeB":
        return

    with ExitStack() as st:
        def sb(name, shape, dt=F32):
            return st.enter_context(nc.sbuf_tensor(un(b, name), list(shape), dt))
        lng = sb("lng", [128, 1024])
        lnb = sb("lnb", [128, 1024])
        p = Phase(kb, f"moeCpre{l}")
        dma(p, nc, "sp", "c0", lng[:], g["ln_g"][l, 1:2, :].partition_broadcast(128), w=["lng"])
        dma(p, nc, "sp", "c1", lnb[:], g["ln_b"][l, 1:2, :].partition_broadcast(128), w=["lnb"])
        p.run()
        sets = []
        for k in range(2):
            sets.append(dict(xt=sb(f"xt{k}", [128, 1024]), g2b=sb(f"g2b{k}", [128, 1024]), rt=sb(f"rt{k}", [128, 8]),
                             di=sb(f"di{k}", [128, 2], I32), y0=sb(f"y0{k}", [128, 1024]), y1=sb(f"y1{k}", [128, 1024]),
                             z=sb(f"z{k}", [128, 1024]), sq=sb(f"sq{k}", [128, 1024]), xo=sb(f"xo{k}", [128, 1024]),
                             stt=sb(f"stt{k}", [128, 8])))
        p = Phase(kb, f"moeC{l}", n_iter=NTILES // 2)
        for k in range(2):
            p.ns = f"#{k}"
            T = sets[k]
            xt, g2b, rt, di, y0, y1, z, sq, xo, stt = [T[n_] for n_ in ["xt", "g2b", "rt", "di", "y0", "y1", "z", "sq", "xo", "stt"]]
            dma(p, nc, "sp", "xt", xt[:], lambda i, k=k: X[bass.ds(i * 256 + k * 128, 128), :], w=["xt"])
            dma(p, nc, "act", "g2b", g2b[:], lambda i, k=k: MODT[bass.ds(i * 2 + k, 1), 5120:6144].partition_broadcast(128), w=["g2b"])
            dma(p, nc, "act", "rt", rt[:], lambda i, k=k: RTD[:, bass.ds(i * 2 + k, 1), :].rearrange("p o c -> p (o c)"), w=["rt"])
            dma(p, nc, "sp", "di", di[:], lambda i, k=k: DESTD[:, bass.ds(i * 2 + k, 1), :].rearrange("p o c -> p (o c)"), w=["di"])
            for kk, yk in enumerate([y0, y1]):
                p.dma("pool", f"ga{kk}", lambda i, kk=kk, yk=yk, di=di: nc.gpsimd.indirect_dma_start(
                    out=yk[:], out_offset=None, in_=YBUF[:, :], in_offset=bass.IndirectOffsetOnAxis(ap=di[:, kk:kk + 1], axis=0)),
                    r=["di"], w=[f"y{kk}"])
            ts(p, nc, "dve", y0[:], y0[:], rt[:, 2:3], None, ALU.mult, r=["y0", "rt"], w=["y0"])
            p.op("dve", lambda i, y0=y0, y1=y1, rt=rt: nc.vector.scalar_tensor_tensor(y0[:], y1[:], rt[:, 3:4], y0[:], ALU.mult, ALU.add),
                 r=["y0", "y1", "rt"], w=["y0"])
            ts(p, nc, "pool", g2b[:], g2b[:], 1.0, None, ALU.add, r=["g2b"], w=["g2b"])
            tt(p, nc, "pool", y0[:], y0[:], g2b[:], ALU.mult, r=["y0", "g2b"], w=["y0"])
            p.op("dve", lambda i, z=z, xt=xt, y0=y0: nc.vector.scalar_tensor_tensor(z[:], xt[:], alpha, y0[:], ALU.mult, ALU.add),
                 r=["xt", "y0"], w=["z"])
            layer_norm_tail(b, p, z, "z", lng, lnb, xo, "xo", dict(sq=sq, st=stt), "ln")
            dma(p, nc, "sp", "xo", lambda i, k=k: X[bass.ds(i * 256 + k * 128, 128), :], xo[:], r=["xo"])
            if k == 0:
                n_half = len(p.ops)
        p.ns = ""
        p.interleave(0, n_half, len(p.ops))
        p.run()


def build_hgrn_layer(b, l, lr, g):
    nc, kb = b.nc, b.kb
    seqs = b.cfg["seqs"]
    NT = sum(seqs)
    NTILES = NT // 128
    depth = b.cfg["depth"]
    LR = depth // 2
    X, MODT, cn = g["X"], g["MODT"], g["cn"]
    identb = g["identb"]
    alpha = g["alpha"]
    seq_tile0 = g["seq_tile0"]
    if "QZ" not in b.dram:
        b.dscr("QZ", [24, 128, NT])
        b.dscr("VT", [NT, D], BF16)
        b.dscr("GT", [NT, D])
        b.dscr("OF", [NT, D])
        b.dscr("OB", [NT, D])
    QZ, VT, GT, OF, OB = [b.dram[k] for k in ["QZ", "VT", "GT", "OF", "OB"]]

    with ExitStack() as st:
        def sb(name, shape, dt=F32):
            return st.enter_context(nc.sbuf_tensor(un(b, name), list(shape), dt))

        def ps(name, shape, dt=F32):
            return st.enter_context(nc.psum_tensor(un(b, name), list(shape), dt))

        win = sb("win", [128, 8, 5120], BF16)
        with ExitStack() as st2:
            stg = [st2.enter_context(nc.sbuf_tensor(un(b, "stg0"), [128, 8, 512], F32)),
                   st2.enter_context(nc.sbuf_tensor(un(b, "stg1"), [128, 8, 512], F32))]
            p = Phase(kb, f"recw{l}")
            load_weight_bf16(b, p, win, g["r_win"][lr].rearrange("(kc p) n -> p kc n", p=128), 5120, "win", stg)
            p.run()
        xg = sb("xg", [128, 4, 1024])
        scb = sb("scb", [128, 1024])
        shb = sb("shb", [128, 1024])
        tmp = [sb("tmp0", [128, 1024]), sb("tmp1", [128, 1024])]
        hb = sb("hb", [128, 4, 1024], BF16)
        hT = sb("hT", [128, 8, 512], BF16)
        fq = sb("fq", [128, 8, 512])
        vo = sb("vo", [128, 4, 1024], BF16)
        go = sb("go", [128, 4, 1024])
        psT = [ps("psT0", [128, 8, 128], BF16), ps("psT1", [128, 8, 128], BF16)]
        pf = [ps(f"pf{k}", [128, 512]) for k in range(4)]
        NG = NT // 512
        p = Phase(kb, f"H1_{l}", n_iter=NG)
        dma(p, nc, "sp", "xg", xg[:], lambda i: X[bass.ds(i * 512, 512), :].rearrange("(t p) d -> p t d", p=128), w=["xg"])
        dma(p, nc, "act", "scb", scb[:], lambda i: MODT[bass.ds(i * 4, 1), 1024:2048].partition_broadcast(128), w=["scb"])
        dma(p, nc, "act", "shb", shb[:], lambda i: MODT[bass.ds(i * 4, 1), 0:1024].partition_broadcast(128), w=["shb"])
        ts(p, nc, "dve", scb[:], scb[:], 1.0, None, ALU.add, r=["scb"], w=["scb"])
        for t in range(4):
            tt(p, nc, "dve", tmp[t % 2][:], xg[:, t, :], scb[:], ALU.mult, r=["xg", "scb"], w=[f"tmp{t % 2}"])
            tt(p, nc, "pool", hb[:, t, :], tmp[t % 2][:], shb[:], ALU.add, r=[f"tmp{t % 2}", "shb"], w=[f"hb{t}"])
            for kc in range(8):
                tr(p, nc, psT[t % 2][:, kc, :], hb[:, t, kc * 128:(kc + 1) * 128], identb[:], r=[f"hb{t}"], w=[f"psT{t % 2}"])
            cp(p, nc, "act", hT[:, :, t * 128:(t + 1) * 128], psT[t % 2][:], r=[f"psT{t % 2}"], w=[f"hT{t}"])
        hTall = [f"hT{t}" for t in range(4)]
        pfi = 0
        for grp in range(3):
            for hh in range(8):
                c = grp * 8 + hh
                ia = pfi % 4
                pfi += 1
                for kc in range(8):
                    mm(p, nc, pf[ia][:], win[:, kc, c * 128:(c + 1) * 128], hT[:, kc, :], kc == 0, kc == 7, r=hTall, w=[f"pf{ia}"])
                cp(p, nc, "act" if hh % 2 == 0 else "dve", fq[:, hh, :], pf[ia][:], r=[f"pf{ia}"], w=[f"fq{hh}"])
            dma(p, nc, ["sp", "act", "pool"][grp], f"fq{grp}",
                lambda i, grp=grp: QZ[grp * 8:grp * 8 + 8, :, bass.ds(i * 512, 512)].rearrange("f p t -> p f t"), fq[:],
                r=[f"fq{hh}" for hh in range(8)])
        for t in range(4):
            for n in range(4):
                ia = pfi % 4
                pfi += 1
                col0 = 3072 + n * 512
                for kc in range(8):
                    mm(p, nc, pf[ia][:], hT[:, kc, t * 128:(t + 1) * 128], win[:, kc, col0:col0 + 512], kc == 0, kc == 7,
                       r=[f"hT{t}"], w=[f"pf{ia}"])
                if n < 2:
                    cp(p, nc, "act", vo[:, t, n * 512:(n + 1) * 512], pf[ia][:], r=[f"pf{ia}"], w=[f"vo{t}"])
                else:
                    cp(p, nc, "dve", go[:, t, (n - 2) * 512:(n - 1) * 512], pf[ia][:], r=[f"pf{ia}"], w=[f"go{t}"])
        dma(p, nc, "sp", "vo", lambda i: VT[bass.ds(i * 512, 512), :].rearrange("(t p) e -> p t e", p=128), vo[:],
            r=[f"vo{t}" for t in range(4)])
        dma(p, nc, "pool", "go", lambda i: GT[bass.ds(i * 512, 512), :].rearrange("(t p) e -> p t e", p=128), go[:],
            r=[f"go{t}" for t in range(4)])
        p.run()
    if b.cfg.get("stop") == "H1":
        return

    with ExitStack() as st:
        def sb(name, shape, dt=F32):
            return st.enter_context(nc.sbuf_tensor(un(b, name), list(shape), dt))

        def ps(name, shape, dt=F32):
            return st.enter_context(nc.psum_tensor(un(b, name), list(shape), dt))

        lbT = sb("lbT", [128, 8])
        omlT = sb("omlT", [128, 8])
        lbe = sb("lbe", [128, max(LR, 1), 8])
        lsum = sb("lsum", [128, 8])
        hm = sb("hm", [64, 2, 64], BF16)
        smask = sb("smask", [128, 1024])
        S = sb("S", [128, 8, 128])
        p = Phase(kb, f"recpre{l}")
        for j in range(LR):
            dma(p, nc, "sp", f"lb{j}", lbe[:, j, :], g["r_lb"][j].rearrange("(h k) -> k h", k=128), w=[f"lbe{j}"], slow=True)
            act(p, nc, lbe[:, j, :], lbe[:, j, :], AF.Exp, r=[f"lbe{j}"], w=[f"lbe{j}"])
        cp(p, nc, "dve", lsum[:], lbe[:, 0, :], r=["lbe0"], w=["lsum"])
        for j in range(1, LR):
            tt(p, nc, "dve", lsum[:], lsum[:], lbe[:, j, :], ALU.add, r=["lsum", f"lbe{j}"], w=["lsum"])
        p.op("dve", lambda i: nc.vector.reciprocal(lsum[:], lsum[:]), r=["lsum"], w=["lsum"])
        p.op("pool", lambda i: nc.gpsimd.memset(lbT[:], 0.0), w=["lbT"])
        for j in range(1, lr + 1):
            tt(p, nc, "dve", lbT[:], lbT[:], lbe[:, j, :], ALU.add, r=["lbT", f"lbe{j}"], w=["lbT"])
        tt(p, nc, "dve", lbT[:], lbT[:], lsum[:], ALU.mult, r=["lbT", "lsum"], w=["lbT"])
        ts(p, nc, "dve", omlT[:], lbT[:], -1.0, 1.0, ALU.mult, ALU.add, r=["lbT"], w=["omlT"])
        dma(p, nc, "sp", "hm", hm[:], cn["hmask"][:, :, :], w=["hm"])
        dma(p, nc, "sp", "smask", smask[:], cn["scanmask"][:, :], w=["smask"])
        p.run()

        from types import SimpleNamespace
        sets = []
        for k in range(2):
            T = SimpleNamespace(qt=sb(f"qt{k}", [128, 8, 128]), zt=sb(f"zt{k}", [128, 8, 128]), vt=sb(f"vt{k}", [64, 2, 1024], BF16),
                                u=sb(f"u{k}", [128, 8, 128]), w_=sb(f"w_{k}", [128, 8, 128]), f_=sb(f"f_{k}", [128, 8, 128]),
                                lf=sb(f"lf{k}", [128, 8, 128]), km=sb(f"km{k}", [128, 8, 128]), bb=sb(f"bb{k}", [128, 8, 128]),
                                bm=sb(f"bm{k}", [128, 8, 128]), Ee=sb(f"Ee{k}", [128, 8, 128]), Ei=sb(f"Ei{k}", [128, 8, 128]),
                                qe=sb(f"qe{k}", [128, 8, 128], BF16), ke=sb(f"ke{k}", [128, 8, 128], BF16), sc=sb(f"sc{k}", [128, 8, 2, 4]),
                                Sc=sb(f"Sc{k}", [128, 8, 128], BF16), AT=sb(f"AT{k}", [64, 8, 64], BF16), keT=sb(f"keT{k}", [64, 8, 128], BF16),
                                oc=sb(f"oc{k}", [64, 1024]))
            sets.append(T)
        psA = ps("psA", [64, 8, 64])
        psKT2 = [ps("psKT0", [64, 8, 128], BF16), ps("psKT1", [64, 8, 128], BF16)]
        pso = [ps("pso0", [64, 512]), ps("pso1", [64, 512])]
        psdS = [ps("psdS0", [128, 4, 128]), ps("psdS1", [128, 4, 128])]
        lb_bc = lbT[:].unsqueeze(2).to_broadcast([128, 8, 128])
        oml_bc = omlT[:].unsqueeze(2).to_broadcast([128, 8, 128])
        fl = lambda t_: t_[:].rearrange("p h t -> p (h t)")
        c4 = lambda t_: t_[:].rearrange("p h (c t) -> p h c t", t=64)

        def rec_body(p, tok0, dirn, OUT, T, marks):
            zbase = 8 if dirn == 0 else 16
            dma(p, nc, "sp", "qt", T.qt[:], lambda i: QZ[0:8, :, bass.ds(tok0(i), 128)].rearrange("f p t -> p f t"), w=["qt"])
            dma(p, nc, "act", "zt", T.zt[:], lambda i: QZ[zbase:zbase + 8, :, bass.ds(tok0(i), 128)].rearrange("f p t -> p f t"), w=["zt"])
            dma(p, nc, "pool", "vt", T.vt[:], lambda i: VT[bass.ds(tok0(i), 128), :].rearrange("(c p) e -> p c e", p=64), w=["vt"])
            act(p, nc, T.u[:], T.zt[:], AF.Exp, scale=-1.0, r=["zt"], w=["u"])
            ts(p, nc, "dve", T.w_[:], T.u[:], 1.0, None, ALU.add, r=["u"], w=["w_"])
            p.op("dve", lambda i: nc.vector.reciprocal(T.w_[:], T.w_[:]), r=["w_"], w=["w_"])
            tt(p, nc, "dve", T.f_[:], T.w_[:], oml_bc, ALU.mult, r=["w_"], w=["f_"])
            tt(p, nc, "dve", T.f_[:], T.f_[:], lb_bc, ALU.add, r=["f_"], w=["f_"])
            act(p, nc, T.lf[:], T.f_[:], AF.Ln, r=["f_"], w=["lf"])
            tt(p, nc, "pool", T.km[:], T.u[:], T.w_[:], ALU.mult, r=["u", "w_"], w=["km"])
            tt(p, nc, "pool", T.km[:], T.km[:], oml_bc, ALU.mult, r=["km"], w=["km"])
            p.op("dve", lambda i: nc.vector.tensor_tensor_scan(fl(T.bb), smask[:], fl(T.lf), 0.0, ALU.mult, ALU.add), r=["lf"], w=["bb"])
            if dirn == 1:
                tt(p, nc, "pool", T.bm[:], T.lf[:], T.bb[:], ALU.subtract, r=["lf", "bb"], w=["bm"])
                tt(p, nc, "pool", c4(T.bb), c4(T.bm), c4(T.bb)[:, :, :, 63:64].to_broadcast([128, 8, 2, 64]), ALU.add, r=["bm", "bb"], w=["bb"])
            endidx = 63 if dirn == 0 else 0
            cref = c4(T.bb)[:, :, :, 31:32]
            bend = c4(T.bb)[:, :, :, endidx:endidx + 1]
            act(p, nc, T.sc[:, :, :, 0:1], cref, AF.Exp, r=["bb"], w=["sc0"])
            act(p, nc, T.sc[:, :, :, 1:2], bend, AF.Exp, r=["bb"], w=["sc1"])
            tt(p, nc, "dve", T.sc[:, :, :, 3:4], bend, cref, ALU.subtract, r=["bb"], w=["sc3"])
            act(p, nc, T.sc[:, :, :, 2:3], T.sc[:, :, :, 3:4], AF.Exp, r=["sc3"], w=["sc2"])
            tt(p, nc, "pool", c4(T.bm), c4(T.bb), cref.to_broadcast([128, 8, 2, 64]), ALU.subtract, r=["bb", "bm"], w=["bm"])
            act(p, nc, T.Ee[:], T.bm[:], AF.Exp, r=["bm"], w=["Ee"])
            act(p, nc, T.Ei[:], T.bm[:], AF.Exp, scale=-1.0, r=["bm"], w=["Ei"])
            tt(p, nc, "dve", T.qe[:], T.qt[:], T.Ee[:], ALU.mult, r=["qt", "Ee"], w=["qe"])
            tt(p, nc, "pool", T.ke[:], T.km[:], T.Ei[:], ALU.mult, r=["km", "Ei"], w=["ke"])
            marks.append(len(p.ops))
            order = [0, 1] if dirn == 0 else [1, 0]
            for c in order:
                cs_ = slice(c * 64, (c + 1) * 64)
                for h in range(8):
                    p.op("act", lambda i, h=h, c=c, T=T: nc.scalar.activation(T.Sc[:, h, :], S[:, h, :], AF.Copy, scale=T.sc[:, h, c, 0:1]),
                         r=[f"!S{h}", "sc0"], w=[f"Sc{h}"])
                for h in range(8):
                    mm(p, nc, psA[:, h, :], T.ke[:, h, cs_], T.qe[:, h, cs_], True, True, r=["ke", "qe"], w=["!psA"])
                for h in range(8):
                    tt(p, nc, "dve", T.AT[:, h, :], psA[:, h, :], hm[:, dirn, :], ALU.mult, r=["!psA"], w=[f"AT{h}"])
                for h in range(8):
                    tr(p, nc, psKT2[h // 4][:, h % 4, :], T.ke[:, h, cs_], identb[:], r=["ke"], w=[f"!psKT{h // 4}"])
                for h in range(8):
                    cp(p, nc, "dve" if h < 4 else "act", T.keT[:, h, :], psKT2[h // 4][:, h % 4, :], r=[f"!psKT{h // 4}"], w=[f"keT{h}"])
                for h in range(8):
                    po = pso[h // 4][:, (h % 4) * 128:(h % 4 + 1) * 128]
                    mm(p, nc, po, T.AT[:, h, :], T.vt[:, c, h * 128:(h + 1) * 128], True, False, r=[f"AT{h}", "vt"], w=[f"!pso{h // 4}"])
                    mm(p, nc, po, T.qe[:, h, cs_], T.Sc[:, h, :], False, True, r=["qe", f"Sc{h}"], w=[f"!pso{h // 4}"])
                for h in range(8):
                    mm(p, nc, psdS[h // 4][:, h % 4, :], T.keT[:, h, :], T.vt[:, c, h * 128:(h + 1) * 128], True, True,
                       r=[f"keT{h}", "vt"], w=[f"!psdS{h // 4}"])
                for h in range(8):
                    ts(p, nc, "pool", S[:, h, :], S[:, h, :], T.sc[:, h, c, 1:2], None, ALU.mult, r=[f"!S{h}", "sc1"], w=[f"!S{h}"])
                for h in range(8):
                    p.op("dve", lambda i, h=h, c=c, T=T: nc.vector.scalar_tensor_tensor(S[:, h, :], psdS[h // 4][:, h % 4, :], T.sc[:, h, c, 2:3],
                                                                                         S[:, h, :], ALU.mult, ALU.add),
                         r=[f"!psdS{h // 4}", f"!S{h}", "sc2"], w=[f"!S{h}"])
                cp(p, nc, "act", T.oc[:, 0:512], pso[0][:], r=["!pso0"], w=["oc0"])
                cp(p, nc, "dve", T.oc[:, 512:1024], pso[1][:], r=["!pso1"], w=["oc1"])
                dma(p, nc, "sp", f"oc", lambda i, c=c: OUT[bass.ds(tok0(i) + c * 64, 64), :], T.oc[:], r=["oc0", "oc1"])

        for s, T_s in enumerate(seqs):
            nb = T_s // 128
            tok_s = seq_tile0[s] * 128
            for dirn in range(2):
                p = Phase(kb, f"recz{l}_{s}_{dirn}")
                p.op("pool", lambda i: nc.gpsimd.memset(S[:], 0.0), w=["S"])
                p.run()
                p = Phase(kb, f"H2_{l}_{s}_{dirn}", n_iter=nb // 2)
                marks = []
                for k in range(2):
                    p.ns = f"#{k}"
                    marks.append(len(p.ops))
                    if dirn == 0:
                        rec_body(p, lambda i, tok_s=tok_s, k=k: i * 256 + (tok_s + k * 128), 0, OF, sets[k], marks)
                    else:
                        rec_body(p, lambda i, tok_s=tok_s, nb=nb, k=k: (tok_s + (nb - 1 - k) * 128) - i * 256, 1, OB, sets[k], marks)
                p.ns = ""
                a0, a1, b0, b1 = marks
                o_ = p.ops
                prepA, chA, prepB, chB = o_[a0:a1], o_[a1:b0], o_[b0:b1], o_[b1:]
                z_ = []
                for q_ in range(max(len(prepA), len(prepB))):
                    if q_ < len(prepA):
                        z_.append(prepA[q_])
                    if q_ < len(prepB):
                        z_.append(prepB[q_])
                p.ops = z_ + chA + chB
                p.run()
    if b.cfg.get("stop") == "H2":
        return

    with ExitStack() as st:
        def sb(name, shape, dt=F32):
            return st.enter_context(nc.sbuf_tensor(un(b, name), list(shape), dt))

        def ps(name, shape, dt=F32):
            return st.enter_context(nc.psum_tensor(un(b, name), list(shape), dt))
        wout = sb("wout", [128, 8, 1024], BF16)
        gnb = sb("gnb", [128, 1024])
        lng = sb("lng", [128, 1024])
        lnb = sb("lnb", [128, 1024])
        with ExitStack() as st2:
            stg = [st2.enter_context(nc.sbuf_tensor(un(b, "stg0"), [128, 8, 512], F32)),
                   st2.enter_context(nc.sbuf_tensor(un(b, "stg1"), [128, 8, 512], F32))]
            p = Phase(kb, f"recw2{l}")
            load_weight_bf16(b, p, wout, g["r_wout"][lr].rearrange("(kc p) n -> p kc n", p=128), 1024, "wout", stg)
            dma(p, nc, "pool", "gnb", gnb[:], g["r_gn"][lr:lr + 1, :].partition_broadcast(128), w=["gnb"])
            dma(p, nc, "pool", "lng", lng[:], g["ln_g"][l, 0:1, :].partition_broadcast(128), w=["lng"])
            dma(p, nc, "pool", "lnb", lnb[:], g["ln_b"][l, 0:1, :].partition_broadcast(128), w=["lnb"])
            p.run()
        from types import SimpleNamespace
        sets = []
        for k in range(2):
            T = SimpleNamespace(of=sb(f"of{k}", [128, 1024]), ob=sb(f"ob{k}", [128, 1024]), gt=sb(f"gt{k}", [128, 1024]),
                                xt=sb(f"xt{k}", [128, 1024]), g1b=sb(f"g1b{k}", [128, 1024]), sq=sb(f"sq{k}", [128, 1024]),
                                ms=sb(f"ms{k}", [128, 8]), on=sb(f"on{k}", [128, 1024], BF16), onT=sb(f"onT{k}", [128, 8, 128], BF16),
                                t1=sb(f"t1{k}", [128, 1024]), z=sb(f"z{k}", [128, 1024]), xo=sb(f"xo{k}", [128, 1024]),
                                stt=sb(f"stt{k}", [128, 8]), psOT=ps(f"psOT{k}", [128, 8, 128], BF16),
                                psY=[ps(f"psY0{k}", [128, 512]), ps(f"psY1{k}", [128, 512])])
            sets.append(T)
        p = Phase(kb, f"H3_{l}", n_iter=NTILES // 2)
        for k in range(2):
            p.ns = f"#{k}"
            T = sets[k]
            dma(p, nc, "sp", "of", T.of[:], lambda i, k=k, T=T: OF[bass.ds(i * 256 + k * 128, 128), :], w=["of"])
            dma(p, nc, "act", "ob", T.ob[:], lambda i, k=k, T=T: OB[bass.ds(i * 256 + k * 128, 128), :], w=["ob"])
            dma(p, nc, "pool", "gt", T.gt[:], lambda i, k=k, T=T: GT[bass.ds(i * 256 + k * 128, 128), :], w=["gt"])
            dma(p, nc, "sp", "xt", T.xt[:], lambda i, k=k, T=T: X[bass.ds(i * 256 + k * 128, 128), :], w=["xt"])
            dma(p, nc, "act", "g1b", T.g1b[:], lambda i, k=k, T=T: MODT[bass.ds(i * 2 + k, 1), 2048:3072].partition_broadcast(128), w=["g1b"])
            tt(p, nc, "dve", T.of[:], T.of[:], T.ob[:], ALU.add, r=["of", "ob"], w=["of"])
            tt(p, nc, "pool", T.sq[:], T.of[:], T.of[:], ALU.mult, r=["of"], w=["sq"])
            p.op("dve", lambda i, k=k, T=T: nc.vector.tensor_reduce(T.ms[:], T.sq[:].rearrange("p (h v) -> p h v", v=128), AX.X, ALU.add), r=["sq"], w=["ms"])
            ts(p, nc, "dve", T.ms[:], T.ms[:], 1.0 / 128.0, None, ALU.mult, r=["ms"], w=["ms"])
            act(p, nc, T.ms[:], T.ms[:], AF.Ln, bias=b.eps_ln[:, 1:2], r=["ms"], w=["ms"])
            act(p, nc, T.ms[:], T.ms[:], AF.Exp, scale=-0.5, r=["ms"], w=["ms"])
            tt(p, nc, "dve", T.sq[:].rearrange("p (h v) -> p h v", v=128), T.of[:].rearrange("p (h v) -> p h v", v=128),
               T.ms[:].unsqueeze(2).to_broadcast([128, 8, 128]), ALU.mult, r=["of", "ms", "sq"], w=["sq"])
            tt(p, nc, "pool", T.sq[:], T.sq[:], gnb[:], ALU.mult, r=["sq"], w=["sq"])
            act(p, nc, T.gt[:], T.gt[:], AF.Silu, r=["gt"], w=["gt"])
            tt(p, nc, "dve", T.on[:], T.sq[:], T.gt[:], ALU.mult, r=["sq", "gt"], w=["on"])
            for kc in range(8):
                tr(p, nc, T.psOT[:, kc, :], T.on[:, kc * 128:(kc + 1) * 128], identb[:], r=["on"], w=["psOT"])
            cp(p, nc, "act", T.onT[:], T.psOT[:], r=["psOT"], w=["onT"])
            for n in range(2):
                for kc in range(8):
                    mm(p, nc, T.psY[n][:], T.onT[:, kc, :], wout[:, kc, n * 512:(n + 1) * 512], kc == 0, kc == 7, r=["onT"], w=[f"psY{n}"])
            ts(p, nc, "pool", T.g1b[:], T.g1b[:], 1.0, None, ALU.add, r=["g1b"], w=["g1b"])
            for n in range(2):
                tt(p, nc, "dve", T.t1[:, n * 512:(n + 1) * 512], T.psY[n][:], T.g1b[:, n * 512:(n + 1) * 512], ALU.mult,
                   r=[f"psY{n}", "g1b"], w=["t1"])
            p.op("dve", lambda i, k=k, T=T: nc.vector.scalar_tensor_tensor(T.z[:], T.xt[:], alpha, T.t1[:], ALU.mult, ALU.add), r=["xt", "t1"], w=["z"])
            layer_norm_tail(b, p, T.z, "z", lng, lnb, T.xo, "xo", dict(sq=T.sq, st=T.stt), "ln")
            dma(p, nc, "sp", "xo", lambda i, k=k, T=T: X[bass.ds(i * 256 + k * 128, 128), :], T.xo[:], r=["xo"])
            if k == 0:
                n_half = len(p.ops)
        p.ns = ""
        p.interleave(0, n_half, len(p.ops))
        p.run()
```

```python
import math
from collections import Counter
from contextlib import ExitStack
import numpy as np
import ml_dtypes
import concourse.bass as bass
import concourse.mybir as mybir
from concourse.bass_utils import run_bass_kernel_spmd

F32 = mybir.dt.float32
BF16 = mybir.dt.bfloat16
I32 = mybir.dt.int32
AF = mybir.ActivationFunctionType
ALU = mybir.AluOpType
AX = mybir.AxisListType

D = 1024
HD = 64
N_GROUPS = 4
EPG = 8
NEXP = 32
DEXP = 512
LN_EPS = 1e-5
RMS_EPS = 1e-6
ROPE_THETA = 10000.0


class KB:
    def __init__(self, nc):
        self.nc = nc
        self.engs = {"pe": nc.tensor, "act": nc.scalar, "dve": nc.vector, "pool": nc.gpsimd, "sp": nc.sync}
        self.sems = {}
        self.base = {}
        for e in self.engs:
            self.sems[("e", e)] = nc.alloc_semaphore(name="S_" + e)
            self.base[("e", e)] = 0
        self.chan_owner = {}
        self.nphase = 0

    def sig(self, key):
        if key not in self.sems:
            self.sems[key] = self.nc.alloc_semaphore(name="C_" + str(key[1]))
            self.base[key] = 0
        return self.sems[key]


class Phase:
    def __init__(self, kb, name, n_iter=None):
        self.kb = kb
        self.name = name
        self.n_iter = n_iter
        self.ops = []

    ns = ""

    def _n(self, name):
        return name[1:] if name.startswith("!") else name + self.ns

    def op(self, eng, fn, r=(), w=(), chan=None):
        self.ops.append(dict(eng=eng, fn=fn, r=tuple(self._n(x) for x in r), w=tuple(self._n(x) for x in w),
                             chan=(chan + self.ns) if chan else None))

    def dma(self, eng, chan, fn, r=(), w=()):
        self.op(eng, fn, r, w, chan=chan)

    def interleave(self, a, b_, c):
        A, Bq = self.ops[a:b_], self.ops[b_:c]
        out = []
        for k in range(max(len(A), len(Bq))):
            if k < len(A):
                out.append(A[k])
            if k < len(Bq):
                out.append(Bq[k])
        self.ops[a:c] = out

    def run(self):
        kb = self.kb
        nc = kb.nc
        ops = self.ops
        m = len(ops)
        if m == 0:
            return
        loop = self.n_iter is not None
        N = self.n_iter if loop else 1
        sig_of = [("c", o["chan"]) if o["chan"] else ("e", o["eng"]) for o in ops]
        for j, o in enumerate(ops):
            if o["chan"]:
                own = kb.chan_owner.setdefault(o["chan"], o["eng"])
                assert own == o["eng"], f"chan {o['chan']} issued from two engines"
        phys = {}
        kcnt = {"sw": 0, "hw": 0}
        for j, o in enumerate(ops):
            sg_ = sig_of[j]
            if sg_ in phys:
                continue
            if sg_[0] == "e":
                phys[sg_] = sg_
            else:
                kind = "sw" if o["eng"] == "pool" else "hw"
                phys[sg_] = ("c", f"{kind}{kcnt[kind]}")
                kcnt[kind] += 1
        per_iter = Counter(sig_of)
        kidx = []
        cnt = Counter()
        for j in range(m):
            kidx.append(cnt[sig_of[j]])
            cnt[sig_of[j]] += 1
        last_w = {}
        readers = {}
        deps = [set() for _ in range(m)]
        for it in ((0, 1) if loop else (0,)):
            for j, o in enumerate(ops):
                d = set()
                for b in o["r"]:
                    if b in last_w:
                        d.add(last_w[b] + ("raw",))
                for b in o["w"]:
                    if b in last_w:
                        d.add(last_w[b] + ("waw",))
                    for rd in readers.get(b, []):
                        d.add(rd + ("war",))
                if it == (1 if loop else 0):
                    deps[j] = {(jj, itt - it, ty) for (itt, jj, ty) in d if (itt, jj) != (it, j)}
                for b in o["r"]:
                    readers.setdefault(b, []).append((it, j))
                for b in o["w"]:
                    last_w[b] = (it, j)
                    readers[b] = []
        waits = []
        for j, o in enumerate(ops):
            need = {}
            for (jj, off, ty) in deps[j]:
                p = ops[jj]
                s = sig_of[jj]
                if s[0] == "e" and sig_of[j][0] == "e" and p["eng"] == o["eng"]:
                    if o["eng"] == "pe" or ty != "raw":
                        continue
                n_s = per_iter[s]
                if s[0] == "c":
                    if off == 0:
                        q = sum(1 for x in range(j) if sig_of[x] == s)
                        const = q * 16
                    else:
                        const = 0
                else:
                    const = (kidx[jj] + 1) if off == 0 else (kidx[jj] + 1 - n_s)
                if s not in need or const > need[s]:
                    need[s] = const
            waits.append(need)
        known = {}
        for j, o in enumerate(ops):
            kn = known.setdefault(o["eng"], {})
            for s in list(waits[j]):
                if s in kn and kn[s] >= waits[j][s]:
                    del waits[j][s]
                else:
                    kn[s] = waits[j][s]
        T0 = {}
        for s, n_s in per_iter.items():
            kb.sig(phys[s])
            inc = 16 if s[0] == "c" else 1
            T0[s] = kb.base[phys[s]] + (n_s * inc if (loop and s[0] == "e") else 0)
        engines_used = []
        for o in ops:
            if o["eng"] not in engines_used:
                engines_used.append(o["eng"])

        def free_ids(E):
            hs, ids = [], []
            while True:
                try:
                    kb.probe_cnt = getattr(kb, "probe_cnt", 0) + 1
                    h = E.alloc_register(f"probe{kb.probe_cnt}")
                except Exception:
                    break
                hs.append(h)
                ids.append(nc.lookup_reg(h).reg_id)
            for h in hs:
                E.free_register(h)
            return set(ids)

        def emit_engine(ename):
            E = kb.engs[ename]
            before = free_ids(E)
            emit_engine_inner(ename)
            leaked = sorted(before - free_ids(E))
            for rid in leaked:
                kb.probe_cnt += 1
                h = nc.add_register(E.engine, f"leak{kb.probe_cnt}", rid)
                E.free_register(h)

        def emit_engine_inner(ename):
            E = kb.engs[ename]
            mine = [j for j, o in enumerate(ops) if o["eng"] == ename]
            wsigs = []
            for j in mine:
                for s in waits[j]:
                    if s not in wsigs:
                        wsigs.append(s)

            def body(i, regs, tmp):
                for j in mine:
                    o = ops[j]
                    for s, const in waits[j].items():
                        if i is None:
                            E.wait_ge(kb.sig(phys[s]), T0[s] + const)
                        elif const == 0:
                            E.wait_ge(kb.sig(phys[s]), regs[s])
                        else:
                            E.reg_add(tmp, regs[s], const)
                            E.wait_ge(kb.sig(phys[s]), tmp)
                    ins = o["fn"](i)
                    s = sig_of[j]
                    ins.then_inc(kb.sig(phys[s]), 16 if s[0] == "c" else 1)

            if loop:
                if ("e", ename) in per_iter:
                    E.sem_inc(kb.sig(("e", ename)), per_iter[("e", ename)])
                regs = {}
                for k, s in enumerate(wsigs):
                    regs[s] = E.alloc_register(f"w{kb.nphase}_{ename}_{k}")
                    E.reg_mov(regs[s], T0[s])
                tmp = E.alloc_register(f"wt{kb.nphase}_{ename}")
                with E.Fori(0, N) as i:
                    body(i, regs, tmp)
                    for s in wsigs:
                        E.reg_add(regs[s], regs[s], per_iter[s] * (16 if s[0] == "c" else 1))
                for s in wsigs:
                    E.free_register(regs[s])
                E.free_register(tmp)
            else:
                body(None, None, None)
            for s, n_s in per_iter.items():
                owner = kb.chan_owner[s[1]] if s[0] == "c" else s[1]
                if owner == ename:
                    inc = 16 if s[0] == "c" else 1
                    E.wait_ge(kb.sig(phys[s]), T0[s] + N * n_s * inc)

        with nc.Block() as blk:
            reg = {"pe": blk.tensor, "act": blk.scalar, "dve": blk.vector, "pool": blk.gpsimd, "sp": blk.sync}
            for ename in engines_used:
                reg[ename](lambda eng, ename=ename: emit_engine(ename))
        for s, n_s in per_iter.items():
            inc = 16 if s[0] == "c" else 1
            kb.base[phys[s]] = T0[s] + N * n_s * inc
        nc.all_engine_barrier()
        kb.nphase += 1


def _bf(a):
    return np.ascontiguousarray(a).astype(ml_dtypes.bfloat16)


def host_constants(cfg):
    seqs = cfg["seqs"]
    NT = sum(seqs)
    c = {}
    c["ident_f"] = np.eye(128, dtype=np.float32)
    c["ident_b"] = _bf(np.eye(128, dtype=np.float32))
    pos = np.concatenate([np.arange(t) for t in seqs]).astype(np.float32)
    inv = np.power(ROPE_THETA, -np.arange(0, HD, 2, dtype=np.float32) / HD).astype(np.float32)
    d = np.arange(128) % 64
    ang = pos[None, :] * inv[d % 32][:, None]
    c["rope_cos"] = np.cos(ang).astype(np.float32)
    sgn = np.where(d < 32, -1.0, 1.0).astype(np.float32)
    c["rope_sin"] = (np.sin(ang) * sgn[:, None]).astype(np.float32)
    j = np.arange(128)[:, None]
    i = np.arange(128)[None, :]
    c["wmask"] = _bf(np.stack([(j >= i), (j <= i)], axis=1).astype(np.float32))
    kc = np.arange(64)
    qc = np.arange(64)
    qwin = np.clip(qc - 8, 0, 48)
    col_ok = (kc[:, None] >= qwin[None, :]) & (kc[:, None] < qwin[None, :] + 16)
    m = np.zeros((128, 9, 128), np.float32)
    for t in range(9):
        for b in range(2):
            for a in range(2):
                ok = col_ok.astype(np.float32)
                if t == 7 and (a == 1 and b == 0):
                    ok = ok * 0
                if t == 8 and not (a == 1 and b == 0):
                    ok = ok * 0
                m[b * 64:(b + 1) * 64, t, a * 64:(a + 1) * 64] = ok
    c["namask"] = m
    s = np.arange(64)[:, None]
    t = np.arange(64)[None, :]
    c["hmask"] = _bf(np.stack([(s <= t), (s >= t)], axis=1).astype(np.float32))
    sm = np.ones((128, 8, 128), np.float32)
    sm[:, :, 0] = 0
    sm[:, :, 64] = 0
    c["scanmask"] = sm.reshape(128, 1024)
    c["iota32"] = np.tile(np.arange(32, dtype=np.float32)[None, :], (128, 1))
    c["iota_blk"] = np.tile((np.arange(256, dtype=np.float32) * 128.0)[None, :], (128, 1))
    c["iota_p"] = np.arange(128, dtype=np.float32).reshape(128, 1)
    c["tri_su"] = _bf((np.arange(128)[:, None] < np.arange(128)[None, :]).astype(np.float32))
    c["ones_b"] = _bf(np.ones((128, 128), np.float32))
    return c


NA_TABLE_DELTA = [-3, -2, -1, 0, 1, 2, 3, -2, 2]


def na_bias_tables(rel_bias):
    L = rel_bias.shape[0]
    kc = np.arange(64)[:, None]
    qc = np.arange(64)[None, :]
    dc = np.clip(kc - qc, -15, 15) + 15
    out = np.zeros((L, 128, 9, 8, 128), np.float32)
    for t, dl in enumerate(NA_TABLE_DELTA):
        for b in range(2):
            for a in range(2):
                dr = 2 * dl + b - a + 7
                g = rel_bias[:, :, dr, :][:, :, dc]
                out[:, b * 64:(b + 1) * 64, t, :, a * 64:(a + 1) * 64] = g.transpose(0, 2, 1, 3)
    return out


def attn_w_layout(w_in):
    L = w_in.shape[0]
    qa0, ka0, va0, qb0, kb0, vb0 = 0, 512, 640, 768, 1280, 1792
    sw = (np.arange(64) + 32) % 64
    cols = []
    for jc in range(4):
        for h in (jc, 4 + jc):
            cols += list(qa0 + h * 64 + np.arange(64))
    for jc in range(4):
        for h in (jc, 4 + jc):
            cols += list(qa0 + h * 64 + sw)
    for h in range(2):
        cols += list(ka0 + h * 64 + np.arange(64))
    for h in range(2):
        cols += list(ka0 + h * 64 + sw)
    cols += list(qb0 + np.arange(512))
    cols += list(kb0 + np.arange(512))
    cols = np.array(cols)
    vcols = np.concatenate([va0 + np.arange(128), vb0 + np.arange(512)])
    return np.ascontiguousarray(w_in[:, :, cols]), np.ascontiguousarray(w_in[:, :, vcols])


class B:
    def __init__(self, cfg):
        self.cfg = cfg
        self.nc = bass.Bass("TRN2", target_bir_lowering=False)
        self.kb = KB(self.nc)
        self.dram = {}
        self.uid = 0

    def din(self, name, shape, dt=F32):
        t = self.nc.dram_tensor(name, list(shape), dt, kind="ExternalInput")
        self.dram[name] = t
        return t

    def dout(self, name, shape, dt=F32):
        t = self.nc.dram_tensor(name, list(shape), dt, kind="ExternalOutput")
        self.dram[name] = t
        return t

    def dscr(self, name, shape, dt=F32):
        if self.cfg.get("debug"):
            return self.dout(name, shape, dt)
        t = self.nc.dram_tensor(name, list(shape), dt)
        self.dram[name] = t
        return t


def un(b, name):
    b.uid += 1
    return f"{name}_{b.uid}"


def _ap(x, i):
    return x(i) if callable(x) else x


def dma(p, nc, eng, chan, out, in_, r=(), w=(), slow=False):
    E = {"sp": nc.sync, "act": nc.scalar, "pool": nc.gpsimd}[eng]
    if slow:
        p.dma(eng, chan, lambda i: E.dma_start(out=_ap(out, i), in_=_ap(in_, i), allow_slow_non_contiguous=True), r=r, w=w)
    else:
        p.dma(eng, chan, lambda i: E.dma_start(out=_ap(out, i), in_=_ap(in_, i)), r=r, w=w)


def eng_of(nc, e):
    return {"dve": nc.vector, "pool": nc.gpsimd, "act": nc.scalar}[e]


def tt(p, nc, e, out, in0, in1, op, r=(), w=()):
    E = eng_of(nc, e)
    p.op(e, lambda i: E.tensor_tensor(_ap(out, i), _ap(in0, i), _ap(in1, i), op), r=r, w=w)


def ts(p, nc, e, out, in0, s1, s2, op0, op1=None, r=(), w=()):
    E = eng_of(nc, e)
    if op1 is None:
        p.op(e, lambda i: E.tensor_scalar(_ap(out, i), _ap(in0, i), s1, None, op0), r=r, w=w)
    else:
        p.op(e, lambda i: E.tensor_scalar(_ap(out, i), _ap(in0, i), s1, s2, op0, op1), r=r, w=w)


def cp(p, nc, e, out, in_, r=(), w=()):
    if e == "act":
        p.op(e, lambda i: nc.scalar.copy(_ap(out, i), _ap(in_, i)), r=r, w=w)
    else:
        E = eng_of(nc, e)
        p.op(e, lambda i: E.tensor_copy(_ap(out, i), _ap(in_, i)), r=r, w=w)


def act(p, nc, out, in_, func, bias=None, scale=None, r=(), w=()):
    kw = {}
    if bias is not None:
        kw["bias"] = bias
    if scale is not None:
        kw["scale"] = scale
    p.op("act", lambda i: nc.scalar.activation(_ap(out, i), _ap(in_, i), func, **kw), r=r, w=w)


def mm(p, nc, out, lhsT, rhs, start, stop, r=(), w=()):
    p.op("pe", lambda i: nc.tensor.matmul(_ap(out, i), lhsT=_ap(lhsT, i), rhs=_ap(rhs, i), start=start, stop=stop), r=r, w=w)


def tr(p, nc, out, in_, ident, r=(), w=()):
    p.op("pe", lambda i: nc.tensor.transpose(_ap(out, i), _ap(in_, i), ident), r=r, w=w)


def load_weight_bf16(b, p, dst, src, ncols, tag, stg, kdim=8):
    nc = b.nc
    engs = ["dve", "act", "pool"]
    c0 = 0
    k = 0
    while c0 < ncols:
        cw = min(512, ncols - c0)
        st = stg[k % 2]
        sname = f"stg{k % 2}"
        dma(p, nc, "sp" if k % 2 == 0 else "act", f"wstg{k % 2}", st[:, :, 0:cw], src[:, :, c0:c0 + cw], w=[sname])
        cp(p, nc, engs[k % 3], dst[:, :, c0:c0 + cw], st[:, :, 0:cw], r=[sname], w=[tag + str(k)])
        c0 += cw
        k += 1


def layer_norm_tail(b, p, z, zname, lng, lnb, out, outname, scr, tg):
    nc = b.nc
    sq, st = scr["sq"], scr["st"]
    p.op("dve", lambda i: nc.vector.tensor_reduce(st[:, 0:1], z[:], AX.X, ALU.add), r=[zname], w=[tg + "s0"])
    tt(p, nc, "pool", sq[:], z[:], z[:], ALU.mult, r=[zname], w=[tg + "sq"])
    p.op("dve", lambda i: nc.vector.tensor_reduce(st[:, 1:2], sq[:], AX.X, ALU.add), r=[tg + "sq"], w=[tg + "s1"])
    ts(p, nc, "dve", st[:, 2:3], st[:, 0:1], 1.0 / D, None, ALU.mult, r=[tg + "s0"], w=[tg + "s2"])
    ts(p, nc, "dve", st[:, 3:4], st[:, 1:2], 1.0 / D, None, ALU.mult, r=[tg + "s1"], w=[tg + "s3"])
    tt(p, nc, "dve", st[:, 4:5], st[:, 2:3], st[:, 2:3], ALU.mult, r=[tg + "s2"], w=[tg + "s4"])
    tt(p, nc, "dve", st[:, 5:6], st[:, 3:4], st[:, 4:5], ALU.subtract, r=[tg + "s3", tg + "s4"], w=[tg + "s5"])
    act(p, nc, st[:, 6:7], st[:, 5:6], AF.Ln, bias=b.eps_ln[:, 0:1], r=[tg + "s5"], w=[tg + "s6"])
    act(p, nc, st[:, 6:7], st[:, 6:7], AF.Exp, scale=-0.5, r=[tg + "s6"], w=[tg + "s6"])
    p.op("dve", lambda i: nc.vector.scalar_tensor_tensor(st[:, 7:8], st[:, 2:3], -1.0, st[:, 6:7], ALU.mult, ALU.mult),
         r=[tg + "s2", tg + "s6"], w=[tg + "s7"])
    act(p, nc, sq[:], z[:], AF.Identity, bias=st[:, 7:8], scale=st[:, 6:7], r=[zname, tg + "s6", tg + "s7", tg + "sq"], w=[tg + "sq"])
    tt(p, nc, "dve", sq[:], sq[:], lng[:], ALU.mult, r=[tg + "sq"], w=[tg + "sq"])
    tt(p, nc, "pool", out[:], sq[:], lnb[:], ALU.add, r=[tg + "sq"], w=[outname])


def build_program(cfg):
    seqs = cfg["seqs"]
    NT = sum(seqs)
    NTILES = NT // 128
    depth = cfg["depth"]
    LA = (depth + 1) // 2
    LR = depth // 2
    stop = cfg.get("stop")
    alpha = float((2 * cfg.get("dn_depth", depth)) ** 0.25)
    b = B(cfg)
    nc = b.nc
    kb = b.kb

    x_in = b.din("x", [NT, D])
    cT_in = b.din("cT", [128, 3, 8])
    ada_w = b.din("ada_w", [depth, D, 6 * D])
    ada_b = b.din("ada_b", [depth, 6 * D])
    ln_g = b.din("ln_g", [depth, 2, D])
    ln_b = b.din("ln_b", [depth, 2, D])
    a_wfm = b.din("attn_wfm", [LA, D, 2304])
    a_wv = b.din("attn_wv", [LA, D, 640])
    a_sink = b.din("attn_sink", [LA, 8])
    a_rb = b.din("na_rb", [LA, 128, 9, 8, 128])
    a_wout = b.din("attn_w_out", [LA, D, D])
    r_win = b.din("rec_w_in", [max(LR, 1), D, 5 * D])
    r_lb = b.din("rec_lb", [max(LR, 1), D])
    r_gn = b.din("rec_gnorm", [max(LR, 1), D])
    r_wout = b.din("rec_w_out", [max(LR, 1), D, D])
    ro_w = b.din("router_w", [depth, D, 36])
    ro_b = b.din("router_b", [depth, 36])
    e_w = b.din("e_w", [depth * NEXP * 128, 12288])
    cn = {}
    for name, shape, dt in [("ident_f", [128, 128], F32), ("ident_b", [128, 128], BF16), ("rope_cos", [128, NT], F32),
                            ("rope_sin", [128, NT], F32), ("wmask", [128, 2, 128], BF16), ("namask", [128, 9, 128], F32),
                            ("hmask", [64, 2, 64], BF16), ("scanmask", [128, 1024], F32), ("iota32", [128, 32], F32),
                            ("iota_blk", [128, 256], F32), ("iota_p", [128, 1], F32), ("tri_su", [128, 128], BF16),
                            ("ones_b", [128, 128], BF16)]:
        cn[name] = b.din(name, shape, dt)
    y_out = b.dout("y", [NT, D])
    X = b.dscr("X", [NT, D])
    MODT = b.dscr("MODT", [NTILES, 6 * D])
    FM = b.dscr("FM", [13, 128, NT], BF16)
    VE = b.dscr("VE", [NT, 720], BF16)

    ODBG = b.dout("ODBG", [NT, D], BF16) if cfg.get("debug") else None
    seq_tile0 = [0]
    for t in seqs[:-1]:
        seq_tile0.append(seq_tile0[-1] + t // 128)

    with ExitStack() as gs:
        def sb(name, shape, dt=F32, stack=gs):
            return stack.enter_context(nc.sbuf_tensor(un(b, name), list(shape), dt))

        def ps(name, shape, dt=F32, stack=gs):
            return stack.enter_context(nc.psum_tensor(un(b, name), list(shape), dt))

        identf = sb("identf", [128, 128])
        identb = sb("identb", [128, 128], BF16)
        CS = sb("CS", [128, 24, 128])
        epst = sb("epst", [128, 2])
        b.eps_ln = epst
        csb = sb("csb", [128, 24])
        p = Phase(kb, "setup")
        p.op("pool", lambda i: nc.gpsimd.memset(epst[:, 0:1], LN_EPS), w=["epst0"])
        p.op("pool", lambda i: nc.gpsimd.memset(epst[:, 1:2], RMS_EPS), w=["epst1"])
        dma(p, nc, "sp", "k0", identf[:], cn["ident_f"][:, :], w=["identf"])
        dma(p, nc, "sp", "k1", identb[:], cn["ident_b"][:, :], w=["identb"])
        dma(p, nc, "sp", "k2", csb[:], cT_in.ap().rearrange("p s k -> p (s k)"), w=["csb"])
        act(p, nc, csb[:], csb[:], AF.Silu, r=["csb"], w=["csb"])
        cp(p, nc, "dve", CS[:], csb[:].unsqueeze(2).to_broadcast([128, 24, 128]), r=["csb"], w=["CS"])
        p.run()

        p = Phase(kb, "xcopy")
        nchunk = max(1, NT // 1024)
        rows = NT // nchunk
        for k in range(nchunk):
            dma(p, nc, ["sp", "act", "pool"][k % 3], f"xc{k % 3}", X[k * rows:(k + 1) * rows, :], x_in[k * rows:(k + 1) * rows, :])
        p.run()

        for l in range(depth):
            with ExitStack() as st:
                aw = sb("aw", [128, 8, 512], stack=st)
                ab = sb("ab", [128, 512], stack=st)
                mo = [sb(f"mo{s}", [128, 512], stack=st) for s in range(3)]
                pm = [ps(f"pm{s}", [128, 512], stack=st) for s in range(3)]
                p = Phase(kb, f"mod{l}", n_iter=12)
                dma(p, nc, "sp", "aw", aw[:], lambda i: ada_w[l].rearrange("(kc p) n -> p kc n", p=128)[:, :, bass.ds(i * 512, 512)], w=["aw"])
                dma(p, nc, "act", "ab", ab[:], lambda i: ada_b[l:l + 1, bass.ds(i * 512, 512)].partition_broadcast(128), w=["ab"])
                for s in range(3):
                    for kc in range(8):
                        mm(p, nc, pm[s][:], CS[:, s * 8 + kc, :], aw[:, kc, :], kc == 0, kc == 7, r=["aw"], w=[f"pm{s}"])
                    tt(p, nc, "dve", mo[s][:], pm[s][:], ab[:], ALU.add, r=[f"pm{s}", "ab"], w=[f"mo{s}"])
                    nts = seqs[s] // 128
                    dma(p, nc, "pool", f"mo{s}", lambda i, s=s, nts=nts: MODT[seq_tile0[s]:seq_tile0[s] + nts, bass.ds(i * 512, 512)],
                        mo[s][0:nts, :], r=[f"mo{s}"])
                p.run()
            if stop == "mod":
                break
            if l % 2 == 0:
                build_attention_layer(b, l, l // 2, dict(X=X, MODT=MODT, FM=FM, VE=VE, a_wfm=a_wfm, a_wv=a_wv, a_sink=a_sink,
                                                         a_rb=a_rb, a_wout=a_wout, ln_g=ln_g, ln_b=ln_b, cn=cn, identb=identb,
                                                         identf=identf, alpha=alpha, seq_tile0=seq_tile0, ODBG=ODBG))
            else:
                build_hgrn_layer(b, l, l // 2, dict(X=X, MODT=MODT, r_win=r_win, r_lb=r_lb, r_gn=r_gn, r_wout=r_wout,
                                                    ln_g=ln_g, ln_b=ln_b, cn=cn, identb=identb, identf=identf, alpha=alpha,
                                                    seq_tile0=seq_tile0))
            if stop in (f"mix{l}", "attw", "A1", "attpre", "A2e") or (stop in ("H1", "H2") and l % 2 == 1):
                break
            build_moe_layer(b, l, dict(X=X, MODT=MODT, ro_w=ro_w, ro_b=ro_b, e_w=e_w, ln_g=ln_g,
                                       ln_b=ln_b, cn=cn, identb=identb, identf=identf, alpha=alpha))
            if stop in (f"moe{l}", "moeD", "moeS", "moeB"):
                break

        p = Phase(kb, "ycopy")
        for k in range(nchunk):
            dma(p, nc, ["sp", "act", "pool"][k % 3], f"yc{k % 3}", y_out[k * rows:(k + 1) * rows, :], X[k * rows:(k + 1) * rows, :])
        p.run()
    return nc


def build_attention_layer(b, l, la, g):
    nc, kb = b.nc, b.kb
    seqs = b.cfg["seqs"]
    NT = sum(seqs)
    X, MODT, FM, VE, cn = g["X"], g["MODT"], g["FM"], g["VE"], g["cn"]
    identb = g["identb"]
    alpha = g["alpha"]
    seq_tile0 = g["seq_tile0"]

    with ExitStack() as st:
        def sb(name, shape, dt=F32):
            return st.enter_context(nc.sbuf_tensor(un(b, name), list(shape), dt))

        def ps(name, shape, dt=F32):
            return st.enter_context(nc.psum_tensor(un(b, name), list(shape), dt))

        wfm = sb("wfm", [128, 8, 2304], BF16)
        wv = sb("wv", [128, 8, 640], BF16)
        stg = [sb("stg0", [128, 8, 512]), sb("stg1", [128, 8, 512])]
        veall = sb("veall", [128, 4, 10, 72], BF16)
        p = Phase(kb, f"attw{l}")
        load_weight_bf16(b, p, wfm, g["a_wfm"][la].rearrange("(kc p) n -> p kc n", p=128), 2304, "wfm", stg)
        load_weight_bf16(b, p, wv, g["a_wv"][la].rearrange("(kc p) n -> p kc n", p=128), 640, "wv", stg)
        p.op("pool", lambda i: nc.gpsimd.memset(veall[:], 1.0), w=["veall"])
        p.run()
        if b.cfg.get("stop") == "attw":
            return

        xg = sb("xg", [128, 4, 1024])
        scb = sb("scb", [128, 1024])
        shb = sb("shb", [128, 1024])
        tmp = [sb("tmp0", [128, 1024]), sb("tmp1", [128, 1024])]
        hb = sb("hb", [128, 4, 1024], BF16)
        hT = sb("hT", [128, 8, 512], BF16)
        ropec = sb("ropec", [128, 512])
        ropes = sb("ropes", [128, 512])
        t1 = sb("t1", [128, 512])
        t2 = sb("t2", [128, 512])
        fmall = sb("fmall", [128, 13, 512], BF16)
        psT = [ps("psT0", [128, 8, 128], BF16), ps("psT1", [128, 8, 128], BF16)]
        pf = [ps(f"pf{k}", [128, 512]) for k in range(3)]
        pva = ps("pva", [128, 512])
        pvb = ps("pvb", [128, 512])

        NG = NT // 512
        p = Phase(kb, f"A1_{l}", n_iter=NG)
        dma(p, nc, "sp", "xg", xg[:], lambda i: X[bass.ds(i * 512, 512), :].rearrange("(t p) d -> p t d", p=128), w=["xg"])
        dma(p, nc, "act", "scb", scb[:], lambda i: MODT[bass.ds(i * 4, 1), 1024:2048].partition_broadcast(128), w=["scb"])
        dma(p, nc, "act", "shb", shb[:], lambda i: MODT[bass.ds(i * 4, 1), 0:1024].partition_broadcast(128), w=["shb"])
        dma(p, nc, "pool", "ropec", ropec[:], lambda i: cn["rope_cos"][:, bass.ds(i * 512, 512)], w=["ropec"])
        dma(p, nc, "pool", "ropes", ropes[:], lambda i: cn["rope_sin"][:, bass.ds(i * 512, 512)], w=["ropes"])
        ts(p, nc, "dve", scb[:], scb[:], 1.0, None, ALU.add, r=["scb"], w=["scb"])
        for t in range(4):
            tt(p, nc, "dve", tmp[t % 2][:], xg[:, t, :], scb[:], ALU.mult, r=["xg", "scb"], w=[f"tmp{t % 2}"])
            tt(p, nc, "pool", hb[:, t, :], tmp[t % 2][:], shb[:], ALU.add, r=[f"tmp{t % 2}", "shb"], w=[f"hb{t}"])
            for kc in range(8):
                tr(p, nc, psT[t % 2][:, kc, :], hb[:, t, kc * 128:(kc + 1) * 128], identb[:], r=[f"hb{t}"], w=[f"psT{t % 2}"])
            cp(p, nc, "act", hT[:, :, t * 128:(t + 1) * 128], psT[t % 2][:], r=[f"psT{t % 2}"], w=[f"hT{t}"])
        hTall = [f"hT{t}" for t in range(4)]
        pfi = 0
        fk = 0
        for (ca, cb_, dst) in [(0, 4, 0), (1, 5, 1), (2, 6, 2), (3, 7, 3), (8, 9, 4)]:
            ia, ib = pfi % 3, (pfi + 1) % 3
            pfi += 2
            for kc in range(8):
                mm(p, nc, pf[ia][:], wfm[:, kc, ca * 128:(ca + 1) * 128], hT[:, kc, :], kc == 0, kc == 7, r=hTall, w=[f"pf{ia}"])
            for kc in range(8):
                mm(p, nc, pf[ib][:], wfm[:, kc, cb_ * 128:(cb_ + 1) * 128], hT[:, kc, :], kc == 0, kc == 7, r=hTall, w=[f"pf{ib}"])
            tt(p, nc, "dve", t1[:], pf[ia][:], ropec[:], ALU.mult, r=[f"pf{ia}", "ropec"], w=["t1"])
            tt(p, nc, "dve", t2[:], pf[ib][:], ropes[:], ALU.mult, r=[f"pf{ib}", "ropes"], w=["t2"])
            tt(p, nc, "pool", fmall[:, dst, :], t1[:], t2[:], ALU.add, r=["t1", "t2"], w=[f"fm{dst}"])
        for c in range(10, 18):
            dst = 5 + (c - 10)
            ia = pfi % 3
            pfi += 1
            for kc in range(8):
                mm(p, nc, pf[ia][:], wfm[:, kc, c * 128:(c + 1) * 128], hT[:, kc, :], kc == 0, kc == 7, r=hTall, w=[f"pf{ia}"])
            cp(p, nc, "act", fmall[:, dst, :], pf[ia][:], r=[f"pf{ia}"], w=[f"fm{dst}"])
        for t in range(4):
            for kc in range(8):
                mm(p, nc, pva[:], hT[:, kc, t * 128:(t + 1) * 128], wv[:, kc, 128:640], kc == 0, kc == 7, r=[f"hT{t}"], w=["pva"])
            for kc in range(8):
                mm(p, nc, pvb[:, 0:128], hT[:, kc, t * 128:(t + 1) * 128], wv[:, kc, 0:128], kc == 0, kc == 7, r=[f"hT{t}"], w=["pvb"])
            cp(p, nc, "dve", veall[:, t, 2:10, 0:64], pva[:].rearrange("p (h d) -> p h d", d=64), r=["pva"], w=[f"ve{t}"])
            cp(p, nc, "act", veall[:, t, 0:2, 0:64], pvb[:, 0:128].rearrange("p (h d) -> p h d", d=64), r=["pvb"], w=[f"ve{t}"])
        dma(p, nc, "sp", "fmall", lambda i: FM[:, :, bass.ds(i * 512, 512)].rearrange("f p t -> p f t"), fmall[:],
            r=[f"fm{d_}" for d_ in range(13)])
        dma(p, nc, "pool", "veall", lambda i: VE[bass.ds(i * 512, 512), :].rearrange("(t p) c -> p t c", p=128),
            veall[:].rearrange("p t h d -> p t (h d)"), r=[f"ve{t}" for t in range(4)])
        p.run()
    if b.cfg.get("stop") == "A1":
        return

    with ExitStack() as st:
        def sb(name, shape, dt=F32):
            return st.enter_context(nc.sbuf_tensor(un(b, name), list(shape), dt))

        def ps(name, shape, dt=F32):
            return st.enter_context(nc.psum_tensor(un(b, name), list(shape), dt))

        wout = sb("wout", [128, 8, 1024], BF16)
        EB = sb("EB", [128, 9, 8, 128], BF16)
        skx = sb("skx", [128, 8])
        wm = sb("wm", [128, 2, 128], BF16)
        lng = sb("lng", [128, 1024])
        lnb = sb("lnb", [128, 1024])
        with ExitStack() as st2:
            stg = [st2.enter_context(nc.sbuf_tensor(un(b, "stg0"), [128, 8, 512], F32)),
                   st2.enter_context(nc.sbuf_tensor(un(b, "stg1"), [128, 8, 512], F32))]
            nam = st2.enter_context(nc.sbuf_tensor(un(b, "nam"), [128, 9, 128], F32))
            rbs = [st2.enter_context(nc.sbuf_tensor(un(b, "rbs0"), [128, 8, 128], F32)),
                   st2.enter_context(nc.sbuf_tensor(un(b, "rbs1"), [128, 8, 128], F32))]
            p = Phase(kb, f"attpre{l}")
            load_weight_bf16(b, p, wout, g["a_wout"][la].rearrange("(kc p) n -> p kc n", p=128), 1024, "wout", stg)
            dma(p, nc, "pool", "nam", nam[:], cn["namask"][:, :, :], w=["nam"])
            dma(p, nc, "pool", "wm", wm[:], cn["wmask"][:, :, :], w=["wm"])
            dma(p, nc, "pool", "skx", skx[:], g["a_sink"][la:la + 1, :].partition_broadcast(128), w=["skx"])
            dma(p, nc, "pool", "lng", lng[:], g["ln_g"][l, 0:1, :].partition_broadcast(128), w=["lng"])
            dma(p, nc, "pool", "lnb", lnb[:], g["ln_b"][l, 0:1, :].partition_broadcast(128), w=["lnb"])
            act(p, nc, skx[:], skx[:], AF.Exp, r=["skx"], w=["skx"])
            for t in range(9):
                rb = rbs[t % 2]
                dma(p, nc, "sp", f"rbs{t % 2}", rb[:], g["a_rb"][la, :, t, :, :], w=[f"rbs{t % 2}"])
                act(p, nc, rb[:], rb[:], AF.Exp, r=[f"rbs{t % 2}"], w=[f"rbs{t % 2}"])
                tt(p, nc, "dve", EB[:, t, :, :], rb[:], nam[:, t, :].unsqueeze(1).to_broadcast([128, 8, 128]), ALU.mult,
                   r=[f"rbs{t % 2}", "nam"], w=[f"EB{t}"])
            p.run()
        if b.cfg.get("stop") == "attpre":
            return

        qa = sb("qa", [128, 4, 128], BF16)
        ka = sb("ka", [128, 3, 128], BF16)
        va = sb("va", [128, 3, 2, 72], BF16)
        qb = sb("qb", [64, 8, 128], BF16)
        kbt = sb("kbt", [64, 8, 5, 128], BF16)
        vb = sb("vb", [128, 5, 8, 72], BF16)
        xt = sb("xt", [128, 1024])
        g1b = sb("g1b", [128, 1024])
        PT = [sb("PT0", [128, 5, 4, 128], BF16), sb("PT1", [128, 5, 4, 128], BF16)]
        o = sb("o", [128, 1024], BF16)
        oT = sb("oT", [128, 8, 128], BF16)
        t1 = sb("t1", [128, 1024])
        z = sb("z", [128, 1024])
        sq = sb("sq", [128, 1024])
        xo = sb("xo", [128, 1024])
        stt = sb("stt", [128, 8])
        den = sb("den", [128, 4, 1])
        pS = [ps("pS0", [128, 4, 128]), ps("pS1", [128, 4, 128])]
        accb = [ps("acc0", [128, 512]), ps("acc1", [128, 512])]
        acc = [a[:].rearrange("p (h d) -> p h d", d=128) for a in accb]
        psOT = ps("psOT", [128, 8, 128], BF16)
        psY = [ps("psY0", [128, 512]), ps("psY1", [128, 512])]
        o3 = o[:].rearrange("p (h d) -> p h d", d=64)

        def block(p, tok0, tile, wl, nl):
            def T(off):
                return (lambda i: (tok0(i) if callable(tok0) else tok0) + off)

            def TL(i):
                return tile(i) if callable(tile) else tile
            dma(p, nc, "sp", "qa", qa[:], lambda i: FM[0:4, :, bass.ds(T(0)(i), 128)].rearrange("f p t -> p f t"), w=["qa"])
            w0, w1 = wl[0], wl[-1]
            nw = w1 - w0 + 1
            dma(p, nc, "sp", "ka", ka[:, w0 + 1:w1 + 2, :].rearrange("p b t -> p (b t)"),
                lambda i: FM[4, :, bass.ds(T(w0 * 128)(i), nw * 128)], w=["ka"])
            dma(p, nc, "pool", "va", va[:, w0 + 1:w1 + 2, :, :].rearrange("p b h d -> p b (h d)"),
                lambda i: VE[bass.ds(T(w0 * 128)(i), nw * 128), 0:144].rearrange("(b p) c -> p b c", p=128), w=["va"])
            dma(p, nc, "sp", "qb", qb[:], lambda i: FM[5:9, :, bass.ds(T(0)(i), 128)].rearrange("f (two d) t -> d (f two) t", two=2), w=["qb"])
            n0 = nl[0][0]
            nn = len(nl)
            dma(p, nc, "act", "kbt", kbt[:, :, 0:nn, :].rearrange("p f b t -> p f (b t)"),
                lambda i: FM[9:13, :, bass.ds(T(n0 * 128)(i), nn * 128)].rearrange("f (two d) t -> d (f two) t", two=2), w=["kbt"])
            dma(p, nc, "pool", "vb", vb[:, 0:nn, :, :].rearrange("p b h d -> p b (h d)"),
                lambda i: VE[bass.ds(T(n0 * 128)(i), nn * 128), 144:720].rearrange("(b p) c -> p b c", p=128), w=["vb"])
            dma(p, nc, "sp", "xt", xt[:], lambda i: X[bass.ds(T(0)(i), 128), :], w=["xt"])
            dma(p, nc, "act", "g1b", g1b[:], lambda i: MODT[bass.ds(TL(i), 1), 2048:3072].partition_broadcast(128), w=["g1b"])
            lvl = b.cfg.get("a2lvl", 9)
            if lvl < 2:
                return
            u = 0
            useg = []
            for gk in range(2):
                a = acc[gk % 2]
                an = f"acc{gk % 2}"
                Pu = PT[gk % 2]
                pn = f"PT{gk % 2}_"
                useg.append([len(p.ops)])
                for di, dl in enumerate(wl):
                    k = u % 2
                    u += 1
                    mm(p, nc, pS[k][:], ka[64 * gk:64 * gk + 64, dl + 1, :], qa[64 * gk:64 * gk + 64, :, :], True, True,
                       r=["ka", "qa"], w=[f"pS{k}"])
                    act(p, nc, Pu[:, di, :, :], pS[k][:], AF.Exp, scale=0.125, r=[f"pS{k}"], w=[pn + str(di)])
                    if dl != 0:
                        tt(p, nc, "dve", Pu[:, di, :, :], Pu[:, di, :, :],
                           wm[:, 0 if dl == -1 else 1, :].unsqueeze(1).to_broadcast([128, 4, 128]),
                           ALU.mult, r=[pn + str(di)], w=[pn + str(di)])
                useg[-1].append(len(p.ops))
                for h in range(4):
                    for di, dl in enumerate(wl):
                        mm(p, nc, a[:, h, 0:66], Pu[:, di, h, :], va[:, dl + 1, gk, 0:66], di == 0, di == len(wl) - 1,
                           r=[pn + str(di), "va"], w=[an])
                tt(p, nc, "dve", den[:], a[:, :, 64:65], skx[:, 4 * gk:4 * gk + 4].unsqueeze(2), ALU.add, r=[an], w=["den"])
                p.op("dve", lambda i: nc.vector.reciprocal(den[:], den[:]), r=["den"], w=["den"])
                tt(p, nc, "dve", o3[:, 4 * gk:4 * gk + 4, :], a[:, :, 0:64], den[:].to_broadcast([128, 4, 64]), ALU.mult,
                   r=[an, "den"], w=["o"])
            if lvl < 3:
                return
            for hg in range(2):
                a = acc[hg % 2]
                an = f"acc{hg % 2}"
                Pu = PT[hg % 2]
                pn = f"PT{hg % 2}_"
                useg[-1].append(len(p.ops))
                useg.append([len(p.ops)])
                for di, (dl, tb) in enumerate(nl):
                    k = u % 2
                    u += 1
                    for uu in range(4):
                        h = 4 * hg + uu
                        mm(p, nc, pS[k][:, uu, :], kbt[:, h, di, :], qb[:, h, :],
                           True, True, r=["kbt", "qb"], w=[f"pS{k}"])
                    act(p, nc, Pu[:, di, :, :], pS[k][:], AF.Exp, scale=0.125, r=[f"pS{k}"], w=[pn + str(di)])
                    tt(p, nc, "dve" if di % 2 == 0 else "pool", Pu[:, di, :, :], Pu[:, di, :, :], EB[:, tb, 4 * hg:4 * hg + 4, :], ALU.mult,
                       r=[pn + str(di)], w=[pn + str(di)])
                useg[-1].append(len(p.ops))
                for uu in range(4):
                    for di, (dl, tb) in enumerate(nl):
                        mm(p, nc, a[:, uu, 0:66], Pu[:, di, uu, :], vb[:, di, 4 * hg + uu, 0:66], di == 0, di == len(nl) - 1,
                           r=[pn + str(di), "vb"], w=[an])
                cp(p, nc, "dve", den[:], a[:, :, 64:65], r=[an], w=["den"])
                p.op("dve", lambda i: nc.vector.reciprocal(den[:], den[:]), r=["den"], w=["den"])
                tt(p, nc, "dve", o3[:, 8 + 4 * hg:8 + 4 * hg + 4, :], a[:, :, 0:64], den[:].to_broadcast([128, 4, 64]), ALU.mult,
                   r=[an, "den"], w=["o"])
            useg[-1].append(len(p.ops))
            if len(useg) == 4 and all(len(x) == 3 for x in useg) and b.cfg.get("a2pipe", True):
                o_ = p.ops
                sem = [o_[x[0]:x[1]] for x in useg]
                pv = [o_[x[1]:x[2]] for x in useg]
                neworder = sem[0] + sem[1] + pv[0] + sem[2] + pv[1] + sem[3] + pv[2] + pv[3]
                p.ops = o_[:useg[0][0]] + neworder + o_[useg[3][2]:]
            if b.cfg.get("debug"):
                dma(p, nc, "pool", "odbg", lambda i: g["ODBG"][bass.ds(T(0)(i), 128), :], o[:], r=["o"])
            if lvl < 4:
                return
            for kc in range(8):
                tr(p, nc, psOT[:, kc, :], o[:, kc * 128:(kc + 1) * 128], identb[:], r=["o"], w=["psOT"])
            cp(p, nc, "act", oT[:], psOT[:], r=["psOT"], w=["oT"])
            for n in range(2):
                for kc in range(8):
                    mm(p, nc, psY[n][:], oT[:, kc, :], wout[:, kc, n * 512:(n + 1) * 512], kc == 0, kc == 7, r=["oT"], w=[f"psY{n}"])
            if lvl < 5:
                return
            ts(p, nc, "pool", g1b[:], g1b[:], 1.0, None, ALU.add, r=["g1b"], w=["g1b"])
            for n in range(2):
                tt(p, nc, "dve", t1[:, n * 512:(n + 1) * 512], psY[n][:], g1b[:, n * 512:(n + 1) * 512], ALU.mult,
                   r=[f"psY{n}", "g1b"], w=["t1"])
            p.op("dve", lambda i: nc.vector.scalar_tensor_tensor(z[:], xt[:], alpha, t1[:], ALU.mult, ALU.add), r=["xt", "t1"], w=["z"])
            layer_norm_tail(b, p, z, "z", lng, lnb, xo, "xo", dict(sq=sq, st=stt), "ln")
            dma(p, nc, "sp", "xo", lambda i: X[bass.ds(T(0)(i), 128), :], xo[:], r=["xo"])

        FULL = lambda dls: [(d_, d_ + 3) for d_ in dls]
        for s, T_s in enumerate(seqs):
            nb = T_s // 128
            tok_s = seq_tile0[s] * 128
            p = Phase(kb, f"A2e_{l}_{s}")
            for j in sorted(set([0, 1, nb - 2, nb - 1]))[:b.cfg.get("a2nblk", 4)]:
                wl = [d_ for d_ in (-1, 0, 1) if 0 <= j + d_ < nb]
                if j == 0:
                    nl = FULL([0, 1, 2, 3])
                elif j == 1:
                    nl = FULL([-1, 0, 1, 2])
                elif j == nb - 2:
                    nl = FULL([-2, -1, 0, 1])
                else:
                    nl = FULL([-3, -2, -1, 0])
                block(p, tok_s + j * 128, seq_tile0[s] + j, wl, nl)
            p.run()
            if b.cfg.get("stop") == "A2e":
                return
            if nb > 4:
                p = Phase(kb, f"A2i_{l}_{s}", n_iter=nb - 4)
                block(p, lambda i, tok_s=tok_s: (i + 2) * 128 + tok_s, lambda i, s=s: i + (2 + seq_tile0[s]),
                      [-1, 0, 1], [(-2, 7), (-1, 2), (0, 3), (1, 4), (2, 8)])
                p.run()


def expert_layout(w, kdim):
    L, E, K, n = w.shape
    return np.ascontiguousarray(w.reshape(L, E, kdim, 128, n).transpose(0, 1, 3, 2, 4)).reshape(L * E * 128, kdim * n)


def prepare_shared(inp, cfg):
    f = lambda k: np.ascontiguousarray(np.asarray(inp[k], dtype=np.float32))
    sh = {}
    for k in ["ada_w", "ada_b", "ln_g", "ln_b", "attn_sink", "attn_w_out", "rec_w_in", "rec_lb", "rec_gnorm", "rec_w_out"]:
        sh[k] = f(k)
    wfm, wv = attn_w_layout(f("attn_w_in"))
    sh["attn_wfm"] = wfm
    sh["attn_wv"] = wv
    sh["na_rb"] = na_bias_tables(f("nat_rel_bias"))
    L = sh["ada_w"].shape[0]
    sh["router_w"] = np.ascontiguousarray(np.concatenate([f("router_w_group"), f("router_w_expert")], axis=2))
    sh["router_b"] = np.ascontiguousarray(np.concatenate([f("router_b_group"), f("router_b_expert").reshape(L, 32)], axis=1))
    sh["e_w"] = np.concatenate([expert_layout(f("expert_w_gate"), 8), expert_layout(f("expert_w_up"), 8),
                                expert_layout(f("expert_w_down"), 4)], axis=1)
    sh.update(host_constants(cfg))
    return sh


def run_cores(inp, cfg, n_cores, pps):
    xp = np.asarray(inp["x_prompt"], dtype=np.float32)
    xs = np.asarray(inp["x_sample"], dtype=np.float32)
    cp_ = np.asarray(inp["c_prompt"], dtype=np.float32)
    cs_ = np.asarray(inp["c_sample"], dtype=np.float32)
    sh = prepare_shared(inp, cfg)
    nc = build_program(cfg)
    in_maps = []
    for c in range(n_cores):
        m = dict(sh)
        xc = np.concatenate([xp[pps * c + k] for k in range(pps)] + [xs[c]], axis=0)
        cc = np.stack([cp_[pps * c + k] for k in range(pps)] + [cs_[c]], axis=0)
        m["x"] = np.ascontiguousarray(xc)
        m["cT"] = np.ascontiguousarray(cc.reshape(3, 8, 128).transpose(2, 0, 1))
        in_maps.append(m)
    res = run_bass_kernel_spmd(nc, in_maps, core_ids=list(range(n_cores)))
    if cfg.get("debug"):
        cfg["_dbg"] = res.results
    TP = cfg["seqs"][0]
    yp = np.zeros_like(xp)
    ys = np.zeros_like(xs)
    for c in range(n_cores):
        y = np.asarray(res.results[c]["y"], dtype=np.float32)
        for k in range(pps):
            yp[pps * c + k] = y[k * TP:(k + 1) * TP]
        ys[c] = y[pps * TP:]
    return yp, ys


def kernel(**inputs):
    cfg = dict(seqs=[2048, 2048, 8192], depth=4)
    yp, ys = run_cores(inputs, cfg, 8, 2)
    return (yp, ys)


def build_moe_layer(b, l, g):
    nc, kb = b.nc, b.kb
    seqs = b.cfg["seqs"]
    NT = sum(seqs)
    NTILES = NT // 128
    NBLK = 2 * NTILES + NEXP
    X, MODT, cn = g["X"], g["MODT"], g["cn"]
    identf = g["identf"]
    alpha = g["alpha"]
    BIG = 1.0e9
    if "HF" not in b.dram:
        b.dscr("HF", [NT, D])
        b.dscr("RTD", [128, NTILES, 8])
        b.dscr("DESTD", [128, NTILES, 2], I32)
        b.dscr("IDXD", [128, NBLK], I32)
        b.dscr("XBUF", [NBLK * 128, D])
        b.dscr("YBUF", [NBLK * 128, D])
    HF, RTD, DESTD, IDXD, XBUF, YBUF = [b.dram[k] for k in ["HF", "RTD", "DESTD", "IDXD", "XBUF", "YBUF"]]

    with ExitStack() as st:
        def sb(name, shape, dt=F32):
            return st.enter_context(nc.sbuf_tensor(un(b, name), list(shape), dt))

        def ps(name, shape, dt=F32):
            return st.enter_context(nc.psum_tensor(un(b, name), list(shape), dt))

        rw = sb("rw", [128, 8, 36])
        rbb = sb("rbb", [128, 36])
        io32 = sb("io32", [128, 32])
        trib = sb("trib", [128, 128], BF16)
        oneb = sb("oneb", [128, 128], BF16)
        carry = sb("carry", [128, 32])
        zt = sb("zt", [128, 1024])
        rt = sb("rt", [128, 8])
        p = Phase(kb, f"moepre{l}")
        p.op("pool", lambda i: nc.gpsimd.memset(rt[:], 0.0), w=["rt"])
        dma(p, nc, "sp", "m0", rw[:], g["ro_w"][l].rearrange("(kc p) n -> p kc n", p=128), w=["rw"])
        dma(p, nc, "sp", "m1", rbb[:], g["ro_b"][l:l + 1, :].partition_broadcast(128), w=["rbb"])
        dma(p, nc, "sp", "m2", io32[:], cn["iota32"][:, :], w=["io32"])
        dma(p, nc, "sp", "m3", trib[:], cn["tri_su"][:, :], w=["trib"])
        dma(p, nc, "sp", "m4", oneb[:], cn["ones_b"][:, :], w=["oneb"])
        p.op("pool", lambda i: nc.gpsimd.memset(carry[:], 0.0), w=["carry"])
        p.op("pool", lambda i: nc.gpsimd.memset(zt[:], 0.0), w=["zt"])
        p.run()
        p = Phase(kb, f"moez{l}", n_iter=NBLK // 4)
        dma(p, nc, "sp", "zf", lambda i: XBUF[bass.ds(i * 512, 512), :].rearrange("(t p) d -> p t d", p=128),
            zt[:].unsqueeze(1).to_broadcast([128, 4, 1024]))
        p.run()

        from types import SimpleNamespace
        sets = []
        for k in range(2):
            T = SimpleNamespace(xt=sb(f"xt{k}", [128, 1024]), scb=sb(f"scb{k}", [128, 1024]), shb=sb(f"shb{k}", [128, 1024]),
                                hf=sb(f"hf{k}", [128, 1024]), hfT=sb(f"hfT{k}", [128, 8, 128]), lg=sb(f"lg{k}", [128, 36]),
                                sm=sb(f"sm{k}", [128, 16]), ge=sb(f"ge{k}", [128, 4]), gmask=sb(f"gmask{k}", [128, 4]),
                                lem=sb(f"lem{k}", [128, 4, 8]), lem2=sb(f"lem2{k}", [128, 32]), oh1=sb(f"oh1{k}", [128, 32]),
                                oh2=sb(f"oh2{k}", [128, 32]), ohb=sb(f"ohb{k}", [128, 32], BF16), tmp32=sb(f"tmp32{k}", [128, 32]),
                                tot=sb(f"tot{k}", [128, 32]), rt=sb(f"rtt{k}", [128, 8]))
            T.lemf = T.lem[:].rearrange("p a b -> p (a b)")
            sets.append(T)
        psAs = [[ps(f"psA0_{k}", [128, 4, 128]), ps(f"psA1_{k}", [128, 4, 128])] for k in range(2)]
        pms = [ps(f"pm{k}", [128, 512]) for k in range(2)]
        p = Phase(kb, f"moeRz{l}")
        for k in range(2):
            p.op("pool", lambda i, k=k: nc.gpsimd.memset(sets[k].rt[:], 0.0), w=[f"rtz{k}"])
        p.run()

        p = Phase(kb, f"moeR{l}", n_iter=NTILES // 2)
        for k in range(2):
            p.ns = f"#{k}"
            T = sets[k]
            psA = psAs[k]
            pl = pms[k][:, 0:64]
            pr = pms[k][:, 64:128]
            pc = pms[k][:, 128:192]
            dma(p, nc, "sp", "xt", T.xt[:], lambda i, k=k, T=T: X[bass.ds(i * 256 + k * 128, 128), :], w=["xt"])
            dma(p, nc, "act", "scb", T.scb[:], lambda i, k=k, T=T: MODT[bass.ds(i * 2 + k, 1), 4096:5120].partition_broadcast(128), w=["scb"])
            dma(p, nc, "act", "shb", T.shb[:], lambda i, k=k, T=T: MODT[bass.ds(i * 2 + k, 1), 3072:4096].partition_broadcast(128), w=["shb"])
            ts(p, nc, "pool", T.scb[:], T.scb[:], 1.0, None, ALU.add, r=["scb"], w=["scb"])
            tt(p, nc, "dve", T.hf[:], T.xt[:], T.scb[:], ALU.mult, r=["xt", "scb"], w=["hf"])
            tt(p, nc, "pool", T.hf[:], T.hf[:], T.shb[:], ALU.add, r=["hf", "shb"], w=["hf"])
            dma(p, nc, "sp", "hfo", lambda i, k=k, T=T: HF[bass.ds(i * 256 + k * 128, 128), :], T.hf[:], r=["hf"])
            for kc in range(8):
                tr(p, nc, psA[kc // 4][:, kc % 4, :], T.hf[:, kc * 128:(kc + 1) * 128], identf[:], r=["hf"], w=[f"psA{kc // 4}"])
            cp(p, nc, "act", T.hfT[:, 0:4, :], psA[0][:], r=["psA0"], w=["hfT0"])
            cp(p, nc, "dve", T.hfT[:, 4:8, :], psA[1][:], r=["psA1"], w=["hfT1"])
            for kc in range(8):
                mm(p, nc, pl[:, 0:36], T.hfT[:, kc, :], rw[:, kc, :], kc == 0, kc == 7, r=["hfT0", "hfT1"], w=["pm"])
            tt(p, nc, "dve", T.lg[:], pl[:, 0:36], rbb[:], ALU.add, r=["pm"], w=["lg"])
            p.op("dve", lambda i, k=k, T=T: nc.vector.tensor_reduce(T.sm[:, 0:1], T.lg[:, 0:4], AX.X, ALU.max), r=["lg"], w=["sm0"])
            ts(p, nc, "dve", T.sm[:, 1:2], T.sm[:, 0:1], -1.0, None, ALU.mult, r=["sm0"], w=["sm1"])
            act(p, nc, T.ge[:], T.lg[:, 0:4], AF.Exp, bias=T.sm[:, 1:2], r=["lg", "sm1"], w=["ge"])
            p.op("dve", lambda i, k=k, T=T: nc.vector.tensor_reduce(T.sm[:, 2:3], T.ge[:], AX.X, ALU.add), r=["ge"], w=["sm2"])
            p.op("dve", lambda i, k=k, T=T: nc.vector.reciprocal(T.sm[:, 3:4], T.sm[:, 2:3]), r=["sm2"], w=["sm3"])
            tt(p, nc, "dve", T.gmask[:], T.lg[:, 0:4], T.sm[:, 0:1].to_broadcast([128, 4]), ALU.is_ge, r=["lg", "sm0"], w=["gmask"])
            ts(p, nc, "dve", T.gmask[:], T.gmask[:], -1.0, BIG, ALU.add, ALU.mult, r=["gmask"], w=["gmask"])
            tt(p, nc, "dve", T.lem[:], T.lg[:, 4:36].rearrange("p (a b) -> p a b", b=8), T.gmask[:].unsqueeze(2).to_broadcast([128, 4, 8]),
               ALU.add, r=["lg", "gmask"], w=["lem"])
            p.op("dve", lambda i, k=k, T=T: nc.vector.tensor_reduce(T.sm[:, 4:5], T.lemf, AX.X, ALU.max), r=["lem"], w=["sm4"])
            tt(p, nc, "dve", T.oh1[:], T.lemf, T.sm[:, 4:5].to_broadcast([128, 32]), ALU.is_ge, r=["lem", "sm4"], w=["oh1"])
            p.op("dve", lambda i, k=k, T=T: nc.vector.scalar_tensor_tensor(T.lem2[:], T.oh1[:], -BIG, T.lemf, ALU.mult, ALU.add), r=["oh1", "lem"], w=["lem2"])
            p.op("dve", lambda i, k=k, T=T: nc.vector.tensor_reduce(T.sm[:, 5:6], T.lem2[:], AX.X, ALU.max), r=["lem2"], w=["sm5"])
            tt(p, nc, "dve", T.oh2[:], T.lem2[:], T.sm[:, 5:6].to_broadcast([128, 32]), ALU.is_ge, r=["lem2", "sm5"], w=["oh2"])
            tt(p, nc, "dve", T.sm[:, 6:7], T.sm[:, 5:6], T.sm[:, 4:5], ALU.subtract, r=["sm4", "sm5"], w=["sm6"])
            act(p, nc, T.sm[:, 7:8], T.sm[:, 6:7], AF.Exp, r=["sm6"], w=["sm7"])
            ts(p, nc, "dve", T.sm[:, 8:9], T.sm[:, 7:8], 1.0, None, ALU.add, r=["sm7"], w=["sm8"])
            p.op("dve", lambda i, k=k, T=T: nc.vector.reciprocal(T.sm[:, 8:9], T.sm[:, 8:9]), r=["sm8"], w=["sm8"])
            tt(p, nc, "dve", T.rt[:, 2:3], T.sm[:, 8:9], T.sm[:, 3:4], ALU.mult, r=["sm8", "sm3"], w=["rt2"])
            tt(p, nc, "dve", T.rt[:, 3:4], T.rt[:, 2:3], T.sm[:, 7:8], ALU.mult, r=["rt2", "sm7"], w=["rt3"])
            tt(p, nc, "dve", T.tmp32[:], T.oh1[:], io32[:], ALU.mult, r=["oh1"], w=["tmp32"])
            p.op("dve", lambda i, k=k, T=T: nc.vector.tensor_reduce(T.rt[:, 0:1], T.tmp32[:], AX.X, ALU.add), r=["tmp32"], w=["rt0"])
            tt(p, nc, "dve", T.tmp32[:], T.oh2[:], io32[:], ALU.mult, r=["oh2", "rt0"], w=["tmp32"])
            p.op("dve", lambda i, k=k, T=T: nc.vector.tensor_reduce(T.rt[:, 1:2], T.tmp32[:], AX.X, ALU.add), r=["tmp32"], w=["rt1"])
            tt(p, nc, "pool", T.ohb[:], T.oh1[:], T.oh2[:], ALU.add, r=["oh1", "oh2"], w=["ohb"])
            mm(p, nc, pr[:, 0:32], trib[:], T.ohb[:], True, True, r=["ohb"], w=["pm"])
            mm(p, nc, pc[:, 0:32], oneb[:], T.ohb[:], True, True, r=["ohb"], w=["pm"])
            tt(p, nc, "dve", T.tot[:], pr[:, 0:32], carry[:], ALU.add, r=["pm", "!carry"], w=["tot"])
            if k == 1:
                tt(p, nc, "dve", T.tot[:], T.tot[:], pms[0][:, 128:160], ALU.add, r=["tot", "!pm#0"], w=["tot"])
            tt(p, nc, "dve", T.tmp32[:], T.oh1[:], T.tot[:], ALU.mult, r=["oh1", "tot", "rt1"], w=["tmp32"])
            p.op("dve", lambda i, k=k, T=T: nc.vector.tensor_reduce(T.rt[:, 4:5], T.tmp32[:], AX.X, ALU.add), r=["tmp32"], w=["rt4"])
            tt(p, nc, "dve", T.tmp32[:], T.oh2[:], T.tot[:], ALU.mult, r=["oh2", "tot", "rt4"], w=["tmp32"])
            p.op("dve", lambda i, k=k, T=T: nc.vector.tensor_reduce(T.rt[:, 5:6], T.tmp32[:], AX.X, ALU.add), r=["tmp32"], w=["rt5"])
            if k == 1:
                tt(p, nc, "dve", carry[:], carry[:], pms[0][:, 128:160], ALU.add, r=["!carry", "!pm#0", "!tot#0", "tot"], w=["!carry"])
                tt(p, nc, "dve", carry[:], carry[:], pc[:, 0:32], ALU.add, r=["!carry", "pm", "tot"], w=["!carry"])
            dma(p, nc, "act", "rto", lambda i, k=k, T=T: RTD[:, bass.ds(i * 2 + k, 1), :].rearrange("p o c -> p (o c)"), T.rt[:],
                r=["rt0", "rt1", "rt2", "rt3", "rt4", "rt5"])
            if k == 0:
                n_half = len(p.ops)
        p.ns = ""
        p.interleave(0, n_half, len(p.ops))
        p.run()

        rta = sb("rta", [128, NTILES, 8])
        cnt = sb("cnt", [128, 32])
        cnti = sb("cnti", [128, 32], I32)
        pend = sb("pend", [128, 32])
        pst = sb("pst", [128, 32])
        onesf = sb("onesf", [128, 32])
        dacc = sb("dacc", [128, NTILES, 2])
        dtmp = sb("dtmp", [128, NTILES, 2])
        dint = sb("dint", [128, NTILES, 2], I32)
        ioblk = sb("ioblk", [128, NBLK])
        bacc = sb("bacc", [128, NBLK])
        btmp = sb("btmp", [128, NBLK])
        iop = sb("iop", [128, 1])
        bint = sb("bint", [128, NBLK], I32)
        p = Phase(kb, f"moeD{l}")
        dma(p, nc, "sp", "d0", rta[:], RTD[:, :, :], w=["rta"])
        dma(p, nc, "sp", "d1", ioblk[:], cn["iota_blk"][:, 0:NBLK], w=["ioblk"])
        dma(p, nc, "sp", "d2", iop[:], cn["iota_p"][:, :], w=["iop"])
        p.op("pool", lambda i: nc.gpsimd.memset(onesf[:], 1.0), w=["onesf"])
        ts(p, nc, "dve", cnt[:], carry[:], 1.0 / 128.0, 0.49609375, ALU.mult, ALU.add, w=["cnt"])
        cp(p, nc, "dve", cnti[:], cnt[:], r=["cnt"], w=["cnti"])
        cp(p, nc, "dve", cnt[:], cnti[:], r=["cnti"], w=["cnt"])
        ts(p, nc, "dve", cnt[:], cnt[:], 128.0, None, ALU.mult, r=["cnt"], w=["cnt"])
        p.op("dve", lambda i: nc.vector.tensor_tensor_scan(pend[:], onesf[:], cnt[:], 0.0, ALU.mult, ALU.add), r=["onesf", "cnt"], w=["pend"])
        tt(p, nc, "dve", pst[:], pend[:], cnt[:], ALU.subtract, r=["pend", "cnt"], w=["pst"])
        cp(p, nc, "dve", dacc[:], rta[:, :, 4:6], r=["rta"], w=["dacc"])
        for e in range(NEXP):
            ts(p, nc, "dve", dtmp[:], rta[:, :, 0:2], float(e), None, ALU.is_equal, r=["rta", "dacc"], w=["dtmp"])
            p.op("dve", lambda i, e=e: nc.vector.scalar_tensor_tensor(dacc[:], dtmp[:], pst[:, e:e + 1], dacc[:], ALU.mult, ALU.add),
                 r=["dtmp", "pst", "dacc"], w=["dacc"])
        cp(p, nc, "dve", dint[:], dacc[:], r=["dacc"], w=["dint"])
        dma(p, nc, "sp", "d3", DESTD[:, :, :], dint[:], r=["dint"])
        p.op("pool", lambda i: nc.gpsimd.memset(bacc[:], 0.0), w=["bacc"])
        for e in range(NEXP):
            ts(p, nc, "dve", btmp[:], ioblk[:], pend[:, e:e + 1], None, ALU.is_ge, r=["ioblk", "pend", "bacc"], w=["btmp"])
            tt(p, nc, "dve", bacc[:], bacc[:], btmp[:], ALU.add, r=["bacc", "btmp"], w=["bacc"])
        ts(p, nc, "dve", bacc[:], bacc[:], float(NEXP - 1), None, ALU.min, r=["bacc"], w=["bacc"])
        OOBV = float(b.cfg["depth"] * NEXP * 128)
        p.op("pool", lambda i: nc.gpsimd.memset(btmp[:, 0:1], 1.0), r=["btmp"], w=["btmp"])
        p.op("pool", lambda i: nc.gpsimd.memset(btmp[:, 1:2], 1.0), r=["btmp"], w=["btmp"])
        tt(p, nc, "dve", btmp[:, 2:NBLK], bacc[:, 2:NBLK], bacc[:, 0:NBLK - 2], ALU.not_equal, r=["bacc", "btmp"], w=["btmp"])
        ts(p, nc, "dve", bacc[:], bacc[:], 128.0, float(l * NEXP * 128), ALU.mult, ALU.add, r=["bacc", "btmp"], w=["bacc"])
        tt(p, nc, "dve", bacc[:], bacc[:], iop[:].to_broadcast([128, NBLK]), ALU.add, r=["bacc", "iop"], w=["bacc"])
        if b.cfg.get("oobskip", True):
            ts(p, nc, "dve", bacc[:], bacc[:], -OOBV, None, ALU.add, r=["bacc"], w=["bacc"])
            tt(p, nc, "dve", bacc[:], bacc[:], btmp[:], ALU.mult, r=["bacc", "btmp"], w=["bacc"])
            ts(p, nc, "dve", bacc[:], bacc[:], OOBV, None, ALU.add, r=["bacc"], w=["bacc"])
        cp(p, nc, "dve", bint[:], bacc[:], r=["bacc"], w=["bint"])
        dma(p, nc, "sp", "d4", IDXD[:, :], bint[:], r=["bint"])
        p.run()
    if b.cfg.get("stop") == "moeD":
        return

    with ExitStack() as st:
        def sb(name, shape, dt=F32):
            return st.enter_context(nc.sbuf_tensor(un(b, name), list(shape), dt))
        hfs = [sb("hf0", [128, 1024]), sb("hf1", [128, 1024])]
        dis = [sb("di0", [128, 2], I32), sb("di1", [128, 2], I32)]
        p = Phase(kb, f"moeS{l}", n_iter=NTILES // 2)
        for q in range(2):
            p.ns = f"#{q}"
            hf, di = hfs[q], dis[q]
            dma(p, nc, "sp", "shf", hf[:], lambda i, q=q: HF[bass.ds(i * 256 + q * 128, 128), :], w=["hf"])
            dma(p, nc, "act", "sdi", di[:], lambda i, q=q: DESTD[:, bass.ds(i * 2 + q, 1), :].rearrange("p o c -> p (o c)"), w=["di"])
            for k in range(2):
                p.dma("pool", f"ssc{k}", lambda i, k=k, hf=hf, di=di: nc.gpsimd.indirect_dma_start(
                    out=XBUF[:, :], out_offset=bass.IndirectOffsetOnAxis(ap=di[:, k:k + 1], axis=0), in_=hf[:], in_offset=None),
                    r=["hf", "di"])
            if q == 0:
                n_half = len(p.ops)
        p.ns = ""
        p.interleave(0, n_half, len(p.ops))
        p.run()
    if b.cfg.get("stop") == "moeS":
        return

    NROWS = b.cfg["depth"] * NEXP * 128
    with ExitStack() as st:
        def sb(name, shape, dt=F32):
            return st.enter_context(nc.sbuf_tensor(un(b, name), list(shape), dt))

        def ps(name, shape, dt=F32):
            return st.enter_context(nc.psum_tensor(un(b, name), list(shape), dt))
        wfs = [sb("wf0", [128, 12288]), sb("wf1", [128, 12288])]
        psA = [ps("psA0", [128, 4, 128]), ps("psA1", [128, 4, 128])]
        psG = ps("psG", [128, 4, 128])
        psU = ps("psU", [128, 4, 128])
        psY = [ps("psY0", [128, 512]), ps("psY1", [128, 512])]
        sets = []
        for k in range(2):
            sets.append(dict(ix=sb(f"ix{k}", [128, 1], I32), wgb=sb(f"wgb{k}", [128, 8, 512], BF16), wub=sb(f"wub{k}", [128, 8, 512], BF16),
                             wdb=sb(f"wdb{k}", [128, 4, 1024], BF16), xb=sb(f"xb{k}", [128, 1024]), xT=sb(f"xT{k}", [128, 8, 128], BF16),
                             sg=sb(f"sg{k}", [128, 4, 128]), hT=sb(f"hT{k}", [128, 4, 128], BF16), yb=sb(f"yb{k}", [128, 1024])))
        p = Phase(kb, f"moeB{l}", n_iter=NBLK // 2)
        segs = []
        for k in range(2):
            p.ns = f"#{k}"
            T = sets[k]
            segs.append(len(p.ops))
            ix, wgb, wub, wdb, xb, xT, sg, hT, yb = [T[n_] for n_ in ["ix", "wgb", "wub", "wdb", "xb", "xT", "sg", "hT", "yb"]]
            dma(p, nc, "sp", "ix", ix[:], lambda i, k=k: IDXD[:, bass.ds(i * 2 + k, 1)], w=["ix"], slow=True)
            dma(p, nc, "act", "xb", xb[:], lambda i, k=k: XBUF[bass.ds(i * 256 + k * 128, 128), :], w=["xb"])
            wf = wfs[k]
            p.dma("pool", "wf", lambda i, ix=ix, wf=wf: nc.gpsimd.indirect_dma_start(
                out=wf[:], out_offset=None, in_=g["e_w"][:, :], in_offset=bass.IndirectOffsetOnAxis(ap=ix[:, 0:1], axis=0),
                bounds_check=NROWS - 1, oob_is_err=False),
                r=["ix"], w=["wf"])
            segs.append(len(p.ops))
            cp(p, nc, "dve", wgb[:].rearrange("p k n -> p (k n)"), wf[:, 0:4096], r=["wf"], w=["wgb"])
            cp(p, nc, "act", wub[:].rearrange("p k n -> p (k n)"), wf[:, 4096:8192], r=["wf"], w=["wub"])
            cp(p, nc, "dve", wdb[:, 0:2, :].rearrange("p k n -> p (k n)"), wf[:, 8192:10240], r=["wf"], w=["wdb0"])
            cp(p, nc, "act", wdb[:, 2:4, :].rearrange("p k n -> p (k n)"), wf[:, 10240:12288], r=["wf"], w=["wdb1"])
            segs.append(len(p.ops))
            for kc in range(8):
                tr(p, nc, psA[kc // 4][:, kc % 4, :], xb[:, kc * 128:(kc + 1) * 128], identf[:], r=["xb"], w=[f"!psA{kc // 4}"])
            cp(p, nc, "act", xT[:, 0:4, :], psA[0][:], r=["!psA0"], w=["xT0"])
            cp(p, nc, "dve", xT[:, 4:8, :], psA[1][:], r=["!psA1"], w=["xT1"])
            for c in range(4):
                for kc in range(8):
                    mm(p, nc, psG[:, c, :], wgb[:, kc, c * 128:(c + 1) * 128], xT[:, kc, :], kc == 0, kc == 7, r=["wgb", "xT0", "xT1"], w=["!psG"])
            for c in range(4):
                for kc in range(8):
                    mm(p, nc, psU[:, c, :], wub[:, kc, c * 128:(c + 1) * 128], xT[:, kc, :], kc == 0, kc == 7, r=["wub", "xT0", "xT1"], w=["!psU"])
            act(p, nc, sg[:], psG[:], AF.Silu, r=["!psG"], w=["sg"])
            tt(p, nc, "dve", hT[:], sg[:], psU[:], ALU.mult, r=["sg", "!psU"], w=["hT"])
            for n in range(2):
                for c in range(4):
                    mm(p, nc, psY[n][:], hT[:, c, :], wdb[:, c, n * 512:(n + 1) * 512], c == 0, c == 3, r=["hT", "wdb0", "wdb1"], w=[f"!psY{n}"])
            cp(p, nc, "act", yb[:, 0:512], psY[0][:], r=["!psY0"], w=["yb0"])
            cp(p, nc, "dve", yb[:, 512:1024], psY[1][:], r=["!psY1"], w=["yb1"])
            dma(p, nc, "sp", "yb", lambda i, k=k: YBUF[bass.ds(i * 256 + k * 128, 128), :], yb[:], r=["yb0", "yb1"])
        p.ns = ""
        a0, a1, a2, b0, b1, b2 = segs
        o_ = p.ops
        p.ops = o_[a0:a1] + o_[b0:b1] + o_[a1:a2] + o_[b1:b2] + o_[a2:b0] + o_[b2:]
        p.run()
    if b.cfg.get("stop") == "moeB":
        return

    with ExitStack() as st:
        def sb(name, shape, dt=F32):
            return st.enter_context(nc.sbuf_tensor(un(b, name), list(shape), dt))
        lng = sb("lng", [128, 1024])
        lnb = sb("lnb", [128, 1024])
        p = Phase(kb, f"moeCpre{l}")
        dma(p, nc, "sp", "c0", lng[:], g["ln_g"][l, 1:2, :].partition_broadcast(128), w=["lng"])
        dma(p, nc, "sp", "c1", lnb[:], g["ln_b"][l, 1:2, :].partition_broadcast(128), w=["lnb"])
        p.run()
        sets = []
        for k in range(2):
            sets.append(dict(xt=sb(f"xt{k}", [128, 1024]), g2b=sb(f"g2b{k}", [128, 1024]), rt=sb(f"rt{k}", [128, 8]),
                             di=sb(f"di{k}", [128, 2], I32), y0=sb(f"y0{k}", [128, 1024]), y1=sb(f"y1{k}", [128, 1024]),
                             z=sb(f"z{k}", [128, 1024]), sq=sb(f"sq{k}", [128, 1024]), xo=sb(f"xo{k}", [128, 1024]),
                             stt=sb(f"stt{k}", [128, 8])))
        p = Phase(kb, f"moeC{l}", n_iter=NTILES // 2)
        for k in range(2):
            p.ns = f"#{k}"
            T = sets[k]
            xt, g2b, rt, di, y0, y1, z, sq, xo, stt = [T[n_] for n_ in ["xt", "g2b", "rt", "di", "y0", "y1", "z", "sq", "xo", "stt"]]
            dma(p, nc, "sp", "xt", xt[:], lambda i, k=k: X[bass.ds(i * 256 + k * 128, 128), :], w=["xt"])
            dma(p, nc, "act", "g2b", g2b[:], lambda i, k=k: MODT[bass.ds(i * 2 + k, 1), 5120:6144].partition_broadcast(128), w=["g2b"])
            dma(p, nc, "act", "rt", rt[:], lambda i, k=k: RTD[:, bass.ds(i * 2 + k, 1), :].rearrange("p o c -> p (o c)"), w=["rt"])
            dma(p, nc, "sp", "di", di[:], lambda i, k=k: DESTD[:, bass.ds(i * 2 + k, 1), :].rearrange("p o c -> p (o c)"), w=["di"])
            for kk, yk in enumerate([y0, y1]):
                p.dma("pool", f"ga{kk}", lambda i, kk=kk, yk=yk, di=di: nc.gpsimd.indirect_dma_start(
                    out=yk[:], out_offset=None, in_=YBUF[:, :], in_offset=bass.IndirectOffsetOnAxis(ap=di[:, kk:kk + 1], axis=0)),
                    r=["di"], w=[f"y{kk}"])
            ts(p, nc, "dve", y0[:], y0[:], rt[:, 2:3], None, ALU.mult, r=["y0", "rt"], w=["y0"])
            p.op("dve", lambda i, y0=y0, y1=y1, rt=rt: nc.vector.scalar_tensor_tensor(y0[:], y1[:], rt[:, 3:4], y0[:], ALU.mult, ALU.add),
                 r=["y0", "y1", "rt"], w=["y0"])
            ts(p, nc, "pool", g2b[:], g2b[:], 1.0, None, ALU.add, r=["g2b"], w=["g2b"])
            tt(p, nc, "pool", y0[:], y0[:], g2b[:], ALU.mult, r=["y0", "g2b"], w=["y0"])
            p.op("dve", lambda i, z=z, xt=xt, y0=y0: nc.vector.scalar_tensor_tensor(z[:], xt[:], alpha, y0[:], ALU.mult, ALU.add),
                 r=["xt", "y0"], w=["z"])
            layer_norm_tail(b, p, z, "z", lng, lnb, xo, "xo", dict(sq=sq, st=stt), "ln")
            dma(p, nc, "sp", "xo", lambda i, k=k: X[bass.ds(i * 256 + k * 128, 128), :], xo[:], r=["xo"])
            if k == 0:
                n_half = len(p.ops)
        p.ns = ""
        p.interleave(0, n_half, len(p.ops))
        p.run()


def build_hgrn_layer(b, l, lr, g):
    nc, kb = b.nc, b.kb
    seqs = b.cfg["seqs"]
    NT = sum(seqs)
    NTILES = NT // 128
    depth = b.cfg["depth"]
    LR = depth // 2
    X, MODT, cn = g["X"], g["MODT"], g["cn"]
    identb = g["identb"]
    alpha = g["alpha"]
    seq_tile0 = g["seq_tile0"]
    if "QZ" not in b.dram:
        b.dscr("QZ", [24, 128, NT])
        b.dscr("VT", [NT, D], BF16)
        b.dscr("GT", [NT, D])
        b.dscr("OF", [NT, D])
        b.dscr("OB", [NT, D])
    QZ, VT, GT, OF, OB = [b.dram[k] for k in ["QZ", "VT", "GT", "OF", "OB"]]

    with ExitStack() as st:
        def sb(name, shape, dt=F32):
            return st.enter_context(nc.sbuf_tensor(un(b, name), list(shape), dt))

        def ps(name, shape, dt=F32):
            return st.enter_context(nc.psum_tensor(un(b, name), list(shape), dt))

        win = sb("win", [128, 8, 5120], BF16)
        with ExitStack() as st2:
            stg = [st2.enter_context(nc.sbuf_tensor(un(b, "stg0"), [128, 8, 512], F32)),
                   st2.enter_context(nc.sbuf_tensor(un(b, "stg1"), [128, 8, 512], F32))]
            p = Phase(kb, f"recw{l}")
            load_weight_bf16(b, p, win, g["r_win"][lr].rearrange("(kc p) n -> p kc n", p=128), 5120, "win", stg)
            p.run()
        xg = sb("xg", [128, 4, 1024])
        scb = sb("scb", [128, 1024])
        shb = sb("shb", [128, 1024])
        tmp = [sb("tmp0", [128, 1024]), sb("tmp1", [128, 1024])]
        hb = sb("hb", [128, 4, 1024], BF16)
        hT = sb("hT", [128, 8, 512], BF16)
        fq = sb("fq", [128, 8, 512])
        vo = sb("vo", [128, 4, 1024], BF16)
        go = sb("go", [128, 4, 1024])
        psT = [ps("psT0", [128, 8, 128], BF16), ps("psT1", [128, 8, 128], BF16)]
        pf = [ps(f"pf{k}", [128, 512]) for k in range(4)]
        NG = NT // 512
        p = Phase(kb, f"H1_{l}", n_iter=NG)
        dma(p, nc, "sp", "xg", xg[:], lambda i: X[bass.ds(i * 512, 512), :].rearrange("(t p) d -> p t d", p=128), w=["xg"])
        dma(p, nc, "act", "scb", scb[:], lambda i: MODT[bass.ds(i * 4, 1), 1024:2048].partition_broadcast(128), w=["scb"])
        dma(p, nc, "act", "shb", shb[:], lambda i: MODT[bass.ds(i * 4, 1), 0:1024].partition_broadcast(128), w=["shb"])
        ts(p, nc, "dve", scb[:], scb[:], 1.0, None, ALU.add, r=["scb"], w=["scb"])
        for t in range(4):
            tt(p, nc, "dve", tmp[t % 2][:], xg[:, t, :], scb[:], ALU.mult, r=["xg", "scb"], w=[f"tmp{t % 2}"])
            tt(p, nc, "pool", hb[:, t, :], tmp[t % 2][:], shb[:], ALU.add, r=[f"tmp{t % 2}", "shb"], w=[f"hb{t}"])
            for kc in range(8):
                tr(p, nc, psT[t % 2][:, kc, :], hb[:, t, kc * 128:(kc + 1) * 128], identb[:], r=[f"hb{t}"], w=[f"psT{t % 2}"])
            cp(p, nc, "act", hT[:, :, t * 128:(t + 1) * 128], psT[t % 2][:], r=[f"psT{t % 2}"], w=[f"hT{t}"])
        hTall = [f"hT{t}" for t in range(4)]
        pfi = 0
        for grp in range(3):
            for hh in range(8):
                c = grp * 8 + hh
                ia = pfi % 4
                pfi += 1
                for kc in range(8):
                    mm(p, nc, pf[ia][:], win[:, kc, c * 128:(c + 1) * 128], hT[:, kc, :], kc == 0, kc == 7, r=hTall, w=[f"pf{ia}"])
                cp(p, nc, "act" if hh % 2 == 0 else "dve", fq[:, hh, :], pf[ia][:], r=[f"pf{ia}"], w=[f"fq{hh}"])
            dma(p, nc, ["sp", "act", "pool"][grp], f"fq{grp}",
                lambda i, grp=grp: QZ[grp * 8:grp * 8 + 8, :, bass.ds(i * 512, 512)].rearrange("f p t -> p f t"), fq[:],
                r=[f"fq{hh}" for hh in range(8)])
        for t in range(4):
            for n in range(4):
                ia = pfi % 4
                pfi += 1
                col0 = 3072 + n * 512
                for kc in range(8):
                    mm(p, nc, pf[ia][:], hT[:, kc, t * 128:(t + 1) * 128], win[:, kc, col0:col0 + 512], kc == 0, kc == 7,
                       r=[f"hT{t}"], w=[f"pf{ia}"])
                if n < 2:
                    cp(p, nc, "act", vo[:, t, n * 512:(n + 1) * 512], pf[ia][:], r=[f"pf{ia}"], w=[f"vo{t}"])
                else:
                    cp(p, nc, "dve", go[:, t, (n - 2) * 512:(n - 1) * 512], pf[ia][:], r=[f"pf{ia}"], w=[f"go{t}"])
        dma(p, nc, "sp", "vo", lambda i: VT[bass.ds(i * 512, 512), :].rearrange("(t p) e -> p t e", p=128), vo[:],
            r=[f"vo{t}" for t in range(4)])
        dma(p, nc, "pool", "go", lambda i: GT[bass.ds(i * 512, 512), :].rearrange("(t p) e -> p t e", p=128), go[:],
            r=[f"go{t}" for t in range(4)])
        p.run()
    if b.cfg.get("stop") == "H1":
        return

    with ExitStack() as st:
        def sb(name, shape, dt=F32):
            return st.enter_context(nc.sbuf_tensor(un(b, name), list(shape), dt))

        def ps(name, shape, dt=F32):
            return st.enter_context(nc.psum_tensor(un(b, name), list(shape), dt))

        lbT = sb("lbT", [128, 8])
        omlT = sb("omlT", [128, 8])
        lbe = sb("lbe", [128, max(LR, 1), 8])
        lsum = sb("lsum", [128, 8])
        hm = sb("hm", [64, 2, 64], BF16)
        smask = sb("smask", [128, 1024])
        S = sb("S", [128, 8, 128])
        p = Phase(kb, f"recpre{l}")
        for j in range(LR):
            dma(p, nc, "sp", f"lb{j}", lbe[:, j, :], g["r_lb"][j].rearrange("(h k) -> k h", k=128), w=[f"lbe{j}"], slow=True)
            act(p, nc, lbe[:, j, :], lbe[:, j, :], AF.Exp, r=[f"lbe{j}"], w=[f"lbe{j}"])
        cp(p, nc, "dve", lsum[:], lbe[:, 0, :], r=["lbe0"], w=["lsum"])
        for j in range(1, LR):
            tt(p, nc, "dve", lsum[:], lsum[:], lbe[:, j, :], ALU.add, r=["lsum", f"lbe{j}"], w=["lsum"])
        p.op("dve", lambda i: nc.vector.reciprocal(lsum[:], lsum[:]), r=["lsum"], w=["lsum"])
        p.op("pool", lambda i: nc.gpsimd.memset(lbT[:], 0.0), w=["lbT"])
        for j in range(1, lr + 1):
            tt(p, nc, "dve", lbT[:], lbT[:], lbe[:, j, :], ALU.add, r=["lbT", f"lbe{j}"], w=["lbT"])
        tt(p, nc, "dve", lbT[:], lbT[:], lsum[:], ALU.mult, r=["lbT", "lsum"], w=["lbT"])
        ts(p, nc, "dve", omlT[:], lbT[:], -1.0, 1.0, ALU.mult, ALU.add, r=["lbT"], w=["omlT"])
        dma(p, nc, "sp", "hm", hm[:], cn["hmask"][:, :, :], w=["hm"])
        dma(p, nc, "sp", "smask", smask[:], cn["scanmask"][:, :], w=["smask"])
        p.run()

        from types import SimpleNamespace
        sets = []
        for k in range(2):
            T = SimpleNamespace(qt=sb(f"qt{k}", [128, 8, 128]), zt=sb(f"zt{k}", [128, 8, 128]), vt=sb(f"vt{k}", [64, 2, 1024], BF16),
                                u=sb(f"u{k}", [128, 8, 128]), w_=sb(f"w_{k}", [128, 8, 128]), f_=sb(f"f_{k}", [128, 8, 128]),
                                lf=sb(f"lf{k}", [128, 8, 128]), km=sb(f"km{k}", [128, 8, 128]), bb=sb(f"bb{k}", [128, 8, 128]),
                                bm=sb(f"bm{k}", [128, 8, 128]), Ee=sb(f"Ee{k}", [128, 8, 128]), Ei=sb(f"Ei{k}", [128, 8, 128]),
                                qe=sb(f"qe{k}", [128, 8, 128], BF16), ke=sb(f"ke{k}", [128, 8, 128], BF16), sc=sb(f"sc{k}", [128, 8, 2, 4]),
                                Sc=sb(f"Sc{k}", [128, 8, 128], BF16), AT=sb(f"AT{k}", [64, 8, 64], BF16), keT=sb(f"keT{k}", [64, 8, 128], BF16),
                                oc=sb(f"oc{k}", [64, 1024]))
            sets.append(T)
        psA = ps("psA", [64, 8, 64])
        psKT2 = [ps("psKT0", [64, 8, 128], BF16), ps("psKT1", [64, 8, 128], BF16)]
        pso = [ps("pso0", [64, 512]), ps("pso1", [64, 512])]
        psdS = [ps("psdS0", [128, 4, 128]), ps("psdS1", [128, 4, 128])]
        lb_bc = lbT[:].unsqueeze(2).to_broadcast([128, 8, 128])
        oml_bc = omlT[:].unsqueeze(2).to_broadcast([128, 8, 128])
        fl = lambda t_: t_[:].rearrange("p h t -> p (h t)")
        c4 = lambda t_: t_[:].rearrange("p h (c t) -> p h c t", t=64)

        def rec_body(p, tok0, dirn, OUT, T, marks):
            zbase = 8 if dirn == 0 else 16
            dma(p, nc, "sp", "qt", T.qt[:], lambda i: QZ[0:8, :, bass.ds(tok0(i), 128)].rearrange("f p t -> p f t"), w=["qt"])
            dma(p, nc, "act", "zt", T.zt[:], lambda i: QZ[zbase:zbase + 8, :, bass.ds(tok0(i), 128)].rearrange("f p t -> p f t"), w=["zt"])
            dma(p, nc, "pool", "vt", T.vt[:], lambda i: VT[bass.ds(tok0(i), 128), :].rearrange("(c p) e -> p c e", p=64), w=["vt"])
            act(p, nc, T.u[:], T.zt[:], AF.Exp, scale=-1.0, r=["zt"], w=["u"])
            ts(p, nc, "dve", T.w_[:], T.u[:], 1.0, None, ALU.add, r=["u"], w=["w_"])
            p.op("dve", lambda i: nc.vector.reciprocal(T.w_[:], T.w_[:]), r=["w_"], w=["w_"])
            tt(p, nc, "dve", T.f_[:], T.w_[:], oml_bc, ALU.mult, r=["w_"], w=["f_"])
            tt(p, nc, "dve", T.f_[:], T.f_[:], lb_bc, ALU.add, r=["f_"], w=["f_"])
            act(p, nc, T.lf[:], T.f_[:], AF.Ln, r=["f_"], w=["lf"])
            tt(p, nc, "pool", T.km[:], T.u[:], T.w_[:], ALU.mult, r=["u", "w_"], w=["km"])
            tt(p, nc, "pool", T.km[:], T.km[:], oml_bc, ALU.mult, r=["km"], w=["km"])
            p.op("dve", lambda i: nc.vector.tensor_tensor_scan(fl(T.bb), smask[:], fl(T.lf), 0.0, ALU.mult, ALU.add), r=["lf"], w=["bb"])
            if dirn == 1:
                tt(p, nc, "pool", T.bm[:], T.lf[:], T.bb[:], ALU.subtract, r=["lf", "bb"], w=["bm"])
                tt(p, nc, "pool", c4(T.bb), c4(T.bm), c4(T.bb)[:, :, :, 63:64].to_broadcast([128, 8, 2, 64]), ALU.add, r=["bm", "bb"], w=["bb"])
            endidx = 63 if dirn == 0 else 0
            cref = c4(T.bb)[:, :, :, 31:32]
            bend = c4(T.bb)[:, :, :, endidx:endidx + 1]
            act(p, nc, T.sc[:, :, :, 0:1], cref, AF.Exp, r=["bb"], w=["sc0"])
            act(p, nc, T.sc[:, :, :, 1:2], bend, AF.Exp, r=["bb"], w=["sc1"])
            tt(p, nc, "dve", T.sc[:, :, :, 3:4], bend, cref, ALU.subtract, r=["bb"], w=["sc3"])
            act(p, nc, T.sc[:, :, :, 2:3], T.sc[:, :, :, 3:4], AF.Exp, r=["sc3"], w=["sc2"])
            tt(p, nc, "pool", c4(T.bm), c4(T.bb), cref.to_broadcast([128, 8, 2, 64]), ALU.subtract, r=["bb", "bm"], w=["bm"])
            act(p, nc, T.Ee[:], T.bm[:], AF.Exp, r=["bm"], w=["Ee"])
            act(p, nc, T.Ei[:], T.bm[:], AF.Exp, scale=-1.0, r=["bm"], w=["Ei"])
            tt(p, nc, "dve", T.qe[:], T.qt[:], T.Ee[:], ALU.mult, r=["qt", "Ee"], w=["qe"])
            tt(p, nc, "pool", T.ke[:], T.km[:], T.Ei[:], ALU.mult, r=["km", "Ei"], w=["ke"])
            marks.append(len(p.ops))
            order = [0, 1] if dirn == 0 else [1, 0]
            for c in order:
                cs_ = slice(c * 64, (c + 1) * 64)
                for h in range(8):
                    p.op("act", lambda i, h=h, c=c, T=T: nc.scalar.activation(T.Sc[:, h, :], S[:, h, :], AF.Copy, scale=T.sc[:, h, c, 0:1]),
                         r=[f"!S{h}", "sc0"], w=[f"Sc{h}"])
                for h in range(8):
                    mm(p, nc, psA[:, h, :], T.ke[:, h, cs_], T.qe[:, h, cs_], True, True, r=["ke", "qe"], w=["!psA"])
                for h in range(8):
                    tt(p, nc, "dve", T.AT[:, h, :], psA[:, h, :], hm[:, dirn, :], ALU.mult, r=["!psA"], w=[f"AT{h}"])
                for h in range(8):
                    tr(p, nc, psKT2[h // 4][:, h % 4, :], T.ke[:, h, cs_], identb[:], r=["ke"], w=[f"!psKT{h // 4}"])
                for h in range(8):
                    cp(p, nc, "dve" if h < 4 else "act", T.keT[:, h, :], psKT2[h // 4][:, h % 4, :], r=[f"!psKT{h // 4}"], w=[f"keT{h}"])
                for h in range(8):
                    po = pso[h // 4][:, (h % 4) * 128:(h % 4 + 1) * 128]
                    mm(p, nc, po, T.AT[:, h, :], T.vt[:, c, h * 128:(h + 1) * 128], True, False, r=[f"AT{h}", "vt"], w=[f"!pso{h // 4}"])
                    mm(p, nc, po, T.qe[:, h, cs_], T.Sc[:, h, :], False, True, r=["qe", f"Sc{h}"], w=[f"!pso{h // 4}"])
                for h in range(8):
                    mm(p, nc, psdS[h // 4][:, h % 4, :], T.keT[:, h, :], T.vt[:, c, h * 128:(h + 1) * 128], True, True,
                       r=[f"keT{h}", "vt"], w=[f"!psdS{h // 4}"])
                for h in range(8):
                    ts(p, nc, "pool", S[:, h, :], S[:, h, :], T.sc[:, h, c, 1:2], None, ALU.mult, r=[f"!S{h}", "sc1"], w=[f"!S{h}"])
                for h in range(8):
                    p.op("dve", lambda i, h=h, c=c, T=T: nc.vector.scalar_tensor_tensor(S[:, h, :], psdS[h // 4][:, h % 4, :], T.sc[:, h, c, 2:3],
                                                                                         S[:, h, :], ALU.mult, ALU.add),
                         r=[f"!psdS{h // 4}", f"!S{h}", "sc2"], w=[f"!S{h}"])
                cp(p, nc, "act", T.oc[:, 0:512], pso[0][:], r=["!pso0"], w=["oc0"])
                cp(p, nc, "dve", T.oc[:, 512:1024], pso[1][:], r=["!pso1"], w=["oc1"])
                dma(p, nc, "sp", f"oc", lambda i, c=c: OUT[bass.ds(tok0(i) + c * 64, 64), :], T.oc[:], r=["oc0", "oc1"])

        for s, T_s in enumerate(seqs):
            nb = T_s // 128
            tok_s = seq_tile0[s] * 128
            for dirn in range(2):
                p = Phase(kb, f"recz{l}_{s}_{dirn}")
                p.op("pool", lambda i: nc.gpsimd.memset(S[:], 0.0), w=["S"])
                p.run()
                p = Phase(kb, f"H2_{l}_{s}_{dirn}", n_iter=nb // 2)
                marks = []
                for k in range(2):
                    p.ns = f"#{k}"
                    marks.append(len(p.ops))
                    if dirn == 0:
                        rec_body(p, lambda i, tok_s=tok_s, k=k: i * 256 + (tok_s + k * 128), 0, OF, sets[k], marks)
                    else:
                        rec_body(p, lambda i, tok_s=tok_s, nb=nb, k=k: (tok_s + (nb - 1 - k) * 128) - i * 256, 1, OB, sets[k], marks)
                p.ns = ""
                a0, a1, b0, b1 = marks
                o_ = p.ops
                prepA, chA, prepB, chB = o_[a0:a1], o_[a1:b0], o_[b0:b1], o_[b1:]
                z_ = []
                for q_ in range(max(len(prepA), len(prepB))):
                    if q_ < len(prepA):
                        z_.append(prepA[q_])
                    if q_ < len(prepB):
                        z_.append(prepB[q_])
                p.ops = z_ + chA + chB
                p.run()
    if b.cfg.get("stop") == "H2":
        return

    with ExitStack() as st:
        def sb(name, shape, dt=F32):
            return st.enter_context(nc.sbuf_tensor(un(b, name), list(shape), dt))

        def ps(name, shape, dt=F32):
            return st.enter_context(nc.psum_tensor(un(b, name), list(shape), dt))
        wout = sb("wout", [128, 8, 1024], BF16)
        gnb = sb("gnb", [128, 1024])
        lng = sb("lng", [128, 1024])
        lnb = sb("lnb", [128, 1024])
        with ExitStack() as st2:
            stg = [st2.enter_context(nc.sbuf_tensor(un(b, "stg0"), [128, 8, 512], F32)),
                   st2.enter_context(nc.sbuf_tensor(un(b, "stg1"), [128, 8, 512], F32))]
            p = Phase(kb, f"recw2{l}")
            load_weight_bf16(b, p, wout, g["r_wout"][lr].rearrange("(kc p) n -> p kc n", p=128), 1024, "wout", stg)
            dma(p, nc, "pool", "gnb", gnb[:], g["r_gn"][lr:lr + 1, :].partition_broadcast(128), w=["gnb"])
            dma(p, nc, "pool", "lng", lng[:], g["ln_g"][l, 0:1, :].partition_broadcast(128), w=["lng"])
            dma(p, nc, "pool", "lnb", lnb[:], g["ln_b"][l, 0:1, :].partition_broadcast(128), w=["lnb"])
            p.run()
        from types import SimpleNamespace
        sets = []
        for k in range(2):
            T = SimpleNamespace(of=sb(f"of{k}", [128, 1024]), ob=sb(f"ob{k}", [128, 1024]), gt=sb(f"gt{k}", [128, 1024]),
                                xt=sb(f"xt{k}", [128, 1024]), g1b=sb(f"g1b{k}", [128, 1024]), sq=sb(f"sq{k}", [128, 1024]),
                                ms=sb(f"ms{k}", [128, 8]), on=sb(f"on{k}", [128, 1024], BF16), onT=sb(f"onT{k}", [128, 8, 128], BF16),
                                t1=sb(f"t1{k}", [128, 1024]), z=sb(f"z{k}", [128, 1024]), xo=sb(f"xo{k}", [128, 1024]),
                                stt=sb(f"stt{k}", [128, 8]), psOT=ps(f"psOT{k}", [128, 8, 128], BF16),
                                psY=[ps(f"psY0{k}", [128, 512]), ps(f"psY1{k}", [128, 512])])
            sets.append(T)
        p = Phase(kb, f"H3_{l}", n_iter=NTILES // 2)
        for k in range(2):
            p.ns = f"#{k}"
            T = sets[k]
            dma(p, nc, "sp", "of", T.of[:], lambda i, k=k, T=T: OF[bass.ds(i * 256 + k * 128, 128), :], w=["of"])
            dma(p, nc, "act", "ob", T.ob[:], lambda i, k=k, T=T: OB[bass.ds(i * 256 + k * 128, 128), :], w=["ob"])
            dma(p, nc, "pool", "gt", T.gt[:], lambda i, k=k, T=T: GT[bass.ds(i * 256 + k * 128, 128), :], w=["gt"])
            dma(p, nc, "sp", "xt", T.xt[:], lambda i, k=k, T=T: X[bass.ds(i * 256 + k * 128, 128), :], w=["xt"])
            dma(p, nc, "act", "g1b", T.g1b[:], lambda i, k=k, T=T: MODT[bass.ds(i * 2 + k, 1), 2048:3072].partition_broadcast(128), w=["g1b"])
            tt(p, nc, "dve", T.of[:], T.of[:], T.ob[:], ALU.add, r=["of", "ob"], w=["of"])
            tt(p, nc, "pool", T.sq[:], T.of[:], T.of[:], ALU.mult, r=["of"], w=["sq"])
            p.op("dve", lambda i, k=k, T=T: nc.vector.tensor_reduce(T.ms[:], T.sq[:].rearrange("p (h v) -> p h v", v=128), AX.X, ALU.add), r=["sq"], w=["ms"])
            ts(p, nc, "dve", T.ms[:], T.ms[:], 1.0 / 128.0, None, ALU.mult, r=["ms"], w=["ms"])
            act(p, nc, T.ms[:], T.ms[:], AF.Ln, bias=b.eps_ln[:, 1:2], r=["ms"], w=["ms"])
            act(p, nc, T.ms[:], T.ms[:], AF.Exp, scale=-0.5, r=["ms"], w=["ms"])
            tt(p, nc, "dve", T.sq[:].rearrange("p (h v) -> p h v", v=128), T.of[:].rearrange("p (h v) -> p h v", v=128),
               T.ms[:].unsqueeze(2).to_broadcast([128, 8, 128]), ALU.mult, r=["of", "ms", "sq"], w=["sq"])
            tt(p, nc, "pool", T.sq[:], T.sq[:], gnb[:], ALU.mult, r=["sq"], w=["sq"])
            act(p, nc, T.gt[:], T.gt[:], AF.Silu, r=["gt"], w=["gt"])
            tt(p, nc, "dve", T.on[:], T.sq[:], T.gt[:], ALU.mult, r=["sq", "gt"], w=["on"])
            for kc in range(8):
                tr(p, nc, T.psOT[:, kc, :], T.on[:, kc * 128:(kc + 1) * 128], identb[:], r=["on"], w=["psOT"])
            cp(p, nc, "act", T.onT[:], T.psOT[:], r=["psOT"], w=["onT"])
            for n in range(2):
                for kc in range(8):
                    mm(p, nc, T.psY[n][:], T.onT[:, kc, :], wout[:, kc, n * 512:(n + 1) * 512], kc == 0, kc == 7, r=["onT"], w=[f"psY{n}"])
            ts(p, nc, "pool", T.g1b[:], T.g1b[:], 1.0, None, ALU.add, r=["g1b"], w=["g1b"])
            for n in range(2):
                tt(p, nc, "dve", T.t1[:, n * 512:(n + 1) * 512], T.psY[n][:], T.g1b[:, n * 512:(n + 1) * 512], ALU.mult,
                   r=[f"psY{n}", "g1b"], w=["t1"])
            p.op("dve", lambda i, k=k, T=T: nc.vector.scalar_tensor_tensor(T.z[:], T.xt[:], alpha, T.t1[:], ALU.mult, ALU.add), r=["xt", "t1"], w=["z"])
            layer_norm_tail(b, p, T.z, "z", lng, lnb, T.xo, "xo", dict(sq=T.sq, st=T.stt), "ln")
            dma(p, nc, "sp", "xo", lambda i, k=k, T=T: X[bass.ds(i * 256 + k * 128, 128), :], T.xo[:], r=["xo"])
            if k == 0:
                n_half = len(p.ops)
        p.ns = ""
        p.interleave(0, n_half, len(p.ops))
        p.run()
```

```python
import math
from collections import Counter
from contextlib import ExitStack
import numpy as np
import ml_dtypes
import concourse.bass as bass
import concourse.mybir as mybir
from concourse.bass_utils import run_bass_kernel_spmd

F32 = mybir.dt.float32
BF16 = mybir.dt.bfloat16
I32 = mybir.dt.int32
AF = mybir.ActivationFunctionType
ALU = mybir.AluOpType
AX = mybir.AxisListType

D = 1024
HD = 64
N_GROUPS = 4
EPG = 8
NEXP = 32
DEXP = 512
LN_EPS = 1e-5
RMS_EPS = 1e-6
ROPE_THETA = 10000.0


class KB:
    def __init__(self, nc):
        self.nc = nc
        self.engs = {"pe": nc.tensor, "act": nc.scalar, "dve": nc.vector, "pool": nc.gpsimd, "sp": nc.sync}
        self.sems = {}
        self.base = {}
        for e in self.engs:
            self.sems[("e", e)] = nc.alloc_semaphore(name="S_" + e)
            self.base[("e", e)] = 0
        self.chan_owner = {}
        self.nphase = 0

    def sig(self, key):
        if key not in self.sems:
            self.sems[key] = self.nc.alloc_semaphore(name="C_" + str(key[1]))
            self.base[key] = 0
        return self.sems[key]


class Phase:
    def __init__(self, kb, name, n_iter=None):
        self.kb = kb
        self.name = name
        self.n_iter = n_iter
        self.ops = []

    ns = ""

    def _n(self, name):
        return name[1:] if name.startswith("!") else name + self.ns

    def op(self, eng, fn, r=(), w=(), chan=None):
        self.ops.append(dict(eng=eng, fn=fn, r=tuple(self._n(x) for x in r), w=tuple(self._n(x) for x in w),
                             chan=(chan + self.ns) if chan else None))

    def dma(self, eng, chan, fn, r=(), w=()):
        self.op(eng, fn, r, w, chan=chan)

    def interleave(self, a, b_, c):
        A, Bq = self.ops[a:b_], self.ops[b_:c]
        out = []
        for k in range(max(len(A), len(Bq))):
            if k < len(A):
                out.append(A[k])
            if k < len(Bq):
                out.append(Bq[k])
        self.ops[a:c] = out

    def run(self):
        kb = self.kb
        nc = kb.nc
        ops = self.ops
        m = len(ops)
        if m == 0:
            return
        loop = self.n_iter is not None
        N = self.n_iter if loop else 1
        sig_of = [("c", o["chan"]) if o["chan"] else ("e", o["eng"]) for o in ops]
        for j, o in enumerate(ops):
            if o["chan"]:
                own = kb.chan_owner.setdefault(o["chan"], o["eng"])
                assert own == o["eng"], f"chan {o['chan']} issued from two engines"
        phys = {}
        kcnt = {"sw": 0, "hw": 0}
        for j, o in enumerate(ops):
            sg_ = sig_of[j]
            if sg_ in phys:
                continue
            if sg_[0] == "e":
                phys[sg_] = sg_
            else:
                kind = "sw" if o["eng"] == "pool" else "hw"
                phys[sg_] = ("c", f"{kind}{kcnt[kind]}")
                kcnt[kind] += 1
        per_iter = Counter(sig_of)
        kidx = []
        cnt = Counter()
        for j in range(m):
            kidx.append(cnt[sig_of[j]])
            cnt[sig_of[j]] += 1
        last_w = {}
        readers = {}
        deps = [set() for _ in range(m)]
        for it in ((0, 1) if loop else (0,)):
            for j, o in enumerate(ops):
                d = set()
                for b in o["r"]:
                    if b in last_w:
                        d.add(last_w[b] + ("raw",))
                for b in o["w"]:
                    if b in last_w:
                        d.add(last_w[b] + ("waw",))
                    for rd in readers.get(b, []):
                        d.add(rd + ("war",))
                if it == (1 if loop else 0):
                    deps[j] = {(jj, itt - it, ty) for (itt, jj, ty) in d if (itt, jj) != (it, j)}
                for b in o["r"]:
                    readers.setdefault(b, []).append((it, j))
                for b in o["w"]:
                    last_w[b] = (it, j)
                    readers[b] = []
        comp_idx = {}
        for j, o in enumerate(ops):
            if not o["chan"]:
                comp_idx.setdefault(o["eng"], []).append(j)
        cpos = {j: k for lst in comp_idx.values() for k, j in enumerate(lst)}
        waits = []
        for j, o in enumerate(ops):
            need = {}
            for (jj, off, ty) in deps[j]:
                p = ops[jj]
                s = sig_of[jj]
                if s[0] == "e" and sig_of[j][0] == "e" and p["eng"] == o["eng"]:
                    if o["eng"] == "pe" or ty != "raw":
                        continue
                    if o["eng"] in ("dve", "act"):
                        lst = comp_idx[o["eng"]]
                        if off == 0:
                            adjacent = (cpos[j] - cpos[jj] == 1)
                        else:
                            adjacent = (cpos[j] == 0 and cpos[jj] == len(lst) - 1)
                        if not adjacent:
                            continue
                n_s = per_iter[s]
                if s[0] == "c":
                    if off == 0:
                        q = sum(1 for x in range(j) if sig_of[x] == s)
                        const = q * 16
                    else:
                        const = 0
                else:
                    const = (kidx[jj] + 1) if off == 0 else (kidx[jj] + 1 - n_s)
                if s not in need or const > need[s]:
                    need[s] = const
            waits.append(need)
        known = {}
        for j, o in enumerate(ops):
            kn = known.setdefault(o["eng"], {})
            for s in list(waits[j]):
                if s in kn and kn[s] >= waits[j][s]:
                    del waits[j][s]
                else:
                    kn[s] = waits[j][s]
        T0 = {}
        for s, n_s in per_iter.items():
            kb.sig(phys[s])
            inc = 16 if s[0] == "c" else 1
            T0[s] = kb.base[phys[s]] + (n_s * inc if (loop and s[0] == "e") else 0)
        engines_used = []
        for o in ops:
            if o["eng"] not in engines_used:
                engines_used.append(o["eng"])

        def free_ids(E):
            hs, ids = [], []
            while True:
                try:
                    kb.probe_cnt = getattr(kb, "probe_cnt", 0) + 1
                    h = E.alloc_register(f"probe{kb.probe_cnt}")
                except Exception:
                    break
                hs.append(h)
                ids.append(nc.lookup_reg(h).reg_id)
            for h in hs:
                E.free_register(h)
            return set(ids)

        def emit_engine(ename):
            E = kb.engs[ename]
            before = free_ids(E)
            emit_engine_inner(ename)
            leaked = sorted(before - free_ids(E))
            for rid in leaked:
                kb.probe_cnt += 1
                h = nc.add_register(E.engine, f"leak{kb.probe_cnt}", rid)
                E.free_register(h)

        def emit_engine_inner(ename):
            E = kb.engs[ename]
            mine = [j for j, o in enumerate(ops) if o["eng"] == ename]
            wsigs = []
            for j in mine:
                for s in waits[j]:
                    if s not in wsigs:
                        wsigs.append(s)

            def body(i, regs, tmp):
                for j in mine:
                    o = ops[j]
                    for s, const in waits[j].items():
                        if i is None:
                            E.wait_ge(kb.sig(phys[s]), T0[s] + const)
                        elif const == 0:
                            E.wait_ge(kb.sig(phys[s]), regs[s])
                        else:
                            E.reg_add(tmp, regs[s], const)
                            E.wait_ge(kb.sig(phys[s]), tmp)
                    ins = o["fn"](i)
                    s = sig_of[j]
                    ins.then_inc(kb.sig(phys[s]), 16 if s[0] == "c" else 1)

            if loop:
                if ("e", ename) in per_iter:
                    E.sem_inc(kb.sig(("e", ename)), per_iter[("e", ename)])
                regs = {}
                for k, s in enumerate(wsigs):
                    regs[s] = E.alloc_register(f"w{kb.nphase}_{ename}_{k}")
                    E.reg_mov(regs[s], T0[s])
                tmp = E.alloc_register(f"wt{kb.nphase}_{ename}")
                with E.Fori(0, N) as i:
                    body(i, regs, tmp)
                    for s in wsigs:
                        E.reg_add(regs[s], regs[s], per_iter[s] * (16 if s[0] == "c" else 1))
                for s in wsigs:
                    E.free_register(regs[s])
                E.free_register(tmp)
            else:
                body(None, None, None)
            for s, n_s in per_iter.items():
                owner = kb.chan_owner[s[1]] if s[0] == "c" else s[1]
                if owner == ename:
                    inc = 16 if s[0] == "c" else 1
                    E.wait_ge(kb.sig(phys[s]), T0[s] + N * n_s * inc)

        with nc.Block() as blk:
            reg = {"pe": blk.tensor, "act": blk.scalar, "dve": blk.vector, "pool": blk.gpsimd, "sp": blk.sync}
            for ename in engines_used:
                reg[ename](lambda eng, ename=ename: emit_engine(ename))
        for s, n_s in per_iter.items():
            inc = 16 if s[0] == "c" else 1
            kb.base[phys[s]] = T0[s] + N * n_s * inc
        nc.all_engine_barrier()
        kb.nphase += 1


def _bf(a):
    return np.ascontiguousarray(a).astype(ml_dtypes.bfloat16)


def host_constants(cfg):
    seqs = cfg["seqs"]
    NT = sum(seqs)
    c = {}
    c["ident_f"] = np.eye(128, dtype=np.float32)
    c["ident_b"] = _bf(np.eye(128, dtype=np.float32))
    pos = np.concatenate([np.arange(t) for t in seqs]).astype(np.float32)
    inv = np.power(ROPE_THETA, -np.arange(0, HD, 2, dtype=np.float32) / HD).astype(np.float32)
    d = np.arange(128) % 64
    ang = pos[None, :] * inv[d % 32][:, None]
    c["rope_cos"] = np.cos(ang).astype(np.float32)
    sgn = np.where(d < 32, -1.0, 1.0).astype(np.float32)
    c["rope_sin"] = (np.sin(ang) * sgn[:, None]).astype(np.float32)
    j = np.arange(128)[:, None]
    i = np.arange(128)[None, :]
    c["wmask"] = _bf(np.stack([(j >= i), (j <= i)], axis=1).astype(np.float32))
    kc = np.arange(64)
    qc = np.arange(64)
    qwin = np.clip(qc - 8, 0, 48)
    col_ok = (kc[:, None] >= qwin[None, :]) & (kc[:, None] < qwin[None, :] + 16)
    m = np.zeros((128, 9, 128), np.float32)
    for t in range(9):
        for b in range(2):
            for a in range(2):
                ok = col_ok.astype(np.float32)
                if t == 7 and (a == 1 and b == 0):
                    ok = ok * 0
                if t == 8 and not (a == 1 and b == 0):
                    ok = ok * 0
                m[b * 64:(b + 1) * 64, t, a * 64:(a + 1) * 64] = ok
    c["namask"] = m
    s = np.arange(64)[:, None]
    t = np.arange(64)[None, :]
    c["hmask"] = _bf(np.stack([(s <= t), (s >= t)], axis=1).astype(np.float32))
    sm = np.ones((128, 8, 128), np.float32)
    sm[:, :, 0] = 0
    sm[:, :, 64] = 0
    c["scanmask"] = sm.reshape(128, 1024)
    c["iota32"] = np.tile(np.arange(32, dtype=np.float32)[None, :], (128, 1))
    c["iota_blk"] = np.tile((np.arange(256, dtype=np.float32) * 128.0)[None, :], (128, 1))
    c["iota_p"] = np.arange(128, dtype=np.float32).reshape(128, 1)
    c["tri_su"] = _bf((np.arange(128)[:, None] < np.arange(128)[None, :]).astype(np.float32))
    c["ones_b"] = _bf(np.ones((128, 128), np.float32))
    return c


NA_TABLE_DELTA = [-3, -2, -1, 0, 1, 2, 3, -2, 2]


def na_bias_tables(rel_bias):
    L = rel_bias.shape[0]
    kc = np.arange(64)[:, None]
    qc = np.arange(64)[None, :]
    dc = np.clip(kc - qc, -15, 15) + 15
    out = np.zeros((L, 128, 9, 8, 128), np.float32)
    for t, dl in enumerate(NA_TABLE_DELTA):
        for b in range(2):
            for a in range(2):
                dr = 2 * dl + b - a + 7
                g = rel_bias[:, :, dr, :][:, :, dc]
                out[:, b * 64:(b + 1) * 64, t, :, a * 64:(a + 1) * 64] = g.transpose(0, 2, 1, 3)
    return out


def attn_w_layout(w_in):
    L = w_in.shape[0]
    qa0, ka0, va0, qb0, kb0, vb0 = 0, 512, 640, 768, 1280, 1792
    sw = (np.arange(64) + 32) % 64
    cols = []
    for jc in range(4):
        for h in (jc, 4 + jc):
            cols += list(qa0 + h * 64 + np.arange(64))
    for jc in range(4):
        for h in (jc, 4 + jc):
            cols += list(qa0 + h * 64 + sw)
    for h in range(2):
        cols += list(ka0 + h * 64 + np.arange(64))
    for h in range(2):
        cols += list(ka0 + h * 64 + sw)
    cols += list(qb0 + np.arange(512))
    cols += list(kb0 + np.arange(512))
    cols = np.array(cols)
    vcols = np.concatenate([va0 + np.arange(128), vb0 + np.arange(512)])
    return np.ascontiguousarray(w_in[:, :, cols]), np.ascontiguousarray(w_in[:, :, vcols])


class B:
    def __init__(self, cfg):
        self.cfg = cfg
        self.nc = bass.Bass("TRN2", target_bir_lowering=False)
        self.kb = KB(self.nc)
        self.dram = {}
        self.uid = 0

    def din(self, name, shape, dt=F32):
        t = self.nc.dram_tensor(name, list(shape), dt, kind="ExternalInput")
        self.dram[name] = t
        return t

    def dout(self, name, shape, dt=F32):
        t = self.nc.dram_tensor(name, list(shape), dt, kind="ExternalOutput")
        self.dram[name] = t
        return t

    def dscr(self, name, shape, dt=F32):
        if self.cfg.get("debug"):
            return self.dout(name, shape, dt)
        t = self.nc.dram_tensor(name, list(shape), dt)
        self.dram[name] = t
        return t


def un(b, name):
    b.uid += 1
    return f"{name}_{b.uid}"


def _ap(x, i):
    return x(i) if callable(x) else x


def dma(p, nc, eng, chan, out, in_, r=(), w=(), slow=False):
    E = {"sp": nc.sync, "act": nc.scalar, "pool": nc.gpsimd}[eng]
    if slow:
        p.dma(eng, chan, lambda i: E.dma_start(out=_ap(out, i), in_=_ap(in_, i), allow_slow_non_contiguous=True), r=r, w=w)
    else:
        p.dma(eng, chan, lambda i: E.dma_start(out=_ap(out, i), in_=_ap(in_, i)), r=r, w=w)


def eng_of(nc, e):
    return {"dve": nc.vector, "pool": nc.gpsimd, "act": nc.scalar}[e]


def tt(p, nc, e, out, in0, in1, op, r=(), w=()):
    E = eng_of(nc, e)
    p.op(e, lambda i: E.tensor_tensor(_ap(out, i), _ap(in0, i), _ap(in1, i), op), r=r, w=w)


def ts(p, nc, e, out, in0, s1, s2, op0, op1=None, r=(), w=()):
    E = eng_of(nc, e)
    if op1 is None:
        p.op(e, lambda i: E.tensor_scalar(_ap(out, i), _ap(in0, i), s1, None, op0), r=r, w=w)
    else:
        p.op(e, lambda i: E.tensor_scalar(_ap(out, i), _ap(in0, i), s1, s2, op0, op1), r=r, w=w)


def cp(p, nc, e, out, in_, r=(), w=()):
    if e == "act":
        p.op(e, lambda i: nc.scalar.copy(_ap(out, i), _ap(in_, i)), r=r, w=w)
    else:
        E = eng_of(nc, e)
        p.op(e, lambda i: E.tensor_copy(_ap(out, i), _ap(in_, i)), r=r, w=w)


def act(p, nc, out, in_, func, bias=None, scale=None, r=(), w=()):
    kw = {}
    if bias is not None:
        kw["bias"] = bias
    if scale is not None:
        kw["scale"] = scale
    p.op("act", lambda i: nc.scalar.activation(_ap(out, i), _ap(in_, i), func, **kw), r=r, w=w)


def mm(p, nc, out, lhsT, rhs, start, stop, r=(), w=()):
    p.op("pe", lambda i: nc.tensor.matmul(_ap(out, i), lhsT=_ap(lhsT, i), rhs=_ap(rhs, i), start=start, stop=stop), r=r, w=w)


def tr(p, nc, out, in_, ident, r=(), w=()):
    p.op("pe", lambda i: nc.tensor.transpose(_ap(out, i), _ap(in_, i), ident), r=r, w=w)


def load_weight_bf16(b, p, dst, src, ncols, tag, stg, kdim=8):
    nc = b.nc
    engs = ["dve", "act", "pool"]
    c0 = 0
    k = 0
    while c0 < ncols:
        cw = min(512, ncols - c0)
        st = stg[k % 2]
        sname = f"stg{k % 2}"
        dma(p, nc, "sp" if k % 2 == 0 else "act", f"wstg{k % 2}", st[:, :, 0:cw], src[:, :, c0:c0 + cw], w=[sname])
        cp(p, nc, engs[k % 3], dst[:, :, c0:c0 + cw], st[:, :, 0:cw], r=[sname], w=[tag + str(k)])
        c0 += cw
        k += 1


def layer_norm_tail(b, p, z, zname, lng, lnb, out, outname, scr, tg):
    nc = b.nc
    sq, st = scr["sq"], scr["st"]
    p.op("dve", lambda i: nc.vector.tensor_reduce(st[:, 0:1], z[:], AX.X, ALU.add), r=[zname], w=[tg + "s0"])
    tt(p, nc, "pool", sq[:], z[:], z[:], ALU.mult, r=[zname], w=[tg + "sq"])
    p.op("dve", lambda i: nc.vector.tensor_reduce(st[:, 1:2], sq[:], AX.X, ALU.add), r=[tg + "sq"], w=[tg + "s1"])
    ts(p, nc, "dve", st[:, 2:3], st[:, 0:1], 1.0 / D, None, ALU.mult, r=[tg + "s0"], w=[tg + "s2"])
    ts(p, nc, "dve", st[:, 3:4], st[:, 1:2], 1.0 / D, None, ALU.mult, r=[tg + "s1"], w=[tg + "s3"])
    tt(p, nc, "dve", st[:, 4:5], st[:, 2:3], st[:, 2:3], ALU.mult, r=[tg + "s2"], w=[tg + "s4"])
    tt(p, nc, "dve", st[:, 5:6], st[:, 3:4], st[:, 4:5], ALU.subtract, r=[tg + "s3", tg + "s4"], w=[tg + "s5"])
    act(p, nc, st[:, 6:7], st[:, 5:6], AF.Ln, bias=b.eps_ln[:, 0:1], r=[tg + "s5"], w=[tg + "s6"])
    act(p, nc, st[:, 6:7], st[:, 6:7], AF.Exp, scale=-0.5, r=[tg + "s6"], w=[tg + "s6"])
    p.op("dve", lambda i: nc.vector.scalar_tensor_tensor(st[:, 7:8], st[:, 2:3], -1.0, st[:, 6:7], ALU.mult, ALU.mult),
         r=[tg + "s2", tg + "s6"], w=[tg + "s7"])
    act(p, nc, sq[:], z[:], AF.Identity, bias=st[:, 7:8], scale=st[:, 6:7], r=[zname, tg + "s6", tg + "s7", tg + "sq"], w=[tg + "sq"])
    tt(p, nc, "dve", sq[:], sq[:], lng[:], ALU.mult, r=[tg + "sq"], w=[tg + "sq"])
    tt(p, nc, "pool", out[:], sq[:], lnb[:], ALU.add, r=[tg + "sq"], w=[outname])


def build_program(cfg):
    seqs = cfg["seqs"]
    NT = sum(seqs)
    NTILES = NT // 128
    depth = cfg["depth"]
    LA = (depth + 1) // 2
    LR = depth // 2
    stop = cfg.get("stop")
    alpha = float((2 * cfg.get("dn_depth", depth)) ** 0.25)
    b = B(cfg)
    nc = b.nc
    kb = b.kb

    x_in = b.din("x", [NT, D])
    cT_in = b.din("cT", [128, 3, 8])
    ada_w = b.din("ada_w", [depth, D, 6 * D])
    ada_b = b.din("ada_b", [depth, 6 * D])
    ln_g = b.din("ln_g", [depth, 2, D])
    ln_b = b.din("ln_b", [depth, 2, D])
    a_wfm = b.din("attn_wfm", [LA, D, 2304])
    a_wv = b.din("attn_wv", [LA, D, 640])
    a_sink = b.din("attn_sink", [LA, 8])
    a_rb = b.din("na_rb", [LA, 128, 9, 8, 128])
    a_wout = b.din("attn_w_out", [LA, D, D])
    r_win = b.din("rec_w_in", [max(LR, 1), D, 5 * D])
    r_lb = b.din("rec_lb", [max(LR, 1), D])
    r_gn = b.din("rec_gnorm", [max(LR, 1), D])
    r_wout = b.din("rec_w_out", [max(LR, 1), D, D])
    ro_w = b.din("router_w", [depth, D, 36])
    ro_b = b.din("router_b", [depth, 36])
    e_w = b.din("e_w", [depth * NEXP * 128, 12288])
    cn = {}
    for name, shape, dt in [("ident_f", [128, 128], F32), ("ident_b", [128, 128], BF16), ("rope_cos", [128, NT], F32),
                            ("rope_sin", [128, NT], F32), ("wmask", [128, 2, 128], BF16), ("namask", [128, 9, 128], F32),
                            ("hmask", [64, 2, 64], BF16), ("scanmask", [128, 1024], F32), ("iota32", [128, 32], F32),
                            ("iota_blk", [128, 256], F32), ("iota_p", [128, 1], F32), ("tri_su", [128, 128], BF16),
                            ("ones_b", [128, 128], BF16)]:
        cn[name] = b.din(name, shape, dt)
    y_out = b.dout("y", [NT, D])
    X = b.dscr("X", [NT, D])
    MODT = b.dscr("MODT", [NTILES, 6 * D])
    FM = b.dscr("FM", [13, 128, NT], BF16)
    VE = b.dscr("VE", [NT, 720], BF16)

    ODBG = b.dout("ODBG", [NT, D], BF16) if cfg.get("debug") else None
    seq_tile0 = [0]
    for t in seqs[:-1]:
        seq_tile0.append(seq_tile0[-1] + t // 128)

    with ExitStack() as gs:
        def sb(name, shape, dt=F32, stack=gs):
            return stack.enter_context(nc.sbuf_tensor(un(b, name), list(shape), dt))

        def ps(name, shape, dt=F32, stack=gs):
            return stack.enter_context(nc.psum_tensor(un(b, name), list(shape), dt))

        identf = sb("identf", [128, 128])
        identb = sb("identb", [128, 128], BF16)
        CS = sb("CS", [128, 24, 128])
        epst = sb("epst", [128, 2])
        b.eps_ln = epst
        csb = sb("csb", [128, 24])
        p = Phase(kb, "setup")
        p.op("pool", lambda i: nc.gpsimd.memset(epst[:, 0:1], LN_EPS), w=["epst0"])
        p.op("pool", lambda i: nc.gpsimd.memset(epst[:, 1:2], RMS_EPS), w=["epst1"])
        dma(p, nc, "sp", "k0", identf[:], cn["ident_f"][:, :], w=["identf"])
        dma(p, nc, "sp", "k1", identb[:], cn["ident_b"][:, :], w=["identb"])
        dma(p, nc, "sp", "k2", csb[:], cT_in.ap().rearrange("p s k -> p (s k)"), w=["csb"])
        act(p, nc, csb[:], csb[:], AF.Silu, r=["csb"], w=["csb"])
        cp(p, nc, "dve", CS[:], csb[:].unsqueeze(2).to_broadcast([128, 24, 128]), r=["csb"], w=["CS"])
        p.run()

        p = Phase(kb, "xcopy")
        nchunk = max(1, NT // 1024)
        rows = NT // nchunk
        for k in range(nchunk):
            dma(p, nc, ["sp", "act", "pool"][k % 3], f"xc{k % 3}", X[k * rows:(k + 1) * rows, :], x_in[k * rows:(k + 1) * rows, :])
        p.run()

        for l in range(depth):
            with ExitStack() as st:
                aw = sb("aw", [128, 8, 512], stack=st)
                ab = sb("ab", [128, 512], stack=st)
                mo = [sb(f"mo{s}", [128, 512], stack=st) for s in range(3)]
                pm = [ps(f"pm{s}", [128, 512], stack=st) for s in range(3)]
                p = Phase(kb, f"mod{l}", n_iter=12)
                dma(p, nc, "sp", "aw", aw[:], lambda i: ada_w[l].rearrange("(kc p) n -> p kc n", p=128)[:, :, bass.ds(i * 512, 512)], w=["aw"])
                dma(p, nc, "act", "ab", ab[:], lambda i: ada_b[l:l + 1, bass.ds(i * 512, 512)].partition_broadcast(128), w=["ab"])
                for s in range(3):
                    for kc in range(8):
                        mm(p, nc, pm[s][:], CS[:, s * 8 + kc, :], aw[:, kc, :], kc == 0, kc == 7, r=["aw"], w=[f"pm{s}"])
                    tt(p, nc, "dve", mo[s][:], pm[s][:], ab[:], ALU.add, r=[f"pm{s}", "ab"], w=[f"mo{s}"])
                    nts = seqs[s] // 128
                    dma(p, nc, "pool", f"mo{s}", lambda i, s=s, nts=nts: MODT[seq_tile0[s]:seq_tile0[s] + nts, bass.ds(i * 512, 512)],
                        mo[s][0:nts, :], r=[f"mo{s}"])
                p.run()
            if stop == "mod":
                break
            if l % 2 == 0:
                build_attention_layer(b, l, l // 2, dict(X=X, MODT=MODT, FM=FM, VE=VE, a_wfm=a_wfm, a_wv=a_wv, a_sink=a_sink,
                                                         a_rb=a_rb, a_wout=a_wout, ln_g=ln_g, ln_b=ln_b, cn=cn, identb=identb,
                                                         identf=identf, alpha=alpha, seq_tile0=seq_tile0, ODBG=ODBG))
            else:
                build_hgrn_layer(b, l, l // 2, dict(X=X, MODT=MODT, r_win=r_win, r_lb=r_lb, r_gn=r_gn, r_wout=r_wout,
                                                    ln_g=ln_g, ln_b=ln_b, cn=cn, identb=identb, identf=identf, alpha=alpha,
                                                    seq_tile0=seq_tile0))
            if stop in (f"mix{l}", "attw", "A1", "attpre", "A2e") or (stop in ("H1", "H2") and l % 2 == 1):
                break
            build_moe_layer(b, l, dict(X=X, MODT=MODT, ro_w=ro_w, ro_b=ro_b, e_w=e_w, ln_g=ln_g,
                                       ln_b=ln_b, cn=cn, identb=identb, identf=identf, alpha=alpha))
            if stop in (f"moe{l}", "moeD", "moeS", "moeB"):
                break

        p = Phase(kb, "ycopy")
        for k in range(nchunk):
            dma(p, nc, ["sp", "act", "pool"][k % 3], f"yc{k % 3}", y_out[k * rows:(k + 1) * rows, :], X[k * rows:(k + 1) * rows, :])
        p.run()
    return nc


def build_attention_layer(b, l, la, g):
    nc, kb = b.nc, b.kb
    seqs = b.cfg["seqs"]
    NT = sum(seqs)
    X, MODT, FM, VE, cn = g["X"], g["MODT"], g["FM"], g["VE"], g["cn"]
    identb = g["identb"]
    alpha = g["alpha"]
    seq_tile0 = g["seq_tile0"]

    with ExitStack() as st:
        def sb(name, shape, dt=F32):
            return st.enter_context(nc.sbuf_tensor(un(b, name), list(shape), dt))

        def ps(name, shape, dt=F32):
            return st.enter_context(nc.psum_tensor(un(b, name), list(shape), dt))

        wfm = sb("wfm", [128, 8, 2304], BF16)
        wv = sb("wv", [128, 8, 640], BF16)
        stg = [sb("stg0", [128, 8, 512]), sb("stg1", [128, 8, 512])]
        veall = sb("veall", [128, 4, 10, 72], BF16)
        p = Phase(kb, f"attw{l}")
        load_weight_bf16(b, p, wfm, g["a_wfm"][la].rearrange("(kc p) n -> p kc n", p=128), 2304, "wfm", stg)
        load_weight_bf16(b, p, wv, g["a_wv"][la].rearrange("(kc p) n -> p kc n", p=128), 640, "wv", stg)
        p.op("pool", lambda i: nc.gpsimd.memset(veall[:], 1.0), w=["veall"])
        p.run()
        if b.cfg.get("stop") == "attw":
            return

        xg = sb("xg", [128, 4, 1024])
        scb = sb("scb", [128, 1024])
        shb = sb("shb", [128, 1024])
        tmp = [sb("tmp0", [128, 1024]), sb("tmp1", [128, 1024])]
        hb = sb("hb", [128, 4, 1024], BF16)
        hT = sb("hT", [128, 8, 512], BF16)
        ropec = sb("ropec", [128, 512])
        ropes = sb("ropes", [128, 512])
        t1 = sb("t1", [128, 512])
        t2 = sb("t2", [128, 512])
        fmall = sb("fmall", [128, 13, 512], BF16)
        psT = [ps("psT0", [128, 8, 128], BF16), ps("psT1", [128, 8, 128], BF16)]
        pf = [ps(f"pf{k}", [128, 512]) for k in range(3)]
        pva = ps("pva", [128, 512])
        pvb = ps("pvb", [128, 512])

        NG = NT // 512
        p = Phase(kb, f"A1_{l}", n_iter=NG)
        dma(p, nc, "sp", "xg", xg[:], lambda i: X[bass.ds(i * 512, 512), :].rearrange("(t p) d -> p t d", p=128), w=["xg"])
        dma(p, nc, "act", "scb", scb[:], lambda i: MODT[bass.ds(i * 4, 1), 1024:2048].partition_broadcast(128), w=["scb"])
        dma(p, nc, "act", "shb", shb[:], lambda i: MODT[bass.ds(i * 4, 1), 0:1024].partition_broadcast(128), w=["shb"])
        dma(p, nc, "pool", "ropec", ropec[:], lambda i: cn["rope_cos"][:, bass.ds(i * 512, 512)], w=["ropec"])
        dma(p, nc, "pool", "ropes", ropes[:], lambda i: cn["rope_sin"][:, bass.ds(i * 512, 512)], w=["ropes"])
        ts(p, nc, "dve", scb[:], scb[:], 1.0, None, ALU.add, r=["scb"], w=["scb"])
        for t in range(4):
            tt(p, nc, "dve", tmp[t % 2][:], xg[:, t, :], scb[:], ALU.mult, r=["xg", "scb"], w=[f"tmp{t % 2}"])
            tt(p, nc, "pool", hb[:, t, :], tmp[t % 2][:], shb[:], ALU.add, r=[f"tmp{t % 2}", "shb"], w=[f"hb{t}"])
            for kc in range(8):
                tr(p, nc, psT[t % 2][:, kc, :], hb[:, t, kc * 128:(kc + 1) * 128], identb[:], r=[f"hb{t}"], w=[f"psT{t % 2}"])
            cp(p, nc, "act", hT[:, :, t * 128:(t + 1) * 128], psT[t % 2][:], r=[f"psT{t % 2}"], w=[f"hT{t}"])
        hTall = [f"hT{t}" for t in range(4)]
        pfi = 0
        fk = 0
        for (ca, cb_, dst) in [(0, 4, 0), (1, 5, 1), (2, 6, 2), (3, 7, 3), (8, 9, 4)]:
            ia, ib = pfi % 3, (pfi + 1) % 3
            pfi += 2
            for kc in range(8):
                mm(p, nc, pf[ia][:], wfm[:, kc, ca * 128:(ca + 1) * 128], hT[:, kc, :], kc == 0, kc == 7, r=hTall, w=[f"pf{ia}"])
            for kc in range(8):
                mm(p, nc, pf[ib][:], wfm[:, kc, cb_ * 128:(cb_ + 1) * 128], hT[:, kc, :], kc == 0, kc == 7, r=hTall, w=[f"pf{ib}"])
            tt(p, nc, "dve", t1[:], pf[ia][:], ropec[:], ALU.mult, r=[f"pf{ia}", "ropec"], w=["t1"])
            tt(p, nc, "dve", t2[:], pf[ib][:], ropes[:], ALU.mult, r=[f"pf{ib}", "ropes"], w=["t2"])
            tt(p, nc, "pool", fmall[:, dst, :], t1[:], t2[:], ALU.add, r=["t1", "t2"], w=[f"fm{dst}"])
        for c in range(10, 18):
            dst = 5 + (c - 10)
            ia = pfi % 3
            pfi += 1
            for kc in range(8):
                mm(p, nc, pf[ia][:], wfm[:, kc, c * 128:(c + 1) * 128], hT[:, kc, :], kc == 0, kc == 7, r=hTall, w=[f"pf{ia}"])
            cp(p, nc, "act", fmall[:, dst, :], pf[ia][:], r=[f"pf{ia}"], w=[f"fm{dst}"])
        for t in range(4):
            for kc in range(8):
                mm(p, nc, pva[:], hT[:, kc, t * 128:(t + 1) * 128], wv[:, kc, 128:640], kc == 0, kc == 7, r=[f"hT{t}"], w=["pva"])
            for kc in range(8):
                mm(p, nc, pvb[:, 0:128], hT[:, kc, t * 128:(t + 1) * 128], wv[:, kc, 0:128], kc == 0, kc == 7, r=[f"hT{t}"], w=["pvb"])
            cp(p, nc, "dve", veall[:, t, 2:10, 0:64], pva[:].rearrange("p (h d) -> p h d", d=64), r=["pva"], w=[f"ve{t}"])
            cp(p, nc, "act", veall[:, t, 0:2, 0:64], pvb[:, 0:128].rearrange("p (h d) -> p h d", d=64), r=["pvb"], w=[f"ve{t}"])
        dma(p, nc, "sp", "fmall", lambda i: FM[:, :, bass.ds(i * 512, 512)].rearrange("f p t -> p f t"), fmall[:],
            r=[f"fm{d_}" for d_ in range(13)])
        dma(p, nc, "pool", "veall", lambda i: VE[bass.ds(i * 512, 512), :].rearrange("(t p) c -> p t c", p=128),
            veall[:].rearrange("p t h d -> p t (h d)"), r=[f"ve{t}" for t in range(4)])
        p.run()
    if b.cfg.get("stop") == "A1":
        return

    with ExitStack() as st:
        def sb(name, shape, dt=F32):
            return st.enter_context(nc.sbuf_tensor(un(b, name), list(shape), dt))

        def ps(name, shape, dt=F32):
            return st.enter_context(nc.psum_tensor(un(b, name), list(shape), dt))

        wout = sb("wout", [128, 8, 1024], BF16)
        EB = sb("EB", [128, 9, 8, 128], BF16)
        skx = sb("skx", [128, 8])
        wm = sb("wm", [128, 2, 128], BF16)
        lng = sb("lng", [128, 1024])
        lnb = sb("lnb", [128, 1024])
        with ExitStack() as st2:
            stg = [st2.enter_context(nc.sbuf_tensor(un(b, "stg0"), [128, 8, 512], F32)),
                   st2.enter_context(nc.sbuf_tensor(un(b, "stg1"), [128, 8, 512], F32))]
            nam = st2.enter_context(nc.sbuf_tensor(un(b, "nam"), [128, 9, 128], F32))
            rbs = [st2.enter_context(nc.sbuf_tensor(un(b, "rbs0"), [128, 8, 128], F32)),
                   st2.enter_context(nc.sbuf_tensor(un(b, "rbs1"), [128, 8, 128], F32))]
            p = Phase(kb, f"attpre{l}")
            load_weight_bf16(b, p, wout, g["a_wout"][la].rearrange("(kc p) n -> p kc n", p=128), 1024, "wout", stg)
            dma(p, nc, "pool", "nam", nam[:], cn["namask"][:, :, :], w=["nam"])
            dma(p, nc, "pool", "wm", wm[:], cn["wmask"][:, :, :], w=["wm"])
            dma(p, nc, "pool", "skx", skx[:], g["a_sink"][la:la + 1, :].partition_broadcast(128), w=["skx"])
            dma(p, nc, "pool", "lng", lng[:], g["ln_g"][l, 0:1, :].partition_broadcast(128), w=["lng"])
            dma(p, nc, "pool", "lnb", lnb[:], g["ln_b"][l, 0:1, :].partition_broadcast(128), w=["lnb"])
            act(p, nc, skx[:], skx[:], AF.Exp, r=["skx"], w=["skx"])
            for t in range(9):
                rb = rbs[t % 2]
                dma(p, nc, "sp", f"rbs{t % 2}", rb[:], g["a_rb"][la, :, t, :, :], w=[f"rbs{t % 2}"])
                act(p, nc, rb[:], rb[:], AF.Exp, r=[f"rbs{t % 2}"], w=[f"rbs{t % 2}"])
                tt(p, nc, "dve", EB[:, t, :, :], rb[:], nam[:, t, :].unsqueeze(1).to_broadcast([128, 8, 128]), ALU.mult,
                   r=[f"rbs{t % 2}", "nam"], w=[f"EB{t}"])
            p.run()
        if b.cfg.get("stop") == "attpre":
            return

        qa = sb("qa", [128, 4, 128], BF16)
        ka = sb("ka", [128, 3, 128], BF16)
        va = sb("va", [128, 3, 2, 72], BF16)
        qb = sb("qb", [64, 8, 128], BF16)
        kbt = sb("kbt", [64, 8, 5, 128], BF16)
        vb = sb("vb", [128, 5, 8, 72], BF16)
        xt = sb("xt", [128, 1024])
        g1b = sb("g1b", [128, 1024])
        PT = [sb("PT0", [128, 5, 4, 128], BF16), sb("PT1", [128, 5, 4, 128], BF16)]
        o = sb("o", [128, 1024], BF16)
        oT = sb("oT", [128, 8, 128], BF16)
        t1 = sb("t1", [128, 1024])
        z = sb("z", [128, 1024])
        sq = sb("sq", [128, 1024])
        xo = sb("xo", [128, 1024])
        stt = sb("stt", [128, 8])
        den = sb("den", [128, 4, 1])
        pS = [ps("pS0", [128, 4, 128]), ps("pS1", [128, 4, 128])]
        accb = [ps("acc0", [128, 512]), ps("acc1", [128, 512])]
        acc = [a[:].rearrange("p (h d) -> p h d", d=128) for a in accb]
        psOT = ps("psOT", [128, 8, 128], BF16)
        psY = [ps("psY0", [128, 512]), ps("psY1", [128, 512])]
        o3 = o[:].rearrange("p (h d) -> p h d", d=64)

        def block(p, tok0, tile, wl, nl):
            def T(off):
                return (lambda i: (tok0(i) if callable(tok0) else tok0) + off)

            def TL(i):
                return tile(i) if callable(tile) else tile
            dma(p, nc, "sp", "qa", qa[:], lambda i: FM[0:4, :, bass.ds(T(0)(i), 128)].rearrange("f p t -> p f t"), w=["qa"])
            w0, w1 = wl[0], wl[-1]
            nw = w1 - w0 + 1
            dma(p, nc, "sp", "ka", ka[:, w0 + 1:w1 + 2, :].rearrange("p b t -> p (b t)"),
                lambda i: FM[4, :, bass.ds(T(w0 * 128)(i), nw * 128)], w=["ka"])
            dma(p, nc, "pool", "va", va[:, w0 + 1:w1 + 2, :, :].rearrange("p b h d -> p b (h d)"),
                lambda i: VE[bass.ds(T(w0 * 128)(i), nw * 128), 0:144].rearrange("(b p) c -> p b c", p=128), w=["va"])
            dma(p, nc, "sp", "qb", qb[:], lambda i: FM[5:9, :, bass.ds(T(0)(i), 128)].rearrange("f (two d) t -> d (f two) t", two=2), w=["qb"])
            n0 = nl[0][0]
            nn = len(nl)
            dma(p, nc, "act", "kbt", kbt[:, :, 0:nn, :].rearrange("p f b t -> p f (b t)"),
                lambda i: FM[9:13, :, bass.ds(T(n0 * 128)(i), nn * 128)].rearrange("f (two d) t -> d (f two) t", two=2), w=["kbt"])
            dma(p, nc, "pool", "vb", vb[:, 0:nn, :, :].rearrange("p b h d -> p b (h d)"),
                lambda i: VE[bass.ds(T(n0 * 128)(i), nn * 128), 144:720].rearrange("(b p) c -> p b c", p=128), w=["vb"])
            dma(p, nc, "sp", "xt", xt[:], lambda i: X[bass.ds(T(0)(i), 128), :], w=["xt"])
            dma(p, nc, "act", "g1b", g1b[:], lambda i: MODT[bass.ds(TL(i), 1), 2048:3072].partition_broadcast(128), w=["g1b"])
            lvl = b.cfg.get("a2lvl", 9)
            if lvl < 2:
                return
            u = 0
            useg = []
            for gk in range(2):
                a = acc[gk % 2]
                an = f"acc{gk % 2}"
                Pu = PT[gk % 2]
                pn = f"PT{gk % 2}_"
                useg.append([len(p.ops)])
                for di, dl in enumerate(wl):
                    k = u % 2
                    u += 1
                    mm(p, nc, pS[k][:], ka[64 * gk:64 * gk + 64, dl + 1, :], qa[64 * gk:64 * gk + 64, :, :], True, True,
                       r=["ka", "qa"], w=[f"pS{k}"])
                    act(p, nc, Pu[:, di, :, :], pS[k][:], AF.Exp, scale=0.125, r=[f"pS{k}"], w=[pn + str(di)])
                    if dl != 0:
                        tt(p, nc, "dve", Pu[:, di, :, :], Pu[:, di, :, :],
                           wm[:, 0 if dl == -1 else 1, :].unsqueeze(1).to_broadcast([128, 4, 128]),
                           ALU.mult, r=[pn + str(di)], w=[pn + str(di)])
                useg[-1].append(len(p.ops))
                for h in range(4):
                    for di, dl in enumerate(wl):
                        mm(p, nc, a[:, h, 0:66], Pu[:, di, h, :], va[:, dl + 1, gk, 0:66], di == 0, di == len(wl) - 1,
                           r=[pn + str(di), "va"], w=[an])
                tt(p, nc, "dve", den[:], a[:, :, 64:65], skx[:, 4 * gk:4 * gk + 4].unsqueeze(2), ALU.add, r=[an], w=["den"])
                p.op("dve", lambda i: nc.vector.reciprocal(den[:], den[:]), r=["den"], w=["den"])
                tt(p, nc, "dve", o3[:, 4 * gk:4 * gk + 4, :], a[:, :, 0:64], den[:].to_broadcast([128, 4, 64]), ALU.mult,
                   r=[an, "den"], w=["o"])
            if lvl < 3:
                return
            for hg in range(2):
                a = acc[hg % 2]
                an = f"acc{hg % 2}"
                Pu = PT[hg % 2]
                pn = f"PT{hg % 2}_"
                useg[-1].append(len(p.ops))
                useg.append([len(p.ops)])
                for di, (dl, tb) in enumerate(nl):
                    k = u % 2
                    u += 1
                    for uu in range(4):
                        h = 4 * hg + uu
                        mm(p, nc, pS[k][:, uu, :], kbt[:, h, di, :], qb[:, h, :],
                           True, True, r=["kbt", "qb"], w=[f"pS{k}"])
                    act(p, nc, Pu[:, di, :, :], pS[k][:], AF.Exp, scale=0.125, r=[f"pS{k}"], w=[pn + str(di)])
                    tt(p, nc, "dve" if di % 2 == 0 else "pool", Pu[:, di, :, :], Pu[:, di, :, :], EB[:, tb, 4 * hg:4 * hg + 4, :], ALU.mult,
                       r=[pn + str(di)], w=[pn + str(di)])
                useg[-1].append(len(p.ops))
                for uu in range(4):
                    for di, (dl, tb) in enumerate(nl):
                        mm(p, nc, a[:, uu, 0:66], Pu[:, di, uu, :], vb[:, di, 4 * hg + uu, 0:66], di == 0, di == len(nl) - 1,
                           r=[pn + str(di), "vb"], w=[an])
                cp(p, nc, "dve", den[:], a[:, :, 64:65], r=[an], w=["den"])
                p.op("dve", lambda i: nc.vector.reciprocal(den[:], den[:]), r=["den"], w=["den"])
                tt(p, nc, "dve", o3[:, 8 + 4 * hg:8 + 4 * hg + 4, :], a[:, :, 0:64], den[:].to_broadcast([128, 4, 64]), ALU.mult,
                   r=[an, "den"], w=["o"])
            useg[-1].append(len(p.ops))
            if len(useg) == 4 and all(len(x) == 3 for x in useg) and b.cfg.get("a2pipe", True):
                o_ = p.ops
                sem = [o_[x[0]:x[1]] for x in useg]
                pv = [o_[x[1]:x[2]] for x in useg]
                neworder = sem[0] + sem[1] + pv[0] + sem[2] + pv[1] + sem[3] + pv[2] + pv[3]
                p.ops = o_[:useg[0][0]] + neworder + o_[useg[3][2]:]
            if b.cfg.get("debug"):
                dma(p, nc, "pool", "odbg", lambda i: g["ODBG"][bass.ds(T(0)(i), 128), :], o[:], r=["o"])
            if lvl < 4:
                return
            for kc in range(8):
                tr(p, nc, psOT[:, kc, :], o[:, kc * 128:(kc + 1) * 128], identb[:], r=["o"], w=["psOT"])
            cp(p, nc, "act", oT[:], psOT[:], r=["psOT"], w=["oT"])
            for n in range(2):
                for kc in range(8):
                    mm(p, nc, psY[n][:], oT[:, kc, :], wout[:, kc, n * 512:(n + 1) * 512], kc == 0, kc == 7, r=["oT"], w=[f"psY{n}"])
            if lvl < 5:
                return
            ts(p, nc, "pool", g1b[:], g1b[:], 1.0, None, ALU.add, r=["g1b"], w=["g1b"])
            for n in range(2):
                tt(p, nc, "dve", t1[:, n * 512:(n + 1) * 512], psY[n][:], g1b[:, n * 512:(n + 1) * 512], ALU.mult,
                   r=[f"psY{n}", "g1b"], w=["t1"])
            p.op("dve", lambda i: nc.vector.scalar_tensor_tensor(z[:], xt[:], alpha, t1[:], ALU.mult, ALU.add), r=["xt", "t1"], w=["z"])
            layer_norm_tail(b, p, z, "z", lng, lnb, xo, "xo", dict(sq=sq, st=stt), "ln")
            dma(p, nc, "sp", "xo", lambda i: X[bass.ds(T(0)(i), 128), :], xo[:], r=["xo"])

        FULL = lambda dls: [(d_, d_ + 3) for d_ in dls]
        for s, T_s in enumerate(seqs):
            nb = T_s // 128
            tok_s = seq_tile0[s] * 128
            p = Phase(kb, f"A2e_{l}_{s}")
            for j in sorted(set([0, 1, nb - 2, nb - 1]))[:b.cfg.get("a2nblk", 4)]:
                wl = [d_ for d_ in (-1, 0, 1) if 0 <= j + d_ < nb]
                if j == 0:
                    nl = FULL([0, 1, 2, 3])
                elif j == 1:
                    nl = FULL([-1, 0, 1, 2])
                elif j == nb - 2:
                    nl = FULL([-2, -1, 0, 1])
                else:
                    nl = FULL([-3, -2, -1, 0])
                block(p, tok_s + j * 128, seq_tile0[s] + j, wl, nl)
            p.run()
            if b.cfg.get("stop") == "A2e":
                return
            if nb > 4:
                p = Phase(kb, f"A2i_{l}_{s}", n_iter=nb - 4)
                block(p, lambda i, tok_s=tok_s: (i + 2) * 128 + tok_s, lambda i, s=s: i + (2 + seq_tile0[s]),
                      [-1, 0, 1], [(-2, 7), (-1, 2), (0, 3), (1, 4), (2, 8)])
                p.run()


def expert_layout(w, kdim):
    L, E, K, n = w.shape
    return np.ascontiguousarray(w.reshape(L, E, kdim, 128, n).transpose(0, 1, 3, 2, 4)).reshape(L * E * 128, kdim * n)


def prepare_shared(inp, cfg):
    f = lambda k: np.ascontiguousarray(np.asarray(inp[k], dtype=np.float32))
    sh = {}
    for k in ["ada_w", "ada_b", "ln_g", "ln_b", "attn_sink", "attn_w_out", "rec_w_in", "rec_lb", "rec_gnorm", "rec_w_out"]:
        sh[k] = f(k)
    wfm, wv = attn_w_layout(f("attn_w_in"))
    sh["attn_wfm"] = wfm
    sh["attn_wv"] = wv
    sh["na_rb"] = na_bias_tables(f("nat_rel_bias"))
    L = sh["ada_w"].shape[0]
    sh["router_w"] = np.ascontiguousarray(np.concatenate([f("router_w_group"), f("router_w_expert")], axis=2))
    sh["router_b"] = np.ascontiguousarray(np.concatenate([f("router_b_group"), f("router_b_expert").reshape(L, 32)], axis=1))
    sh["e_w"] = np.concatenate([expert_layout(f("expert_w_gate"), 8), expert_layout(f("expert_w_up"), 8),
                                expert_layout(f("expert_w_down"), 4)], axis=1)
    sh.update(host_constants(cfg))
    return sh


def run_cores(inp, cfg, n_cores, pps):
    xp = np.asarray(inp["x_prompt"], dtype=np.float32)
    xs = np.asarray(inp["x_sample"], dtype=np.float32)
    cp_ = np.asarray(inp["c_prompt"], dtype=np.float32)
    cs_ = np.asarray(inp["c_sample"], dtype=np.float32)
    sh = prepare_shared(inp, cfg)
    nc = build_program(cfg)
    in_maps = []
    for c in range(n_cores):
        m = dict(sh)
        xc = np.concatenate([xp[pps * c + k] for k in range(pps)] + [xs[c]], axis=0)
        cc = np.stack([cp_[pps * c + k] for k in range(pps)] + [cs_[c]], axis=0)
        m["x"] = np.ascontiguousarray(xc)
        m["cT"] = np.ascontiguousarray(cc.reshape(3, 8, 128).transpose(2, 0, 1))
        in_maps.append(m)
    res = run_bass_kernel_spmd(nc, in_maps, core_ids=list(range(n_cores)))
    if cfg.get("debug"):
        cfg["_dbg"] = res.results
    TP = cfg["seqs"][0]
    yp = np.zeros_like(xp)
    ys = np.zeros_like(xs)
    for c in range(n_cores):
        y = np.asarray(res.results[c]["y"], dtype=np.float32)
        for k in range(pps):
            yp[pps * c + k] = y[k * TP:(k + 1) * TP]
        ys[c] = y[pps * TP:]
    return yp, ys


def kernel(**inputs):
    cfg = dict(seqs=[2048, 2048, 8192], depth=4)
    yp, ys = run_cores(inputs, cfg, 8, 2)
    return (yp, ys)


def build_moe_layer(b, l, g):
    nc, kb = b.nc, b.kb
    seqs = b.cfg["seqs"]
    NT = sum(seqs)
    NTILES = NT // 128
    NBLK = 2 * NTILES + NEXP
    X, MODT, cn = g["X"], g["MODT"], g["cn"]
    identf = g["identf"]
    alpha = g["alpha"]
    BIG = 1.0e9
    if "HF" not in b.dram:
        b.dscr("HF", [NT, D])
        b.dscr("RTD", [128, NTILES, 8])
        b.dscr("DESTD", [128, NTILES, 2], I32)
        b.dscr("IDXD", [128, NBLK], I32)
        b.dscr("XBUF", [NBLK * 128, D])
        b.dscr("YBUF", [NBLK * 128, D])
    HF, RTD, DESTD, IDXD, XBUF, YBUF = [b.dram[k] for k in ["HF", "RTD", "DESTD", "IDXD", "XBUF", "YBUF"]]

    with ExitStack() as st:
        def sb(name, shape, dt=F32):
            return st.enter_context(nc.sbuf_tensor(un(b, name), list(shape), dt))

        def ps(name, shape, dt=F32):
            return st.enter_context(nc.psum_tensor(un(b, name), list(shape), dt))

        rw = sb("rw", [128, 8, 36])
        rbb = sb("rbb", [128, 36])
        io32 = sb("io32", [128, 32])
        trib = sb("trib", [128, 128], BF16)
        oneb = sb("oneb", [128, 128], BF16)
        carry = sb("carry", [128, 32])
        zt = sb("zt", [128, 1024])
        rt = sb("rt", [128, 8])
        p = Phase(kb, f"moepre{l}")
        p.op("pool", lambda i: nc.gpsimd.memset(rt[:], 0.0), w=["rt"])
        dma(p, nc, "sp", "m0", rw[:], g["ro_w"][l].rearrange("(kc p) n -> p kc n", p=128), w=["rw"])
        dma(p, nc, "sp", "m1", rbb[:], g["ro_b"][l:l + 1, :].partition_broadcast(128), w=["rbb"])
        dma(p, nc, "sp", "m2", io32[:], cn["iota32"][:, :], w=["io32"])
        dma(p, nc, "sp", "m3", trib[:], cn["tri_su"][:, :], w=["trib"])
        dma(p, nc, "sp", "m4", oneb[:], cn["ones_b"][:, :], w=["oneb"])
        p.op("pool", lambda i: nc.gpsimd.memset(carry[:], 0.0), w=["carry"])
        p.op("pool", lambda i: nc.gpsimd.memset(zt[:], 0.0), w=["zt"])
        p.run()
        p = Phase(kb, f"moez{l}", n_iter=NBLK)
        dma(p, nc, "sp", "zf", lambda i: XBUF[bass.ds(i * 128, 128), :], zt[:])
        p.run()

        from types import SimpleNamespace
        sets = []
        for k in range(2):
            T = SimpleNamespace(xt=sb(f"xt{k}", [128, 1024]), scb=sb(f"scb{k}", [128, 1024]), shb=sb(f"shb{k}", [128, 1024]),
                                hf=sb(f"hf{k}", [128, 1024]), hfT=sb(f"hfT{k}", [128, 8, 128]), lg=sb(f"lg{k}", [128, 36]),
                                sm=sb(f"sm{k}", [128, 16]), ge=sb(f"ge{k}", [128, 4]), gmask=sb(f"gmask{k}", [128, 4]),
                                lem=sb(f"lem{k}", [128, 4, 8]), lem2=sb(f"lem2{k}", [128, 32]), oh1=sb(f"oh1{k}", [128, 32]),
                                oh2=sb(f"oh2{k}", [128, 32]), ohb=sb(f"ohb{k}", [128, 32], BF16), tmp32=sb(f"tmp32{k}", [128, 32]),
                                tot=sb(f"tot{k}", [128, 32]), rt=sb(f"rtt{k}", [128, 8]))
            T.lemf = T.lem[:].rearrange("p a b -> p (a b)")
            sets.append(T)
        psAs = [[ps(f"psA0_{k}", [128, 4, 128]), ps(f"psA1_{k}", [128, 4, 128])] for k in range(2)]
        pms = [ps(f"pm{k}", [128, 512]) for k in range(2)]
        p = Phase(kb, f"moeRz{l}")
        for k in range(2):
            p.op("pool", lambda i, k=k: nc.gpsimd.memset(sets[k].rt[:], 0.0), w=[f"rtz{k}"])
        p.run()

        p = Phase(kb, f"moeR{l}", n_iter=NTILES // 2)
        for k in range(2):
            p.ns = f"#{k}"
            T = sets[k]
            psA = psAs[k]
            pl = pms[k][:, 0:64]
            pr = pms[k][:, 64:128]
            pc = pms[k][:, 128:192]
            dma(p, nc, "sp", "xt", T.xt[:], lambda i, k=k, T=T: X[bass.ds(i * 256 + k * 128, 128), :], w=["xt"])
            dma(p, nc, "act", "scb", T.scb[:], lambda i, k=k, T=T: MODT[bass.ds(i * 2 + k, 1), 4096:5120].partition_broadcast(128), w=["scb"])
            dma(p, nc, "act", "shb", T.shb[:], lambda i, k=k, T=T: MODT[bass.ds(i * 2 + k, 1), 3072:4096].partition_broadcast(128), w=["shb"])
            ts(p, nc, "pool", T.scb[:], T.scb[:], 1.0, None, ALU.add, r=["scb"], w=["scb"])
            tt(p, nc, "dve", T.hf[:], T.xt[:], T.scb[:], ALU.mult, r=["xt", "scb"], w=["hf"])
            tt(p, nc, "pool", T.hf[:], T.hf[:], T.shb[:], ALU.add, r=["hf", "shb"], w=["hf"])
            dma(p, nc, "sp", "hfo", lambda i, k=k, T=T: HF[bass.ds(i * 256 + k * 128, 128), :], T.hf[:], r=["hf"])
            for kc in range(8):
                tr(p, nc, psA[kc // 4][:, kc % 4, :], T.hf[:, kc * 128:(kc + 1) * 128], identf[:], r=["hf"], w=[f"psA{kc // 4}"])
            cp(p, nc, "act", T.hfT[:, 0:4, :], psA[0][:], r=["psA0"], w=["hfT0"])
            cp(p, nc, "dve", T.hfT[:, 4:8, :], psA[1][:], r=["psA1"], w=["hfT1"])
            for kc in range(8):
                mm(p, nc, pl[:, 0:36], T.hfT[:, kc, :], rw[:, kc, :], kc == 0, kc == 7, r=["hfT0", "hfT1"], w=["pm"])
            tt(p, nc, "dve", T.lg[:], pl[:, 0:36], rbb[:], ALU.add, r=["pm"], w=["lg"])
            p.op("dve", lambda i, k=k, T=T: nc.vector.tensor_reduce(T.sm[:, 0:1], T.lg[:, 0:4], AX.X, ALU.max), r=["lg"], w=["sm0"])
            ts(p, nc, "dve", T.sm[:, 1:2], T.sm[:, 0:1], -1.0, None, ALU.mult, r=["sm0"], w=["sm1"])
            act(p, nc, T.ge[:], T.lg[:, 0:4], AF.Exp, bias=T.sm[:, 1:2], r=["lg", "sm1"], w=["ge"])
            p.op("dve", lambda i, k=k, T=T: nc.vector.tensor_reduce(T.sm[:, 2:3], T.ge[:], AX.X, ALU.add), r=["ge"], w=["sm2"])
            p.op("dve", lambda i, k=k, T=T: nc.vector.reciprocal(T.sm[:, 3:4], T.sm[:, 2:3]), r=["sm2"], w=["sm3"])
            tt(p, nc, "dve", T.gmask[:], T.lg[:, 0:4], T.sm[:, 0:1].to_broadcast([128, 4]), ALU.is_ge, r=["lg", "sm0"], w=["gmask"])
            ts(p, nc, "dve", T.gmask[:], T.gmask[:], -1.0, BIG, ALU.add, ALU.mult, r=["gmask"], w=["gmask"])
            tt(p, nc, "dve", T.lem[:], T.lg[:, 4:36].rearrange("p (a b) -> p a b", b=8), T.gmask[:].unsqueeze(2).to_broadcast([128, 4, 8]),
               ALU.add, r=["lg", "gmask"], w=["lem"])
            p.op("dve", lambda i, k=k, T=T: nc.vector.tensor_reduce(T.sm[:, 4:5], T.lemf, AX.X, ALU.max), r=["lem"], w=["sm4"])
            tt(p, nc, "dve", T.oh1[:], T.lemf, T.sm[:, 4:5].to_broadcast([128, 32]), ALU.is_ge, r=["lem", "sm4"], w=["oh1"])
            p.op("dve", lambda i, k=k, T=T: nc.vector.scalar_tensor_tensor(T.lem2[:], T.oh1[:], -BIG, T.lemf, ALU.mult, ALU.add), r=["oh1", "lem"], w=["lem2"])
            p.op("dve", lambda i, k=k, T=T: nc.vector.tensor_reduce(T.sm[:, 5:6], T.lem2[:], AX.X, ALU.max), r=["lem2"], w=["sm5"])
            tt(p, nc, "dve", T.oh2[:], T.lem2[:], T.sm[:, 5:6].to_broadcast([128, 32]), ALU.is_ge, r=["lem2", "sm5"], w=["oh2"])
            tt(p, nc, "dve", T.sm[:, 6:7], T.sm[:, 5:6], T.sm[:, 4:5], ALU.subtract, r=["sm4", "sm5"], w=["sm6"])
            act(p, nc, T.sm[:, 7:8], T.sm[:, 6:7], AF.Exp, r=["sm6"], w=["sm7"])
            ts(p, nc, "dve", T.sm[:, 8:9], T.sm[:, 7:8], 1.0, None, ALU.add, r=["sm7"], w=["sm8"])
            p.op("dve", lambda i, k=k, T=T: nc.vector.reciprocal(T.sm[:, 8:9], T.sm[:, 8:9]), r=["sm8"], w=["sm8"])
            tt(p, nc, "dve", T.rt[:, 2:3], T.sm[:, 8:9], T.sm[:, 3:4], ALU.mult, r=["sm8", "sm3"], w=["rt2"])
            tt(p, nc, "dve", T.rt[:, 3:4], T.rt[:, 2:3], T.sm[:, 7:8], ALU.mult, r=["rt2", "sm7"], w=["rt3"])
            tt(p, nc, "dve", T.tmp32[:], T.oh1[:], io32[:], ALU.mult, r=["oh1"], w=["tmp32"])
            p.op("dve", lambda i, k=k, T=T: nc.vector.tensor_reduce(T.rt[:, 0:1], T.tmp32[:], AX.X, ALU.add), r=["tmp32"], w=["rt0"])
            tt(p, nc, "dve", T.tmp32[:], T.oh2[:], io32[:], ALU.mult, r=["oh2", "rt0"], w=["tmp32"])
            p.op("dve", lambda i, k=k, T=T: nc.vector.tensor_reduce(T.rt[:, 1:2], T.tmp32[:], AX.X, ALU.add), r=["tmp32"], w=["rt1"])
            tt(p, nc, "pool", T.ohb[:], T.oh1[:], T.oh2[:], ALU.add, r=["oh1", "oh2"], w=["ohb"])
            mm(p, nc, pr[:, 0:32], trib[:], T.ohb[:], True, True, r=["ohb"], w=["pm"])
            mm(p, nc, pc[:, 0:32], oneb[:], T.ohb[:], True, True, r=["ohb"], w=["pm"])
            tt(p, nc, "dve", T.tot[:], pr[:, 0:32], carry[:], ALU.add, r=["pm", "!carry"], w=["tot"])
            if k == 1:
                tt(p, nc, "dve", T.tot[:], T.tot[:], pms[0][:, 128:160], ALU.add, r=["tot", "!pm#0"], w=["tot"])
            tt(p, nc, "dve", T.tmp32[:], T.oh1[:], T.tot[:], ALU.mult, r=["oh1", "tot", "rt1"], w=["tmp32"])
            p.op("dve", lambda i, k=k, T=T: nc.vector.tensor_reduce(T.rt[:, 4:5], T.tmp32[:], AX.X, ALU.add), r=["tmp32"], w=["rt4"])
            tt(p, nc, "dve", T.tmp32[:], T.oh2[:], T.tot[:], ALU.mult, r=["oh2", "tot", "rt4"], w=["tmp32"])
            p.op("dve", lambda i, k=k, T=T: nc.vector.tensor_reduce(T.rt[:, 5:6], T.tmp32[:], AX.X, ALU.add), r=["tmp32"], w=["rt5"])
            if k == 1:
                tt(p, nc, "dve", carry[:], carry[:], pms[0][:, 128:160], ALU.add, r=["!carry", "!pm#0", "!tot#0", "tot"], w=["!carry"])
                tt(p, nc, "dve", carry[:], carry[:], pc[:, 0:32], ALU.add, r=["!carry", "pm", "tot"], w=["!carry"])
            dma(p, nc, "act", "rto", lambda i, k=k, T=T: RTD[:, bass.ds(i * 2 + k, 1), :].rearrange("p o c -> p (o c)"), T.rt[:],
                r=["rt0", "rt1", "rt2", "rt3", "rt4", "rt5"])
            if k == 0:
                n_half = len(p.ops)
        p.ns = ""
        p.interleave(0, n_half, len(p.ops))
        p.run()

        rta = sb("rta", [128, NTILES, 8])
        cnt = sb("cnt", [128, 32])
        cnti = sb("cnti", [128, 32], I32)
        pend = sb("pend", [128, 32])
        pst = sb("pst", [128, 32])
        onesf = sb("onesf", [128, 32])
        dacc = sb("dacc", [128, NTILES, 2])
        dtmp = sb("dtmp", [128, NTILES, 2])
        dint = sb("dint", [128, NTILES, 2], I32)
        ioblk = sb("ioblk", [128, NBLK])
        bacc = sb("bacc", [128, NBLK])
        btmp = sb("btmp", [128, NBLK])
        iop = sb("iop", [128, 1])
        bint = sb("bint", [128, NBLK], I32)
        p = Phase(kb, f"moeD{l}")
        dma(p, nc, "sp", "d0", rta[:], RTD[:, :, :], w=["rta"])
        dma(p, nc, "sp", "d1", ioblk[:], cn["iota_blk"][:, 0:NBLK], w=["ioblk"])
        dma(p, nc, "sp", "d2", iop[:], cn["iota_p"][:, :], w=["iop"])
        p.op("pool", lambda i: nc.gpsimd.memset(onesf[:], 1.0), w=["onesf"])
        ts(p, nc, "dve", cnt[:], carry[:], 1.0 / 128.0, 0.49609375, ALU.mult, ALU.add, w=["cnt"])
        cp(p, nc, "dve", cnti[:], cnt[:], r=["cnt"], w=["cnti"])
        cp(p, nc, "dve", cnt[:], cnti[:], r=["cnti"], w=["cnt"])
        ts(p, nc, "dve", cnt[:], cnt[:], 128.0, None, ALU.mult, r=["cnt"], w=["cnt"])
        p.op("dve", lambda i: nc.vector.tensor_tensor_scan(pend[:], onesf[:], cnt[:], 0.0, ALU.mult, ALU.add), r=["onesf", "cnt"], w=["pend"])
        tt(p, nc, "dve", pst[:], pend[:], cnt[:], ALU.subtract, r=["pend", "cnt"], w=["pst"])
        cp(p, nc, "dve", dacc[:], rta[:, :, 4:6], r=["rta"], w=["dacc"])
        for e in range(NEXP):
            ts(p, nc, "dve", dtmp[:], rta[:, :, 0:2], float(e), None, ALU.is_equal, r=["rta", "dacc"], w=["dtmp"])
            p.op("dve", lambda i, e=e: nc.vector.scalar_tensor_tensor(dacc[:], dtmp[:], pst[:, e:e + 1], dacc[:], ALU.mult, ALU.add),
                 r=["dtmp", "pst", "dacc"], w=["dacc"])
        cp(p, nc, "dve", dint[:], dacc[:], r=["dacc"], w=["dint"])
        dma(p, nc, "sp", "d3", DESTD[:, :, :], dint[:], r=["dint"])
        p.op("pool", lambda i: nc.gpsimd.memset(bacc[:], 0.0), w=["bacc"])
        for e in range(NEXP):
            ts(p, nc, "dve", btmp[:], ioblk[:], pend[:, e:e + 1], None, ALU.is_ge, r=["ioblk", "pend", "bacc"], w=["btmp"])
            tt(p, nc, "dve", bacc[:], bacc[:], btmp[:], ALU.add, r=["bacc", "btmp"], w=["bacc"])
        ts(p, nc, "dve", bacc[:], bacc[:], float(NEXP - 1), None, ALU.min, r=["bacc"], w=["bacc"])
        OOBV = float(b.cfg["depth"] * NEXP * 128)
        p.op("pool", lambda i: nc.gpsimd.memset(btmp[:, 0:1], 1.0), r=["btmp"], w=["btmp"])
        p.op("pool", lambda i: nc.gpsimd.memset(btmp[:, 1:2], 1.0), r=["btmp"], w=["btmp"])
        tt(p, nc, "dve", btmp[:, 2:NBLK], bacc[:, 2:NBLK], bacc[:, 0:NBLK - 2], ALU.not_equal, r=["bacc", "btmp"], w=["btmp"])
        ts(p, nc, "dve", bacc[:], bacc[:], 128.0, float(l * NEXP * 128), ALU.mult, ALU.add, r=["bacc", "btmp"], w=["bacc"])
        tt(p, nc, "dve", bacc[:], bacc[:], iop[:].to_broadcast([128, NBLK]), ALU.add, r=["bacc", "iop"], w=["bacc"])
        if b.cfg.get("oobskip", True):
            ts(p, nc, "dve", bacc[:], bacc[:], -OOBV, None, ALU.add, r=["bacc"], w=["bacc"])
            tt(p, nc, "dve", bacc[:], bacc[:], btmp[:], ALU.mult, r=["bacc", "btmp"], w=["bacc"])
            ts(p, nc, "dve", bacc[:], bacc[:], OOBV, None, ALU.add, r=["bacc"], w=["bacc"])
        cp(p, nc, "dve", bint[:], bacc[:], r=["bacc"], w=["bint"])
        dma(p, nc, "sp", "d4", IDXD[:, :], bint[:], r=["bint"])
        p.run()
    if b.cfg.get("stop") == "moeD":
        return

    with ExitStack() as st:
        def sb(name, shape, dt=F32):
            return st.enter_context(nc.sbuf_tensor(un(b, name), list(shape), dt))
        hf = sb("hf", [128, 1024])
        di = sb("di", [128, 2], I32)
        p = Phase(kb, f"moeS{l}", n_iter=NTILES)
        dma(p, nc, "sp", "hf", hf[:], lambda i: HF[bass.ds(i * 128, 128), :], w=["hf"])
        dma(p, nc, "act", "di", di[:], lambda i: DESTD[:, bass.ds(i, 1), :].rearrange("p o c -> p (o c)"), w=["di"])
        for k in range(2):
            p.dma("pool", f"sc{k}", lambda i, k=k: nc.gpsimd.indirect_dma_start(
                out=XBUF[:, :], out_offset=bass.IndirectOffsetOnAxis(ap=di[:, k:k + 1], axis=0), in_=hf[:], in_offset=None),
                r=["hf", "di"])
        p.run()
    if b.cfg.get("stop") == "moeS":
        return

    NROWS = b.cfg["depth"] * NEXP * 128
    with ExitStack() as st:
        def sb(name, shape, dt=F32):
            return st.enter_context(nc.sbuf_tensor(un(b, name), list(shape), dt))

        def ps(name, shape, dt=F32):
            return st.enter_context(nc.psum_tensor(un(b, name), list(shape), dt))
        wfs = [sb("wf0", [128, 12288]), sb("wf1", [128, 12288])]
        psA = [ps("psA0", [128, 4, 128]), ps("psA1", [128, 4, 128])]
        psG = ps("psG", [128, 4, 128])
        psU = ps("psU", [128, 4, 128])
        psY = [ps("psY0", [128, 512]), ps("psY1", [128, 512])]
        sets = []
        for k in range(2):
            sets.append(dict(ix=sb(f"ix{k}", [128, 1], I32), wgb=sb(f"wgb{k}", [128, 8, 512], BF16), wub=sb(f"wub{k}", [128, 8, 512], BF16),
                             wdb=sb(f"wdb{k}", [128, 4, 1024], BF16), xb=sb(f"xb{k}", [128, 1024]), xT=sb(f"xT{k}", [128, 8, 128], BF16),
                             sg=sb(f"sg{k}", [128, 4, 128]), hT=sb(f"hT{k}", [128, 4, 128], BF16), yb=sb(f"yb{k}", [128, 1024])))
        p = Phase(kb, f"moeB{l}", n_iter=NBLK // 2)
        segs = []
        for k in range(2):
            p.ns = f"#{k}"
            T = sets[k]
            segs.append(len(p.ops))
            ix, wgb, wub, wdb, xb, xT, sg, hT, yb = [T[n_] for n_ in ["ix", "wgb", "wub", "wdb", "xb", "xT", "sg", "hT", "yb"]]
            dma(p, nc, "sp", "ix", ix[:], lambda i, k=k: IDXD[:, bass.ds(i * 2 + k, 1)], w=["ix"], slow=True)
            dma(p, nc, "act", "xb", xb[:], lambda i, k=k: XBUF[bass.ds(i * 256 + k * 128, 128), :], w=["xb"])
            wf = wfs[k]
            p.dma("pool", "wf", lambda i, ix=ix, wf=wf: nc.gpsimd.indirect_dma_start(
                out=wf[:], out_offset=None, in_=g["e_w"][:, :], in_offset=bass.IndirectOffsetOnAxis(ap=ix[:, 0:1], axis=0),
                bounds_check=NROWS - 1, oob_is_err=False),
                r=["ix"], w=["wf"])
            segs.append(len(p.ops))
            cp(p, nc, "dve", wgb[:].rearrange("p k n -> p (k n)"), wf[:, 0:4096], r=["wf"], w=["wgb"])
            cp(p, nc, "act", wub[:].rearrange("p k n -> p (k n)"), wf[:, 4096:8192], r=["wf"], w=["wub"])
            cp(p, nc, "dve", wdb[:, 0:2, :].rearrange("p k n -> p (k n)"), wf[:, 8192:10240], r=["wf"], w=["wdb0"])
            cp(p, nc, "act", wdb[:, 2:4, :].rearrange("p k n -> p (k n)"), wf[:, 10240:12288], r=["wf"], w=["wdb1"])
            segs.append(len(p.ops))
            for kc in range(8):
                tr(p, nc, psA[kc // 4][:, kc % 4, :], xb[:, kc * 128:(kc + 1) * 128], identf[:], r=["xb"], w=[f"!psA{kc // 4}"])
            cp(p, nc, "act", xT[:, 0:4, :], psA[0][:], r=["!psA0"], w=["xT0"])
            cp(p, nc, "dve", xT[:, 4:8, :], psA[1][:], r=["!psA1"], w=["xT1"])
            for c in range(4):
                for kc in range(8):
                    mm(p, nc, psG[:, c, :], wgb[:, kc, c * 128:(c + 1) * 128], xT[:, kc, :], kc == 0, kc == 7, r=["wgb", "xT0", "xT1"], w=["!psG"])
            for c in range(4):
                for kc in range(8):
                    mm(p, nc, psU[:, c, :], wub[:, kc, c * 128:(c + 1) * 128], xT[:, kc, :], kc == 0, kc == 7, r=["wub", "xT0", "xT1"], w=["!psU"])
            act(p, nc, sg[:], psG[:], AF.Silu, r=["!psG"], w=["sg"])
            tt(p, nc, "dve", hT[:], sg[:], psU[:], ALU.mult, r=["sg", "!psU"], w=["hT"])
            for n in range(2):
                for c in range(4):
                    mm(p, nc, psY[n][:], hT[:, c, :], wdb[:, c, n * 512:(n + 1) * 512], c == 0, c == 3, r=["hT", "wdb0", "wdb1"], w=[f"!psY{n}"])
            cp(p, nc, "act", yb[:, 0:512], psY[0][:], r=["!psY0"], w=["yb0"])
            cp(p, nc, "dve", yb[:, 512:1024], psY[1][:], r=["!psY1"], w=["yb1"])
            dma(p, nc, "sp", "yb", lambda i, k=k: YBUF[bass.ds(i * 256 + k * 128, 128), :], yb[:], r=["yb0", "yb1"])
        p.ns = ""
        a0, a1, a2, b0, b1, b2 = segs
        o_ = p.ops
        p.ops = o_[a0:a1] + o_[b0:b1] + o_[a1:a2] + o_[b1:b2] + o_[a2:b0] + o_[b2:]
        p.run()
    if b.cfg.get("stop") == "moeB":
        return

    with ExitStack() as st:
        def sb(name, shape, dt=F32):
            return st.enter_context(nc.sbuf_tensor(un(b, name), list(shape), dt))
        lng = sb("lng", [128, 1024])
        lnb = sb("lnb", [128, 1024])
        p = Phase(kb, f"moeCpre{l}")
        dma(p, nc, "sp", "c0", lng[:], g["ln_g"][l, 1:2, :].partition_broadcast(128), w=["lng"])
        dma(p, nc, "sp", "c1", lnb[:], g["ln_b"][l, 1:2, :].partition_broadcast(128), w=["lnb"])
        p.run()
        sets = []
        for k in range(2):
            sets.append(dict(xt=sb(f"xt{k}", [128, 1024]), g2b=sb(f"g2b{k}", [128, 1024]), rt=sb(f"rt{k}", [128, 8]),
                             di=sb(f"di{k}", [128, 2], I32), y0=sb(f"y0{k}", [128, 1024]), y1=sb(f"y1{k}", [128, 1024]),
                             z=sb(f"z{k}", [128, 1024]), sq=sb(f"sq{k}", [128, 1024]), xo=sb(f"xo{k}", [128, 1024]),
                             stt=sb(f"stt{k}", [128, 8])))
        p = Phase(kb, f"moeC{l}", n_iter=NTILES // 2)
        for k in range(2):
            p.ns = f"#{k}"
            T = sets[k]
            xt, g2b, rt, di, y0, y1, z, sq, xo, stt = [T[n_] for n_ in ["xt", "g2b", "rt", "di", "y0", "y1", "z", "sq", "xo", "stt"]]
            dma(p, nc, "sp", "xt", xt[:], lambda i, k=k: X[bass.ds(i * 256 + k * 128, 128), :], w=["xt"])
            dma(p, nc, "act", "g2b", g2b[:], lambda i, k=k: MODT[bass.ds(i * 2 + k, 1), 5120:6144].partition_broadcast(128), w=["g2b"])
            dma(p, nc, "act", "rt", rt[:], lambda i, k=k: RTD[:, bass.ds(i * 2 + k, 1), :].rearrange("p o c -> p (o c)"), w=["rt"])
            dma(p, nc, "sp", "di", di[:], lambda i, k=k: DESTD[:, bass.ds(i * 2 + k, 1), :].rearrange("p o c -> p (o c)"), w=["di"])
            for kk, yk in enumerate([y0, y1]):
                p.dma("pool", f"ga{kk}", lambda i, kk=kk, yk=yk, di=di: nc.gpsimd.indirect_dma_start(
                    out=yk[:], out_offset=None, in_=YBUF[:, :], in_offset=bass.IndirectOffsetOnAxis(ap=di[:, kk:kk + 1], axis=0)),
                    r=["di"], w=[f"y{kk}"])
            ts(p, nc, "dve", y0[:], y0[:], rt[:, 2:3], None, ALU.mult, r=["y0", "rt"], w=["y0"])
            p.op("dve", lambda i, y0=y0, y1=y1, rt=rt: nc.vector.scalar_tensor_tensor(y0[:], y1[:], rt[:, 3:4], y0[:], ALU.mult, ALU.add),
                 r=["y0", "y1", "rt"], w=["y0"])
            ts(p, nc, "pool", g2b[:], g2b[:], 1.0, None, ALU.add, r=["g2b"], w=["g2b"])
            tt(p, nc, "pool", y0[:], y0[:], g2b[:], ALU.mult, r=["y0", "g2b"], w=["y0"])
            p.op("dve", lambda i, z=z, xt=xt, y0=y0: nc.vector.scalar_tensor_tensor(z[:], xt[:], alpha, y0[:], ALU.mult, ALU.add),
                 r=["xt", "y0"], w=["z"])
            layer_norm_tail(b, p, z, "z", lng, lnb, xo, "xo", dict(sq=sq, st=stt), "ln")
            dma(p, nc, "sp", "xo", lambda i, k=k: X[bass.ds(i * 256 + k * 128, 128), :], xo[:], r=["xo"])
            if k == 0:
                n_half = len(p.ops)
        p.ns = ""
        p.interleave(0, n_half, len(p.ops))
        p.run()


def build_hgrn_layer(b, l, lr, g):
    nc, kb = b.nc, b.kb
    seqs = b.cfg["seqs"]
    NT = sum(seqs)
    NTILES = NT // 128
    depth = b.cfg["depth"]
    LR = depth // 2
    X, MODT, cn = g["X"], g["MODT"], g["cn"]
    identb = g["identb"]
    alpha = g["alpha"]
    seq_tile0 = g["seq_tile0"]
    if "QZ" not in b.dram:
        b.dscr("QZ", [24, 128, NT])
        b.dscr("VT", [NT, D], BF16)
        b.dscr("GT", [NT, D])
        b.dscr("OF", [NT, D])
        b.dscr("OB", [NT, D])
    QZ, VT, GT, OF, OB = [b.dram[k] for k in ["QZ", "VT", "GT", "OF", "OB"]]

    with ExitStack() as st:
        def sb(name, shape, dt=F32):
            return st.enter_context(nc.sbuf_tensor(un(b, name), list(shape), dt))

        def ps(name, shape, dt=F32):
            return st.enter_context(nc.psum_tensor(un(b, name), list(shape), dt))

        win = sb("win", [128, 8, 5120], BF16)
        with ExitStack() as st2:
            stg = [st2.enter_context(nc.sbuf_tensor(un(b, "stg0"), [128, 8, 512], F32)),
                   st2.enter_context(nc.sbuf_tensor(un(b, "stg1"), [128, 8, 512], F32))]
            p = Phase(kb, f"recw{l}")
            load_weight_bf16(b, p, win, g["r_win"][lr].rearrange("(kc p) n -> p kc n", p=128), 5120, "win", stg)
            p.run()
        xg = sb("xg", [128, 4, 1024])
        scb = sb("scb", [128, 1024])
        shb = sb("shb", [128, 1024])
        tmp = [sb("tmp0", [128, 1024]), sb("tmp1", [128, 1024])]
        hb = sb("hb", [128, 4, 1024], BF16)
        hT = sb("hT", [128, 8, 512], BF16)
        fq = sb("fq", [128, 8, 512])
        vo = sb("vo", [128, 4, 1024], BF16)
        go = sb("go", [128, 4, 1024])
        psT = [ps("psT0", [128, 8, 128], BF16), ps("psT1", [128, 8, 128], BF16)]
        pf = [ps(f"pf{k}", [128, 512]) for k in range(4)]
        NG = NT // 512
        p = Phase(kb, f"H1_{l}", n_iter=NG)
        dma(p, nc, "sp", "xg", xg[:], lambda i: X[bass.ds(i * 512, 512), :].rearrange("(t p) d -> p t d", p=128), w=["xg"])
        dma(p, nc, "act", "scb", scb[:], lambda i: MODT[bass.ds(i * 4, 1), 1024:2048].partition_broadcast(128), w=["scb"])
        dma(p, nc, "act", "shb", shb[:], lambda i: MODT[bass.ds(i * 4, 1), 0:1024].partition_broadcast(128), w=["shb"])
        ts(p, nc, "dve", scb[:], scb[:], 1.0, None, ALU.add, r=["scb"], w=["scb"])
        for t in range(4):
            tt(p, nc, "dve", tmp[t % 2][:], xg[:, t, :], scb[:], ALU.mult, r=["xg", "scb"], w=[f"tmp{t % 2}"])
            tt(p, nc, "pool", hb[:, t, :], tmp[t % 2][:], shb[:], ALU.add, r=[f"tmp{t % 2}", "shb"], w=[f"hb{t}"])
            for kc in range(8):
                tr(p, nc, psT[t % 2][:, kc, :], hb[:, t, kc * 128:(kc + 1) * 128], identb[:], r=[f"hb{t}"], w=[f"psT{t % 2}"])
            cp(p, nc, "act", hT[:, :, t * 128:(t + 1) * 128], psT[t % 2][:], r=[f"psT{t % 2}"], w=[f"hT{t}"])
        hTall = [f"hT{t}" for t in range(4)]
        pfi = 0
        for grp in range(3):
            for hh in range(8):
                c = grp * 8 + hh
                ia = pfi % 4
                pfi += 1
                for kc in range(8):
                    mm(p, nc, pf[ia][:], win[:, kc, c * 128:(c + 1) * 128], hT[:, kc, :], kc == 0, kc == 7, r=hTall, w=[f"pf{ia}"])
                cp(p, nc, "act" if hh % 2 == 0 else "dve", fq[:, hh, :], pf[ia][:], r=[f"pf{ia}"], w=[f"fq{hh}"])
            dma(p, nc, ["sp", "act", "pool"][grp], f"fq{grp}",
                lambda i, grp=grp: QZ[grp * 8:grp * 8 + 8, :, bass.ds(i * 512, 512)].rearrange("f p t -> p f t"), fq[:],
                r=[f"fq{hh}" for hh in range(8)])
        for t in range(4):
            for n in range(4):
                ia = pfi % 4
                pfi += 1
                col0 = 3072 + n * 512
                for kc in range(8):
                    mm(p, nc, pf[ia][:], hT[:, kc, t * 128:(t + 1) * 128], win[:, kc, col0:col0 + 512], kc == 0, kc == 7,
                       r=[f"hT{t}"], w=[f"pf{ia}"])
                if n < 2:
                    cp(p, nc, "act", vo[:, t, n * 512:(n + 1) * 512], pf[ia][:], r=[f"pf{ia}"], w=[f"vo{t}"])
                else:
                    cp(p, nc, "dve", go[:, t, (n - 2) * 512:(n - 1) * 512], pf[ia][:], r=[f"pf{ia}"], w=[f"go{t}"])
        dma(p, nc, "sp", "vo", lambda i: VT[bass.ds(i * 512, 512), :].rearrange("(t p) e -> p t e", p=128), vo[:],
            r=[f"vo{t}" for t in range(4)])
        dma(p, nc, "pool", "go", lambda i: GT[bass.ds(i * 512, 512), :].rearrange("(t p) e -> p t e", p=128), go[:],
            r=[f"go{t}" for t in range(4)])
        p.run()
    if b.cfg.get("stop") == "H1":
        return

    with ExitStack() as st:
        def sb(name, shape, dt=F32):
            return st.enter_context(nc.sbuf_tensor(un(b, name), list(shape), dt))

        def ps(name, shape, dt=F32):
            return st.enter_context(nc.psum_tensor(un(b, name), list(shape), dt))

        lbT = sb("lbT", [128, 8])
        omlT = sb("omlT", [128, 8])
        lbe = sb("lbe", [128, max(LR, 1), 8])
        lsum = sb("lsum", [128, 8])
        hm = sb("hm", [64, 2, 64], BF16)
        smask = sb("smask", [128, 1024])
        S = sb("S", [128, 8, 128])
        p = Phase(kb, f"recpre{l}")
        for j in range(LR):
            dma(p, nc, "sp", f"lb{j}", lbe[:, j, :], g["r_lb"][j].rearrange("(h k) -> k h", k=128), w=[f"lbe{j}"], slow=True)
            act(p, nc, lbe[:, j, :], lbe[:, j, :], AF.Exp, r=[f"lbe{j}"], w=[f"lbe{j}"])
        cp(p, nc, "dve", lsum[:], lbe[:, 0, :], r=["lbe0"], w=["lsum"])
        for j in range(1, LR):
            tt(p, nc, "dve", lsum[:], lsum[:], lbe[:, j, :], ALU.add, r=["lsum", f"lbe{j}"], w=["lsum"])
        p.op("dve", lambda i: nc.vector.reciprocal(lsum[:], lsum[:]), r=["lsum"], w=["lsum"])
        p.op("pool", lambda i: nc.gpsimd.memset(lbT[:], 0.0), w=["lbT"])
        for j in range(1, lr + 1):
            tt(p, nc, "dve", lbT[:], lbT[:], lbe[:, j, :], ALU.add, r=["lbT", f"lbe{j}"], w=["lbT"])
        tt(p, nc, "dve", lbT[:], lbT[:], lsum[:], ALU.mult, r=["lbT", "lsum"], w=["lbT"])
        ts(p, nc, "dve", omlT[:], lbT[:], -1.0, 1.0, ALU.mult, ALU.add, r=["lbT"], w=["omlT"])
        dma(p, nc, "sp", "hm", hm[:], cn["hmask"][:, :, :], w=["hm"])
        dma(p, nc, "sp", "smask", smask[:], cn["scanmask"][:, :], w=["smask"])
        p.run()

        from types import SimpleNamespace
        sets = []
        for k in range(2):
            T = SimpleNamespace(qt=sb(f"qt{k}", [128, 8, 128]), zt=sb(f"zt{k}", [128, 8, 128]), vt=sb(f"vt{k}", [64, 2, 1024], BF16),
                                u=sb(f"u{k}", [128, 8, 128]), w_=sb(f"w_{k}", [128, 8, 128]), f_=sb(f"f_{k}", [128, 8, 128]),
                                lf=sb(f"lf{k}", [128, 8, 128]), km=sb(f"km{k}", [128, 8, 128]), bb=sb(f"bb{k}", [128, 8, 128]),
                                bm=sb(f"bm{k}", [128, 8, 128]), Ee=sb(f"Ee{k}", [128, 8, 128]), Ei=sb(f"Ei{k}", [128, 8, 128]),
                                qe=sb(f"qe{k}", [128, 8, 128], BF16), ke=sb(f"ke{k}", [128, 8, 128], BF16), sc=sb(f"sc{k}", [128, 8, 2, 4]),
                                Sc=sb(f"Sc{k}", [128, 8, 128], BF16), AT=sb(f"AT{k}", [64, 8, 64], BF16), keT=sb(f"keT{k}", [64, 8, 128], BF16),
                                oc=sb(f"oc{k}", [64, 1024]))
            sets.append(T)
        psA = ps("psA", [64, 8, 64])
        psKT2 = [ps("psKT0", [64, 8, 128], BF16), ps("psKT1", [64, 8, 128], BF16)]
        pso = [ps("pso0", [64, 512]), ps("pso1", [64, 512])]
        psdS = [ps("psdS0", [128, 4, 128]), ps("psdS1", [128, 4, 128])]
        lb_bc = lbT[:].unsqueeze(2).to_broadcast([128, 8, 128])
        oml_bc = omlT[:].unsqueeze(2).to_broadcast([128, 8, 128])
        fl = lambda t_: t_[:].rearrange("p h t -> p (h t)")
        c4 = lambda t_: t_[:].rearrange("p h (c t) -> p h c t", t=64)

        def rec_body(p, tok0, dirn, OUT, T, marks):
            zbase = 8 if dirn == 0 else 16
            dma(p, nc, "sp", "qt", T.qt[:], lambda i: QZ[0:8, :, bass.ds(tok0(i), 128)].rearrange("f p t -> p f t"), w=["qt"])
            dma(p, nc, "act", "zt", T.zt[:], lambda i: QZ[zbase:zbase + 8, :, bass.ds(tok0(i), 128)].rearrange("f p t -> p f t"), w=["zt"])
            dma(p, nc, "pool", "vt", T.vt[:], lambda i: VT[bass.ds(tok0(i), 128), :].rearrange("(c p) e -> p c e", p=64), w=["vt"])
            act(p, nc, T.u[:], T.zt[:], AF.Exp, scale=-1.0, r=["zt"], w=["u"])
            ts(p, nc, "dve", T.w_[:], T.u[:], 1.0, None, ALU.add, r=["u"], w=["w_"])
            p.op("dve", lambda i: nc.vector.reciprocal(T.w_[:], T.w_[:]), r=["w_"], w=["w_"])
            tt(p, nc, "dve", T.f_[:], T.w_[:], oml_bc, ALU.mult, r=["w_"], w=["f_"])
            tt(p, nc, "dve", T.f_[:], T.f_[:], lb_bc, ALU.add, r=["f_"], w=["f_"])
            act(p, nc, T.lf[:], T.f_[:], AF.Ln, r=["f_"], w=["lf"])
            tt(p, nc, "pool", T.km[:], T.u[:], T.w_[:], ALU.mult, r=["u", "w_"], w=["km"])
            tt(p, nc, "pool", T.km[:], T.km[:], oml_bc, ALU.mult, r=["km"], w=["km"])
            p.op("dve", lambda i: nc.vector.tensor_tensor_scan(fl(T.bb), smask[:], fl(T.lf), 0.0, ALU.mult, ALU.add), r=["lf"], w=["bb"])
            if dirn == 1:
                tt(p, nc, "pool", T.bm[:], T.lf[:], T.bb[:], ALU.subtract, r=["lf", "bb"], w=["bm"])
                tt(p, nc, "pool", c4(T.bb), c4(T.bm), c4(T.bb)[:, :, :, 63:64].to_broadcast([128, 8, 2, 64]), ALU.add, r=["bm", "bb"], w=["bb"])
            endidx = 63 if dirn == 0 else 0
            cref = c4(T.bb)[:, :, :, 31:32]
            bend = c4(T.bb)[:, :, :, endidx:endidx + 1]
            act(p, nc, T.sc[:, :, :, 0:1], cref, AF.Exp, r=["bb"], w=["sc0"])
            act(p, nc, T.sc[:, :, :, 1:2], bend, AF.Exp, r=["bb"], w=["sc1"])
            tt(p, nc, "dve", T.sc[:, :, :, 3:4], bend, cref, ALU.subtract, r=["bb"], w=["sc3"])
            act(p, nc, T.sc[:, :, :, 2:3], T.sc[:, :, :, 3:4], AF.Exp, r=["sc3"], w=["sc2"])
            tt(p, nc, "pool", c4(T.bm), c4(T.bb), cref.to_broadcast([128, 8, 2, 64]), ALU.subtract, r=["bb", "bm"], w=["bm"])
            act(p, nc, T.Ee[:], T.bm[:], AF.Exp, r=["bm"], w=["Ee"])
            act(p, nc, T.Ei[:], T.bm[:], AF.Exp, scale=-1.0, r=["bm"], w=["Ei"])
            tt(p, nc, "dve", T.qe[:], T.qt[:], T.Ee[:], ALU.mult, r=["qt", "Ee"], w=["qe"])
            tt(p, nc, "pool", T.ke[:], T.km[:], T.Ei[:], ALU.mult, r=["km", "Ei"], w=["ke"])
            marks.append(len(p.ops))
            order = [0, 1] if dirn == 0 else [1, 0]
            for c in order:
                cs_ = slice(c * 64, (c + 1) * 64)
                for h in range(8):
                    p.op("act", lambda i, h=h, c=c, T=T: nc.scalar.activation(T.Sc[:, h, :], S[:, h, :], AF.Copy, scale=T.sc[:, h, c, 0:1]),
                         r=[f"!S{h}", "sc0"], w=[f"Sc{h}"])
                for h in range(8):
                    mm(p, nc, psA[:, h, :], T.ke[:, h, cs_], T.qe[:, h, cs_], True, True, r=["ke", "qe"], w=["!psA"])
                for h in range(8):
                    tt(p, nc, "dve", T.AT[:, h, :], psA[:, h, :], hm[:, dirn, :], ALU.mult, r=["!psA"], w=[f"AT{h}"])
                for h in range(8):
                    tr(p, nc, psKT2[h // 4][:, h % 4, :], T.ke[:, h, cs_], identb[:], r=["ke"], w=[f"!psKT{h // 4}"])
                for h in range(8):
                    cp(p, nc, "dve" if h < 4 else "act", T.keT[:, h, :], psKT2[h // 4][:, h % 4, :], r=[f"!psKT{h // 4}"], w=[f"keT{h}"])
                for h in range(8):
                    po = pso[h // 4][:, (h % 4) * 128:(h % 4 + 1) * 128]
                    mm(p, nc, po, T.AT[:, h, :], T.vt[:, c, h * 128:(h + 1) * 128], True, False, r=[f"AT{h}", "vt"], w=[f"!pso{h // 4}"])
                    mm(p, nc, po, T.qe[:, h, cs_], T.Sc[:, h, :], False, True, r=["qe", f"Sc{h}"], w=[f"!pso{h // 4}"])
                for h in range(8):
                    mm(p, nc, psdS[h // 4][:, h % 4, :], T.keT[:, h, :], T.vt[:, c, h * 128:(h + 1) * 128], True, True,
                       r=[f"keT{h}", "vt"], w=[f"!psdS{h // 4}"])
                for h in range(8):
                    ts(p, nc, "pool", S[:, h, :], S[:, h, :], T.sc[:, h, c, 1:2], None, ALU.mult, r=[f"!S{h}", "sc1"], w=[f"!S{h}"])
                for h in range(8):
                    p.op("dve", lambda i, h=h, c=c, T=T: nc.vector.scalar_tensor_tensor(S[:, h, :], psdS[h // 4][:, h % 4, :], T.sc[:, h, c, 2:3],
                                                                                         S[:, h, :], ALU.mult, ALU.add),
                         r=[f"!psdS{h // 4}", f"!S{h}", "sc2"], w=[f"!S{h}"])
                cp(p, nc, "act", T.oc[:, 0:512], pso[0][:], r=["!pso0"], w=["oc0"])
                cp(p, nc, "dve", T.oc[:, 512:1024], pso[1][:], r=["!pso1"], w=["oc1"])
                dma(p, nc, "sp", f"oc", lambda i, c=c: OUT[bass.ds(tok0(i) + c * 64, 64), :], T.oc[:], r=["oc0", "oc1"])

        for s, T_s in enumerate(seqs):
            nb = T_s // 128
            tok_s = seq_tile0[s] * 128
            for dirn in range(2):
                p = Phase(kb, f"recz{l}_{s}_{dirn}")
                p.op("pool", lambda i: nc.gpsimd.memset(S[:], 0.0), w=["S"])
                p.run()
                p = Phase(kb, f"H2_{l}_{s}_{dirn}", n_iter=nb // 2)
                marks = []
                for k in range(2):
                    p.ns = f"#{k}"
                    marks.append(len(p.ops))
                    if dirn == 0:
                        rec_body(p, lambda i, tok_s=tok_s, k=k: i * 256 + (tok_s + k * 128), 0, OF, sets[k], marks)
                    else:
                        rec_body(p, lambda i, tok_s=tok_s, nb=nb, k=k: (tok_s + (nb - 1 - k) * 128) - i * 256, 1, OB, sets[k], marks)
                p.ns = ""
                a0, a1, b0, b1 = marks
                o_ = p.ops
                prepA, chA, prepB, chB = o_[a0:a1], o_[a1:b0], o_[b0:b1], o_[b1:]
                z_ = []
                for q_ in range(max(len(prepA), len(prepB))):
                    if q_ < len(prepA):
                        z_.append(prepA[q_])
                    if q_ < len(prepB):
                        z_.append(prepB[q_])
                p.ops = z_ + chA + chB
                p.run()
    if b.cfg.get("stop") == "H2":
        return

    with ExitStack() as st:
        def sb(name, shape, dt=F32):
            return st.enter_context(nc.sbuf_tensor(un(b, name), list(shape), dt))

        def ps(name, shape, dt=F32):
            return st.enter_context(nc.psum_tensor(un(b, name), list(shape), dt))
        wout = sb("wout", [128, 8, 1024], BF16)
        gnb = sb("gnb", [128, 1024])
        lng = sb("lng", [128, 1024])
        lnb = sb("lnb", [128, 1024])
        with ExitStack() as st2:
            stg = [st2.enter_context(nc.sbuf_tensor(un(b, "stg0"), [128, 8, 512], F32)),
                   st2.enter_context(nc.sbuf_tensor(un(b, "stg1"), [128, 8, 512], F32))]
            p = Phase(kb, f"recw2{l}")
            load_weight_bf16(b, p, wout, g["r_wout"][lr].rearrange("(kc p) n -> p kc n", p=128), 1024, "wout", stg)
            dma(p, nc, "pool", "gnb", gnb[:], g["r_gn"][lr:lr + 1, :].partition_broadcast(128), w=["gnb"])
            dma(p, nc, "pool", "lng", lng[:], g["ln_g"][l, 0:1, :].partition_broadcast(128), w=["lng"])
            dma(p, nc, "pool", "lnb", lnb[:], g["ln_b"][l, 0:1, :].partition_broadcast(128), w=["lnb"])
            p.run()
        from types import SimpleNamespace
        sets = []
        for k in range(2):
            T = SimpleNamespace(of=sb(f"of{k}", [128, 1024]), ob=sb(f"ob{k}", [128, 1024]), gt=sb(f"gt{k}", [128, 1024]),
                                xt=sb(f"xt{k}", [128, 1024]), g1b=sb(f"g1b{k}", [128, 1024]), sq=sb(f"sq{k}", [128, 1024]),
                                ms=sb(f"ms{k}", [128, 8]), on=sb(f"on{k}", [128, 1024], BF16), onT=sb(f"onT{k}", [128, 8, 128], BF16),
                                t1=sb(f"t1{k}", [128, 1024]), z=sb(f"z{k}", [128, 1024]), xo=sb(f"xo{k}", [128, 1024]),
                                stt=sb(f"stt{k}", [128, 8]), psOT=ps(f"psOT{k}", [128, 8, 128], BF16),
                                psY=[ps(f"psY0{k}", [128, 512]), ps(f"psY1{k}", [128, 512])])
            sets.append(T)
        p = Phase(kb, f"H3_{l}", n_iter=NTILES // 2)
        for k in range(2):
            p.ns = f"#{k}"
            T = sets[k]
            dma(p, nc, "sp", "of", T.of[:], lambda i, k=k, T=T: OF[bass.ds(i * 256 + k * 128, 128), :], w=["of"])
            dma(p, nc, "act", "ob", T.ob[:], lambda i, k=k, T=T: OB[bass.ds(i * 256 + k * 128, 128), :], w=["ob"])
            dma(p, nc, "pool", "gt", T.gt[:], lambda i, k=k, T=T: GT[bass.ds(i * 256 + k * 128, 128), :], w=["gt"])
            dma(p, nc, "sp", "xt", T.xt[:], lambda i, k=k, T=T: X[bass.ds(i * 256 + k * 128, 128), :], w=["xt"])
            dma(p, nc, "act", "g1b", T.g1b[:], lambda i, k=k, T=T: MODT[bass.ds(i * 2 + k, 1), 2048:3072].partition_broadcast(128), w=["g1b"])
            tt(p, nc, "dve", T.of[:], T.of[:], T.ob[:], ALU.add, r=["of", "ob"], w=["of"])
            tt(p, nc, "pool", T.sq[:], T.of[:], T.of[:], ALU.mult, r=["of"], w=["sq"])
            p.op("dve", lambda i, k=k, T=T: nc.vector.tensor_reduce(T.ms[:], T.sq[:].rearrange("p (h v) -> p h v", v=128), AX.X, ALU.add), r=["sq"], w=["ms"])
            ts(p, nc, "dve", T.ms[:], T.ms[:], 1.0 / 128.0, None, ALU.mult, r=["ms"], w=["ms"])
            act(p, nc, T.ms[:], T.ms[:], AF.Ln, bias=b.eps_ln[:, 1:2], r=["ms"], w=["ms"])
            act(p, nc, T.ms[:], T.ms[:], AF.Exp, scale=-0.5, r=["ms"], w=["ms"])
            tt(p, nc, "dve", T.sq[:].rearrange("p (h v) -> p h v", v=128), T.of[:].rearrange("p (h v) -> p h v", v=128),
               T.ms[:].unsqueeze(2).to_broadcast([128, 8, 128]), ALU.mult, r=["of", "ms", "sq"], w=["sq"])
            tt(p, nc, "pool", T.sq[:], T.sq[:], gnb[:], ALU.mult, r=["sq"], w=["sq"])
            act(p, nc, T.gt[:], T.gt[:], AF.Silu, r=["gt"], w=["gt"])
            tt(p, nc, "dve", T.on[:], T.sq[:], T.gt[:], ALU.mult, r=["sq", "gt"], w=["on"])
            for kc in range(8):
                tr(p, nc, T.psOT[:, kc, :], T.on[:, kc * 128:(kc + 1) * 128], identb[:], r=["on"], w=["psOT"])
            cp(p, nc, "act", T.onT[:], T.psOT[:], r=["psOT"], w=["onT"])
            for n in range(2):
                for kc in range(8):
                    mm(p, nc, T.psY[n][:], T.onT[:, kc, :], wout[:, kc, n * 512:(n + 1) * 512], kc == 0, kc == 7, r=["onT"], w=[f"psY{n}"])
            ts(p, nc, "pool", T.g1b[:], T.g1b[:], 1.0, None, ALU.add, r=["g1b"], w=["g1b"])
            for n in range(2):
                tt(p, nc, "dve", T.t1[:, n * 512:(n + 1) * 512], T.psY[n][:], T.g1b[:, n * 512:(n + 1) * 512], ALU.mult,
                   r=[f"psY{n}", "g1b"], w=["t1"])
            p.op("dve", lambda i, k=k, T=T: nc.vector.scalar_tensor_tensor(T.z[:], T.xt[:], alpha, T.t1[:], ALU.mult, ALU.add), r=["xt", "t1"], w=["z"])
            layer_norm_tail(b, p, T.z, "z", lng, lnb, T.xo, "xo", dict(sq=T.sq, st=T.stt), "ln")
            dma(p, nc, "sp", "xo", lambda i, k=k, T=T: X[bass.ds(i * 256 + k * 128, 128), :], T.xo[:], r=["xo"])
            if k == 0:
                n_half = len(p.ops)
        p.ns = ""
        p.interleave(0, n_half, len(p.ops))
        p.run()
```
